# Optimizing a Trainium2 kernel written in Bass

```python
import math
import jax
import jax.numpy as jnp
from jax import lax
import numpy as np

D_MODEL = 1024
BATCH = 2
SEQ = 16384
DEPTH = 4

GRID_W = 64
CTX_LEN = 256
N_MIXERS = 4
EPS = 1e-6

GMLP_CHUNK = 128
GMLP_WIDTH = 2 * D_MODEL
GMLP_GROUPS = 8

LRU_WIDTH = D_MODEL
LRU_HEADS = 8
LRU_HEAD_DIM = LRU_WIDTH // LRU_HEADS
LRU_CONV = 4
LRU_C = 8.0

HYENA_WIDTH = D_MODEL
HYENA_ORDER = 2
HYENA_CONV = 3
HYENA_BANDS = 16
HYENA_EMB = 2 * HYENA_BANDS + 1
HYENA_FILTER_HIDDEN = 64
HYENA_FAST_DECAY = 0.3
HYENA_SLOW_DECAY = 1.5
HYENA_DECAY_TARGET = 1e-2

POOL_WIDTH = D_MODEL
POOL_WINDOWS = (2, 4, 8, 16)
POOL_GROUP = POOL_WIDTH // len(POOL_WINDOWS)

D_FF = 2816
N_EXPERTS = 8
TOP_K = 2
EXPERT_FF = 3584

kernel_name = 'hybrid_interleaved_diffusion_block'


def _n_uses(first, period):
    return len(range(first, DEPTH, period))


def rmsnorm(x, g):
    x32 = x.astype(jnp.float32)
    y = x32 * lax.rsqrt(jnp.mean(x32 * x32, axis=-1, keepdims=True) + EPS)
    return (y * g.astype(jnp.float32)).astype(x.dtype)


def layernorm(x, g):
    x32 = x.astype(jnp.float32)
    mu = jnp.mean(x32, axis=-1, keepdims=True)
    xc = x32 - mu
    y = xc * lax.rsqrt(jnp.mean(xc * xc, axis=-1, keepdims=True) + EPS)
    return (y * g.astype(jnp.float32)).astype(x.dtype)


def modulate(x, shift, scale):
    return x * (1.0 + scale) + shift


def grid_pos_embed(rows, dtype):
    r, col = jnp.meshgrid(jnp.arange(rows, dtype=jnp.float32),
                          jnp.arange(GRID_W, dtype=jnp.float32), indexing='ij')
    quarter = D_MODEL // 4
    omega = 1.0 / (10000.0 ** (jnp.arange(quarter, dtype=jnp.float32) / quarter))

    def sincos(p):
        ang = p.reshape(-1, 1) * omega[None, :]
        return jnp.concatenate([jnp.sin(ang), jnp.cos(ang)], axis=-1)

    return jnp.concatenate([sincos(r), sincos(col)], axis=-1).astype(dtype)


def depthwise_conv_centred(x, w, b):
    K = w.shape[0]
    L = x.shape[1]
    left = K // 2
    xp = jnp.pad(x, ((0, 0), (left, K - 1 - left), (0, 0)))
    y = b
    for k in range(K):
        y = y + xp[:, k:k + L] * w[k]
    return y


def gmlp_mixer(h, w_in, g_v, w_s, b_s, w_out):
    B, L, _ = h.shape
    u, v = jnp.split(jax.nn.gelu(h @ w_in), 2, axis=-1)
    v = layernorm(v, g_v)
    v = v.reshape(B, L // GMLP_CHUNK, GMLP_CHUNK, GMLP_GROUPS, GMLP_WIDTH // GMLP_GROUPS)
    v = jnp.einsum('gts,bnsgc->bntgc', w_s, v) + b_s.T[:, :, None]
    return (u * v.reshape(B, L, GMLP_WIDTH)) @ w_out


def rglru_scan(x, w_a, b_a, w_x, b_x, lam, h0, reverse):
    B, L, W = x.shape
    xh = x.reshape(B, L, LRU_HEADS, LRU_HEAD_DIM)
    r = jax.nn.sigmoid(jnp.einsum('blhi,hij->blhj', xh, w_a).reshape(B, L, W).astype(jnp.float32) + b_a)
    i = jax.nn.sigmoid(jnp.einsum('blhi,hij->blhj', xh, w_x).reshape(B, L, W).astype(jnp.float32) + b_x)
    log_a = -LRU_C * r * jax.nn.softplus(-lam.astype(jnp.float32))
    a = jnp.exp(log_a)
    b = jnp.sqrt(-jnp.expm1(2.0 * log_a)) * (i * x.astype(jnp.float32))
    if h0 is not None:
        first = L - 1 if reverse else 0
        b = b.at[:, first].add(a[:, first] * h0)

    def combine(e1, e2):
        a1, b1 = e1
        a2, b2 = e2
        return a1 * a2, a2 * b1 + b2

    _, hs = lax.associative_scan(combine, (a, b), axis=1, reverse=reverse)
    return hs


def lru_mixer(h_lat, h_ctx, w_in, conv_w, conv_b, w_a, b_a, w_x, b_x, lam, w_out, ctx_out):
    def scans(xb, s_f, s_b):
        hf = rglru_scan(xb, w_a[0], b_a[0], w_x[0], b_x[0], lam[0], s_f, False)
        hb = rglru_scan(xb, w_a[1], b_a[1], w_x[1], b_x[1], lam[1], s_b, True)
        return hf, hb

    gate_c, xb_c = jnp.split(h_ctx @ w_in, 2, axis=-1)
    hf_c, hb_c = scans(depthwise_conv_centred(xb_c, conv_w, conv_b), None, None)
    gate_l, xb_l = jnp.split(h_lat @ w_in, 2, axis=-1)
    hf_l, hb_l = scans(depthwise_conv_centred(xb_l, conv_w, conv_b), hf_c[:, -1], hb_c[:, 0])
    g_l = jax.nn.gelu(gate_l)
    y_lat = (g_l * (hf_l + hb_l).astype(g_l.dtype)) @ w_out
    y_ctx = None
    if ctx_out:
        g_c = jax.nn.gelu(gate_c)
        y_ctx = (g_c * (hf_c + hb_c).astype(g_c.dtype)) @ w_out
    return y_lat, y_ctx


def hyena_filters(L, f_w1, f_b1, f_w2, f_b2, f_w3, f_b3, f_freq, f_wout):
    f32 = jnp.float32
    t = jnp.linspace(0.0, 1.0, L, dtype=f32)[:, None]
    w = 2.0 * math.pi * jnp.arange(L, dtype=f32)[:, None] / L
    bands = jnp.linspace(1e-4, HYENA_BANDS - 1, HYENA_BANDS, dtype=f32)[None, :]
    z = jnp.concatenate([t, jnp.cos(bands * w), jnp.sin(-bands * w)], axis=-1)
    fr = f_freq.astype(f32)
    hdn = jnp.sin(fr[0] * (z @ f_w1.astype(f32) + f_b1.astype(f32)))
    hdn = jnp.sin(fr[1] * (hdn @ f_w2.astype(f32) + f_b2.astype(f32)))
    hdn = jnp.sin(fr[2] * (hdn @ f_w3.astype(f32) + f_b3.astype(f32)))
    filt = (hdn @ f_wout.astype(f32)).reshape(L, HYENA_ORDER, 2, HYENA_WIDTH)
    max_decay = math.log(HYENA_DECAY_TARGET) / HYENA_FAST_DECAY
    min_decay = math.log(HYENA_DECAY_TARGET) / HYENA_SLOW_DECAY
    deltas = jnp.abs(jnp.linspace(min_decay, max_decay, HYENA_WIDTH, dtype=f32))
    window = jnp.exp(-t * deltas[None, :])
    return filt * window[:, None, None, :]


def two_sided_long_conv(v, h_fwd, h_bwd, bias):
    B, L, C = v.shape
    v32 = v.astype(jnp.float32)
    k = jnp.concatenate([h_fwd, jnp.zeros((1, C), jnp.float32), h_bwd[:0:-1]], axis=0)
    n = 2 * L
    y = jnp.fft.irfft(jnp.fft.rfft(v32, n=n, axis=1) * jnp.fft.rfft(k, n=n, axis=0)[None], n=n, axis=1)[:, :L]
    return y + v32 * bias.astype(jnp.float32)


def hyena_mixer(h, w_in, conv_w, conv_b, f_w1, f_b1, f_w2, f_b2, f_w3, f_b3, f_freq, f_wout, bias, w_out):
    B, L, _ = h.shape
    z = depthwise_conv_centred(h @ w_in, conv_w, conv_b).astype(jnp.float32)
    x1, x2, v = jnp.split(z, 3, axis=-1)
    filt = hyena_filters(L, f_w1, f_b1, f_w2, f_b2, f_w3, f_b3, f_freq, f_wout)
    y = v
    for o, gate in enumerate((x1, x2)):
        y = gate * two_sided_long_conv(y, filt[:, o, 0], filt[:, o, 1], bias[o])
    return y.astype(h.dtype) @ w_out


def pool_mixer(h, w_in, w_g, b_g, scale, w_out):
    B, L, _ = h.shape
    G = len(POOL_WINDOWS)
    p = (h @ w_in).astype(jnp.float32).reshape(B, L, G, POOL_GROUP)
    S = jnp.concatenate([jnp.zeros((B, 1, G, POOL_GROUP), jnp.float32), jnp.cumsum(p, axis=1)], axis=1)
    t = jnp.arange(L)
    pooled = []
    for g, win in enumerate(POOL_WINDOWS):
        lo = jnp.clip(t - win // 2, 0, L)
        hi = jnp.clip(t + win // 2, 0, L)
        cnt = (hi - lo).astype(jnp.float32)[None, :, None]
        Sg = S[:, :, g]
        pooled.append((Sg[:, hi] - Sg[:, lo]) / cnt - p[:, :, g])
    q = jnp.stack(pooled, axis=2)
    y = jnp.einsum('blgc,gcd->blgd', q, w_g.astype(jnp.float32)) + b_g.astype(jnp.float32)
    y = y.reshape(B, L, POOL_WIDTH) * scale.astype(jnp.float32)
    return y.astype(h.dtype) @ w_out


def swiglu(h, w_gate, w_up, w_down):
    return (jax.nn.silu(h @ w_gate) * (h @ w_up)) @ w_down


def moe_ffn(h, w_router, w_gate, w_up, w_down):
    shape = h.shape
    hf = h.reshape(-1, shape[-1])
    logits = (hf @ w_router).astype(jnp.float32)
    top_val, top_idx = lax.top_k(logits, TOP_K)
    probs = jax.nn.softmax(top_val, axis=-1)
    combine = jnp.sum(jax.nn.one_hot(top_idx, N_EXPERTS, dtype=jnp.float32) * probs[..., None], axis=1)
    out = jnp.zeros_like(hf)
    for e in range(N_EXPERTS):
        out = out + combine[:, e:e + 1].astype(hf.dtype) * swiglu(hf, w_gate[e], w_up[e], w_down[e])
    return out.reshape(shape)


def setup_inputs(seed: int = 0) -> dict:
    key = jax.random.key(seed)
    keys = iter(jax.random.split(key, 64))
    f32 = jnp.float32

    def nrm(shape, scale):
        return jax.random.normal(next(keys), shape, f32) * scale

    D = D_MODEL
    nA, nB, nC, nD = (_n_uses(k, N_MIXERS) for k in range(N_MIXERS))
    nF, nM = _n_uses(0, 2), _n_uses(1, 2)
    inp = {}
    inp['x'] = nrm((BATCH, SEQ, D), 1.0)
    inp['c'] = nrm((BATCH, D), 1.0)
    inp['ctx'] = nrm((BATCH, CTX_LEN, D), 1.0)
    inp['c_ctx'] = nrm((D,), 1.0)
    inp['ada_w'] = nrm((DEPTH, D, 6 * D), 0.5 * D ** -0.5)
    inp['ada_b'] = nrm((DEPTH, 6 * D), 0.02)
    inp['norm_g'] = 1.0 + nrm((DEPTH, 4, D), 0.05)
    inp['gmlp_w_in'] = nrm((nA, D, 2 * GMLP_WIDTH), D ** -0.5)
    inp['gmlp_g_v'] = 1.0 + nrm((nA, GMLP_WIDTH), 0.05)
    inp['gmlp_w_s'] = nrm((nA, GMLP_GROUPS, GMLP_CHUNK, GMLP_CHUNK), GMLP_CHUNK ** -0.5)
    inp['gmlp_b_s'] = 1.0 + nrm((nA, GMLP_GROUPS, GMLP_CHUNK), 0.02)
    inp['gmlp_w_out'] = nrm((nA, GMLP_WIDTH, D), GMLP_WIDTH ** -0.5)
    inp['lru_w_in'] = nrm((nB, D, 2 * LRU_WIDTH), D ** -0.5)
    inp['lru_conv_w'] = nrm((nB, LRU_CONV, LRU_WIDTH), LRU_CONV ** -0.5)
    inp['lru_conv_b'] = nrm((nB, LRU_WIDTH), 0.02)
    inp['lru_w_a'] = nrm((nB, 2, LRU_HEADS, LRU_HEAD_DIM, LRU_HEAD_DIM), LRU_HEAD_DIM ** -0.5)
    inp['lru_b_a'] = nrm((nB, 2, LRU_WIDTH), 0.02)
    inp['lru_w_x'] = nrm((nB, 2, LRU_HEADS, LRU_HEAD_DIM, LRU_HEAD_DIM), LRU_HEAD_DIM ** -0.5)
    inp['lru_b_x'] = nrm((nB, 2, LRU_WIDTH), 0.02)
    u = jax.random.uniform(next(keys), (nB, 2, LRU_WIDTH), f32, minval=0.9, maxval=0.999)
    a = u ** (1.0 / LRU_C)
    inp['lru_lam'] = jnp.log(a) - jnp.log1p(-a)
    inp['lru_w_out'] = nrm((nB, LRU_WIDTH, D), LRU_WIDTH ** -0.5)
    inp['hyena_w_in'] = nrm((nC, D, 3 * HYENA_WIDTH), D ** -0.5)
    inp['hyena_conv_w'] = nrm((nC, HYENA_CONV, 3 * HYENA_WIDTH), HYENA_CONV ** -0.5)
    inp['hyena_conv_b'] = nrm((nC, 3 * HYENA_WIDTH), 0.02)
    inp['hyena_f_w1'] = nrm((nC, HYENA_EMB, HYENA_FILTER_HIDDEN), HYENA_EMB ** -0.5)
    inp['hyena_f_b1'] = nrm((nC, HYENA_FILTER_HIDDEN), 0.02)
    inp['hyena_f_w2'] = nrm((nC, HYENA_FILTER_HIDDEN, HYENA_FILTER_HIDDEN), HYENA_FILTER_HIDDEN ** -0.5)
    inp['hyena_f_b2'] = nrm((nC, HYENA_FILTER_HIDDEN), 0.02)
    inp['hyena_f_w3'] = nrm((nC, HYENA_FILTER_HIDDEN, HYENA_FILTER_HIDDEN), HYENA_FILTER_HIDDEN ** -0.5)
    inp['hyena_f_b3'] = nrm((nC, HYENA_FILTER_HIDDEN), 0.02)
    inp['hyena_f_freq'] = 1.0 + nrm((nC, 3, HYENA_FILTER_HIDDEN), 0.05)
    inp['hyena_f_wout'] = nrm((nC, HYENA_FILTER_HIDDEN, HYENA_ORDER * 2 * HYENA_WIDTH), 0.1 * HYENA_FILTER_HIDDEN ** -0.5)
    inp['hyena_bias'] = nrm((nC, HYENA_ORDER, HYENA_WIDTH), 0.1)
    inp['hyena_w_out'] = nrm((nC, HYENA_WIDTH, D), HYENA_WIDTH ** -0.5)
    inp['pool_w_in'] = nrm((nD, D, POOL_WIDTH), D ** -0.5)
    inp['pool_w_g'] = nrm((nD, len(POOL_WINDOWS), POOL_GROUP, POOL_GROUP), POOL_GROUP ** -0.5)
    inp['pool_b_g'] = nrm((nD, len(POOL_WINDOWS), POOL_GROUP), 0.02)
    inp['pool_scale'] = 1.0 + nrm((nD, POOL_WIDTH), 0.05)
    inp['pool_w_out'] = nrm((nD, POOL_WIDTH, D), POOL_WIDTH ** -0.5)
    inp['ffn_w_gate'] = nrm((nF, D, D_FF), D ** -0.5)
    inp['ffn_w_up'] = nrm((nF, D, D_FF), D ** -0.5)
    inp['ffn_w_down'] = nrm((nF, D_FF, D), D_FF ** -0.5)
    inp['moe_w_router'] = nrm((nM, D, N_EXPERTS), D ** -0.5)
    inp['moe_w_gate'] = nrm((nM, N_EXPERTS, D, EXPERT_FF), D ** -0.5)
    inp['moe_w_up'] = nrm((nM, N_EXPERTS, D, EXPERT_FF), D ** -0.5)
    inp['moe_w_down'] = nrm((nM, N_EXPERTS, EXPERT_FF, D), EXPERT_FF ** -0.5)
    return inp


def reference(x, c, ctx, c_ctx, ada_w, ada_b, norm_g,
              gmlp_w_in, gmlp_g_v, gmlp_w_s, gmlp_b_s, gmlp_w_out,
              lru_w_in, lru_conv_w, lru_conv_b, lru_w_a, lru_b_a, lru_w_x, lru_b_x, lru_lam, lru_w_out,
              hyena_w_in, hyena_conv_w, hyena_conv_b, hyena_f_w1, hyena_f_b1, hyena_f_w2, hyena_f_b2,
              hyena_f_w3, hyena_f_b3, hyena_f_freq, hyena_f_wout, hyena_bias, hyena_w_out,
              pool_w_in, pool_w_g, pool_b_g, pool_scale, pool_w_out,
              ffn_w_gate, ffn_w_up, ffn_w_down,
              moe_w_router, moe_w_gate, moe_w_up, moe_w_down):
    B, L, _ = x.shape
    rows = L // GRID_W
    x = x + grid_pos_embed(rows, x.dtype)[None]
    xc = ctx
    last_ctx = max([i for i in range(DEPTH) if i % N_MIXERS == 1], default=-1)

    for i in range(DEPTH):
        kind, j = i % N_MIXERS, i // N_MIXERS
        update_ctx = i < last_ctx
        read_ctx = i <= last_ctx
        sh1, sc1, gt1, sh2, sc2, gt2 = jnp.split((jax.nn.silu(c) @ ada_w[i] + ada_b[i])[:, None, :], 6, axis=-1)
        h = modulate(rmsnorm(x, norm_g[i, 0]), sh1, sc1)
        hc = None
        if read_ctx:
            csh1, csc1, cgt1, csh2, csc2, cgt2 = jnp.split(jax.nn.silu(c_ctx) @ ada_w[i] + ada_b[i], 6, axis=-1)
            hc = modulate(rmsnorm(xc, norm_g[i, 0]), csh1, csc1)

        y_c = None
        if kind == 0:
            p = (gmlp_w_in[j], gmlp_g_v[j], gmlp_w_s[j], gmlp_b_s[j], gmlp_w_out[j])
            y = gmlp_mixer(h, *p)
            if update_ctx:
                y_c = gmlp_mixer(hc, *p)
        elif kind == 1:
            y, y_c = lru_mixer(h, hc, lru_w_in[j], lru_conv_w[j], lru_conv_b[j], lru_w_a[j], lru_b_a[j],
                               lru_w_x[j], lru_b_x[j], lru_lam[j], lru_w_out[j], update_ctx)
        elif kind == 2:
            p = (hyena_w_in[j], hyena_conv_w[j], hyena_conv_b[j], hyena_f_w1[j], hyena_f_b1[j],
                 hyena_f_w2[j], hyena_f_b2[j], hyena_f_w3[j], hyena_f_b3[j], hyena_f_freq[j],
                 hyena_f_wout[j], hyena_bias[j], hyena_w_out[j])
            y = hyena_mixer(h, *p)
            if update_ctx:
                y_c = hyena_mixer(hc, *p)
        else:
            p = (pool_w_in[j], pool_w_g[j], pool_b_g[j], pool_scale[j], pool_w_out[j])
            y = pool_mixer(h, *p)
            if update_ctx:
                y_c = pool_mixer(hc, *p)

        x = x + gt1 * rmsnorm(y, norm_g[i, 1])
        if update_ctx:
            xc = xc + cgt1 * rmsnorm(y_c, norm_g[i, 1])

        k = i // 2

        def channel_mixer(t):
            if i % 2 == 0:
                return swiglu(t, ffn_w_gate[k], ffn_w_up[k], ffn_w_down[k])
            return moe_ffn(t, moe_w_router[k], moe_w_gate[k], moe_w_up[k], moe_w_down[k])

        x = x + gt2 * rmsnorm(channel_mixer(modulate(rmsnorm(x, norm_g[i, 2]), sh2, sc2)), norm_g[i, 3])
        if update_ctx:
            xc = xc + cgt2 * rmsnorm(channel_mixer(modulate(rmsnorm(xc, norm_g[i, 2]), csh2, csc2)), norm_g[i, 3])
    return x
```

```python
import os, math
import ml_dtypes
import numpy as np
from contextlib import ExitStack
import concourse.bass as bass
import concourse.mybir as mybir
from concourse.bass_utils import run_bass_kernel_spmd

F32 = mybir.dt.float32
BF16 = mybir.dt.bfloat16
I32 = mybir.dt.int32
AF = mybir.ActivationFunctionType
ALU = mybir.AluOpType
AX = mybir.AxisListType

COMPUTE = ("tensor", "vector", "scalar", "gpsimd")
DMAQ = ("sync", "gpsimd")
SAME_ENGINE_SYNC = True


class Prog:
    def __init__(self, name="k"):
        self.nc = bass.Bass("TRN2", target_bir_lowering=False)
        self.es = ExitStack()
        self.ops = []
        self.last_w = {}
        self.readers = {}
        self.n_dma_sems = {"sync": 24, "gpsimd": 24}
        self.names = set()

    def dram(self, name, shape, dt, kind):
        return self.nc.dram_tensor(name, list(shape), dt, kind=kind).ap()

    def sb(self, name, shape, dt):
        assert name not in self.names, name
        self.names.add(name)
        return self.es.enter_context(self.nc.sbuf_tensor(name, list(shape), dt))

    def ps(self, name, shape, dt):
        assert name not in self.names, name
        self.names.add(name)
        return self.es.enter_context(self.nc.psum_tensor(name, list(shape), dt))

    def op(self, eng, fn, r=(), w=(), dma=False):
        i = len(self.ops)
        deps = set()
        for k in r:
            if k in self.last_w:
                deps.add(self.last_w[k])
        for k in w:
            if k in self.last_w:
                deps.add(self.last_w[k])
            for j in self.readers.get(k, ()):
                deps.add(j)
        deps.discard(i)
        self.ops.append(dict(eng=eng, fn=fn, deps=sorted(deps), dma=dma))
        for k in r:
            self.readers.setdefault(k, []).append(i)
        for k in w:
            self.last_w[k] = i
            self.readers[k] = []
        return i

    def dma(self, q, out, in_, r=(), w=(), **kw):
        return self.op(q, lambda e: e.dma_start(out=out, in_=in_, **kw), r, w, dma=True)

    def emit(self):
        nc = self.nc
        ops = self.ops
        es = self.es
        eng_ops = {}
        for i, o in enumerate(ops):
            lst = eng_ops.setdefault(o["eng"], [])
            o["pos"] = len(lst)
            lst.append(i)
        dcount = {}
        for i, o in enumerate(ops):
            if o["dma"]:
                q = o["eng"]
                o["dnum"] = dcount.get(q, 0)
                dcount[q] = o["dnum"] + 1
        seen = {e: {} for e in eng_ops}
        seen_dma = {e: set() for e in eng_ops}
        for i, o in enumerate(ops):
            X = o["eng"]
            waits = []
            for j in o["deps"]:
                p = ops[j]
                if p["dma"]:
                    if j in seen_dma[X]:
                        continue
                    seen_dma[X].add(j)
                    waits.append(j)
                else:
                    E = p["eng"]
                    if E == X and (E == "tensor" or not SAME_ENGINE_SYNC):
                        continue
                    if seen[X].get(E, -1) >= p["pos"]:
                        continue
                    seen[X][E] = p["pos"]
                    waits.append(j)
            o["waits"] = waits
            for j in waits:
                ops[j]["sig"] = True
        sigcount = {}
        for i, o in enumerate(ops):
            if o["dma"]:
                continue
            if o.get("sig"):
                E = o["eng"]
                sigcount[E] = sigcount.get(E, 0) + 1
                o["signum"] = sigcount[E]
        csem = {e: es.enter_context(nc.semaphore("c_" + e)) for e in COMPUTE}
        dsem = {q: [es.enter_context(nc.semaphore("d_%s_%d" % (q, k))) for k in range(self.n_dma_sems[q])]
                for q in dcount}
        self.stats = {e: len(v) for e, v in eng_ops.items()}
        self.stats["sig"] = dict(sigcount)

        def waitspec(j):
            p = ops[j]
            if p["dma"]:
                K = self.n_dma_sems[p["eng"]]
                return dsem[p["eng"]][p["dnum"] % K], 16 * (p["dnum"] // K + 1)
            return csem[p["eng"]], p["signum"]

        def run_engine(ename, e):
            for i in eng_ops.get(ename, []):
                o = ops[i]
                for j in o["waits"]:
                    s, v = waitspec(j)
                    e.wait_ge(s, v)
                if o["dma"]:
                    K = self.n_dma_sems[ename]
                    m = o["dnum"]
                    if m >= K:
                        e.wait_ge(dsem[ename][m % K], 16 * (m // K))
                    ins = o["fn"](e)
                    ins.then_inc(dsem[ename][m % K], 16)
                else:
                    ins = o["fn"](e)
                    if o.get("sig"):
                        ins.then_inc(csem[ename], 1)
            if ename in dsem:
                K = self.n_dma_sems[ename]
                n = dcount[ename]
                for k in range(min(K, n)):
                    cnt = (n - 1 - k) // K + 1
                    e.wait_ge(dsem[ename][k], 16 * cnt)

        with nc.Block() as block:
            @block.sync
            def _(e):
                run_engine("sync", e)

            @block.tensor
            def _(e):
                run_engine("tensor", e)

            @block.vector
            def _(e):
                run_engine("vector", e)

            @block.scalar
            def _(e):
                run_engine("scalar", e)

            @block.gpsimd
            def _(e):
                run_engine("gpsimd", e)
        self.es.close()
        return nc
D = 1024
EPS = 1e-6


class Tok(Prog):
    def __init__(self, TB):
        super().__init__()
        self.TB = TB
        self.NT = TB // 128
        self.ident_d = self.dram("ident", [128, 128], F32, "ExternalInput")
        self.ident = self.sb("ident_sb", [128, 128], BF16)
        self.dma("gpsimd", self.ident[:], self.ident_d, w=["ident"])
        self.ones = self.sb("ones", [1, 128], F32)
        self.op("vector", lambda e: e.memset(self.ones[:], 1.0), w=["ones"])
        self.psA = [self.ps("psA%d" % i, [128, 512], F32) for i in range(3)]
        self.psY = [self.ps("psY%d" % i, [128, 1024], F32) for i in range(2)]
        self.psT = self.ps("psT", [128, 1024], BF16)
        self.wsl = [self.sb("wsl%d" % i, [128, 8192], BF16) for i in range(3)]
        self.wi = 0
        self.ai = 0
        self.yi = 0
        self.tmpf = [self.sb("tmpf%d" % i, [128, 1024], F32) for i in range(2)]
        self.ti = 0
        self.hbf = [self.sb("hbf%d" % i, [128, 1024], BF16) for i in range(2)]
        self.hi = 0
        self.stat = self.sb("stat", [128, 64], F32)
        self.si = 0
        self.rowbuf = self.sb("rowbuf", [1, 2048], F32)
        self.si2 = 0
        self.csb = self.sb("csb", [128, 8], F32)
        self.csl = self.sb("csl", [128, 8], BF16)
        self.junk = self.sb("junk", [128, 1024], BF16)

    def nw(self):
        i = self.wi % 3
        self.wi += 1
        return self.wsl[i], "wsl%d" % i

    def nA(self):
        i = self.ai % 3
        self.ai += 1
        return self.psA[i], "psA%d" % i

    def nY(self):
        i = self.yi % 2
        self.yi += 1
        return self.psY[i], "psY%d" % i

    def ntmp(self):
        i = self.ti % 2
        self.ti += 1
        return self.tmpf[i], "tmpf%d" % i

    def nh(self):
        i = self.hi % 2
        self.hi += 1
        return self.hbf[i], "hbf%d" % i

    def nstat(self, n=1):
        if self.si + n > 64:
            self.si = 0
        c = self.si
        self.si += n
        return c

    def load_w(self, src, a, b):
        t, key = self.nw()
        dst = t[:, 0:a * b].rearrange("p (a b) -> p a b", a=a)
        self.dma("gpsimd", dst, src, w=[key])
        return dst, key

    def bc_dram(self, dst, dkey, src1d, off, n, q="sync"):
        ap = bass.AP(tensor=src1d.tensor, offset=off, ap=[[0, 128], [1, n]])
        self.dma(q, dst, ap, w=[dkey])

    def ada_mod(self, pref, cvec, ada_w, ada_b, normg, segs):
        self.dma("sync", self.csb[:], cvec, w=["csb"])
        self.op("scalar", lambda e: e.activation(out=self.csl[:], in_=self.csb[:], func=AF.Silu), r=["csb"], w=["csl"])
        wv = ada_w.rearrange("(kc p) n -> p kc n", p=128)
        bv = ada_b.rearrange("(o n) -> o n", o=1)
        rb = self.rowbuf
        out = {}
        for (seg, name, kind, gidx) in segs:
            t = self.get_tile(pref + name)
            tk = pref + name
            for hb in range(2):
                nb = seg * 2 + hb
                sl = self.si2 % 2
                self.si2 += 1
                rk = "row%d" % sl
                self.dma("sync", rb[0:1, sl * 1024 + 512:sl * 1024 + 1024], bv[0:1, nb * 512:(nb + 1) * 512], w=[rk + "b"])
                wt, wk = self.load_w(wv[:, :, nb * 512:(nb + 1) * 512], 8, 512)
                pa, pk = self.nA()
                for kc in range(8):
                    self.op("tensor", lambda e, kc=kc, wt=wt, pa=pa: e.matmul(pa[0:1, :], lhsT=self.csl[:, kc:kc + 1], rhs=wt[:, kc, :], start=(kc == 0), stop=(kc == 7)),
                            r=[wk, "csl"], w=[pk])
                self.op("vector", lambda e, sl=sl, pa=pa: e.tensor_tensor(out=rb[0:1, sl * 1024:sl * 1024 + 512], in0=pa[0:1, :], in1=rb[0:1, sl * 1024 + 512:sl * 1024 + 1024], op=ALU.add),
                        r=[pk, rk + "b"], w=[rk])
                pb, pbk = self.nA()
                self.op("tensor", lambda e, sl=sl, pb=pb: e.matmul(pb[:, :], lhsT=self.ones[0:1, :], rhs=rb[0:1, sl * 1024:sl * 1024 + 512], start=True, stop=True),
                        r=[rk, "ones"], w=[pbk])
                self.op("scalar", lambda e, hb=hb, pb=pb, t=t: e.copy(out=t[:, hb * 512:(hb + 1) * 512], in_=pb[:, :]), r=[pbk], w=[tk])
            if kind != "raw":
                tmp, tmk = self.ntmp()
                self.bc_dram(tmp[:], tmk, normg, gidx * D, D)
                if kind == "mul1p":
                    self.op("vector", lambda e, t=t, tmp=tmp: e.scalar_tensor_tensor(out=t[:], in0=t[:], scalar=1.0, in1=tmp[:], op0=ALU.add, op1=ALU.mult), r=[tk, tmk], w=[tk])
                else:
                    self.op("vector", lambda e, t=t, tmp=tmp: e.tensor_tensor(out=t[:], in0=t[:], in1=tmp[:], op=ALU.mult), r=[tk, tmk], w=[tk])
            out[name] = (t, tk)
        return out

    def get_tile(self, name, shape=None, dt=F32):
        if not hasattr(self, "_tiles"):
            self._tiles = {}
        if name not in self._tiles:
            self._tiles[name] = self.sb(name, shape or [128, D], dt)
        return self._tiles[name]

    def rstd_of(self, srcs, skeys):
        n = len(srcs)
        c = self.nstat(n + 2)
        st = self.stat
        self.op("vector", lambda e: e.memset(st[:, c:c + n], 0.0), w=["stat"])
        for i, s in enumerate(srcs):
            self.op("scalar", lambda e, s=s, i=i: e.activation(out=self.junk[:, 0:s.shape[-1]], in_=s, func=AF.Square, accum_out=st[:, c + i:c + i + 1]),
                    r=list(skeys), w=["junk", "stat"])
        if n == 2:
            self.op("vector", lambda e: e.tensor_tensor(out=st[:, c:c + 1], in0=st[:, c:c + 1], in1=st[:, c + 1:c + 2], op=ALU.add), r=["stat"], w=["stat"])
        self.op("scalar", lambda e: e.activation(out=st[:, c + n:c + n + 1], in_=st[:, c:c + 1], func=AF.Sqrt, scale=1.0 / D, bias=EPS), r=["stat"], w=["stat"])
        self.op("vector", lambda e: e.reciprocal(out=st[:, c + n + 1:c + n + 2], in_=st[:, c + n:c + n + 1]), r=["stat"], w=["stat"])
        return st[:, c + n + 1:c + n + 2]

    def norm_mod(self, xt, xkey, Gt, SHt, out_bf, okey):
        rs = self.rstd_of([xt], [xkey])
        tmp, tk = self.ntmp()
        self.op("vector", lambda e: e.scalar_tensor_tensor(out=tmp[:], in0=xt, scalar=rs, in1=Gt[0][:], op0=ALU.mult, op1=ALU.mult),
                r=[xkey, "stat", Gt[1]], w=[tk])
        self.op("gpsimd", lambda e: e.tensor_tensor(out=out_bf, in0=tmp[:], in1=SHt[0][:], op=ALU.add), r=[tk, SHt[1]], w=[okey])

    def transpose_to(self, src_bf, skey, dstT, dkey, tcol, nk=8):
        pv = self.psT[:, 0:nk * 128].rearrange("p (a b) -> p a b", a=nk)
        for kc in range(nk):
            self.op("tensor", lambda e, kc=kc: e.transpose(out=pv[:, kc, :], in_=src_bf[:, kc * 128:(kc + 1) * 128], identity=self.ident[:]),
                    r=[skey, "ident"], w=["psT"])
        self.op("vector", lambda e: e.tensor_copy(out=dstT[:, 0:nk, tcol:tcol + 128], in_=pv), r=["psT"], w=[dkey])

    def post_res(self, ypsum, ykey, xt, xkey, GTt):
        rs = self.rstd_of([ypsum[:, 0:512], ypsum[:, 512:1024]], [ykey])
        tmp, tk = self.ntmp()
        self.op("vector", lambda e: e.scalar_tensor_tensor(out=tmp[:], in0=ypsum[:, :], scalar=rs, in1=GTt[0][:], op0=ALU.mult, op1=ALU.mult),
                r=[ykey, "stat", GTt[1]], w=[tk])
        self.op("gpsimd", lambda e: e.tensor_tensor(out=xt, in0=xt, in1=tmp[:], op=ALU.add), r=[tk, xkey], w=[xkey])

    def post_res_sb(self, ysb, ykey, xt, xkey, GTt):
        rs = self.rstd_of([ysb], [ykey, "uT"])
        tmp, tk = self.ntmp()
        self.op("vector", lambda e: e.scalar_tensor_tensor(out=tmp[:], in0=ysb, scalar=rs, in1=GTt[0][:], op0=ALU.mult, op1=ALU.mult),
                r=[ykey, "stat", GTt[1]], w=[tk])
        self.op("gpsimd", lambda e: e.tensor_tensor(out=xt, in0=xt, in1=tmp[:], op=ALU.add), r=[tk, xkey], w=[xkey])

    def lin_fm(self, W, KCn, hT, hkey, t0, ntok, nchunks, evac, group=4):
        wv = W.rearrange("(kc p) n -> p kc n", p=128)
        cols_per_load = 8192 // KCn // 128 * 128
        cols_per_load = min(cols_per_load, 512)
        i = 0
        while i < len(nchunks):
            n0 = nchunks[i]
            run = [n0]
            while len(run) * 128 < cols_per_load and i + len(run) < len(nchunks) and nchunks[i + len(run)] == run[-1] + 1:
                run.append(run[-1] + 1)
            wt, wk = self.load_w(wv[:, :, n0 * 128:(n0 + len(run)) * 128], KCn, len(run) * 128)
            for j, ncx in enumerate(run):
                pa, pk = self.nA()
                for kc in range(KCn):
                    self.op("tensor", lambda e, kc=kc, j=j, wt=wt, pa=pa: e.matmul(pa[:, 0:ntok], lhsT=wt[:, kc, j * 128:(j + 1) * 128], rhs=hT[:, kc, t0:t0 + ntok], start=(kc == 0), stop=(kc == KCn - 1)),
                            r=[wk, hkey], w=[pk])
                evac(ncx, pa[:, 0:ntok], pk)
            i += len(run)

    def gelu_evac(self, ps, pk, out, okey):
        n = ps.shape[-1]
        tmp, tk = self.ntmp()
        t = tmp[:, 0:n]
        self.op("scalar", lambda e: e.activation(out=t, in_=ps, func=AF.Square), r=[pk], w=[tk])
        self.op("vector", lambda e: e.tensor_scalar(out=t, in0=t, scalar1=0.044715, scalar2=1.0, op0=ALU.mult, op1=ALU.add), r=[tk], w=[tk])
        self.op("vector", lambda e: e.tensor_tensor(out=t, in0=t, in1=ps, op=ALU.mult), r=[tk, pk], w=[tk])
        self.op("scalar", lambda e: e.activation(out=t, in_=t, func=AF.Sigmoid, scale=1.5957691216057308), r=[tk], w=[tk])
        self.op("vector", lambda e: e.tensor_tensor(out=out, in0=t, in1=ps, op=ALU.mult), r=[tk, pk], w=[okey])

    def load_block(self, X, srcs, pos=None):
        for ti, s in enumerate(srcs):
            self.dma("sync", X[:, ti, :], s, w=["X%d" % ti])
            if pos is not None:
                tmp, tk = self.ntmp()
                self.dma("sync", tmp[:], pos[ti], w=[tk])
                self.op("gpsimd", lambda e, ti=ti, tmp=tmp: e.tensor_tensor(out=X[:, ti, :], in0=X[:, ti, :], in1=tmp[:], op=ALU.add), r=[tk, "X%d" % ti], w=["X%d" % ti])

    def pre_T(self, X, nt, Gt, SHt, hT, want32=None):
        for ti in range(nt):
            hb, hk = self.nh()
            if want32 is None:
                self.norm_mod(X[:, ti, :], "X%d" % ti, Gt, SHt, hb[:], hk)
            else:
                h32, h32k = want32
                self.norm_mod(X[:, ti, :], "X%d" % ti, Gt, SHt, h32[:, ti, :], h32k + str(ti))
                self.op("scalar", lambda e, ti=ti, hb=hb: e.copy(out=hb[:], in_=h32[:, ti, :]), r=[h32k + str(ti)], w=[hk])
            self.transpose_to(hb, hk, hT, "hT", ti * 128)

    def gmlp(self, X, nt, c, mods):
        ntok = nt * 128
        hT, uT, vg = c["hT"], c["uT"], c["vg"]
        w_in, w_out = c["w_in"], c["w_out"]
        def ev_u(ncx, ps, pk):
            self.gelu_evac(ps, pk, uT[:, ncx, 0:ntok], "uT")
        self.lin_fm(w_in, 8, hT, "hT", 0, ntok, list(range(16)), ev_u)
        wv = w_in.rearrange("(kc p) n -> p kc n", p=128)
        for cb in range(4):
            wt, wk = self.load_w(wv[:, :, 2048 + cb * 512:2048 + (cb + 1) * 512], 8, 512)
            for ti in range(nt):
                pa, pk = self.nA()
                for kc in range(8):
                    self.op("tensor", lambda e, kc=kc, ti=ti, wt=wt, pa=pa: e.matmul(pa[:, :], lhsT=hT[:, kc, ti * 128:(ti + 1) * 128], rhs=wt[:, kc, :], start=(kc == 0), stop=(kc == 7)),
                            r=[wk, "hT"], w=[pk])
                self.gelu_evac(pa[:, :], pk, vg[:, ti, cb * 512:(cb + 1) * 512], "vg%d" % ti)
        bst = c["bst"]
        wsT = c["wsT"]
        for ti in range(nt):
            for cb in range(4):
                self.op("vector", lambda e, ti=ti, cb=cb: e.bn_stats(out=bst[:, cb * 6:(cb + 1) * 6], in_=vg[:, ti, cb * 512:(cb + 1) * 512]), r=["vg%d" % ti], w=["bst"])
            self.op("vector", lambda e: e.bn_aggr(out=bst[:, 24:26], in_=bst[:, 0:24]), r=["bst"], w=["bst"])
            self.op("scalar", lambda e: e.activation(out=bst[:, 26:27], in_=bst[:, 25:26], func=AF.Sqrt, scale=1.0, bias=EPS), r=["bst"], w=["bst"])
            self.op("vector", lambda e: e.reciprocal(out=bst[:, 27:28], in_=bst[:, 26:27]), r=["bst"], w=["bst"])
            vn, vk = c["vn"][ti % 2], "vn%d" % (ti % 2)
            for hf in range(2):
                tmp, tk = self.ntmp()
                self.op("vector", lambda e, ti=ti, hf=hf, tmp=tmp: e.tensor_scalar(out=tmp[:], in0=vg[:, ti, hf * 1024:(hf + 1) * 1024], scalar1=bst[:, 24:25], scalar2=bst[:, 27:28], op0=ALU.subtract, op1=ALU.mult),
                        r=["vg%d" % ti, "bst"], w=[tk])
                self.op("gpsimd", lambda e, hf=hf, tmp=tmp, vn=vn: e.tensor_tensor(out=vn[:, hf * 1024:(hf + 1) * 1024], in0=tmp[:], in1=c["GV"][:, hf * 1024:(hf + 1) * 1024], op=ALU.mult),
                        r=[tk, "GV"], w=[vk])
            for q4 in range(4):
                pa, pk = self.nA()
                for j in range(4):
                    cc = q4 * 4 + j
                    self.op("tensor", lambda e, j=j, cc=cc, pa=pa, vn=vn: e.matmul(pa[:, j * 128:(j + 1) * 128], lhsT=vn[:, cc * 128:(cc + 1) * 128], rhs=wsT[:, cc // 2, :], start=True, stop=True),
                            r=[vk, "wsT"], w=[pk])
                tmp, tk = self.ntmp()
                self.op("vector", lambda e, q4=q4, pa=pa, tmp=tmp: e.tensor_tensor(out=tmp[:, 0:512], in0=pa[:, :], in1=c["BS"][:, q4 * 512:(q4 + 1) * 512], op=ALU.add),
                        r=[pk, "BS"], w=[tk])
                self.op("gpsimd", lambda e, q4=q4, ti=ti, tmp=tmp: e.tensor_tensor(out=uT[:, q4 * 4:(q4 + 1) * 4, ti * 128:(ti + 1) * 128], in0=tmp[:, 0:512].rearrange("p (a b) -> p a b", a=4),
                                                                                    in1=uT[:, q4 * 4:(q4 + 1) * 4, ti * 128:(ti + 1) * 128], op=ALU.mult),
                        r=[tk, "uT"], w=["uT"])
        wo = w_out.rearrange("(kc p) n -> p kc n", p=128)
        wts = [self.load_w(wo[:, :, hf * 512:(hf + 1) * 512], 16, 512) for hf in range(2)]
        for ti in range(nt):
            py, pyk = self.nY()
            for hf in range(2):
                wt, wk = wts[hf]
                for kc in range(16):
                    self.op("tensor", lambda e, kc=kc, hf=hf, ti=ti, wt=wt, py=py: e.matmul(py[:, hf * 512:(hf + 1) * 512], lhsT=uT[:, kc, ti * 128:(ti + 1) * 128], rhs=wt[:, kc, :], start=(kc == 0), stop=(kc == 15)),
                            r=[wk, "uT"], w=[pyk])
            self.post_res(py, pyk, X[:, ti, :], "X%d" % ti, mods["GT1"])

    def mixer_out(self, X, nt, mTs, mkeys, w_out, KCn, GT):
        wo = w_out.rearrange("(kc p) n -> p kc n", p=128)
        wts = [self.load_w(wo[:, :, hf * 512:(hf + 1) * 512], KCn, 512) for hf in range(2)]
        nm = len(mTs)
        for ti in range(nt):
            py, pyk = self.nY()
            for hf in range(2):
                wt, wk = wts[hf]
                idx = 0
                for mi in range(nm):
                    for kc in range(KCn):
                        self.op("tensor", lambda e, kc=kc, hf=hf, ti=ti, wt=wt, py=py, mi=mi, idx=idx: e.matmul(py[:, hf * 512:(hf + 1) * 512], lhsT=mTs[mi][:, kc, ti * 128:(ti + 1) * 128], rhs=wt[:, kc, :], start=(idx == 0), stop=(idx == nm * KCn - 1)),
                                r=[wk, mkeys[mi]], w=[pyk])
                        idx += 1
            self.post_res(py, pyk, X[:, ti, :], "X%d" % ti, GT)

    def ffn(self, X, nt, c, GT, wg, wu, wd, FF, comb=None, first=True):
        ntok = nt * 128
        hT, Y2 = c["hT"], c["Y2"]
        nfb = (FF + 511) // 512
        wdv = wd.rearrange("(kc p) n -> p kc n", p=128)
        for fb in range(nfb):
            nch = min(4, FF // 128 - fb * 4)
            sg, sgk = c["sg"][fb % 2], "sg%d" % (fb % 2)
            gT, gk = c["gT"][fb % 2], "gT%d" % (fb % 2)
            chunks = list(range(fb * 4, fb * 4 + nch))
            def ev_g(ncx, ps, pk, sg=sg, sgk=sgk, fb=fb):
                self.op("scalar", lambda e: e.activation(out=sg[:, ncx - fb * 4, 0:ntok], in_=ps, func=AF.Silu), r=[pk], w=[sgk])
            self.lin_fm(wg, 8, hT, "hT", 0, ntok, chunks, ev_g)
            def ev_u(ncx, ps, pk, sg=sg, sgk=sgk, gT=gT, gk=gk, fb=fb):
                self.op("vector", lambda e: e.tensor_tensor(out=gT[:, ncx - fb * 4, 0:ntok], in0=sg[:, ncx - fb * 4, 0:ntok], in1=ps, op=ALU.mult), r=[pk, sgk], w=[gk])
            self.lin_fm(wu, 8, hT, "hT", 0, ntok, chunks, ev_u)
            wt, wk = self.load_w(wdv[:, fb * 4:fb * 4 + nch, :], nch, 1024)
            for ti in range(nt):
                py, pyk = self.nY()
                for hf in range(2):
                    for kc in range(nch):
                        self.op("tensor", lambda e, kc=kc, hf=hf, ti=ti, wt=wt, py=py, gT=gT: e.matmul(py[:, hf * 512:(hf + 1) * 512], lhsT=gT[:, kc, ti * 128:(ti + 1) * 128], rhs=wt[:, kc, hf * 512:(hf + 1) * 512], start=(kc == 0), stop=(kc == nch - 1)),
                                r=[wk, gk], w=[pyk])
                yk = "Y2_%d" % ti
                if comb is None:
                    if first and fb == 0:
                        self.op("scalar", lambda e, ti=ti, py=py: e.copy(out=Y2[:, ti, :], in_=py[:, :]), r=[pyk], w=[yk, "uT"])
                    else:
                        self.op("vector", lambda e, ti=ti, py=py: e.tensor_tensor(out=Y2[:, ti, :], in0=py[:, :], in1=Y2[:, ti, :], op=ALU.add), r=[pyk, yk], w=[yk])
                else:
                    cap, ck = comb
                    if first and fb == 0:
                        self.op("vector", lambda e, ti=ti, py=py: e.tensor_scalar(out=Y2[:, ti, :], in0=py[:, :], scalar1=cap(ti), scalar2=None, op0=ALU.mult), r=[pyk, ck], w=[yk, "uT"])
                    else:
                        self.op("vector", lambda e, ti=ti, py=py: e.scalar_tensor_tensor(out=Y2[:, ti, :], in0=py[:, :], scalar=cap(ti), in1=Y2[:, ti, :], op0=ALU.mult, op1=ALU.add), r=[pyk, yk, ck], w=[yk])

    def ffn_post(self, X, nt, c, GT):
        for ti in range(nt):
            self.post_res_sb(c["Y2"][:, ti, :], "Y2_%d" % ti, X[:, ti, :], "X%d" % ti, GT)

    def out_block(self, X, nt, xo_rows, ho_rows=None, Gn=None, SHn=None):
        for ti in range(nt):
            if ho_rows is not None:
                hb, hk = self.nh()
                self.norm_mod(X[:, ti, :], "X%d" % ti, Gn, SHn, hb[:], hk)
                self.dma("sync", ho_rows[ti], hb[:], r=[hk])
            if xo_rows is not None:
                self.dma("sync", xo_rows[ti], X[:, ti, :], r=["X%d" % ti])

def build_S0(n_lat_tiles=32, TB=512):
    P = Tok(TB)
    NT = P.NT
    NTOK = n_lat_tiles * 128
    dI = lambda n, s, dt=F32: P.dram(n, s, dt, "ExternalInput")
    dO = lambda n, s, dt=F32: P.dram(n, s, dt, "ExternalOutput")
    x_d, pos_d, ctx_d = dI("x", [NTOK, D]), dI("pos", [NTOK, D]), dI("ctx", [128, D])
    c_d, cc_d = dI("c", [128, 8]), dI("c_ctx", [128, 8])
    aw0, ab0, ng0 = dI("ada_w0", [D, 6 * D]), dI("ada_b0", [6 * D]), dI("ng0", [4 * D])
    aw1, ab1, ng1 = dI("ada_w1", [D, 6 * D]), dI("ada_b1", [6 * D]), dI("ng1", [4 * D])
    g_win, g_gv, g_wsT, g_bs, g_wout = dI("g_win", [D, 4096]), dI("g_gv", [2048]), dI("g_wsT", [128, 1024]), dI("g_bs", [2048]), dI("g_wout", [2048, D])
    f_wg, f_wu, f_wd = dI("f_wg", [D, 2816]), dI("f_wu", [D, 2816]), dI("f_wd", [2816, D])
    xo, ho = dO("xo", [NTOK, D]), dO("ho", [NTOK, D], BF16)
    xco, hco = dO("xco", [128, D]), dO("hco", [128, D], BF16)
    X = P.sb("X", [128, NT, D], F32)
    c = dict(hT=P.sb("hT", [128, 8, TB], BF16), uT=None, vg=P.sb("vg", [128, NT, 2048], BF16),
             vn=[P.sb("vn%d" % i, [128, 2048], BF16) for i in range(2)], bst=P.sb("bst", [128, 32], F32),
             Y2=None, sg=[P.sb("sg%d" % i, [128, 4, TB], BF16) for i in range(2)],
             gT=[P.sb("gT%d" % i, [128, 4, TB], BF16) for i in range(2)],
             w_in=g_win, w_out=g_wout)
    uY = P.sb("uY", [128, NT * D], F32)
    c["Y2"] = uY[:].rearrange("p (a b) -> p a b", a=NT)
    c["uT"] = uY[:].bitcast(BF16).rearrange("p (a b) -> p a b", a=16)
    c["GV"] = P.sb("GV", [128, 2048], F32)
    c["BS"] = P.sb("BS", [128, 2048], F32)
    P.bc_dram(c["GV"][:], "GV", g_gv, 0, 2048)
    P.bc_dram(c["BS"][:], "BS", g_bs, 0, 2048)
    wsT = P.sb("wsT", [128, 8, 128], BF16)
    P.dma("gpsimd", wsT[:].rearrange("p a b -> p (a b)"), g_wsT, w=["wsT"])
    c["wsT"] = wsT

    def run_pass(pref, cvec, blocks):
        m = P.ada_mod("m", cvec, aw0, ab0, ng0, [(0, "SH1", "raw", 0), (1, "G1", "mul1p", 0), (2, "GT1", "mul", 1),
                                                  (3, "SH2", "raw", 0), (4, "G2", "mul1p", 2), (5, "GT2", "mul", 3)])
        mn = P.ada_mod("n", cvec, aw1, ab1, ng1, [(0, "SH1", "raw", 0), (1, "G1", "mul1p", 0)])
        for (srcs, poss, xos, hos) in blocks:
            nt = len(srcs)
            P.load_block(X, srcs, poss)
            P.pre_T(X, nt, m["G1"], m["SH1"], c["hT"])
            P.gmlp(X, nt, c, m)
            P.pre_T(X, nt, m["G2"], m["SH2"], c["hT"])
            P.ffn(X, nt, c, m["GT2"], f_wg, f_wu, f_wd, 2816)
            P.ffn_post(X, nt, c, m["GT2"])
            P.out_block(X, nt, xos, hos, mn["G1"], mn["SH1"])

    rows = lambda d, t: d[t * 128:(t + 1) * 128, :]
    blocks = []
    for b in range(n_lat_tiles // NT):
        ts = list(range(b * NT, (b + 1) * NT))
        blocks.append(([rows(x_d, t) for t in ts], [rows(pos_d, t) for t in ts], [rows(xo, t) for t in ts], [rows(ho, t) for t in ts]))
    run_pass("l", c_d, blocks)
    run_pass("c", cc_d, [([ctx_d], None, [xco], [hco])])
    return P

def tok_router(P, nt, c):
    h32, hT32, wr32, lg, mx8, comb, identf = c["h32"], c["hT32"], c["wr32"], c["lg"], c["mx8"], c["comb"], c["identf"]
    for ti in range(nt):
        py, pyk = P.nY()
        pv = py[:, :].rearrange("p (a b) -> p a b", a=8)
        for kc in range(8):
            P.op("tensor", lambda e, kc=kc, ti=ti, pv=pv: e.transpose(out=pv[:, kc, :], in_=h32[:, ti, kc * 128:(kc + 1) * 128], identity=identf[:]),
                 r=["h32_%d" % ti, "identf"], w=[pyk])
        P.op("vector", lambda e, pv=pv: e.tensor_copy(out=hT32[:], in_=pv), r=[pyk], w=["hT32"])
        pa, pk = P.nA()
        for kc in range(8):
            P.op("tensor", lambda e, kc=kc, pa=pa: e.matmul(pa[:, 0:8], lhsT=hT32[:, kc, :], rhs=wr32[:, kc, :], start=(kc == 0), stop=(kc == 7)),
                 r=["hT32", "wr32"], w=[pk])
        P.op("vector", lambda e, pa=pa: e.tensor_copy(out=lg[:, 0:8], in_=pa[:, 0:8]), r=[pk], w=["lg"])
        P.op("vector", lambda e: e.max(out=mx8[:, 0:8], in_=lg[:, 0:8]), r=["lg"], w=["mx8"])
        P.op("vector", lambda e: e.tensor_tensor(out=mx8[:, 8:9], in0=mx8[:, 0:1], in1=mx8[:, 1:2], op=ALU.subtract), r=["mx8"], w=["mx8"])
        P.op("scalar", lambda e: e.activation(out=mx8[:, 9:10], in_=mx8[:, 8:9], func=AF.Sigmoid, scale=1.0), r=["mx8"], w=["mx8"])
        P.op("scalar", lambda e: e.activation(out=mx8[:, 10:11], in_=mx8[:, 8:9], func=AF.Sigmoid, scale=-1.0), r=["mx8"], w=["mx8"])
        P.op("vector", lambda e: e.tensor_scalar(out=lg[:, 8:16], in0=lg[:, 0:8], scalar1=mx8[:, 0:1], scalar2=mx8[:, 9:10], op0=ALU.is_equal, op1=ALU.mult), r=["lg", "mx8"], w=["lg"])
        P.op("vector", lambda e: e.tensor_scalar(out=lg[:, 16:24], in0=lg[:, 0:8], scalar1=mx8[:, 1:2], scalar2=mx8[:, 10:11], op0=ALU.is_equal, op1=ALU.mult), r=["lg", "mx8"], w=["lg"])
        P.op("vector", lambda e, ti=ti: e.tensor_tensor(out=comb[:, ti, :], in0=lg[:, 8:16], in1=lg[:, 16:24], op=ALU.add), r=["lg"], w=["comb"])


def build_TOK2(kind, n_tiles=32, TB=512):
    P = Tok(TB)
    NT = P.NT
    NTOK = n_tiles * 128
    dI = lambda n, s, dt=F32: P.dram(n, s, dt, "ExternalInput")
    dO = lambda n, s, dt=F32: P.dram(n, s, dt, "ExternalOutput")
    moe = kind in ("S2", "S5")
    has_next = kind in ("S2", "S4")
    x_d = dI("x", [NTOK, D])
    c_d = dI("c", [128, 8])
    aw, ab, ng = dI("ada_w", [D, 6 * D]), dI("ada_b", [6 * D]), dI("ng", [4 * D])
    if has_next:
        awn, abn, ngn = dI("ada_wn", [D, 6 * D]), dI("ada_bn", [6 * D]), dI("ngn", [4 * D])
        ho = dO("ho", [NTOK, D], BF16)
    xo = dO("xo", [NTOK, D])
    w_out = dI("w_out", [D, D])
    FF = 3584 if moe else 2816
    if moe:
        wr_d = dI("w_router", [128, 64])
        wg_d, wu_d, wd_d = dI("wg", [8, D, FF]), dI("wu", [8, D, FF]), dI("wd", [8, FF, D])
    else:
        wg_d, wu_d, wd_d = dI("wg", [D, FF]), dI("wu", [D, FF]), dI("wd", [FF, D])
    X = P.sb("X", [128, NT, D], F32)
    c = dict(hT=P.sb("hT", [128, 8, TB], BF16), Y2=P.sb("Y2", [128, NT, D], F32),
             sg=[P.sb("sg%d" % i, [128, 4, TB], BF16) for i in range(2)],
             gT=[P.sb("gT%d" % i, [128, 4, TB], BF16) for i in range(2)])
    if moe:
        c["h32"] = P.sb("h32", [128, NT, D], F32)
        c["hT32"] = P.sb("hT32", [128, 8, 128], F32)
        c["wr32"] = P.sb("wr32", [128, 8, 8], F32)
        c["lg"] = P.sb("lg", [128, 24], F32)
        c["mx8"] = P.sb("mx8", [128, 16], F32)
        c["comb"] = P.sb("comb", [128, NT, 8], F32)
        c["identf"] = P.sb("identf", [128, 128], F32)
        P.dma("sync", c["identf"][:], P.ident_d, w=["identf"])
        P.dma("sync", c["wr32"][:].rearrange("p a b -> p (a b)"), wr_d, w=["wr32"])
    if kind == "S2":
        mf_d, mb_d = dI("mf", [D, NTOK], BF16), dI("mb", [D, NTOK], BF16)
        mTs = [P.sb("mT0", [128, 8, TB], BF16), P.sb("mT1", [128, 8, TB], BF16)]
        msrc = [mf_d, mb_d]
    elif kind == "S4":
        mf_d = dI("mf", [D, NTOK], BF16)
        mTs = [P.sb("mT0", [128, 8, TB], BF16)]
        msrc = [mf_d]
    else:
        HW = 8
        h3_d = dI("h3T", [D, NTOK + 2 * HW], BF16)
        pw_in = dI("p_win", [D, D])
        pwg_d = dI("p_wg", [4, 256, 256])
        pbg_d, psc_d = dI("p_bg", [128, 8]), dI("p_scale", [128, 8])
        pfix_d = dI("p_fix", [128, 64])
        hTh = P.sb("hTh", [128, 8, TB + 2 * HW], BF16)
        pT = P.sb("pT", [128, TB + 2 * HW], F32)
        sA = P.sb("sA", [128, TB + 2 * HW], F32)
        sB = P.sb("sB", [128, TB + 2 * HW], F32)
        qT = P.sb("qT", [128, 8, TB], BF16)
        yT = P.sb("yT", [128, 8, TB], BF16)
        wgs = P.sb("wgs", [128, 4, 2, 256], BF16)
        P.dma("gpsimd", wgs[:].rearrange("p g c d -> p (g c) d"), pwg_d.rearrange("g (cc p) d -> p (g cc) d", p=128), w=["wgs"])
        pbg, psc, pfix = P.sb("pbg", [128, 8], F32), P.sb("psc", [128, 8], F32), P.sb("pfix", [128, 4, 16], F32)
        P.dma("sync", pbg[:], pbg_d, w=["pbg"])
        P.dma("sync", psc[:], psc_d, w=["psc"])
        P.dma("sync", pfix[:].rearrange("p a b -> p (a b)"), pfix_d, w=["pfix"])

    segs = [(2, "GT1", "mul", 1), (3, "SH2", "raw", 0), (4, "G2", "mul1p", 2), (5, "GT2", "mul", 3)]
    m = P.ada_mod("m", c_d, aw, ab, ng, segs)
    if has_next:
        mn = P.ada_mod("n", c_d, awn, abn, ngn, [(0, "SH1", "raw", 0), (1, "G1", "mul1p", 0)])
    rows = lambda d, t: d[t * 128:(t + 1) * 128, :]
    nblk = n_tiles // NT
    for b in range(nblk):
        ts = list(range(b * NT, (b + 1) * NT))
        nt = NT
        t0 = b * TB
        P.load_block(X, [rows(x_d, t) for t in ts], None)
        if kind in ("S2", "S4"):
            for mi, src in enumerate(msrc):
                P.dma("sync", mTs[mi][:], src.rearrange("(kc p) t -> p kc t", p=128)[:, :, t0:t0 + TB], w=["mT%d" % mi])
            P.mixer_out(X, nt, mTs, ["mT%d" % i for i in range(len(mTs))], w_out, 8, m["GT1"])
        else:
            NH = TB + 2 * HW
            P.dma("sync", hTh[:], h3_d.rearrange("(kc p) t -> p kc t", p=128)[:, :, t0:t0 + NH], w=["hTh"])
            wv = pw_in.rearrange("(kc p) n -> p kc n", p=128)
            for cg in range(2):
                wt, wk = P.load_w(wv[:, :, cg * 512:(cg + 1) * 512], 8, 512)
                for j in range(4):
                    cc = cg * 4 + j
                    win = (2, 4, 8, 16)[cc // 2]
                    for hf in range(2):
                        pa, pk = P.nA()
                        c0 = hf * (NH // 2)
                        for kc in range(8):
                            P.op("tensor", lambda e, kc=kc, j=j, wt=wt, pa=pa, c0=c0: e.matmul(pa[:, 0:NH // 2], lhsT=wt[:, kc, j * 128:(j + 1) * 128], rhs=hTh[:, kc, c0:c0 + NH // 2], start=(kc == 0), stop=(kc == 7)),
                                 r=[wk, "hTh"], w=[pk])
                        P.op("scalar", lambda e, pa=pa, c0=c0: e.copy(out=pT[:, c0:c0 + NH // 2], in_=pa[:, 0:NH // 2]), r=[pk], w=["pT"])
                    P.op("vector", lambda e: e.tensor_tensor(out=sA[:, 1:NH], in0=pT[:, 0:NH - 1], in1=pT[:, 1:NH], op=ALU.add), r=["pT", "sB"], w=["sA"])
                    cur, ck, oth, ok = sA, "sA", sB, "sB"
                    sh = 1
                    lo, hi = 1, NH
                    while sh * 2 < win:
                        l2, h2 = lo + sh, hi - sh
                        P.op("vector", lambda e, cur=cur, oth=oth, sh=sh, l2=l2, h2=h2: e.tensor_tensor(out=oth[:, l2:h2], in0=cur[:, l2 - sh:h2 - sh], in1=cur[:, l2 + sh:h2 + sh], op=ALU.add),
                             r=[ck], w=[ok])
                        cur, ck, oth, ok = oth, ok, cur, ck
                        lo, hi = l2, h2
                        sh *= 2
                    wi = cc // 2
                    P.op("vector", lambda e, cur=cur, win=win: e.tensor_scalar(out=cur[:, HW:HW + TB], in0=cur[:, HW:HW + TB], scalar1=1.0 / win, scalar2=None, op0=ALU.mult), r=[ck], w=[ck])
                    if b == 0:
                        P.op("vector", lambda e, cur=cur, wi=wi: e.tensor_tensor(out=cur[:, HW:HW + 8], in0=cur[:, HW:HW + 8], in1=pfix[:, wi, 0:8], op=ALU.mult), r=[ck, "pfix"], w=[ck])
                    if b == nblk - 1:
                        P.op("vector", lambda e, cur=cur, wi=wi: e.tensor_tensor(out=cur[:, HW + TB - 8:HW + TB], in0=cur[:, HW + TB - 8:HW + TB], in1=pfix[:, wi, 8:16], op=ALU.mult), r=[ck, "pfix"], w=[ck])
                    P.op("vector", lambda e, cur=cur, cc=cc: e.tensor_tensor(out=qT[:, cc, :], in0=cur[:, HW:HW + TB], in1=pT[:, HW:HW + TB], op=ALU.subtract), r=[ck, "pT"], w=["qT", "sA", "sB"])
            for cc in range(8):
                g = cc // 2
                pa, pk = P.nA()
                for ci in range(2):
                    P.op("tensor", lambda e, ci=ci, g=g, cc=cc, pa=pa: e.matmul(pa[:, :], lhsT=wgs[:, g, ci, (cc % 2) * 128:(cc % 2 + 1) * 128], rhs=qT[:, 2 * g + ci, :], start=(ci == 0), stop=(ci == 1)),
                         r=["wgs", "qT"], w=[pk])
                P.op("vector", lambda e, cc=cc, pa=pa: e.tensor_scalar(out=yT[:, cc, :], in0=pa[:, :], scalar1=pbg[:, cc:cc + 1], scalar2=psc[:, cc:cc + 1], op0=ALU.add, op1=ALU.mult), r=[pk, "pbg", "psc"], w=["yT"])
            P.mixer_out(X, nt, [yT], ["yT"], w_out, 8, m["GT1"])
        if moe:
            P.pre_T(X, nt, m["G2"], m["SH2"], c["hT"], want32=(c["h32"], "h32_"))
            tok_router(P, nt, c)
            for ex in range(8):
                comb = (lambda ti, ex=ex: c["comb"][:, ti, ex:ex + 1], "comb")
                P.ffn(X, nt, c, m["GT2"], wg_d[ex], wu_d[ex], wd_d[ex], FF, comb=comb, first=(ex == 0))
        else:
            P.pre_T(X, nt, m["G2"], m["SH2"], c["hT"])
            P.ffn(X, nt, c, m["GT2"], wg_d, wu_d, wd_d, FF)
        P.ffn_post(X, nt, c, m["GT2"])
        if has_next:
            P.out_block(X, nt, [rows(xo, t) for t in ts], [rows(ho, t) for t in ts], mn["G1"], mn["SH1"])
        else:
            P.out_block(X, nt, [rows(xo, t) for t in ts])
    return P

def build_S1(L=16384, CTX=256):
    P = Prog()
    SEQP = 2 + CTX + 4 + L + 2
    dI = lambda n, s, dt=F32: P.dram(n, s, dt, "ExternalInput")
    hseq = dI("hseq", [4, D, SEQP], BF16)
    wg_d, wx_d = dI("w_gate", [D, 128]), dI("w_xb", [D, 128])
    cw_d, cb_d = dI("conv_w", [4 * 128]), dI("conv_b", [128, 1])
    wa_d, wxx_d = dI("w_a", [2, 128, 128]), dI("w_x", [2, 128, 128])
    ba_d, bx_d, lam_d = dI("b_a", [128, 2]), dI("b_x", [128, 2]), dI("lam", [128, 2])
    m_d = P.dram("m", [4, 128, L], BF16, "ExternalOutput")
    Wg = P.sb("Wg", [128, 8, 128], BF16)
    Wx = P.sb("Wx", [128, 8, 128], BF16)
    Wxk = P.sb("Wxk", [128, 8, 4, 128], BF16)
    cw = P.sb("cw", [128, 4, 128], F32)
    cb = P.sb("cb", [128, 1], F32)
    wa = P.sb("wa", [128, 2, 128], BF16)
    wxx = P.sb("wxx", [128, 2, 128], BF16)
    ba, bx, lam, cl = P.sb("ba", [128, 2], F32), P.sb("bx", [128, 2], F32), P.sb("lam_s", [128, 2], F32), P.sb("cl", [128, 2], F32)
    st = P.sb("st", [128, 1], F32)
    P.dma("gpsimd", Wg[:], wg_d.rearrange("(kc p) n -> p kc n", p=128), w=["Wg"])
    P.dma("gpsimd", Wx[:], wx_d.rearrange("(kc p) n -> p kc n", p=128), w=["Wx"])
    P.dma("sync", cw[:].rearrange("p a b -> p (a b)"), bass.AP(tensor=cw_d.tensor, offset=0, ap=[[0, 128], [1, 512]]), w=["cw"])
    P.dma("sync", cb[:], cb_d, w=["cb"])
    P.dma("gpsimd", wa[:], wa_d.rearrange("d i j -> i d j"), w=["wa"])
    P.dma("gpsimd", wxx[:], wxx_d.rearrange("d i j -> i d j"), w=["wxx"])
    P.dma("sync", ba[:], ba_d, w=["ba"])
    P.dma("sync", bx[:], bx_d, w=["bx"])
    P.dma("sync", lam[:], lam_d, w=["lam"])
    for k in range(4):
        for kc in range(8):
            P.op("vector", lambda e, k=k, kc=kc: e.tensor_tensor(out=Wxk[:, kc, k, :], in0=Wx[:, kc, :], in1=cw[:, k, :], op=ALU.mult), r=["Wx", "cw"], w=["Wxk"])
    P.op("scalar", lambda e: e.activation(out=cl[:], in_=lam[:], func=AF.Exp, scale=-1.0), r=["lam"], w=["cl"])
    P.op("scalar", lambda e: e.activation(out=cl[:], in_=cl[:], func=AF.Ln, scale=1.0, bias=1.0), r=["cl"], w=["cl"])
    P.op("vector", lambda e: e.tensor_scalar(out=cl[:], in0=cl[:], scalar1=-8.0, scalar2=None, op0=ALU.mult), r=["cl"], w=["cl"])
    ps = [P.ps("ps%d" % i, [128, 512], F32) for i in range(6)]
    pi = [0]
    def nps():
        i = pi[0] % 6
        pi[0] += 1
        return ps[i], "ps%d" % i
    NB = 2
    hc = [P.sb("hc%d" % i, [128, 8, 516], BF16) for i in range(NB)]
    bufs = {}
    for nm, dt in (("xc32", F32), ("xcb", BF16), ("r", F32), ("ii", F32), ("a", F32), ("sq", F32), ("bb", F32), ("hs", F32), ("t1", F32), ("mo", BF16)):
        bufs[nm] = [P.sb("%s%d" % (nm, i), [128, 512], dt) for i in range(NB)]
    ci = 0
    for s in range(4):
        d = s // 2
        P.op("vector", lambda e: e.memset(st[:], 0.0), r=[], w=["st"])
        chunks = [(2, CTX, False, 0)] + [(2 + CTX + 4 + t0, min(512, L - t0), True, t0) for t0 in range(0, L, 512)]
        for (c0, n, is_lat, t0) in chunks:
            sl = ci % NB
            ci += 1
            B = {k: (v[sl], "%s%d" % (k, sl)) for k, v in bufs.items()}
            h, hk = hc[sl], "hc%d" % sl
            P.dma("sync", h[:, :, 0:n + 4], hseq[s].rearrange("(kc p) t -> p kc t", p=128)[:, :, c0 - 2:c0 + n + 2], w=[hk])
            px, pxk = nps()
            idx = 0
            for k in range(4):
                off = (k - 2) if d == 0 else (2 - k)
                for kc in range(8):
                    P.op("tensor", lambda e, k=k, kc=kc, off=off, px=px, h=h, n=n, idx=idx: e.matmul(px[:, 0:n], lhsT=Wxk[:, kc, k, :], rhs=h[:, kc, 2 + off:2 + off + n], start=(idx == 0), stop=(idx == 31)),
                         r=["Wxk", hk], w=[pxk])
                    idx += 1
            xc32, xk = B["xc32"]
            xcb, xbk = B["xcb"]
            P.op("scalar", lambda e, px=px, xc32=xc32, n=n: e.activation(out=xc32[:, 0:n], in_=px[:, 0:n], func=AF.Identity, bias=cb[:, 0:1], scale=1.0), r=[pxk, "cb"], w=[xk])
            P.op("vector", lambda e, xc32=xc32, xcb=xcb, n=n: e.tensor_copy(out=xcb[:, 0:n], in_=xc32[:, 0:n]), r=[xk], w=[xbk])
            pr, prk = nps()
            P.op("tensor", lambda e, pr=pr, xcb=xcb, n=n, d=d: e.matmul(pr[:, 0:n], lhsT=wa[:, d, :], rhs=xcb[:, 0:n], start=True, stop=True), r=["wa", xbk], w=[prk])
            pI, pik = nps()
            P.op("tensor", lambda e, pI=pI, xcb=xcb, n=n, d=d: e.matmul(pI[:, 0:n], lhsT=wxx[:, d, :], rhs=xcb[:, 0:n], start=True, stop=True), r=["wxx", xbk], w=[pik])
            r_, rk = B["r"]
            i_, ik = B["ii"]
            a_, ak = B["a"]
            sq, sqk = B["sq"]
            bb, bbk = B["bb"]
            hs, hsk = B["hs"]
            P.op("scalar", lambda e, pr=pr, r_=r_, n=n, d=d: e.activation(out=r_[:, 0:n], in_=pr[:, 0:n], func=AF.Sigmoid, bias=ba[:, d:d + 1], scale=1.0), r=[prk, "ba"], w=[rk])
            P.op("scalar", lambda e, pI=pI, i_=i_, n=n, d=d: e.activation(out=i_[:, 0:n], in_=pI[:, 0:n], func=AF.Sigmoid, bias=bx[:, d:d + 1], scale=1.0), r=[pik, "bx"], w=[ik])
            P.op("scalar", lambda e, r_=r_, a_=a_, n=n, d=d: e.activation(out=a_[:, 0:n], in_=r_[:, 0:n], func=AF.Exp, scale=cl[:, d:d + 1]), r=[rk, "cl"], w=[ak])
            P.op("scalar", lambda e, a_=a_, sq=sq, n=n: e.activation(out=sq[:, 0:n], in_=a_[:, 0:n], func=AF.Square), r=[ak], w=[sqk])
            P.op("scalar", lambda e, sq=sq, n=n: e.activation(out=sq[:, 0:n], in_=sq[:, 0:n], func=AF.Sqrt, scale=-1.0, bias=1.0), r=[sqk], w=[sqk])
            P.op("vector", lambda e, i_=i_, xc32=xc32, bb=bb, n=n: e.tensor_tensor(out=bb[:, 0:n], in0=i_[:, 0:n], in1=xc32[:, 0:n], op=ALU.mult), r=[ik, xk], w=[bbk])
            P.op("vector", lambda e, sq=sq, bb=bb, n=n: e.tensor_tensor(out=bb[:, 0:n], in0=bb[:, 0:n], in1=sq[:, 0:n], op=ALU.mult), r=[sqk, bbk], w=[bbk])
            P.op("vector", lambda e, a_=a_, bb=bb, hs=hs, n=n: e.tensor_tensor_scan(out=hs[:, 0:n], data0=a_[:, 0:n], data1=bb[:, 0:n], initial=st[:, 0:1], op0=ALU.mult, op1=ALU.add), r=[ak, bbk, "st"], w=[hsk])
            P.op("vector", lambda e, hs=hs, n=n: e.tensor_copy(out=st[:, 0:1], in_=hs[:, n - 1:n]), r=[hsk], w=["st"])
            if is_lat:
                pg, pgk = nps()
                for kc in range(8):
                    P.op("tensor", lambda e, kc=kc, pg=pg, h=h, n=n: e.matmul(pg[:, 0:n], lhsT=Wg[:, kc, :], rhs=h[:, kc, 2:2 + n], start=(kc == 0), stop=(kc == 7)), r=["Wg", hk], w=[pgk])
                t1, t1k = B["t1"]
                mo, mok = B["mo"]
                g = pg[:, 0:n]
                t = t1[:, 0:n]
                P.op("scalar", lambda e, t=t, g=g: e.activation(out=t, in_=g, func=AF.Square), r=[pgk], w=[t1k])
                P.op("vector", lambda e, t=t: e.tensor_scalar(out=t, in0=t, scalar1=0.044715, scalar2=1.0, op0=ALU.mult, op1=ALU.add), r=[t1k], w=[t1k])
                P.op("vector", lambda e, t=t, g=g: e.tensor_tensor(out=t, in0=t, in1=g, op=ALU.mult), r=[t1k, pgk], w=[t1k])
                P.op("scalar", lambda e, t=t: e.activation(out=t, in_=t, func=AF.Sigmoid, scale=1.5957691216057308), r=[t1k], w=[t1k])
                P.op("vector", lambda e, t=t, g=g: e.tensor_tensor(out=t, in0=t, in1=g, op=ALU.mult), r=[t1k, pgk], w=[t1k])
                P.op("vector", lambda e, t=t, hs=hs, mo=mo, n=n: e.tensor_tensor(out=mo[:, 0:n], in0=t, in1=hs[:, 0:n], op=ALU.mult), r=[t1k, hsk], w=[mok])
                P.dma("sync", m_d[s][:, t0:t0 + n], mo[:, 0:n], r=[mok])
    return P

def build_S3a(n_tiles=32):
    P = Prog()
    NTOK = n_tiles * 128
    dI = lambda n, s, dt=F32: P.dram(n, s, dt, "ExternalInput")
    hT_d = dI("hT", [D, NTOK + 2], BF16)
    w_d, cw_d, cb_d = dI("w_in", [D, 3072]), dI("conv_w", [3 * 3072]), dI("conv_b", [3072])
    z_d = P.dram("z", [NTOK, 3072], F32, "ExternalOutput")
    hT = P.sb("hT_s", [128, 8, NTOK + 2], BF16)
    P.dma("sync", hT[:], hT_d.rearrange("(kc p) t -> p kc t", p=128), w=["hT"])
    W = [P.sb("W%d" % i, [128, 8, 512], BF16) for i in range(2)]
    Wk = [P.sb("Wk%d" % i, [128, 3, 8, 512], BF16) for i in range(2)]
    cwb = [P.sb("cwb%d" % i, [128, 3, 512], F32) for i in range(2)]
    bb = [P.sb("bb%d" % i, [128, 512], F32) for i in range(2)]
    zo = [P.sb("zo%d" % i, [128, 512], F32) for i in range(3)]
    ps = [P.ps("ps%d" % i, [128, 512], F32) for i in range(4)]
    wv = w_d.rearrange("(kc p) n -> p kc n", p=128)
    n = 0
    for cb in range(6):
        s = cb % 2
        P.dma("gpsimd", W[s][:], wv[:, :, cb * 512:(cb + 1) * 512], w=["W%d" % s])
        for k in range(3):
            P.dma("sync", cwb[s][:, k, :], bass.AP(tensor=cw_d.tensor, offset=k * 3072 + cb * 512, ap=[[0, 128], [1, 512]]), w=["cwb%d_%d" % (s, k)])
        P.dma("sync", bb[s][:], bass.AP(tensor=cb_d.tensor, offset=cb * 512, ap=[[0, 128], [1, 512]]), w=["bb%d" % s])
        for k in range(3):
            for kc in range(8):
                eng = "vector" if (kc % 2 == 0) else "gpsimd"
                P.op(eng, lambda e, s=s, k=k, kc=kc: e.tensor_tensor(out=Wk[s][:, k, kc, :], in0=W[s][:, kc, :], in1=cwb[s][:, k, :], op=ALU.mult),
                     r=["W%d" % s, "cwb%d_%d" % (s, k)], w=["Wk%d_%d" % (s, k)])
        for ti in range(n_tiles):
            pa, pk = ps[n % 4], "ps%d" % (n % 4)
            zt, zk = zo[n % 3], "zo%d" % (n % 3)
            n += 1
            idx = 0
            for k in range(3):
                for kc in range(8):
                    P.op("tensor", lambda e, s=s, k=k, kc=kc, ti=ti, pa=pa, idx=idx: e.matmul(pa[:, :], lhsT=hT[:, kc, ti * 128 + k:ti * 128 + k + 128], rhs=Wk[s][:, k, kc, :], start=(idx == 0), stop=(idx == 23)),
                         r=["hT", "Wk%d_%d" % (s, k)], w=[pk])
                    idx += 1
            P.op("vector", lambda e, pa=pa, zt=zt, s=s: e.tensor_tensor(out=zt[:], in0=pa[:, :], in1=bb[s][:], op=ALU.add), r=[pk, "bb%d" % s], w=[zk])
            P.dma("sync", z_d[ti * 128:(ti + 1) * 128, cb * 512:(cb + 1) * 512], zt[:], r=[zk])
    return P


def build_S3b(L=16384, NCH=128):
    P = Prog()
    NB = L // 128
    NC2 = 2 * NB
    L2 = 2 * L
    NBLK = L2 // 512
    dI = lambda n, s, dt=F32: P.dram(n, s, dt, "ExternalInput")
    vrev_d, vnat_d = dI("vrev", [NCH, 128, NC2]), dI("vnat", [NCH, 128, NC2])
    x1_d, x2_d = dI("x1nat", [NCH, 128, NC2]), dI("x2rev", [NCH, 128, NC2])
    zT_d = [dI("zT1", [33, L2]), dI("zT2", [33, L2])]
    win_d = [dI("win1", [128, L2]), dI("win2", [128, L2])]
    w1_d, w2_d, w3_d = dI("f_w1", [33, 64]), dI("f_w2", [64, 64]), dI("f_w3", [64, 64])
    fb_d, fr_d = dI("f_b", [64, 3]), dI("f_freq", [64, 3])
    wo_d = dI("f_wout", [64, 4 * 128])
    hb_d = dI("hbias", [2 * 128])
    J_d = dI("J", [128, 128])
    y2_d = P.dram("y2rev", [NCH, 128, NC2], BF16, "ExternalOutput")
    Ktab = [P.dram("Ktab%d" % i, [128, L2], BF16, "Internal") for i in range(2)]
    Y1d = P.dram("Y1d", [NCH, 128, NC2], BF16, "Internal")
    w1, w2, w3 = P.sb("w1", [33, 64], F32), P.sb("w2", [64, 64], F32), P.sb("w3", [64, 64], F32)
    fb, fr, wo = P.sb("fb", [64, 3], F32), P.sb("fr", [64, 3], F32), P.sb("wo", [64, 4, 128], F32)
    hbias = P.sb("hbias_s", [128, 2, 128], F32)
    J = P.sb("J_s", [128, 128], BF16)
    for t, d_, k in ((w1, w1_d, "w1"), (w2, w2_d, "w2"), (w3, w3_d, "w3"), (fb, fb_d, "fb"), (fr, fr_d, "fr")):
        P.dma("sync", t[:], d_, w=[k])
    P.dma("sync", wo[:].rearrange("p a b -> p (a b)"), wo_d, w=["wo"])
    P.dma("sync", hbias[:].rearrange("p a b -> p (a b)"), bass.AP(tensor=hb_d.tensor, offset=0, ap=[[0, 128], [1, 256]]), w=["hbias"])
    P.dma("gpsimd", J[:], J_d, w=["J"])
    ps = [P.ps("ps%d" % i, [128, 512], F32) for i in range(8)]
    pi = [0]
    def nps():
        i = pi[0] % 8
        pi[0] += 1
        return ps[i], "ps%d" % i
    zt = [P.sb("zt%d" % i, [33, 512], F32) for i in range(2)]
    wn = [P.sb("wn%d" % i, [128, 512], F32) for i in range(2)]
    hd = [P.sb("hd%d" % i, [64, 512], F32) for i in range(2)]
    wi = [P.sb("wi%d" % i, [64, 512], I32) for i in range(2)]
    g1 = [P.sb("g1_%d" % i, [64, 512], F32) for i in range(2)]
    kb = [P.sb("kb%d" % i, [128, 512], BF16) for i in range(2)]
    TWO_PI = 6.283185307179586
    n = 0
    for tab in range(2):
        for blk in range(NBLK):
            s = n % 2
            n += 1
            m0 = blk * 512
            P.dma("sync", zt[s][:], zT_d[tab][:, m0:m0 + 512], w=["zt%d" % s])
            P.dma("sync", wn[s][:], win_d[tab][:, m0:m0 + 512], w=["wn%d" % s])
            src, skey, W_ = zt[s], "zt%d" % s, [w1, w2, w3]
            for ly in range(3):
                pa, pk = nps()
                nk = 33 if ly == 0 else 64
                P.op("tensor", lambda e, pa=pa, src=src, ly=ly, W_=W_, nk=nk: e.matmul(pa[0:64, :], lhsT=W_[ly][0:nk, :], rhs=src[0:nk, :], start=True, stop=True),
                     r=[skey, "w%d" % (ly + 1)], w=[pk])
                h, hk = hd[s], "hd%d" % s
                P.op("vector", lambda e, pa=pa, h=h, ly=ly: e.tensor_scalar(out=h[:], in0=pa[0:64, :], scalar1=fb[:, ly:ly + 1], scalar2=fr[:, ly:ly + 1], op0=ALU.add, op1=ALU.mult), r=[pk, "fb", "fr"], w=[hk])
                P.op("vector", lambda e, h=h: e.tensor_scalar(out=h[:], in0=h[:], scalar1=1.0 / TWO_PI, scalar2=None, op0=ALU.mult), r=[hk], w=[hk])
                P.op("vector", lambda e, h=h, s=s: e.tensor_copy(out=wi[s][:], in_=h[:]), r=[hk], w=["wi%d" % s])
                P.op("vector", lambda e, s=s: e.tensor_copy(out=g1[s][:], in_=wi[s][:]), r=["wi%d" % s], w=["g1_%d" % s])
                P.op("vector", lambda e, h=h, s=s: e.tensor_tensor(out=h[:], in0=h[:], in1=g1[s][:], op=ALU.subtract), r=[hk, "g1_%d" % s], w=[hk])
                P.op("vector", lambda e, h=h, s=s: e.tensor_scalar(out=g1[s][:], in0=h[:], scalar1=0.5, scalar2=None, op0=ALU.is_gt), r=[hk], w=["g1_%d" % s])
                P.op("vector", lambda e, h=h, s=s: e.tensor_tensor(out=h[:], in0=h[:], in1=g1[s][:], op=ALU.subtract), r=[hk, "g1_%d" % s], w=[hk])
                P.op("vector", lambda e, h=h, s=s: e.tensor_scalar(out=g1[s][:], in0=h[:], scalar1=-0.5, scalar2=None, op0=ALU.is_lt), r=[hk], w=["g1_%d" % s])
                P.op("vector", lambda e, h=h, s=s: e.tensor_tensor(out=h[:], in0=h[:], in1=g1[s][:], op=ALU.add), r=[hk, "g1_%d" % s], w=[hk])
                P.op("scalar", lambda e, h=h: e.activation(out=h[:], in_=h[:], func=AF.Sin, scale=TWO_PI), r=[hk], w=[hk])
                src, skey = h, hk
            pa, pk = nps()
            o = tab
            if tab == 0:
                dirs = [(1, 0, 512)] if blk < NBLK // 2 - 1 else ([(1, 0, 511), (0, 511, 512)] if blk == NBLK // 2 - 1 else [(0, 0, 512)])
            else:
                dirs = [(0, 0, 512)] if blk < NBLK // 2 else [(1, 0, 512)]
            for (dr, ca, cb_) in dirs:
                P.op("tensor", lambda e, pa=pa, src=src, o=o, dr=dr, ca=ca, cb_=cb_: e.matmul(pa[:, ca:cb_], lhsT=wo[:, o * 2 + dr, :], rhs=src[:, ca:cb_], start=True, stop=True),
                     r=[skey, "wo"], w=[pk])
            P.op("vector", lambda e, pa=pa, s=s: e.tensor_tensor(out=kb[s][:], in0=pa[:, :], in1=wn[s][:], op=ALU.mult), r=[pk, "wn%d" % s], w=["kb%d" % s])
            P.dma("sync", Ktab[tab][:, m0:m0 + 512], kb[s][:], r=["kb%d" % s], w=["Ktab%d" % tab])
    HALF = NB * 128
    T = [P.sb("T%d" % i, [128, HALF], BF16) for i in range(3)]
    vb = [P.sb("vb%d" % i, [128, NC2], BF16) for i in range(2)]
    gA = [P.sb("gA%d" % i, [128, NC2], F32) for i in range(2)]
    gB = [P.sb("gB%d" % i, [128, NC2], F32) for i in range(2)]
    tt = [P.sb("tt%d" % i, [128, NC2], F32) for i in range(2)]
    yo = [P.sb("yo%d" % i, [128, NC2], BF16) for i in range(2)]
    tn = 0
    n = 0
    for stage in range(2):
        for c in range(NCH):
            s = n % 2
            n += 1
            if stage == 0:
                P.dma("gpsimd", vb[s][:], vrev_d[c], w=["vb%d" % s])
                P.dma("sync", gA[s][:], vnat_d[c], w=["gA%d" % s])
                P.dma("sync", gB[s][:], x1_d[c], w=["gB%d" % s])
            else:
                P.dma("sync", vb[s][:], Y1d[c], r=["Y1d"], w=["vb%d" % s])
                P.dma("sync", gB[s][:], x2_d[c], w=["gB%d" % s])
                pj, pjk = nps()
                P.op("tensor", lambda e, pj=pj, s=s: e.matmul(pj[:, 0:NC2], lhsT=J[:], rhs=vb[s][:], start=True, stop=True), r=["J", "vb%d" % s], w=[pjk])
                P.op("scalar", lambda e, pj=pj, s=s: e.copy(out=gA[s][:], in_=pj[:, 0:NC2]), r=[pjk], w=["gA%d" % s])
            pa, pk = nps()
            pav = pa[:, 0:NC2].rearrange("p (b i) -> p b i", b=2)
            vv = vb[s][:].rearrange("p (b i) -> p b i", b=2)
            order = [NB - 1] + [k for k in range(2 * NB - 1) if k != NB - 1]
            loaded = {}
            cnt = 0
            for k in order:
                hf = 0 if k < NB else 1
                if hf not in loaded:
                    ts_ = tn % 3
                    tn += 1
                    nel = HALF if hf == 0 else HALF - 128
                    P.dma("gpsimd" if False else "sync", T[ts_][:, 0:nel], bass.AP(tensor=Ktab[stage].tensor, offset=c * L2 + hf * HALF, ap=[[1, 128], [1, nel]]),
                          r=["Ktab%d" % stage], w=["T%d" % ts_])
                    loaded[hf] = ts_
                ts_ = loaded[hf]
                delta = (k - (NB - 1)) if stage == 0 else ((NB - 1) - k)
                i0, i1 = max(0, delta), min(NB, NB + delta)
                kk = k - hf * NB
                P.op("tensor", lambda e, pav=pav, vv=vv, ts_=ts_, kk=kk, i0=i0, i1=i1, delta=delta, cnt=cnt: e.matmul(pav[:, :, i0:i1], lhsT=T[ts_][:, kk * 128:(kk + 1) * 128], rhs=vv[:, :, i0 - delta:i1 - delta], start=(cnt == 0), stop=(cnt == 2 * NB - 2)),
                     r=["T%d" % ts_, "vb%d" % s], w=[pk])
                cnt += 1
            P.op("vector", lambda e, pa=pa, s=s, stage=stage, c=c: e.scalar_tensor_tensor(out=tt[s][:], in0=gA[s][:], scalar=hbias[:, stage, c:c + 1], in1=pa[:, 0:NC2], op0=ALU.mult, op1=ALU.add),
                 r=[pk, "gA%d" % s, "hbias"], w=["tt%d" % s])
            P.op("gpsimd", lambda e, s=s: e.tensor_tensor(out=yo[s][:], in0=tt[s][:], in1=gB[s][:], op=ALU.mult), r=["tt%d" % s, "gB%d" % s], w=["yo%d" % s])
            if stage == 0:
                P.dma("sync", Y1d[c], yo[s][:], r=["yo%d" % s], w=["Y1d"])
            else:
                P.dma("sync", y2_d[c], yo[s][:], r=["yo%d" % s])
    return P

def hyena_consts(L, ch0, nch):
    f32 = np.float32
    t = np.linspace(0.0, 1.0, L, dtype=f32)[:, None]
    w = (2.0 * np.pi * np.arange(L, dtype=f32)[:, None] / L).astype(f32)
    bands = np.linspace(1e-4, 15, 16, dtype=f32)[None, :]
    z = np.concatenate([t, np.cos(bands * w), np.sin(-bands * w)], axis=-1).astype(f32)
    max_decay = math.log(1e-2) / 0.3
    min_decay = math.log(1e-2) / 1.5
    deltas = np.abs(np.linspace(min_decay, max_decay, 1024, dtype=f32))
    window = np.exp(-t * deltas[None, ch0:ch0 + nch]).astype(f32)
    m = np.arange(2 * L)
    d1 = np.clip(np.abs(m - (L - 1)), 0, L - 1)
    d2 = np.clip(np.abs((L - 1) - m), 0, L - 1)
    zT1 = np.ascontiguousarray(z[d1].T)
    zT2 = np.ascontiguousarray(z[d2].T)
    win1 = np.zeros((128, 2 * L), f32)
    win2 = np.zeros((128, 2 * L), f32)
    win1[:nch] = window[d1].T
    win2[:nch] = window[d2].T
    return zT1, zT2, win1, win2

def _pos_embed(rows):
    f32 = np.float32
    r, col = np.meshgrid(np.arange(rows, dtype=f32), np.arange(64, dtype=f32), indexing='ij')
    quarter = D // 4
    omega = (1.0 / (f32(10000.0) ** (np.arange(quarter, dtype=f32) / f32(quarter)))).astype(f32)
    def sincos(p):
        ang = (p.reshape(-1, 1) * omega[None, :]).astype(f32)
        return np.concatenate([np.sin(ang), np.cos(ang)], axis=-1)
    return np.concatenate([sincos(r), sincos(col)], axis=-1).astype(f32)


_LAST_DBG = {}


def _run(P, in_maps):
    nc = P.emit()
    res = run_bass_kernel_spmd(nc, in_maps, core_ids=list(range(8)))
    return res.results


def _col8(v):
    return np.ascontiguousarray(np.asarray(v, np.float32).reshape(8, 128).T)


def kernel(**inp):
    f32 = np.float32
    bf = ml_dtypes.bfloat16
    inp = {k: np.asarray(v) for k, v in inp.items()}
    B, L = 2, 16384
    TPC = 4096
    ident = np.eye(128, dtype=f32)
    pos = _pos_embed(L // 64)
    dbg = _LAST_DBG
    dbg.clear()
    cb = lambda k: (k // 4, (k % 4) * TPC)
    wsT = np.ascontiguousarray(inp['gmlp_w_s'][0].transpose(2, 0, 1)).reshape(128, 1024)
    bs16 = np.ascontiguousarray(np.repeat(inp['gmlp_b_s'][0], 2, axis=0).reshape(-1))
    maps = []
    for k in range(8):
        b, t0 = cb(k)
        bc_, half = (k % 4) // 2, (k % 4) % 2
        maps.append(dict(ident=ident, x=inp['x'][b, t0:t0 + TPC], pos=pos[t0:t0 + TPC], ctx=inp['ctx'][bc_, half * 128:(half + 1) * 128],
                         c=_col8(inp['c'][b]), c_ctx=_col8(inp['c_ctx']),
                         ada_w0=inp['ada_w'][0], ada_b0=inp['ada_b'][0], ng0=inp['norm_g'][0].reshape(-1),
                         ada_w1=inp['ada_w'][1], ada_b1=inp['ada_b'][1], ng1=inp['norm_g'][1].reshape(-1),
                         g_win=inp['gmlp_w_in'][0], g_gv=inp['gmlp_g_v'][0], g_wsT=wsT, g_bs=bs16, g_wout=inp['gmlp_w_out'][0],
                         f_wg=inp['ffn_w_gate'][0], f_wu=inp['ffn_w_up'][0], f_wd=inp['ffn_w_down'][0]))
    r = _run(build_S0(), maps)
    x1 = np.stack([np.concatenate([r[b * 4 + j]['xo'] for j in range(4)], 0) for b in range(2)])
    h1 = np.stack([np.concatenate([r[b * 4 + j]['ho'] for j in range(4)], 0) for b in range(2)])
    h1c = np.stack([np.concatenate([r[bc_ * 2 + half]['hco'] for half in range(2)], 0) for bc_ in range(2)])
    dbg['x0'] = x1
    del r
    CTX = 256
    SEQP = 2 + CTX + 4 + L + 2
    hseq = np.zeros((4, D, SEQP), dtype=bf)
    for d in range(2):
        for b in range(2):
            cs, ls = h1c[b], h1[b]
            if d == 1:
                cs, ls = cs[::-1], ls[::-1]
            hseq[d * 2 + b, :, 2:2 + CTX] = cs.T
            hseq[d * 2 + b, :, 2 + CTX + 4:2 + CTX + 4 + L] = ls.T
    maps = []
    for k in range(8):
        sl = slice(k * 128, (k + 1) * 128)
        maps.append(dict(hseq=hseq, w_gate=np.ascontiguousarray(inp['lru_w_in'][0][:, sl]), w_xb=np.ascontiguousarray(inp['lru_w_in'][0][:, 1024 + k * 128:1024 + (k + 1) * 128]),
                         conv_w=np.ascontiguousarray(inp['lru_conv_w'][0][:, sl]).reshape(-1), conv_b=np.ascontiguousarray(inp['lru_conv_b'][0][sl].reshape(128, 1)),
                         w_a=np.ascontiguousarray(inp['lru_w_a'][0][:, k]), w_x=np.ascontiguousarray(inp['lru_w_x'][0][:, k]),
                         b_a=np.ascontiguousarray(inp['lru_b_a'][0][:, sl].T), b_x=np.ascontiguousarray(inp['lru_b_x'][0][:, sl].T),
                         lam=np.ascontiguousarray(inp['lru_lam'][0][:, sl].T)))
    r = _run(build_S1(L=L, CTX=CTX), maps)
    del hseq
    mf = [np.concatenate([r[k]['m'][b] for k in range(8)], 0) for b in range(2)]
    mb = [np.concatenate([r[k]['m'][2 + b][:, ::-1] for k in range(8)], 0) for b in range(2)]
    del r
    def wr_lay(w):
        return np.ascontiguousarray(w.reshape(8, 128, 8).transpose(1, 0, 2).reshape(128, 64))
    maps = []
    for k in range(8):
        b, t0 = cb(k)
        maps.append(dict(ident=ident, x=x1[b, t0:t0 + TPC], c=_col8(inp['c'][b]),
                         ada_w=inp['ada_w'][1], ada_b=inp['ada_b'][1], ng=inp['norm_g'][1].reshape(-1),
                         ada_wn=inp['ada_w'][2], ada_bn=inp['ada_b'][2], ngn=inp['norm_g'][2].reshape(-1),
                         w_out=inp['lru_w_out'][0], w_router=wr_lay(inp['moe_w_router'][0]),
                         wg=inp['moe_w_gate'][0], wu=inp['moe_w_up'][0], wd=inp['moe_w_down'][0],
                         mf=np.ascontiguousarray(mf[b][:, t0:t0 + TPC]), mb=np.ascontiguousarray(mb[b][:, t0:t0 + TPC])))
    r = _run(build_TOK2("S2"), maps)
    x2 = np.stack([np.concatenate([r[b * 4 + j]['xo'] for j in range(4)], 0) for b in range(2)])
    h2 = np.stack([np.concatenate([r[b * 4 + j]['ho'] for j in range(4)], 0) for b in range(2)])
    dbg['x1'] = x2
    del r, mf, mb, x1, h1
    maps = []
    for k in range(8):
        b, t0 = cb(k)
        hp = np.zeros((D, TPC + 2), dtype=bf)
        lo, hi = max(t0 - 1, 0), min(t0 + TPC + 1, L)
        hp[:, lo - (t0 - 1):hi - (t0 - 1)] = h2[b, lo:hi].T
        maps.append(dict(hT=hp, w_in=inp['hyena_w_in'][0], conv_w=inp['hyena_conv_w'][0].reshape(-1), conv_b=inp['hyena_conv_b'][0]))
    r = _run(build_S3a(), maps)
    z = np.stack([np.concatenate([r[b * 4 + j]['z'] for j in range(4)], 0) for b in range(2)])
    del r, h2
    NB = L // 128
    def lay(a, rev):
        t = a.reshape(2, NB, 128, 128).transpose(3, 2, 0, 1)
        if rev:
            t = t[:, ::-1]
        return np.ascontiguousarray(t.reshape(128, 128, 2 * NB))
    fbias = np.ascontiguousarray(np.stack([inp['hyena_f_b1'][0], inp['hyena_f_b2'][0], inp['hyena_f_b3'][0]], 1))
    ffreq = np.ascontiguousarray(inp['hyena_f_freq'][0].T)
    Jm = np.ascontiguousarray(np.eye(128, dtype=f32)[::-1])
    maps = []
    for k in range(8):
        ch0 = k * 128
        zT1, zT2, win1, win2 = hyena_consts(L, ch0, 128)
        wo = np.ascontiguousarray(inp['hyena_f_wout'][0].reshape(64, 4, 1024)[:, :, ch0:ch0 + 128]).reshape(64, 512)
        maps.append(dict(vrev=lay(z[:, :, 2048 + ch0:2048 + ch0 + 128], True), vnat=lay(z[:, :, 2048 + ch0:2048 + ch0 + 128], False),
                         x1nat=lay(z[:, :, ch0:ch0 + 128], False), x2rev=lay(z[:, :, 1024 + ch0:1024 + ch0 + 128], True),
                         zT1=zT1, zT2=zT2, win1=win1, win2=win2, f_w1=inp['hyena_f_w1'][0], f_w2=inp['hyena_f_w2'][0], f_w3=inp['hyena_f_w3'][0],
                         f_b=fbias, f_freq=ffreq, f_wout=wo, hbias=np.ascontiguousarray(inp['hyena_bias'][0][:, ch0:ch0 + 128]).reshape(-1), J=Jm))
    r = _run(build_S3b(L=L, NCH=128), maps)
    del z
    m2 = np.zeros((2, D, L), dtype=bf)
    for k in range(8):
        y = r[k]['y2rev'][:, ::-1]
        m2[:, k * 128:(k + 1) * 128, :] = y.reshape(128, 128, 2, NB).transpose(2, 0, 3, 1).reshape(2, 128, L)
    del r
    maps = []
    for k in range(8):
        b, t0 = cb(k)
        maps.append(dict(ident=ident, x=x2[b, t0:t0 + TPC], c=_col8(inp['c'][b]),
                         ada_w=inp['ada_w'][2], ada_b=inp['ada_b'][2], ng=inp['norm_g'][2].reshape(-1),
                         ada_wn=inp['ada_w'][3], ada_bn=inp['ada_b'][3], ngn=inp['norm_g'][3].reshape(-1),
                         w_out=inp['hyena_w_out'][0], wg=inp['ffn_w_gate'][1], wu=inp['ffn_w_up'][1], wd=inp['ffn_w_down'][1],
                         mf=np.ascontiguousarray(m2[b][:, t0:t0 + TPC])))
    r = _run(build_TOK2("S4"), maps)
    x3 = np.stack([np.concatenate([r[b * 4 + j]['xo'] for j in range(4)], 0) for b in range(2)])
    h3 = np.stack([np.concatenate([r[b * 4 + j]['ho'] for j in range(4)], 0) for b in range(2)])
    dbg['x2'] = x3
    del r, m2, x2
    HW = 8
    maps = []
    for k in range(8):
        b, t0 = cb(k)
        hp = np.zeros((D, TPC + 2 * HW), dtype=bf)
        lo, hi = max(t0 - HW, 0), min(t0 + TPC + HW, L)
        hp[:, lo - (t0 - HW):hi - (t0 - HW)] = h3[b, lo:hi].T
        fix = np.ones((4, 16), f32)
        for wi_, win in enumerate((2, 4, 8, 16)):
            for j in range(8):
                if k % 4 == 0:
                    t = j
                    fix[wi_, j] = win / float(min(t + win // 2, L) - max(t - win // 2, 0))
                if k % 4 == 3:
                    t = L - 8 + j
                    fix[wi_, 8 + j] = win / float(min(t + win // 2, L) - max(t - win // 2, 0))
        maps.append(dict(ident=ident, x=x3[b, t0:t0 + TPC], c=_col8(inp['c'][b]),
                         ada_w=inp['ada_w'][3], ada_b=inp['ada_b'][3], ng=inp['norm_g'][3].reshape(-1),
                         w_out=inp['pool_w_out'][0], w_router=wr_lay(inp['moe_w_router'][1]),
                         wg=inp['moe_w_gate'][1], wu=inp['moe_w_up'][1], wd=inp['moe_w_down'][1],
                         h3T=hp, p_win=inp['pool_w_in'][0], p_wg=inp['pool_w_g'][0], p_bg=_col8(inp['pool_b_g'][0].reshape(-1)),
                         p_scale=_col8(inp['pool_scale'][0]), p_fix=np.ascontiguousarray(np.broadcast_to(fix.reshape(1, 64), (128, 64)))))
    r = _run(build_TOK2("S5"), maps)
    out = np.stack([np.concatenate([r[b * 4 + j]['xo'] for j in range(4)], 0) for b in range(2)]).astype(f32)
    return out
```

```python
import os, math
import ml_dtypes
import numpy as np
from contextlib import ExitStack
import concourse.bass as bass
import concourse.mybir as mybir
from concourse.bass_utils import run_bass_kernel_spmd

F32 = mybir.dt.float32
BF16 = mybir.dt.bfloat16
I32 = mybir.dt.int32
AF = mybir.ActivationFunctionType
ALU = mybir.AluOpType
AX = mybir.AxisListType

COMPUTE = ("tensor", "vector", "scalar", "gpsimd")
DMAQ = ("sync", "gpsimd")
SAME_ENGINE_SYNC = True


class Prog:
    def __init__(self, name="k"):
        self.nc = bass.Bass("TRN2", target_bir_lowering=False)
        self.es = ExitStack()
        self.ops = []
        self.last_w = {}
        self.readers = {}
        self.n_dma_sems = {"sync": 24, "gpsimd": 24, "scalar": 16}
        self.names = set()

    def dram(self, name, shape, dt, kind):
        return self.nc.dram_tensor(name, list(shape), dt, kind=kind).ap()

    def sb(self, name, shape, dt):
        assert name not in self.names, name
        self.names.add(name)
        return self.es.enter_context(self.nc.sbuf_tensor(name, list(shape), dt))

    def ps(self, name, shape, dt):
        assert name not in self.names, name
        self.names.add(name)
        return self.es.enter_context(self.nc.psum_tensor(name, list(shape), dt))

    def op(self, eng, fn, r=(), w=(), dma=False):
        i = len(self.ops)
        deps = set()
        for k in r:
            if k in self.last_w:
                deps.add(self.last_w[k])
        for k in w:
            if k in self.last_w:
                deps.add(self.last_w[k])
            for j in self.readers.get(k, ()):
                deps.add(j)
        deps.discard(i)
        self.ops.append(dict(eng=eng, fn=fn, deps=sorted(deps), dma=dma))
        for k in r:
            self.readers.setdefault(k, []).append(i)
        for k in w:
            self.last_w[k] = i
            self.readers[k] = []
        return i

    def dma(self, q, out, in_, r=(), w=(), **kw):
        return self.op(q, lambda e: e.dma_start(out=out, in_=in_, **kw), r, w, dma=True)

    def emit(self):
        nc = self.nc
        ops = self.ops
        es = self.es
        eng_ops = {}
        for i, o in enumerate(ops):
            lst = eng_ops.setdefault(o["eng"], [])
            o["pos"] = len(lst)
            lst.append(i)
        dcount = {}
        for i, o in enumerate(ops):
            if o["dma"]:
                q = o["eng"]
                o["dnum"] = dcount.get(q, 0)
                dcount[q] = o["dnum"] + 1
        seen = {e: {} for e in eng_ops}
        seen_dma = {e: set() for e in eng_ops}
        for i, o in enumerate(ops):
            X = o["eng"]
            waits = []
            best = {}
            for j in o["deps"]:
                p = ops[j]
                if p["dma"]:
                    if j in seen_dma[X]:
                        continue
                    seen_dma[X].add(j)
                    waits.append(j)
                else:
                    E = p["eng"]
                    if E == X and (E == "tensor" or not SAME_ENGINE_SYNC):
                        continue
                    if E not in best or ops[best[E]]["pos"] < p["pos"]:
                        best[E] = j
            for E, j in best.items():
                p = ops[j]
                if seen[X].get(E, -1) >= p["pos"]:
                    continue
                seen[X][E] = p["pos"]
                waits.append(j)
            o["waits"] = waits
            for j in waits:
                ops[j]["sig"] = True
        sigcount = {}
        for i, o in enumerate(ops):
            if o["dma"]:
                continue
            if o.get("sig"):
                E = o["eng"]
                sigcount[E] = sigcount.get(E, 0) + 1
                o["signum"] = sigcount[E]
        csem = {e: es.enter_context(nc.semaphore("c_" + e)) for e in COMPUTE}
        dsem = {q: [es.enter_context(nc.semaphore("d_%s_%d" % (q, k))) for k in range(self.n_dma_sems[q])]
                for q in dcount}
        self.stats = {e: len(v) for e, v in eng_ops.items()}
        self.stats["sig"] = dict(sigcount)

        def waitspec(j):
            p = ops[j]
            if p["dma"]:
                K = self.n_dma_sems[p["eng"]]
                return dsem[p["eng"]][p["dnum"] % K], 16 * (p["dnum"] // K + 1)
            return csem[p["eng"]], p["signum"]

        def run_engine(ename, e):
            for i in eng_ops.get(ename, []):
                o = ops[i]
                for j in o["waits"]:
                    s, v = waitspec(j)
                    e.wait_ge(s, v)
                if o["dma"]:
                    K = self.n_dma_sems[ename]
                    m = o["dnum"]
                    if m >= K:
                        e.wait_ge(dsem[ename][m % K], 16 * (m // K))
                    ins = o["fn"](e)
                    ins.then_inc(dsem[ename][m % K], 16)
                else:
                    ins = o["fn"](e)
                    if o.get("sig"):
                        ins.then_inc(csem[ename], 1)
            if ename in dsem:
                K = self.n_dma_sems[ename]
                n = dcount[ename]
                for k in range(min(K, n)):
                    cnt = (n - 1 - k) // K + 1
                    e.wait_ge(dsem[ename][k], 16 * cnt)

        with nc.Block() as block:
            @block.sync
            def _(e):
                run_engine("sync", e)

            @block.tensor
            def _(e):
                run_engine("tensor", e)

            @block.vector
            def _(e):
                run_engine("vector", e)

            @block.scalar
            def _(e):
                run_engine("scalar", e)

            @block.gpsimd
            def _(e):
                run_engine("gpsimd", e)
        self.es.close()
        return nc
D = 1024
EPS = 1e-6


class Tok(Prog):
    def __init__(self, TB):
        super().__init__()
        self.TB = TB
        self.NT = TB // 128
        self.ident_d = self.dram("ident", [128, 128], F32, "ExternalInput")
        self.ident = self.sb("ident_sb", [128, 128], BF16)
        self.dma("gpsimd", self.ident[:], self.ident_d, w=["ident"])
        self.ones = self.sb("ones", [1, 128], F32)
        self.op("vector", lambda e: e.memset(self.ones[:], 1.0), w=["ones"])
        self.psA = [self.ps("psA%d" % i, [128, 512], F32) for i in range(3)]
        self.psY = [self.ps("psY%d" % i, [128, 1024], F32) for i in range(2)]
        self.psT = self.ps("psT", [128, 1024], BF16)
        self.wsl = [self.sb("wsl%d" % i, [128, 8192], BF16) for i in range(3)]
        self.wi = 0
        self.ai = 0
        self.yi = 0
        self.tmpf = [self.sb("tmpf%d" % i, [128, 1024], F32) for i in range(2)]
        self.ti = 0
        self.hbf = [self.sb("hbf%d" % i, [128, 1024], BF16) for i in range(2)]
        self.hi = 0
        self.stat = self.sb("stat", [128, 64], F32)
        self.si = 0
        self.rowbuf = self.sb("rowbuf", [1, 2048], F32)
        self.si2 = 0
        self.csb = self.sb("csb", [128, 8], F32)
        self.csl = self.sb("csl", [128, 8], BF16)
        self.junk = self.sb("junk", [128, 1024], BF16)

    def nw(self):
        i = self.wi % 3
        self.wi += 1
        return self.wsl[i], "wsl%d" % i

    def nA(self):
        i = self.ai % 3
        self.ai += 1
        return self.psA[i], "psA%d" % i

    def nY(self):
        i = self.yi % 2
        self.yi += 1
        return self.psY[i], "psY%d" % i

    def ntmp(self):
        i = self.ti % 2
        self.ti += 1
        return self.tmpf[i], "tmpf%d" % i

    def nh(self):
        i = self.hi % 2
        self.hi += 1
        return self.hbf[i], "hbf%d" % i

    def nstat(self, n=1):
        if self.si + n > 64:
            self.si = 0
        c = self.si
        self.si += n
        return c

    def load_w(self, src, a, b):
        t, key = self.nw()
        dst = t[:, 0:a * b].rearrange("p (a b) -> p a b", a=a)
        self.dma("gpsimd", dst, src, w=[key])
        return dst, key

    def bc_dram(self, dst, dkey, src1d, off, n, q="sync"):
        ap = bass.AP(tensor=src1d.tensor, offset=off, ap=[[0, 128], [1, n]])
        self.dma(q, dst, ap, w=[dkey])

    def ada_mod(self, pref, cvec, ada_w, ada_b, normg, segs):
        self.dma("sync", self.csb[:], cvec, w=["csb"])
        self.op("scalar", lambda e: e.activation(out=self.csl[:], in_=self.csb[:], func=AF.Silu), r=["csb"], w=["csl"])
        wv = ada_w.rearrange("(kc p) n -> p kc n", p=128)
        bv = ada_b.rearrange("(o n) -> o n", o=1)
        rb = self.rowbuf
        out = {}
        for (seg, name, kind, gidx) in segs:
            t = self.get_tile(pref + name)
            tk = pref + name
            for hb in range(2):
                nb = seg * 2 + hb
                sl = self.si2 % 2
                self.si2 += 1
                rk = "row%d" % sl
                self.dma("sync", rb[0:1, sl * 1024 + 512:sl * 1024 + 1024], bv[0:1, nb * 512:(nb + 1) * 512], w=[rk + "b"])
                wt, wk = self.load_w(wv[:, :, nb * 512:(nb + 1) * 512], 8, 512)
                pa, pk = self.nA()
                for kc in range(8):
                    self.op("tensor", lambda e, kc=kc, wt=wt, pa=pa: e.matmul(pa[0:1, :], lhsT=self.csl[:, kc:kc + 1], rhs=wt[:, kc, :], start=(kc == 0), stop=(kc == 7)),
                            r=[wk, "csl"], w=[pk])
                self.op("vector", lambda e, sl=sl, pa=pa: e.tensor_tensor(out=rb[0:1, sl * 1024:sl * 1024 + 512], in0=pa[0:1, :], in1=rb[0:1, sl * 1024 + 512:sl * 1024 + 1024], op=ALU.add),
                        r=[pk, rk + "b"], w=[rk])
                pb, pbk = self.nA()
                self.op("tensor", lambda e, sl=sl, pb=pb: e.matmul(pb[:, :], lhsT=self.ones[0:1, :], rhs=rb[0:1, sl * 1024:sl * 1024 + 512], start=True, stop=True),
                        r=[rk, "ones"], w=[pbk])
                self.op("scalar", lambda e, hb=hb, pb=pb, t=t: e.copy(out=t[:, hb * 512:(hb + 1) * 512], in_=pb[:, :]), r=[pbk], w=[tk])
            if kind != "raw":
                tmp, tmk = self.ntmp()
                self.bc_dram(tmp[:], tmk, normg, gidx * D, D)
                if kind == "mul1p":
                    self.op("vector", lambda e, t=t, tmp=tmp: e.scalar_tensor_tensor(out=t[:], in0=t[:], scalar=1.0, in1=tmp[:], op0=ALU.add, op1=ALU.mult), r=[tk, tmk], w=[tk])
                else:
                    self.op("vector", lambda e, t=t, tmp=tmp: e.tensor_tensor(out=t[:], in0=t[:], in1=tmp[:], op=ALU.mult), r=[tk, tmk], w=[tk])
            out[name] = (t, tk)
        return out

    def get_tile(self, name, shape=None, dt=F32):
        if not hasattr(self, "_tiles"):
            self._tiles = {}
        if name not in self._tiles:
            self._tiles[name] = self.sb(name, shape or [128, D], dt)
        return self._tiles[name]

    def rstd_of(self, srcs, skeys):
        n = len(srcs)
        c = self.nstat(n + 2)
        st = self.stat
        self.op("vector", lambda e: e.memset(st[:, c:c + n], 0.0), w=["stat"])
        for i, s in enumerate(srcs):
            self.op("scalar", lambda e, s=s, i=i: e.activation(out=self.junk[:, 0:s.shape[-1]], in_=s, func=AF.Square, accum_out=st[:, c + i:c + i + 1]),
                    r=list(skeys), w=["junk", "stat"])
        if n == 2:
            self.op("vector", lambda e: e.tensor_tensor(out=st[:, c:c + 1], in0=st[:, c:c + 1], in1=st[:, c + 1:c + 2], op=ALU.add), r=["stat"], w=["stat"])
        self.op("scalar", lambda e: e.activation(out=st[:, c + n:c + n + 1], in_=st[:, c:c + 1], func=AF.Sqrt, scale=1.0 / D, bias=EPS), r=["stat"], w=["stat"])
        self.op("vector", lambda e: e.reciprocal(out=st[:, c + n + 1:c + n + 2], in_=st[:, c + n:c + n + 1]), r=["stat"], w=["stat"])
        return st[:, c + n + 1:c + n + 2]

    def norm_mod(self, xt, xkey, Gt, SHt, out_bf, okey):
        rs = self.rstd_of([xt], [xkey])
        tmp, tk = self.ntmp()
        self.op("vector", lambda e: e.scalar_tensor_tensor(out=tmp[:], in0=xt, scalar=rs, in1=Gt[0][:], op0=ALU.mult, op1=ALU.mult),
                r=[xkey, "stat", Gt[1]], w=[tk])
        self.op("gpsimd", lambda e: e.tensor_tensor(out=out_bf, in0=tmp[:], in1=SHt[0][:], op=ALU.add), r=[tk, SHt[1]], w=[okey])

    def transpose_to(self, src_bf, skey, dstT, dkey, tcol, nk=8):
        pv = self.psT[:, 0:nk * 128].rearrange("p (a b) -> p a b", a=nk)
        for kc in range(nk):
            self.op("tensor", lambda e, kc=kc: e.transpose(out=pv[:, kc, :], in_=src_bf[:, kc * 128:(kc + 1) * 128], identity=self.ident[:]),
                    r=[skey, "ident"], w=["psT"])
        self.op("vector", lambda e: e.tensor_copy(out=dstT[:, 0:nk, tcol:tcol + 128], in_=pv), r=["psT"], w=[dkey])

    def post_res(self, ypsum, ykey, xt, xkey, GTt):
        rs = self.rstd_of([ypsum[:, 0:512], ypsum[:, 512:1024]], [ykey])
        tmp, tk = self.ntmp()
        self.op("vector", lambda e: e.scalar_tensor_tensor(out=tmp[:], in0=ypsum[:, :], scalar=rs, in1=GTt[0][:], op0=ALU.mult, op1=ALU.mult),
                r=[ykey, "stat", GTt[1]], w=[tk])
        self.op("gpsimd", lambda e: e.tensor_tensor(out=xt, in0=xt, in1=tmp[:], op=ALU.add), r=[tk, xkey], w=[xkey])

    def post_res_sb(self, ysb, ykey, xt, xkey, GTt):
        rs = self.rstd_of([ysb], [ykey, "uT"])
        tmp, tk = self.ntmp()
        self.op("vector", lambda e: e.scalar_tensor_tensor(out=tmp[:], in0=ysb, scalar=rs, in1=GTt[0][:], op0=ALU.mult, op1=ALU.mult),
                r=[ykey, "stat", GTt[1]], w=[tk])
        self.op("gpsimd", lambda e: e.tensor_tensor(out=xt, in0=xt, in1=tmp[:], op=ALU.add), r=[tk, xkey], w=[xkey])

    def lin_fm(self, W, KCn, hT, hkey, t0, ntok, nchunks, evac, group=4):
        wv = W.rearrange("(kc p) n -> p kc n", p=128)
        cols_per_load = 8192 // KCn // 128 * 128
        cols_per_load = min(cols_per_load, 512)
        i = 0
        while i < len(nchunks):
            n0 = nchunks[i]
            run = [n0]
            while len(run) * 128 < cols_per_load and i + len(run) < len(nchunks) and nchunks[i + len(run)] == run[-1] + 1:
                run.append(run[-1] + 1)
            wt, wk = self.load_w(wv[:, :, n0 * 128:(n0 + len(run)) * 128], KCn, len(run) * 128)
            for j, ncx in enumerate(run):
                pa, pk = self.nA()
                for kc in range(KCn):
                    self.op("tensor", lambda e, kc=kc, j=j, wt=wt, pa=pa: e.matmul(pa[:, 0:ntok], lhsT=wt[:, kc, j * 128:(j + 1) * 128], rhs=hT[:, kc, t0:t0 + ntok], start=(kc == 0), stop=(kc == KCn - 1)),
                            r=[wk, hkey], w=[pk])
                evac(ncx, pa[:, 0:ntok], pk)
            i += len(run)

    def gelu_evac(self, ps, pk, out, okey):
        n = ps.shape[-1]
        tmp, tk = self.ntmp()
        t = tmp[:, 0:n]
        self.op("scalar", lambda e: e.activation(out=t, in_=ps, func=AF.Square), r=[pk], w=[tk])
        self.op("vector", lambda e: e.tensor_scalar(out=t, in0=t, scalar1=0.044715, scalar2=1.0, op0=ALU.mult, op1=ALU.add), r=[tk], w=[tk])
        self.op("vector", lambda e: e.tensor_tensor(out=t, in0=t, in1=ps, op=ALU.mult), r=[tk, pk], w=[tk])
        self.op("scalar", lambda e: e.activation(out=t, in_=t, func=AF.Sigmoid, scale=1.5957691216057308), r=[tk], w=[tk])
        self.op("vector", lambda e: e.tensor_tensor(out=out, in0=t, in1=ps, op=ALU.mult), r=[tk, pk], w=[okey])

    def load_block(self, X, srcs, pos=None):
        for ti, s in enumerate(srcs):
            self.dma("sync", X[:, ti, :], s, w=["X%d" % ti])
            if pos is not None:
                tmp, tk = self.ntmp()
                self.dma("sync", tmp[:], pos[ti], w=[tk])
                self.op("gpsimd", lambda e, ti=ti, tmp=tmp: e.tensor_tensor(out=X[:, ti, :], in0=X[:, ti, :], in1=tmp[:], op=ALU.add), r=[tk, "X%d" % ti], w=["X%d" % ti])

    def pre_T(self, X, nt, Gt, SHt, hT, want32=None):
        for ti in range(nt):
            hb, hk = self.nh()
            if want32 is None:
                self.norm_mod(X[:, ti, :], "X%d" % ti, Gt, SHt, hb[:], hk)
            else:
                h32, h32k = want32
                self.norm_mod(X[:, ti, :], "X%d" % ti, Gt, SHt, h32[:, ti, :], h32k + str(ti))
                self.op("scalar", lambda e, ti=ti, hb=hb: e.copy(out=hb[:], in_=h32[:, ti, :]), r=[h32k + str(ti)], w=[hk])
            self.transpose_to(hb, hk, hT, "hT", ti * 128)

    def gmlp(self, X, nt, c, mods):
        ntok = nt * 128
        hT, uT, vg = c["hT"], c["uT"], c["vg"]
        w_in, w_out = c["w_in"], c["w_out"]
        def ev_u(ncx, ps, pk):
            self.gelu_evac(ps, pk, uT[:, ncx, 0:ntok], "uT")
        self.lin_fm(w_in, 8, hT, "hT", 0, ntok, list(range(16)), ev_u)
        wv = w_in.rearrange("(kc p) n -> p kc n", p=128)
        for cb in range(4):
            wt, wk = self.load_w(wv[:, :, 2048 + cb * 512:2048 + (cb + 1) * 512], 8, 512)
            for ti in range(nt):
                pa, pk = self.nA()
                for kc in range(8):
                    self.op("tensor", lambda e, kc=kc, ti=ti, wt=wt, pa=pa: e.matmul(pa[:, :], lhsT=hT[:, kc, ti * 128:(ti + 1) * 128], rhs=wt[:, kc, :], start=(kc == 0), stop=(kc == 7)),
                            r=[wk, "hT"], w=[pk])
                self.gelu_evac(pa[:, :], pk, vg[:, ti, cb * 512:(cb + 1) * 512], "vg%d" % ti)
        bst = c["bst"]
        wsT = c["wsT"]
        for ti in range(nt):
            for cb in range(4):
                self.op("vector", lambda e, ti=ti, cb=cb: e.bn_stats(out=bst[:, cb * 6:(cb + 1) * 6], in_=vg[:, ti, cb * 512:(cb + 1) * 512]), r=["vg%d" % ti], w=["bst"])
            self.op("vector", lambda e: e.bn_aggr(out=bst[:, 24:26], in_=bst[:, 0:24]), r=["bst"], w=["bst"])
            self.op("scalar", lambda e: e.activation(out=bst[:, 26:27], in_=bst[:, 25:26], func=AF.Sqrt, scale=1.0, bias=EPS), r=["bst"], w=["bst"])
            self.op("vector", lambda e: e.reciprocal(out=bst[:, 27:28], in_=bst[:, 26:27]), r=["bst"], w=["bst"])
            vn, vk = c["vn"][ti % 2], "vn%d" % (ti % 2)
            for hf in range(2):
                tmp, tk = self.ntmp()
                self.op("vector", lambda e, ti=ti, hf=hf, tmp=tmp: e.tensor_scalar(out=tmp[:], in0=vg[:, ti, hf * 1024:(hf + 1) * 1024], scalar1=bst[:, 24:25], scalar2=bst[:, 27:28], op0=ALU.subtract, op1=ALU.mult),
                        r=["vg%d" % ti, "bst"], w=[tk])
                self.op("gpsimd", lambda e, hf=hf, tmp=tmp, vn=vn: e.tensor_tensor(out=vn[:, hf * 1024:(hf + 1) * 1024], in0=tmp[:], in1=c["GV"][:, hf * 1024:(hf + 1) * 1024], op=ALU.mult),
                        r=[tk, "GV"], w=[vk])
            for q4 in range(4):
                pa, pk = self.nA()
                for j in range(4):
                    cc = q4 * 4 + j
                    self.op("tensor", lambda e, j=j, cc=cc, pa=pa, vn=vn: e.matmul(pa[:, j * 128:(j + 1) * 128], lhsT=vn[:, cc * 128:(cc + 1) * 128], rhs=wsT[:, cc // 2, :], start=True, stop=True),
                            r=[vk, "wsT"], w=[pk])
                tmp, tk = self.ntmp()
                self.op("vector", lambda e, q4=q4, pa=pa, tmp=tmp: e.tensor_tensor(out=tmp[:, 0:512], in0=pa[:, :], in1=c["BS"][:, q4 * 512:(q4 + 1) * 512], op=ALU.add),
                        r=[pk, "BS"], w=[tk])
                self.op("gpsimd", lambda e, q4=q4, ti=ti, tmp=tmp: e.tensor_tensor(out=uT[:, q4 * 4:(q4 + 1) * 4, ti * 128:(ti + 1) * 128], in0=tmp[:, 0:512].rearrange("p (a b) -> p a b", a=4),
                                                                                    in1=uT[:, q4 * 4:(q4 + 1) * 4, ti * 128:(ti + 1) * 128], op=ALU.mult),
                        r=[tk, "uT"], w=["uT"])
        wo = w_out.rearrange("(kc p) n -> p kc n", p=128)
        wts = [self.load_w(wo[:, :, hf * 512:(hf + 1) * 512], 16, 512) for hf in range(2)]
        for ti in range(nt):
            py, pyk = self.nY()
            for hf in range(2):
                wt, wk = wts[hf]
                for kc in range(16):
                    self.op("tensor", lambda e, kc=kc, hf=hf, ti=ti, wt=wt, py=py: e.matmul(py[:, hf * 512:(hf + 1) * 512], lhsT=uT[:, kc, ti * 128:(ti + 1) * 128], rhs=wt[:, kc, :], start=(kc == 0), stop=(kc == 15)),
                            r=[wk, "uT"], w=[pyk])
            self.post_res(py, pyk, X[:, ti, :], "X%d" % ti, mods["GT1"])

    def mixer_out(self, X, nt, mTs, mkeys, w_out, KCn, GT):
        wo = w_out.rearrange("(kc p) n -> p kc n", p=128)
        wts = [self.load_w(wo[:, :, hf * 512:(hf + 1) * 512], KCn, 512) for hf in range(2)]
        nm = len(mTs)
        for ti in range(nt):
            py, pyk = self.nY()
            for hf in range(2):
                wt, wk = wts[hf]
                idx = 0
                for mi in range(nm):
                    for kc in range(KCn):
                        self.op("tensor", lambda e, kc=kc, hf=hf, ti=ti, wt=wt, py=py, mi=mi, idx=idx: e.matmul(py[:, hf * 512:(hf + 1) * 512], lhsT=mTs[mi][:, kc, ti * 128:(ti + 1) * 128], rhs=wt[:, kc, :], start=(idx == 0), stop=(idx == nm * KCn - 1)),
                                r=[wk, mkeys[mi]], w=[pyk])
                        idx += 1
            self.post_res(py, pyk, X[:, ti, :], "X%d" % ti, GT)

    def ffn(self, X, nt, c, GT, wg, wu, wd, FF, comb=None, first=True):
        ntok = nt * 128
        hT, Y2 = c["hT"], c["Y2"]
        nfb = (FF + 511) // 512
        wdv = wd.rearrange("(kc p) n -> p kc n", p=128)
        for fb in range(nfb):
            nch = min(4, FF // 128 - fb * 4)
            sg, sgk = c["sg"][fb % 2], "sg%d" % (fb % 2)
            gT, gk = c["gT"][fb % 2], "gT%d" % (fb % 2)
            chunks = list(range(fb * 4, fb * 4 + nch))
            def ev_g(ncx, ps, pk, sg=sg, sgk=sgk, fb=fb):
                self.op("scalar", lambda e: e.activation(out=sg[:, ncx - fb * 4, 0:ntok], in_=ps, func=AF.Silu), r=[pk], w=[sgk])
            self.lin_fm(wg, 8, hT, "hT", 0, ntok, chunks, ev_g)
            def ev_u(ncx, ps, pk, sg=sg, sgk=sgk, gT=gT, gk=gk, fb=fb):
                self.op("vector", lambda e: e.tensor_tensor(out=gT[:, ncx - fb * 4, 0:ntok], in0=sg[:, ncx - fb * 4, 0:ntok], in1=ps, op=ALU.mult), r=[pk, sgk], w=[gk])
            self.lin_fm(wu, 8, hT, "hT", 0, ntok, chunks, ev_u)
            wt, wk = self.load_w(wdv[:, fb * 4:fb * 4 + nch, :], nch, 1024)
            for ti in range(nt):
                py, pyk = self.nY()
                for hf in range(2):
                    for kc in range(nch):
                        self.op("tensor", lambda e, kc=kc, hf=hf, ti=ti, wt=wt, py=py, gT=gT: e.matmul(py[:, hf * 512:(hf + 1) * 512], lhsT=gT[:, kc, ti * 128:(ti + 1) * 128], rhs=wt[:, kc, hf * 512:(hf + 1) * 512], start=(kc == 0), stop=(kc == nch - 1)),
                                r=[wk, gk], w=[pyk])
                yk = "Y2_%d" % ti
                if comb is None:
                    if first and fb == 0:
                        self.op("scalar", lambda e, ti=ti, py=py: e.copy(out=Y2[:, ti, :], in_=py[:, :]), r=[pyk], w=[yk, "uT"])
                    else:
                        self.op("vector", lambda e, ti=ti, py=py: e.tensor_tensor(out=Y2[:, ti, :], in0=py[:, :], in1=Y2[:, ti, :], op=ALU.add), r=[pyk, yk], w=[yk])
                else:
                    cap, ck = comb
                    if first and fb == 0:
                        self.op("vector", lambda e, ti=ti, py=py: e.tensor_scalar(out=Y2[:, ti, :], in0=py[:, :], scalar1=cap(ti), scalar2=None, op0=ALU.mult), r=[pyk, ck], w=[yk, "uT"])
                    else:
                        self.op("vector", lambda e, ti=ti, py=py: e.scalar_tensor_tensor(out=Y2[:, ti, :], in0=py[:, :], scalar=cap(ti), in1=Y2[:, ti, :], op0=ALU.mult, op1=ALU.add), r=[pyk, yk, ck], w=[yk])

    def ffn_post(self, X, nt, c, GT):
        for ti in range(nt):
            self.post_res_sb(c["Y2"][:, ti, :], "Y2_%d" % ti, X[:, ti, :], "X%d" % ti, GT)

    def out_block(self, X, nt, xo_rows, ho_rows=None, Gn=None, SHn=None):
        for ti in range(nt):
            if ho_rows is not None:
                hb, hk = self.nh()
                self.norm_mod(X[:, ti, :], "X%d" % ti, Gn, SHn, hb[:], hk)
                self.dma("sync", ho_rows[ti], hb[:], r=[hk])
            if xo_rows is not None:
                self.dma("sync", xo_rows[ti], X[:, ti, :], r=["X%d" % ti])

def build_S0(n_lat_tiles=32, TB=512):
    P = Tok(TB)
    NT = P.NT
    NTOK = n_lat_tiles * 128
    dI = lambda n, s, dt=F32: P.dram(n, s, dt, "ExternalInput")
    dO = lambda n, s, dt=F32: P.dram(n, s, dt, "ExternalOutput")
    x_d, pos_d, ctx_d = dI("x", [NTOK, D]), dI("pos", [NTOK, D]), dI("ctx", [128, D])
    c_d, cc_d = dI("c", [128, 8]), dI("c_ctx", [128, 8])
    aw0, ab0, ng0 = dI("ada_w0", [D, 6 * D]), dI("ada_b0", [6 * D]), dI("ng0", [4 * D])
    aw1, ab1, ng1 = dI("ada_w1", [D, 6 * D]), dI("ada_b1", [6 * D]), dI("ng1", [4 * D])
    g_win, g_gv, g_wsT, g_bs, g_wout = dI("g_win", [D, 4096]), dI("g_gv", [2048]), dI("g_wsT", [128, 1024]), dI("g_bs", [2048]), dI("g_wout", [2048, D])
    f_wg, f_wu, f_wd = dI("f_wg", [D, 2816]), dI("f_wu", [D, 2816]), dI("f_wd", [2816, D])
    xo, ho = dO("xo", [NTOK, D]), dO("ho", [NTOK, D], BF16)
    xco, hco = dO("xco", [128, D]), dO("hco", [128, D], BF16)
    X = P.sb("X", [128, NT, D], F32)
    c = dict(hT=P.sb("hT", [128, 8, TB], BF16), uT=None, vg=P.sb("vg", [128, NT, 2048], BF16),
             vn=[P.sb("vn%d" % i, [128, 2048], BF16) for i in range(2)], bst=P.sb("bst", [128, 32], F32),
             Y2=None, sg=[P.sb("sg%d" % i, [128, 4, TB], BF16) for i in range(2)],
             gT=[P.sb("gT%d" % i, [128, 4, TB], BF16) for i in range(2)],
             w_in=g_win, w_out=g_wout)
    uY = P.sb("uY", [128, NT * D], F32)
    c["Y2"] = uY[:].rearrange("p (a b) -> p a b", a=NT)
    c["uT"] = uY[:].bitcast(BF16).rearrange("p (a b) -> p a b", a=16)
    c["GV"] = P.sb("GV", [128, 2048], F32)
    c["BS"] = P.sb("BS", [128, 2048], F32)
    P.bc_dram(c["GV"][:], "GV", g_gv, 0, 2048)
    P.bc_dram(c["BS"][:], "BS", g_bs, 0, 2048)
    wsT = P.sb("wsT", [128, 8, 128], BF16)
    P.dma("gpsimd", wsT[:].rearrange("p a b -> p (a b)"), g_wsT, w=["wsT"])
    c["wsT"] = wsT

    def run_pass(pref, cvec, blocks):
        m = P.ada_mod("m", cvec, aw0, ab0, ng0, [(0, "SH1", "raw", 0), (1, "G1", "mul1p", 0), (2, "GT1", "mul", 1),
                                                  (3, "SH2", "raw", 0), (4, "G2", "mul1p", 2), (5, "GT2", "mul", 3)])
        mn = P.ada_mod("n", cvec, aw1, ab1, ng1, [(0, "SH1", "raw", 0), (1, "G1", "mul1p", 0)])
        for (srcs, poss, xos, hos) in blocks:
            nt = len(srcs)
            P.load_block(X, srcs, poss)
            P.pre_T(X, nt, m["G1"], m["SH1"], c["hT"])
            P.gmlp(X, nt, c, m)
            P.pre_T(X, nt, m["G2"], m["SH2"], c["hT"])
            P.ffn(X, nt, c, m["GT2"], f_wg, f_wu, f_wd, 2816)
            P.ffn_post(X, nt, c, m["GT2"])
            P.out_block(X, nt, xos, hos, mn["G1"], mn["SH1"])

    rows = lambda d, t: d[t * 128:(t + 1) * 128, :]
    blocks = []
    for b in range(n_lat_tiles // NT):
        ts = list(range(b * NT, (b + 1) * NT))
        blocks.append(([rows(x_d, t) for t in ts], [rows(pos_d, t) for t in ts], [rows(xo, t) for t in ts], [rows(ho, t) for t in ts]))
    run_pass("l", c_d, blocks)
    run_pass("c", cc_d, [([ctx_d], None, [xco], [hco])])
    return P

def tok_router(P, nt, c):
    h32, hT32, wr32, lg, mx8, comb, identf = c["h32"], c["hT32"], c["wr32"], c["lg"], c["mx8"], c["comb"], c["identf"]
    for ti in range(nt):
        py, pyk = P.nY()
        pv = py[:, :].rearrange("p (a b) -> p a b", a=8)
        for kc in range(8):
            P.op("tensor", lambda e, kc=kc, ti=ti, pv=pv: e.transpose(out=pv[:, kc, :], in_=h32[:, ti, kc * 128:(kc + 1) * 128], identity=identf[:]),
                 r=["h32_%d" % ti, "identf"], w=[pyk])
        P.op("vector", lambda e, pv=pv: e.tensor_copy(out=hT32[:], in_=pv), r=[pyk], w=["hT32"])
        pa, pk = P.nA()
        for kc in range(8):
            P.op("tensor", lambda e, kc=kc, pa=pa: e.matmul(pa[:, 0:8], lhsT=hT32[:, kc, :], rhs=wr32[:, kc, :], start=(kc == 0), stop=(kc == 7)),
                 r=["hT32", "wr32"], w=[pk])
        P.op("vector", lambda e, pa=pa: e.tensor_copy(out=lg[:, 0:8], in_=pa[:, 0:8]), r=[pk], w=["lg"])
        P.op("vector", lambda e: e.max(out=mx8[:, 0:8], in_=lg[:, 0:8]), r=["lg"], w=["mx8"])
        P.op("vector", lambda e: e.tensor_tensor(out=mx8[:, 8:9], in0=mx8[:, 0:1], in1=mx8[:, 1:2], op=ALU.subtract), r=["mx8"], w=["mx8"])
        P.op("scalar", lambda e: e.activation(out=mx8[:, 9:10], in_=mx8[:, 8:9], func=AF.Sigmoid, scale=1.0), r=["mx8"], w=["mx8"])
        P.op("scalar", lambda e: e.activation(out=mx8[:, 10:11], in_=mx8[:, 8:9], func=AF.Sigmoid, scale=-1.0), r=["mx8"], w=["mx8"])
        P.op("vector", lambda e: e.tensor_scalar(out=lg[:, 8:16], in0=lg[:, 0:8], scalar1=mx8[:, 0:1], scalar2=mx8[:, 9:10], op0=ALU.is_equal, op1=ALU.mult), r=["lg", "mx8"], w=["lg"])
        P.op("vector", lambda e: e.tensor_scalar(out=lg[:, 16:24], in0=lg[:, 0:8], scalar1=mx8[:, 1:2], scalar2=mx8[:, 10:11], op0=ALU.is_equal, op1=ALU.mult), r=["lg", "mx8"], w=["lg"])
        P.op("vector", lambda e, ti=ti: e.tensor_tensor(out=comb[:, ti, :], in0=lg[:, 8:16], in1=lg[:, 16:24], op=ALU.add), r=["lg"], w=["comb"])


def build_TOK2(kind, n_tiles=32, TB=512):
    P = Tok(TB)
    NT = P.NT
    NTOK = n_tiles * 128
    dI = lambda n, s, dt=F32: P.dram(n, s, dt, "ExternalInput")
    dO = lambda n, s, dt=F32: P.dram(n, s, dt, "ExternalOutput")
    moe = kind in ("S2", "S5")
    has_next = kind in ("S2", "S4")
    x_d = dI("x", [NTOK, D])
    c_d = dI("c", [128, 8])
    aw, ab, ng = dI("ada_w", [D, 6 * D]), dI("ada_b", [6 * D]), dI("ng", [4 * D])
    if has_next:
        awn, abn, ngn = dI("ada_wn", [D, 6 * D]), dI("ada_bn", [6 * D]), dI("ngn", [4 * D])
        ho = dO("ho", [NTOK, D], BF16)
    xo = dO("xo", [NTOK, D])
    w_out = dI("w_out", [D, D])
    FF = 3584 if moe else 2816
    if moe:
        wr_d = dI("w_router", [128, 64])
        wg_d, wu_d, wd_d = dI("wg", [8, D, FF]), dI("wu", [8, D, FF]), dI("wd", [8, FF, D])
    else:
        wg_d, wu_d, wd_d = dI("wg", [D, FF]), dI("wu", [D, FF]), dI("wd", [FF, D])
    X = P.sb("X", [128, NT, D], F32)
    c = dict(hT=P.sb("hT", [128, 8, TB], BF16), Y2=P.sb("Y2", [128, NT, D], F32),
             sg=[P.sb("sg%d" % i, [128, 4, TB], BF16) for i in range(2)],
             gT=[P.sb("gT%d" % i, [128, 4, TB], BF16) for i in range(2)])
    if moe:
        c["h32"] = P.sb("h32", [128, NT, D], F32)
        c["hT32"] = P.sb("hT32", [128, 8, 128], F32)
        c["wr32"] = P.sb("wr32", [128, 8, 8], F32)
        c["lg"] = P.sb("lg", [128, 24], F32)
        c["mx8"] = P.sb("mx8", [128, 16], F32)
        c["comb"] = P.sb("comb", [128, NT, 8], F32)
        c["identf"] = P.sb("identf", [128, 128], F32)
        P.dma("sync", c["identf"][:], P.ident_d, w=["identf"])
        P.dma("sync", c["wr32"][:].rearrange("p a b -> p (a b)"), wr_d, w=["wr32"])
    if kind == "S2":
        mf_d, mb_d = dI("mf", [D, NTOK], BF16), dI("mb", [D, NTOK], BF16)
        mTs = [P.sb("mT0", [128, 8, TB], BF16), P.sb("mT1", [128, 8, TB], BF16)]
        msrc = [mf_d, mb_d]
    elif kind == "S4":
        mf_d = dI("mf", [D, NTOK], BF16)
        mTs = [P.sb("mT0", [128, 8, TB], BF16)]
        msrc = [mf_d]
    else:
        HW = 8
        h3_d = dI("h3T", [D, NTOK + 2 * HW], BF16)
        pw_in = dI("p_win", [D, D])
        pwg_d = dI("p_wg", [4, 256, 256])
        pbg_d, psc_d = dI("p_bg", [128, 8]), dI("p_scale", [128, 8])
        pfix_d = dI("p_fix", [128, 64])
        hTh = P.sb("hTh", [128, 8, TB + 2 * HW], BF16)
        pT = P.sb("pT", [128, TB + 2 * HW], F32)
        sA = P.sb("sA", [128, TB + 2 * HW], F32)
        sB = P.sb("sB", [128, TB + 2 * HW], F32)
        qT = P.sb("qT", [128, 8, TB], BF16)
        yT = P.sb("yT", [128, 8, TB], BF16)
        wgs = P.sb("wgs", [128, 4, 2, 256], BF16)
        P.dma("gpsimd", wgs[:].rearrange("p g c d -> p (g c) d"), pwg_d.rearrange("g (cc p) d -> p (g cc) d", p=128), w=["wgs"])
        pbg, psc, pfix = P.sb("pbg", [128, 8], F32), P.sb("psc", [128, 8], F32), P.sb("pfix", [128, 4, 16], F32)
        P.dma("sync", pbg[:], pbg_d, w=["pbg"])
        P.dma("sync", psc[:], psc_d, w=["psc"])
        P.dma("sync", pfix[:].rearrange("p a b -> p (a b)"), pfix_d, w=["pfix"])

    segs = [(2, "GT1", "mul", 1), (3, "SH2", "raw", 0), (4, "G2", "mul1p", 2), (5, "GT2", "mul", 3)]
    m = P.ada_mod("m", c_d, aw, ab, ng, segs)
    if has_next:
        mn = P.ada_mod("n", c_d, awn, abn, ngn, [(0, "SH1", "raw", 0), (1, "G1", "mul1p", 0)])
    rows = lambda d, t: d[t * 128:(t + 1) * 128, :]
    nblk = n_tiles // NT
    for b in range(nblk):
        ts = list(range(b * NT, (b + 1) * NT))
        nt = NT
        t0 = b * TB
        P.load_block(X, [rows(x_d, t) for t in ts], None)
        if kind in ("S2", "S4"):
            for mi, src in enumerate(msrc):
                P.dma("sync", mTs[mi][:], src.rearrange("(kc p) t -> p kc t", p=128)[:, :, t0:t0 + TB], w=["mT%d" % mi])
            P.mixer_out(X, nt, mTs, ["mT%d" % i for i in range(len(mTs))], w_out, 8, m["GT1"])
        else:
            NH = TB + 2 * HW
            P.dma("sync", hTh[:], h3_d.rearrange("(kc p) t -> p kc t", p=128)[:, :, t0:t0 + NH], w=["hTh"])
            wv = pw_in.rearrange("(kc p) n -> p kc n", p=128)
            for cg in range(2):
                wt, wk = P.load_w(wv[:, :, cg * 512:(cg + 1) * 512], 8, 512)
                for j in range(4):
                    cc = cg * 4 + j
                    win = (2, 4, 8, 16)[cc // 2]
                    for hf in range(2):
                        pa, pk = P.nA()
                        c0 = hf * (NH // 2)
                        for kc in range(8):
                            P.op("tensor", lambda e, kc=kc, j=j, wt=wt, pa=pa, c0=c0: e.matmul(pa[:, 0:NH // 2], lhsT=wt[:, kc, j * 128:(j + 1) * 128], rhs=hTh[:, kc, c0:c0 + NH // 2], start=(kc == 0), stop=(kc == 7)),
                                 r=[wk, "hTh"], w=[pk])
                        P.op("scalar", lambda e, pa=pa, c0=c0: e.copy(out=pT[:, c0:c0 + NH // 2], in_=pa[:, 0:NH // 2]), r=[pk], w=["pT"])
                    P.op("vector", lambda e: e.tensor_tensor(out=sA[:, 1:NH], in0=pT[:, 0:NH - 1], in1=pT[:, 1:NH], op=ALU.add), r=["pT", "sB"], w=["sA"])
                    cur, ck, oth, ok = sA, "sA", sB, "sB"
                    sh = 1
                    lo, hi = 1, NH
                    while sh * 2 < win:
                        l2, h2 = lo + sh, hi - sh
                        P.op("vector", lambda e, cur=cur, oth=oth, sh=sh, l2=l2, h2=h2: e.tensor_tensor(out=oth[:, l2:h2], in0=cur[:, l2 - sh:h2 - sh], in1=cur[:, l2 + sh:h2 + sh], op=ALU.add),
                             r=[ck], w=[ok])
                        cur, ck, oth, ok = oth, ok, cur, ck
                        lo, hi = l2, h2
                        sh *= 2
                    wi = cc // 2
                    P.op("vector", lambda e, cur=cur, win=win: e.tensor_scalar(out=cur[:, HW:HW + TB], in0=cur[:, HW:HW + TB], scalar1=1.0 / win, scalar2=None, op0=ALU.mult), r=[ck], w=[ck])
                    if b == 0:
                        P.op("vector", lambda e, cur=cur, wi=wi: e.tensor_tensor(out=cur[:, HW:HW + 8], in0=cur[:, HW:HW + 8], in1=pfix[:, wi, 0:8], op=ALU.mult), r=[ck, "pfix"], w=[ck])
                    if b == nblk - 1:
                        P.op("vector", lambda e, cur=cur, wi=wi: e.tensor_tensor(out=cur[:, HW + TB - 8:HW + TB], in0=cur[:, HW + TB - 8:HW + TB], in1=pfix[:, wi, 8:16], op=ALU.mult), r=[ck, "pfix"], w=[ck])
                    P.op("vector", lambda e, cur=cur, cc=cc: e.tensor_tensor(out=qT[:, cc, :], in0=cur[:, HW:HW + TB], in1=pT[:, HW:HW + TB], op=ALU.subtract), r=[ck, "pT"], w=["qT", "sA", "sB"])
            for cc in range(8):
                g = cc // 2
                pa, pk = P.nA()
                for ci in range(2):
                    P.op("tensor", lambda e, ci=ci, g=g, cc=cc, pa=pa: e.matmul(pa[:, :], lhsT=wgs[:, g, ci, (cc % 2) * 128:(cc % 2 + 1) * 128], rhs=qT[:, 2 * g + ci, :], start=(ci == 0), stop=(ci == 1)),
                         r=["wgs", "qT"], w=[pk])
                P.op("vector", lambda e, cc=cc, pa=pa: e.tensor_scalar(out=yT[:, cc, :], in0=pa[:, :], scalar1=pbg[:, cc:cc + 1], scalar2=psc[:, cc:cc + 1], op0=ALU.add, op1=ALU.mult), r=[pk, "pbg", "psc"], w=["yT"])
            P.mixer_out(X, nt, [yT], ["yT"], w_out, 8, m["GT1"])
        if moe:
            P.pre_T(X, nt, m["G2"], m["SH2"], c["hT"], want32=(c["h32"], "h32_"))
            tok_router(P, nt, c)
            for ex in range(8):
                comb = (lambda ti, ex=ex: c["comb"][:, ti, ex:ex + 1], "comb")
                P.ffn(X, nt, c, m["GT2"], wg_d[ex], wu_d[ex], wd_d[ex], FF, comb=comb, first=(ex == 0))
        else:
            P.pre_T(X, nt, m["G2"], m["SH2"], c["hT"])
            P.ffn(X, nt, c, m["GT2"], wg_d, wu_d, wd_d, FF)
        P.ffn_post(X, nt, c, m["GT2"])
        if has_next:
            P.out_block(X, nt, [rows(xo, t) for t in ts], [rows(ho, t) for t in ts], mn["G1"], mn["SH1"])
        else:
            P.out_block(X, nt, [rows(xo, t) for t in ts])
    return P

def build_S1(L=16384, CTX=256):
    P = Prog()
    SEQP = 2 + CTX + 4 + L + 2
    dI = lambda n, s, dt=F32: P.dram(n, s, dt, "ExternalInput")
    hseq = dI("hseq", [4, D, SEQP], BF16)
    wg_d, wx_d = dI("w_gate", [D, 128]), dI("w_xb", [D, 128])
    cw_d, cb_d = dI("conv_w", [4 * 128]), dI("conv_b", [128, 1])
    wa_d, wxx_d = dI("w_a", [2, 128, 128]), dI("w_x", [2, 128, 128])
    ba_d, bx_d, lam_d = dI("b_a", [128, 2]), dI("b_x", [128, 2]), dI("lam", [128, 2])
    m_d = P.dram("m", [4, 128, L], BF16, "ExternalOutput")
    Wg = P.sb("Wg", [128, 8, 128], BF16)
    Wx = P.sb("Wx", [128, 8, 128], BF16)
    Wxk = P.sb("Wxk", [128, 8, 4, 128], BF16)
    cw = P.sb("cw", [128, 4, 128], F32)
    cb = P.sb("cb", [128, 1], F32)
    wa = P.sb("wa", [128, 2, 128], BF16)
    wxx = P.sb("wxx", [128, 2, 128], BF16)
    ba, bx, lam, cl = P.sb("ba", [128, 2], F32), P.sb("bx", [128, 2], F32), P.sb("lam_s", [128, 2], F32), P.sb("cl", [128, 2], F32)
    st = P.sb("st", [128, 1], F32)
    P.dma("gpsimd", Wg[:], wg_d.rearrange("(kc p) n -> p kc n", p=128), w=["Wg"])
    P.dma("gpsimd", Wx[:], wx_d.rearrange("(kc p) n -> p kc n", p=128), w=["Wx"])
    P.dma("sync", cw[:].rearrange("p a b -> p (a b)"), bass.AP(tensor=cw_d.tensor, offset=0, ap=[[0, 128], [1, 512]]), w=["cw"])
    P.dma("sync", cb[:], cb_d, w=["cb"])
    P.dma("gpsimd", wa[:], wa_d.rearrange("d i j -> i d j"), w=["wa"])
    P.dma("gpsimd", wxx[:], wxx_d.rearrange("d i j -> i d j"), w=["wxx"])
    P.dma("sync", ba[:], ba_d, w=["ba"])
    P.dma("sync", bx[:], bx_d, w=["bx"])
    P.dma("sync", lam[:], lam_d, w=["lam"])
    for k in range(4):
        for kc in range(8):
            P.op("vector", lambda e, k=k, kc=kc: e.tensor_tensor(out=Wxk[:, kc, k, :], in0=Wx[:, kc, :], in1=cw[:, k, :], op=ALU.mult), r=["Wx", "cw"], w=["Wxk"])
    P.op("scalar", lambda e: e.activation(out=cl[:], in_=lam[:], func=AF.Exp, scale=-1.0), r=["lam"], w=["cl"])
    P.op("scalar", lambda e: e.activation(out=cl[:], in_=cl[:], func=AF.Ln, scale=1.0, bias=1.0), r=["cl"], w=["cl"])
    P.op("vector", lambda e: e.tensor_scalar(out=cl[:], in0=cl[:], scalar1=-8.0, scalar2=None, op0=ALU.mult), r=["cl"], w=["cl"])
    ps = [P.ps("ps%d" % i, [128, 512], F32) for i in range(6)]
    pi = [0]
    def nps():
        i = pi[0] % 6
        pi[0] += 1
        return ps[i], "ps%d" % i
    NB = 2
    hc = [P.sb("hc%d" % i, [128, 8, 516], BF16) for i in range(NB)]
    bufs = {}
    for nm, dt in (("xc32", F32), ("xcb", BF16), ("r", F32), ("ii", F32), ("a", F32), ("sq", F32), ("bb", F32), ("hs", F32), ("t1", F32), ("mo", BF16)):
        bufs[nm] = [P.sb("%s%d" % (nm, i), [128, 512], dt) for i in range(NB)]
    ci = 0
    for s in range(4):
        d = s // 2
        P.op("vector", lambda e: e.memset(st[:], 0.0), r=[], w=["st"])
        chunks = [(2, CTX, False, 0)] + [(2 + CTX + 4 + t0, min(512, L - t0), True, t0) for t0 in range(0, L, 512)]
        for (c0, n, is_lat, t0) in chunks:
            sl = ci % NB
            ci += 1
            B = {k: (v[sl], "%s%d" % (k, sl)) for k, v in bufs.items()}
            h, hk = hc[sl], "hc%d" % sl
            P.dma("sync", h[:, :, 0:n + 4], hseq[s].rearrange("(kc p) t -> p kc t", p=128)[:, :, c0 - 2:c0 + n + 2], w=[hk])
            px, pxk = nps()
            idx = 0
            for k in range(4):
                off = (k - 2) if d == 0 else (2 - k)
                for kc in range(8):
                    P.op("tensor", lambda e, k=k, kc=kc, off=off, px=px, h=h, n=n, idx=idx: e.matmul(px[:, 0:n], lhsT=Wxk[:, kc, k, :], rhs=h[:, kc, 2 + off:2 + off + n], start=(idx == 0), stop=(idx == 31)),
                         r=["Wxk", hk], w=[pxk])
                    idx += 1
            xc32, xk = B["xc32"]
            xcb, xbk = B["xcb"]
            P.op("scalar", lambda e, px=px, xc32=xc32, n=n: e.activation(out=xc32[:, 0:n], in_=px[:, 0:n], func=AF.Identity, bias=cb[:, 0:1], scale=1.0), r=[pxk, "cb"], w=[xk])
            P.op("vector", lambda e, xc32=xc32, xcb=xcb, n=n: e.tensor_copy(out=xcb[:, 0:n], in_=xc32[:, 0:n]), r=[xk], w=[xbk])
            pr, prk = nps()
            P.op("tensor", lambda e, pr=pr, xcb=xcb, n=n, d=d: e.matmul(pr[:, 0:n], lhsT=wa[:, d, :], rhs=xcb[:, 0:n], start=True, stop=True), r=["wa", xbk], w=[prk])
            pI, pik = nps()
            P.op("tensor", lambda e, pI=pI, xcb=xcb, n=n, d=d: e.matmul(pI[:, 0:n], lhsT=wxx[:, d, :], rhs=xcb[:, 0:n], start=True, stop=True), r=["wxx", xbk], w=[pik])
            r_, rk = B["r"]
            i_, ik = B["ii"]
            a_, ak = B["a"]
            sq, sqk = B["sq"]
            bb, bbk = B["bb"]
            hs, hsk = B["hs"]
            P.op("scalar", lambda e, pr=pr, r_=r_, n=n, d=d: e.activation(out=r_[:, 0:n], in_=pr[:, 0:n], func=AF.Sigmoid, bias=ba[:, d:d + 1], scale=1.0), r=[prk, "ba"], w=[rk])
            P.op("scalar", lambda e, pI=pI, i_=i_, n=n, d=d: e.activation(out=i_[:, 0:n], in_=pI[:, 0:n], func=AF.Sigmoid, bias=bx[:, d:d + 1], scale=1.0), r=[pik, "bx"], w=[ik])
            P.op("scalar", lambda e, r_=r_, a_=a_, n=n, d=d: e.activation(out=a_[:, 0:n], in_=r_[:, 0:n], func=AF.Exp, scale=cl[:, d:d + 1]), r=[rk, "cl"], w=[ak])
            P.op("scalar", lambda e, a_=a_, sq=sq, n=n: e.activation(out=sq[:, 0:n], in_=a_[:, 0:n], func=AF.Square), r=[ak], w=[sqk])
            P.op("scalar", lambda e, sq=sq, n=n: e.activation(out=sq[:, 0:n], in_=sq[:, 0:n], func=AF.Sqrt, scale=-1.0, bias=1.0), r=[sqk], w=[sqk])
            P.op("vector", lambda e, i_=i_, xc32=xc32, bb=bb, n=n: e.tensor_tensor(out=bb[:, 0:n], in0=i_[:, 0:n], in1=xc32[:, 0:n], op=ALU.mult), r=[ik, xk], w=[bbk])
            P.op("vector", lambda e, sq=sq, bb=bb, n=n: e.tensor_tensor(out=bb[:, 0:n], in0=bb[:, 0:n], in1=sq[:, 0:n], op=ALU.mult), r=[sqk, bbk], w=[bbk])
            P.op("vector", lambda e, a_=a_, bb=bb, hs=hs, n=n: e.tensor_tensor_scan(out=hs[:, 0:n], data0=a_[:, 0:n], data1=bb[:, 0:n], initial=st[:, 0:1], op0=ALU.mult, op1=ALU.add), r=[ak, bbk, "st"], w=[hsk])
            P.op("vector", lambda e, hs=hs, n=n: e.tensor_copy(out=st[:, 0:1], in_=hs[:, n - 1:n]), r=[hsk], w=["st"])
            if is_lat:
                pg, pgk = nps()
                for kc in range(8):
                    P.op("tensor", lambda e, kc=kc, pg=pg, h=h, n=n: e.matmul(pg[:, 0:n], lhsT=Wg[:, kc, :], rhs=h[:, kc, 2:2 + n], start=(kc == 0), stop=(kc == 7)), r=["Wg", hk], w=[pgk])
                t1, t1k = B["t1"]
                mo, mok = B["mo"]
                g = pg[:, 0:n]
                t = t1[:, 0:n]
                P.op("scalar", lambda e, t=t, g=g: e.activation(out=t, in_=g, func=AF.Square), r=[pgk], w=[t1k])
                P.op("vector", lambda e, t=t: e.tensor_scalar(out=t, in0=t, scalar1=0.044715, scalar2=1.0, op0=ALU.mult, op1=ALU.add), r=[t1k], w=[t1k])
                P.op("vector", lambda e, t=t, g=g: e.tensor_tensor(out=t, in0=t, in1=g, op=ALU.mult), r=[t1k, pgk], w=[t1k])
                P.op("scalar", lambda e, t=t: e.activation(out=t, in_=t, func=AF.Sigmoid, scale=1.5957691216057308), r=[t1k], w=[t1k])
                P.op("vector", lambda e, t=t, g=g: e.tensor_tensor(out=t, in0=t, in1=g, op=ALU.mult), r=[t1k, pgk], w=[t1k])
                P.op("vector", lambda e, t=t, hs=hs, mo=mo, n=n: e.tensor_tensor(out=mo[:, 0:n], in0=t, in1=hs[:, 0:n], op=ALU.mult), r=[t1k, hsk], w=[mok])
                P.dma("sync", m_d[s][:, t0:t0 + n], mo[:, 0:n], r=[mok])
    return P

def build_S3a(n_tiles=32):
    P = Prog()
    NTOK = n_tiles * 128
    dI = lambda n, s, dt=F32: P.dram(n, s, dt, "ExternalInput")
    hT_d = dI("hT", [D, NTOK + 2], BF16)
    w_d, cw_d, cb_d = dI("w_in", [D, 3072]), dI("conv_w", [3 * 3072]), dI("conv_b", [3072])
    z_d = P.dram("z", [NTOK, 3072], F32, "ExternalOutput")
    hT = P.sb("hT_s", [128, 8, NTOK + 2], BF16)
    P.dma("sync", hT[:], hT_d.rearrange("(kc p) t -> p kc t", p=128), w=["hT"])
    W = [P.sb("W%d" % i, [128, 8, 512], BF16) for i in range(2)]
    Wk = [P.sb("Wk%d" % i, [128, 3, 8, 512], BF16) for i in range(2)]
    cwb = [P.sb("cwb%d" % i, [128, 3, 512], F32) for i in range(2)]
    bb = [P.sb("bb%d" % i, [128, 512], F32) for i in range(2)]
    zo = [P.sb("zo%d" % i, [128, 512], F32) for i in range(3)]
    ps = [P.ps("ps%d" % i, [128, 512], F32) for i in range(4)]
    wv = w_d.rearrange("(kc p) n -> p kc n", p=128)
    n = 0
    for cb in range(6):
        s = cb % 2
        P.dma("gpsimd", W[s][:], wv[:, :, cb * 512:(cb + 1) * 512], w=["W%d" % s])
        for k in range(3):
            P.dma("sync", cwb[s][:, k, :], bass.AP(tensor=cw_d.tensor, offset=k * 3072 + cb * 512, ap=[[0, 128], [1, 512]]), w=["cwb%d_%d" % (s, k)])
        P.dma("sync", bb[s][:], bass.AP(tensor=cb_d.tensor, offset=cb * 512, ap=[[0, 128], [1, 512]]), w=["bb%d" % s])
        for k in range(3):
            for kc in range(8):
                eng = "vector" if (kc % 2 == 0) else "gpsimd"
                P.op(eng, lambda e, s=s, k=k, kc=kc: e.tensor_tensor(out=Wk[s][:, k, kc, :], in0=W[s][:, kc, :], in1=cwb[s][:, k, :], op=ALU.mult),
                     r=["W%d" % s, "cwb%d_%d" % (s, k)], w=["Wk%d_%d" % (s, k)])
        for ti in range(n_tiles):
            pa, pk = ps[n % 4], "ps%d" % (n % 4)
            zt, zk = zo[n % 3], "zo%d" % (n % 3)
            n += 1
            idx = 0
            for k in range(3):
                for kc in range(8):
                    P.op("tensor", lambda e, s=s, k=k, kc=kc, ti=ti, pa=pa, idx=idx: e.matmul(pa[:, :], lhsT=hT[:, kc, ti * 128 + k:ti * 128 + k + 128], rhs=Wk[s][:, k, kc, :], start=(idx == 0), stop=(idx == 23)),
                         r=["hT", "Wk%d_%d" % (s, k)], w=[pk])
                    idx += 1
            P.op("vector", lambda e, pa=pa, zt=zt, s=s: e.tensor_tensor(out=zt[:], in0=pa[:, :], in1=bb[s][:], op=ALU.add), r=[pk, "bb%d" % s], w=[zk])
            P.dma("sync", z_d[ti * 128:(ti + 1) * 128, cb * 512:(cb + 1) * 512], zt[:], r=[zk])
    return P


def build_S3b(L=16384, NCH=128):
    P = Prog()
    NB = L // 128
    NC2 = 2 * NB
    L2 = 2 * L
    NBLK = L2 // 512
    dI = lambda n, s, dt=F32: P.dram(n, s, dt, "ExternalInput")
    vrev_d, vnat_d = dI("vrev", [NCH, 128, NC2]), dI("vnat", [NCH, 128, NC2])
    x1_d, x2_d = dI("x1nat", [NCH, 128, NC2]), dI("x2rev", [NCH, 128, NC2])
    zT_d = [dI("zT1", [33, L2]), dI("zT2", [33, L2])]
    win_d = [dI("win1", [128, L2]), dI("win2", [128, L2])]
    w1_d, w2_d, w3_d = dI("f_w1", [33, 64]), dI("f_w2", [64, 64]), dI("f_w3", [64, 64])
    fb_d, fr_d = dI("f_b", [64, 3]), dI("f_freq", [64, 3])
    wo_d = dI("f_wout", [64, 4 * 128])
    hb_d = dI("hbias", [2 * 128])
    J_d = dI("J", [128, 128])
    y2_d = P.dram("y2rev", [NCH, 128, NC2], BF16, "ExternalOutput")
    Ktab = [P.dram("Ktab%d" % i, [128, L2], BF16, "Internal") for i in range(2)]
    Y1d = P.dram("Y1d", [NCH, 128, NC2], BF16, "Internal")
    w1, w2, w3 = P.sb("w1", [33, 64], F32), P.sb("w2", [64, 64], F32), P.sb("w3", [64, 64], F32)
    fb, fr, wo = P.sb("fb", [64, 3], F32), P.sb("fr", [64, 3], F32), P.sb("wo", [64, 4, 128], F32)
    hbias = P.sb("hbias_s", [128, 2, 128], F32)
    J = P.sb("J_s", [128, 128], BF16)
    for t, d_, k in ((w1, w1_d, "w1"), (w2, w2_d, "w2"), (w3, w3_d, "w3"), (fb, fb_d, "fb"), (fr, fr_d, "fr")):
        P.dma("sync", t[:], d_, w=[k])
    P.dma("sync", wo[:].rearrange("p a b -> p (a b)"), wo_d, w=["wo"])
    P.dma("sync", hbias[:].rearrange("p a b -> p (a b)"), bass.AP(tensor=hb_d.tensor, offset=0, ap=[[0, 128], [1, 256]]), w=["hbias"])
    P.dma("gpsimd", J[:], J_d, w=["J"])
    ps = [P.ps("ps%d" % i, [128, 512], F32) for i in range(8)]
    pi = [0]
    def nps():
        i = pi[0] % 8
        pi[0] += 1
        return ps[i], "ps%d" % i
    zt = [P.sb("zt%d" % i, [33, 512], F32) for i in range(2)]
    wn = [P.sb("wn%d" % i, [128, 512], F32) for i in range(2)]
    hd = [P.sb("hd%d" % i, [64, 512], F32) for i in range(2)]
    wi = [P.sb("wi%d" % i, [64, 512], I32) for i in range(2)]
    g1 = [P.sb("g1_%d" % i, [64, 512], F32) for i in range(2)]
    kb = [P.sb("kb%d" % i, [128, 512], BF16) for i in range(2)]
    TWO_PI = 6.283185307179586
    n = 0
    for tab in range(2):
        for blk in range(NBLK):
            s = n % 2
            n += 1
            m0 = blk * 512
            P.dma("sync", zt[s][:], zT_d[tab][:, m0:m0 + 512], w=["zt%d" % s])
            P.dma("sync", wn[s][:], win_d[tab][:, m0:m0 + 512], w=["wn%d" % s])
            src, skey, W_ = zt[s], "zt%d" % s, [w1, w2, w3]
            for ly in range(3):
                pa, pk = nps()
                nk = 33 if ly == 0 else 64
                P.op("tensor", lambda e, pa=pa, src=src, ly=ly, W_=W_, nk=nk: e.matmul(pa[0:64, :], lhsT=W_[ly][0:nk, :], rhs=src[0:nk, :], start=True, stop=True),
                     r=[skey, "w%d" % (ly + 1)], w=[pk])
                h, hk = hd[s], "hd%d" % s
                P.op("vector", lambda e, pa=pa, h=h, ly=ly: e.tensor_scalar(out=h[:], in0=pa[0:64, :], scalar1=fb[:, ly:ly + 1], scalar2=fr[:, ly:ly + 1], op0=ALU.add, op1=ALU.mult), r=[pk, "fb", "fr"], w=[hk])
                P.op("vector", lambda e, h=h: e.tensor_scalar(out=h[:], in0=h[:], scalar1=1.0 / TWO_PI, scalar2=None, op0=ALU.mult), r=[hk], w=[hk])
                P.op("vector", lambda e, h=h, s=s: e.tensor_copy(out=wi[s][:], in_=h[:]), r=[hk], w=["wi%d" % s])
                P.op("vector", lambda e, s=s: e.tensor_copy(out=g1[s][:], in_=wi[s][:]), r=["wi%d" % s], w=["g1_%d" % s])
                P.op("vector", lambda e, h=h, s=s: e.tensor_tensor(out=h[:], in0=h[:], in1=g1[s][:], op=ALU.subtract), r=[hk, "g1_%d" % s], w=[hk])
                P.op("vector", lambda e, h=h, s=s: e.tensor_scalar(out=g1[s][:], in0=h[:], scalar1=0.5, scalar2=None, op0=ALU.is_gt), r=[hk], w=["g1_%d" % s])
                P.op("vector", lambda e, h=h, s=s: e.tensor_tensor(out=h[:], in0=h[:], in1=g1[s][:], op=ALU.subtract), r=[hk, "g1_%d" % s], w=[hk])
                P.op("vector", lambda e, h=h, s=s: e.tensor_scalar(out=g1[s][:], in0=h[:], scalar1=-0.5, scalar2=None, op0=ALU.is_lt), r=[hk], w=["g1_%d" % s])
                P.op("vector", lambda e, h=h, s=s: e.tensor_tensor(out=h[:], in0=h[:], in1=g1[s][:], op=ALU.add), r=[hk, "g1_%d" % s], w=[hk])
                P.op("scalar", lambda e, h=h: e.activation(out=h[:], in_=h[:], func=AF.Sin, scale=TWO_PI), r=[hk], w=[hk])
                src, skey = h, hk
            pa, pk = nps()
            o = tab
            if tab == 0:
                dirs = [(1, 0, 512)] if blk < NBLK // 2 - 1 else ([(1, 0, 511), (0, 511, 512)] if blk == NBLK // 2 - 1 else [(0, 0, 512)])
            else:
                dirs = [(0, 0, 512)] if blk < NBLK // 2 else [(1, 0, 512)]
            for (dr, ca, cb_) in dirs:
                P.op("tensor", lambda e, pa=pa, src=src, o=o, dr=dr, ca=ca, cb_=cb_: e.matmul(pa[:, ca:cb_], lhsT=wo[:, o * 2 + dr, :], rhs=src[:, ca:cb_], start=True, stop=True),
                     r=[skey, "wo"], w=[pk])
            P.op("vector", lambda e, pa=pa, s=s: e.tensor_tensor(out=kb[s][:], in0=pa[:, :], in1=wn[s][:], op=ALU.mult), r=[pk, "wn%d" % s], w=["kb%d" % s])
            P.dma("sync", Ktab[tab][:, m0:m0 + 512], kb[s][:], r=["kb%d" % s], w=["Ktab%d" % tab])
    HALF = NB * 128
    T = [P.sb("T%d" % i, [128, HALF], BF16) for i in range(4)]
    vb = [P.sb("vb%d" % i, [128, NC2], BF16) for i in range(2)]
    gA = [P.sb("gA%d" % i, [128, NC2], F32) for i in range(2)]
    gB = [P.sb("gB%d" % i, [128, NC2], F32) for i in range(2)]
    tt = [P.sb("tt%d" % i, [128, NC2], F32) for i in range(2)]
    yo = [P.sb("yo%d" % i, [128, NC2], BF16) for i in range(2)]
    tn = 0
    n = 0
    for stage in range(2):
        for c in range(NCH):
            s = n % 2
            n += 1
            if stage == 0:
                P.dma("gpsimd", vb[s][:], vrev_d[c], w=["vb%d" % s])
                P.dma("sync", gA[s][:], vnat_d[c], w=["gA%d" % s])
                P.dma("sync", gB[s][:], x1_d[c], w=["gB%d" % s])
            else:
                P.dma("sync", vb[s][:], Y1d[c], r=["Y1d"], w=["vb%d" % s])
                P.dma("sync", gB[s][:], x2_d[c], w=["gB%d" % s])
                pj, pjk = nps()
                P.op("tensor", lambda e, pj=pj, s=s: e.matmul(pj[:, 0:NC2], lhsT=J[:], rhs=vb[s][:], start=True, stop=True), r=["J", "vb%d" % s], w=[pjk])
                P.op("scalar", lambda e, pj=pj, s=s: e.copy(out=gA[s][:], in_=pj[:, 0:NC2]), r=[pjk], w=["gA%d" % s])
            pa, pk = nps()
            pav = pa[:, 0:NC2].rearrange("p (b i) -> p b i", b=2)
            vv = vb[s][:].rearrange("p (b i) -> p b i", b=2)
            order = [NB - 1] + [k for k in range(2 * NB - 1) if k != NB - 1]
            loaded = {}
            cnt = 0
            for k in order:
                hf = 0 if k < NB else 1
                if hf not in loaded:
                    ts_ = tn % 4
                    tn += 1
                    nel = HALF if hf == 0 else HALF - 128
                    P.dma(("sync", "scalar")[tn % 2], T[ts_][:, 0:nel], bass.AP(tensor=Ktab[stage].tensor, offset=c * L2 + hf * HALF, ap=[[1, 128], [1, nel]]),
                          r=["Ktab%d" % stage], w=["T%d" % ts_])
                    loaded[hf] = ts_
                ts_ = loaded[hf]
                delta = (k - (NB - 1)) if stage == 0 else ((NB - 1) - k)
                i0, i1 = max(0, delta), min(NB, NB + delta)
                kk = k - hf * NB
                P.op("tensor", lambda e, pav=pav, vv=vv, ts_=ts_, kk=kk, i0=i0, i1=i1, delta=delta, cnt=cnt: e.matmul(pav[:, :, i0:i1], lhsT=T[ts_][:, kk * 128:(kk + 1) * 128], rhs=vv[:, :, i0 - delta:i1 - delta], start=(cnt == 0), stop=(cnt == 2 * NB - 2)),
                     r=["T%d" % ts_, "vb%d" % s], w=[pk])
                cnt += 1
            P.op("vector", lambda e, pa=pa, s=s, stage=stage, c=c: e.scalar_tensor_tensor(out=tt[s][:], in0=gA[s][:], scalar=hbias[:, stage, c:c + 1], in1=pa[:, 0:NC2], op0=ALU.mult, op1=ALU.add),
                 r=[pk, "gA%d" % s, "hbias"], w=["tt%d" % s])
            P.op("gpsimd", lambda e, s=s: e.tensor_tensor(out=yo[s][:], in0=tt[s][:], in1=gB[s][:], op=ALU.mult), r=["tt%d" % s, "gB%d" % s], w=["yo%d" % s])
            if stage == 0:
                P.dma("sync", Y1d[c], yo[s][:], r=["yo%d" % s], w=["Y1d"])
            else:
                P.dma("sync", y2_d[c], yo[s][:], r=["yo%d" % s])
    return P

def hyena_consts(L, ch0, nch):
    f32 = np.float32
    t = np.linspace(0.0, 1.0, L, dtype=f32)[:, None]
    w = (2.0 * np.pi * np.arange(L, dtype=f32)[:, None] / L).astype(f32)
    bands = np.linspace(1e-4, 15, 16, dtype=f32)[None, :]
    z = np.concatenate([t, np.cos(bands * w), np.sin(-bands * w)], axis=-1).astype(f32)
    max_decay = math.log(1e-2) / 0.3
    min_decay = math.log(1e-2) / 1.5
    deltas = np.abs(np.linspace(min_decay, max_decay, 1024, dtype=f32))
    window = np.exp(-t * deltas[None, ch0:ch0 + nch]).astype(f32)
    m = np.arange(2 * L)
    d1 = np.clip(np.abs(m - (L - 1)), 0, L - 1)
    d2 = np.clip(np.abs((L - 1) - m), 0, L - 1)
    zT1 = np.ascontiguousarray(z[d1].T)
    zT2 = np.ascontiguousarray(z[d2].T)
    win1 = np.zeros((128, 2 * L), f32)
    win2 = np.zeros((128, 2 * L), f32)
    win1[:nch] = window[d1].T
    win2[:nch] = window[d2].T
    return zT1, zT2, win1, win2

def _pos_embed(rows):
    f32 = np.float32
    r, col = np.meshgrid(np.arange(rows, dtype=f32), np.arange(64, dtype=f32), indexing='ij')
    quarter = D // 4
    omega = (1.0 / (f32(10000.0) ** (np.arange(quarter, dtype=f32) / f32(quarter)))).astype(f32)
    def sincos(p):
        ang = (p.reshape(-1, 1) * omega[None, :]).astype(f32)
        return np.concatenate([np.sin(ang), np.cos(ang)], axis=-1)
    return np.concatenate([sincos(r), sincos(col)], axis=-1).astype(f32)


_LAST_DBG = {}


def _run(P, in_maps):
    nc = P.emit()
    res = run_bass_kernel_spmd(nc, in_maps, core_ids=list(range(8)))
    return res.results


def _col8(v):
    return np.ascontiguousarray(np.asarray(v, np.float32).reshape(8, 128).T)


def kernel(**inp):
    f32 = np.float32
    bf = ml_dtypes.bfloat16
    inp = {k: np.asarray(v) for k, v in inp.items()}
    B, L = 2, 16384
    TPC = 4096
    ident = np.eye(128, dtype=f32)
    pos = _pos_embed(L // 64)
    dbg = _LAST_DBG
    dbg.clear()
    cb = lambda k: (k // 4, (k % 4) * TPC)
    wsT = np.ascontiguousarray(inp['gmlp_w_s'][0].transpose(2, 0, 1)).reshape(128, 1024)
    bs16 = np.ascontiguousarray(np.repeat(inp['gmlp_b_s'][0], 2, axis=0).reshape(-1))
    maps = []
    for k in range(8):
        b, t0 = cb(k)
        bc_, half = (k % 4) // 2, (k % 4) % 2
        maps.append(dict(ident=ident, x=inp['x'][b, t0:t0 + TPC], pos=pos[t0:t0 + TPC], ctx=inp['ctx'][bc_, half * 128:(half + 1) * 128],
                         c=_col8(inp['c'][b]), c_ctx=_col8(inp['c_ctx']),
                         ada_w0=inp['ada_w'][0], ada_b0=inp['ada_b'][0], ng0=inp['norm_g'][0].reshape(-1),
                         ada_w1=inp['ada_w'][1], ada_b1=inp['ada_b'][1], ng1=inp['norm_g'][1].reshape(-1),
                         g_win=inp['gmlp_w_in'][0], g_gv=inp['gmlp_g_v'][0], g_wsT=wsT, g_bs=bs16, g_wout=inp['gmlp_w_out'][0],
                         f_wg=inp['ffn_w_gate'][0], f_wu=inp['ffn_w_up'][0], f_wd=inp['ffn_w_down'][0]))
    r = _run(build_S0(), maps)
    x1 = np.stack([np.concatenate([r[b * 4 + j]['xo'] for j in range(4)], 0) for b in range(2)])
    h1 = np.stack([np.concatenate([r[b * 4 + j]['ho'] for j in range(4)], 0) for b in range(2)])
    h1c = np.stack([np.concatenate([r[bc_ * 2 + half]['hco'] for half in range(2)], 0) for bc_ in range(2)])
    dbg['x0'] = x1
    del r
    CTX = 256
    SEQP = 2 + CTX + 4 + L + 2
    hseq = np.zeros((4, D, SEQP), dtype=bf)
    for d in range(2):
        for b in range(2):
            cs, ls = h1c[b], h1[b]
            if d == 1:
                cs, ls = cs[::-1], ls[::-1]
            hseq[d * 2 + b, :, 2:2 + CTX] = cs.T
            hseq[d * 2 + b, :, 2 + CTX + 4:2 + CTX + 4 + L] = ls.T
    maps = []
    for k in range(8):
        sl = slice(k * 128, (k + 1) * 128)
        maps.append(dict(hseq=hseq, w_gate=np.ascontiguousarray(inp['lru_w_in'][0][:, sl]), w_xb=np.ascontiguousarray(inp['lru_w_in'][0][:, 1024 + k * 128:1024 + (k + 1) * 128]),
                         conv_w=np.ascontiguousarray(inp['lru_conv_w'][0][:, sl]).reshape(-1), conv_b=np.ascontiguousarray(inp['lru_conv_b'][0][sl].reshape(128, 1)),
                         w_a=np.ascontiguousarray(inp['lru_w_a'][0][:, k]), w_x=np.ascontiguousarray(inp['lru_w_x'][0][:, k]),
                         b_a=np.ascontiguousarray(inp['lru_b_a'][0][:, sl].T), b_x=np.ascontiguousarray(inp['lru_b_x'][0][:, sl].T),
                         lam=np.ascontiguousarray(inp['lru_lam'][0][:, sl].T)))
    r = _run(build_S1(L=L, CTX=CTX), maps)
    del hseq
    mf = [np.concatenate([r[k]['m'][b] for k in range(8)], 0) for b in range(2)]
    mb = [np.concatenate([r[k]['m'][2 + b][:, ::-1] for k in range(8)], 0) for b in range(2)]
    del r
    def wr_lay(w):
        return np.ascontiguousarray(w.reshape(8, 128, 8).transpose(1, 0, 2).reshape(128, 64))
    maps = []
    for k in range(8):
        b, t0 = cb(k)
        maps.append(dict(ident=ident, x=x1[b, t0:t0 + TPC], c=_col8(inp['c'][b]),
                         ada_w=inp['ada_w'][1], ada_b=inp['ada_b'][1], ng=inp['norm_g'][1].reshape(-1),
                         ada_wn=inp['ada_w'][2], ada_bn=inp['ada_b'][2], ngn=inp['norm_g'][2].reshape(-1),
                         w_out=inp['lru_w_out'][0], w_router=wr_lay(inp['moe_w_router'][0]),
                         wg=inp['moe_w_gate'][0], wu=inp['moe_w_up'][0], wd=inp['moe_w_down'][0],
                         mf=np.ascontiguousarray(mf[b][:, t0:t0 + TPC]), mb=np.ascontiguousarray(mb[b][:, t0:t0 + TPC])))
    r = _run(build_TOK2("S2"), maps)
    x2 = np.stack([np.concatenate([r[b * 4 + j]['xo'] for j in range(4)], 0) for b in range(2)])
    h2 = np.stack([np.concatenate([r[b * 4 + j]['ho'] for j in range(4)], 0) for b in range(2)])
    dbg['x1'] = x2
    del r, mf, mb, x1, h1
    maps = []
    for k in range(8):
        b, t0 = cb(k)
        hp = np.zeros((D, TPC + 2), dtype=bf)
        lo, hi = max(t0 - 1, 0), min(t0 + TPC + 1, L)
        hp[:, lo - (t0 - 1):hi - (t0 - 1)] = h2[b, lo:hi].T
        maps.append(dict(hT=hp, w_in=inp['hyena_w_in'][0], conv_w=inp['hyena_conv_w'][0].reshape(-1), conv_b=inp['hyena_conv_b'][0]))
    r = _run(build_S3a(), maps)
    z = np.stack([np.concatenate([r[b * 4 + j]['z'] for j in range(4)], 0) for b in range(2)])
    del r, h2
    NB = L // 128
    def lay(a, rev):
        t = a.reshape(2, NB, 128, 128).transpose(3, 2, 0, 1)
        if rev:
            t = t[:, ::-1]
        return np.ascontiguousarray(t.reshape(128, 128, 2 * NB))
    fbias = np.ascontiguousarray(np.stack([inp['hyena_f_b1'][0], inp['hyena_f_b2'][0], inp['hyena_f_b3'][0]], 1))
    ffreq = np.ascontiguousarray(inp['hyena_f_freq'][0].T)
    Jm = np.ascontiguousarray(np.eye(128, dtype=f32)[::-1])
    maps = []
    for k in range(8):
        ch0 = k * 128
        zT1, zT2, win1, win2 = hyena_consts(L, ch0, 128)
        wo = np.ascontiguousarray(inp['hyena_f_wout'][0].reshape(64, 4, 1024)[:, :, ch0:ch0 + 128]).reshape(64, 512)
        maps.append(dict(vrev=lay(z[:, :, 2048 + ch0:2048 + ch0 + 128], True), vnat=lay(z[:, :, 2048 + ch0:2048 + ch0 + 128], False),
                         x1nat=lay(z[:, :, ch0:ch0 + 128], False), x2rev=lay(z[:, :, 1024 + ch0:1024 + ch0 + 128], True),
                         zT1=zT1, zT2=zT2, win1=win1, win2=win2, f_w1=inp['hyena_f_w1'][0], f_w2=inp['hyena_f_w2'][0], f_w3=inp['hyena_f_w3'][0],
                         f_b=fbias, f_freq=ffreq, f_wout=wo, hbias=np.ascontiguousarray(inp['hyena_bias'][0][:, ch0:ch0 + 128]).reshape(-1), J=Jm))
    r = _run(build_S3b(L=L, NCH=128), maps)
    del z
    m2 = np.zeros((2, D, L), dtype=bf)
    for k in range(8):
        y = r[k]['y2rev'][:, ::-1]
        m2[:, k * 128:(k + 1) * 128, :] = y.reshape(128, 128, 2, NB).transpose(2, 0, 3, 1).reshape(2, 128, L)
    del r
    maps = []
    for k in range(8):
        b, t0 = cb(k)
        maps.append(dict(ident=ident, x=x2[b, t0:t0 + TPC], c=_col8(inp['c'][b]),
                         ada_w=inp['ada_w'][2], ada_b=inp['ada_b'][2], ng=inp['norm_g'][2].reshape(-1),
                         ada_wn=inp['ada_w'][3], ada_bn=inp['ada_b'][3], ngn=inp['norm_g'][3].reshape(-1),
                         w_out=inp['hyena_w_out'][0], wg=inp['ffn_w_gate'][1], wu=inp['ffn_w_up'][1], wd=inp['ffn_w_down'][1],
                         mf=np.ascontiguousarray(m2[b][:, t0:t0 + TPC])))
    r = _run(build_TOK2("S4"), maps)
    x3 = np.stack([np.concatenate([r[b * 4 + j]['xo'] for j in range(4)], 0) for b in range(2)])
    h3 = np.stack([np.concatenate([r[b * 4 + j]['ho'] for j in range(4)], 0) for b in range(2)])
    dbg['x2'] = x3
    del r, m2, x2
    HW = 8
    maps = []
    for k in range(8):
        b, t0 = cb(k)
        hp = np.zeros((D, TPC + 2 * HW), dtype=bf)
        lo, hi = max(t0 - HW, 0), min(t0 + TPC + HW, L)
        hp[:, lo - (t0 - HW):hi - (t0 - HW)] = h3[b, lo:hi].T
        fix = np.ones((4, 16), f32)
        for wi_, win in enumerate((2, 4, 8, 16)):
            for j in range(8):
                if k % 4 == 0:
                    t = j
                    fix[wi_, j] = win / float(min(t + win // 2, L) - max(t - win // 2, 0))
                if k % 4 == 3:
                    t = L - 8 + j
                    fix[wi_, 8 + j] = win / float(min(t + win // 2, L) - max(t - win // 2, 0))
        maps.append(dict(ident=ident, x=x3[b, t0:t0 + TPC], c=_col8(inp['c'][b]),
                         ada_w=inp['ada_w'][3], ada_b=inp['ada_b'][3], ng=inp['norm_g'][3].reshape(-1),
                         w_out=inp['pool_w_out'][0], w_router=wr_lay(inp['moe_w_router'][1]),
                         wg=inp['moe_w_gate'][1], wu=inp['moe_w_up'][1], wd=inp['moe_w_down'][1],
                         h3T=hp, p_win=inp['pool_w_in'][0], p_wg=inp['pool_w_g'][0], p_bg=_col8(inp['pool_b_g'][0].reshape(-1)),
                         p_scale=_col8(inp['pool_scale'][0]), p_fix=np.ascontiguousarray(np.broadcast_to(fix.reshape(1, 64), (128, 64)))))
    r = _run(build_TOK2("S5"), maps)
    out = np.stack([np.concatenate([r[b * 4 + j]['xo'] for j in range(4)], 0) for b in range(2)]).astype(f32)
    return out
```

```python
import os, math
import ml_dtypes
import numpy as np
from contextlib import ExitStack
import concourse.bass as bass
import concourse.mybir as mybir
from concourse.bass_utils import run_bass_kernel_spmd

F32 = mybir.dt.float32
BF16 = mybir.dt.bfloat16
I32 = mybir.dt.int32
AF = mybir.ActivationFunctionType
ALU = mybir.AluOpType
AX = mybir.AxisListType

COMPUTE = ("tensor", "vector", "scalar", "gpsimd")
DMAQ = ("sync", "gpsimd")
SAME_ENGINE_SYNC = True


class Prog:
    def __init__(self, name="k"):
        self.nc = bass.Bass("TRN2", target_bir_lowering=False)
        self.es = ExitStack()
        self.ops = []
        self.last_w = {}
        self.readers = {}
        self.n_dma_sems = {"sync": 24, "gpsimd": 24, "scalar": 16}
        self.names = set()

    def dram(self, name, shape, dt, kind):
        return self.nc.dram_tensor(name, list(shape), dt, kind=kind).ap()

    def sb(self, name, shape, dt):
        assert name not in self.names, name
        self.names.add(name)
        return self.es.enter_context(self.nc.sbuf_tensor(name, list(shape), dt))

    def ps(self, name, shape, dt):
        assert name not in self.names, name
        self.names.add(name)
        return self.es.enter_context(self.nc.psum_tensor(name, list(shape), dt))

    def op(self, eng, fn, r=(), w=(), dma=False):
        i = len(self.ops)
        deps = set()
        for k in r:
            if k in self.last_w:
                deps.add(self.last_w[k])
        for k in w:
            if k in self.last_w:
                deps.add(self.last_w[k])
            for j in self.readers.get(k, ()):
                deps.add(j)
        deps.discard(i)
        self.ops.append(dict(eng=eng, fn=fn, deps=sorted(deps), dma=dma))
        for k in r:
            self.readers.setdefault(k, []).append(i)
        for k in w:
            self.last_w[k] = i
            self.readers[k] = []
        return i

    def dma(self, q, out, in_, r=(), w=(), **kw):
        return self.op(q, lambda e: e.dma_start(out=out, in_=(in_() if callable(in_) else in_), **kw), r, w, dma=True)

    def emit(self):
        nc = self.nc
        ops = self.ops
        es = self.es
        eng_ops = {}
        for i, o in enumerate(ops):
            lst = eng_ops.setdefault(o["eng"], [])
            o["pos"] = len(lst)
            lst.append(i)
        dcount = {}
        for i, o in enumerate(ops):
            if o["dma"]:
                q = o["eng"]
                o["dnum"] = dcount.get(q, 0)
                dcount[q] = o["dnum"] + 1
        seen = {e: {} for e in eng_ops}
        seen_dma = {e: set() for e in eng_ops}
        for i, o in enumerate(ops):
            X = o["eng"]
            waits = []
            best = {}
            for j in o["deps"]:
                p = ops[j]
                if p["dma"]:
                    if j in seen_dma[X]:
                        continue
                    seen_dma[X].add(j)
                    waits.append(j)
                else:
                    E = p["eng"]
                    if E == X and (E == "tensor" or not SAME_ENGINE_SYNC):
                        continue
                    if E not in best or ops[best[E]]["pos"] < p["pos"]:
                        best[E] = j
            for E, j in best.items():
                p = ops[j]
                if seen[X].get(E, -1) >= p["pos"]:
                    continue
                seen[X][E] = p["pos"]
                waits.append(j)
            o["waits"] = waits
            for j in waits:
                ops[j]["sig"] = True
        sigcount = {}
        for i, o in enumerate(ops):
            if o["dma"]:
                continue
            if o.get("sig"):
                E = o["eng"]
                sigcount[E] = sigcount.get(E, 0) + 1
                o["signum"] = sigcount[E]
        csem = {e: es.enter_context(nc.semaphore("c_" + e)) for e in COMPUTE}
        dsem = {q: [es.enter_context(nc.semaphore("d_%s_%d" % (q, k))) for k in range(self.n_dma_sems[q])]
                for q in dcount}
        self.stats = {e: len(v) for e, v in eng_ops.items()}
        self.stats["sig"] = dict(sigcount)

        def waitspec(j):
            p = ops[j]
            if p["dma"]:
                K = self.n_dma_sems[p["eng"]]
                return dsem[p["eng"]][p["dnum"] % K], 16 * (p["dnum"] // K + 1)
            return csem[p["eng"]], p["signum"]

        def run_engine(ename, e):
            for i in eng_ops.get(ename, []):
                o = ops[i]
                for j in o["waits"]:
                    s, v = waitspec(j)
                    e.wait_ge(s, v)
                if o["dma"]:
                    K = self.n_dma_sems[ename]
                    m = o["dnum"]
                    if m >= K:
                        e.wait_ge(dsem[ename][m % K], 16 * (m // K))
                    ins = o["fn"](e)
                    ins.then_inc(dsem[ename][m % K], 16)
                else:
                    ins = o["fn"](e)
                    if o.get("sig"):
                        ins.then_inc(csem[ename], 1)
            if ename in dsem:
                K = self.n_dma_sems[ename]
                n = dcount[ename]
                for k in range(min(K, n)):
                    cnt = (n - 1 - k) // K + 1
                    e.wait_ge(dsem[ename][k], 16 * cnt)

        with nc.Block() as block:
            @block.sync
            def _(e):
                run_engine("sync", e)

            @block.tensor
            def _(e):
                run_engine("tensor", e)

            @block.vector
            def _(e):
                run_engine("vector", e)

            @block.scalar
            def _(e):
                run_engine("scalar", e)

            @block.gpsimd
            def _(e):
                run_engine("gpsimd", e)
        self.es.close()
        return nc
D = 1024
EPS = 1e-6


class Tok(Prog):
    def __init__(self, TB, nwsl=3):
        super().__init__()
        self.nwsl = nwsl
        self.TB = TB
        self.NT = TB // 128
        self.ident_d = self.dram("ident", [128, 128], F32, "ExternalInput")
        self.ident = self.sb("ident_sb", [128, 128], BF16)
        self.dma("gpsimd", self.ident[:], self.ident_d, w=["ident"])
        self.ones = self.sb("ones", [1, 128], F32)
        self.op("vector", lambda e: e.memset(self.ones[:], 1.0), w=["ones"])
        self.psA = [self.ps("psA%d" % i, [128, 512], F32) for i in range(3)]
        self.psY = [self.ps("psY%d" % i, [128, 1024], F32) for i in range(2)]
        self.psT = self.ps("psT", [128, 1024], BF16)
        self.wsl = [self.sb("wsl%d" % i, [128, 8192], BF16) for i in range(nwsl)]
        self.wi = 0
        self.ai = 0
        self.yi = 0
        self.tmpf = [self.sb("tmpf%d" % i, [128, 1024], F32) for i in range(2)]
        self.ti = 0
        self.hbf = [self.sb("hbf%d" % i, [128, 1024], BF16) for i in range(2)]
        self.hi = 0
        self.stat = self.sb("stat", [128, 64], F32)
        self.si = 0
        self.nrow = 1 if nwsl == 2 else 2
        self.rowbuf = self.sb("rowbuf", [1, 1024 * self.nrow], F32)
        self.si2 = 0
        self.csb = self.sb("csb", [128, 8], F32)
        self.csl = self.sb("csl", [128, 8], BF16)
        self.junk = self.sb("junk", [128, 1024], BF16)

    def nw(self):
        i = self.wi % self.nwsl
        self.wi += 1
        return self.wsl[i], "wsl%d" % i

    def nA(self):
        i = self.ai % 3
        self.ai += 1
        return self.psA[i], "psA%d" % i

    def nY(self):
        i = self.yi % 2
        self.yi += 1
        return self.psY[i], "psY%d" % i

    def ntmp(self):
        i = self.ti % 2
        self.ti += 1
        return self.tmpf[i], "tmpf%d" % i

    def nh(self):
        i = self.hi % 2
        self.hi += 1
        return self.hbf[i], "hbf%d" % i

    def nstat(self, n=1):
        if self.si + n > 64:
            self.si = 0
        c = self.si
        self.si += n
        return c

    def load_w(self, src, a, b):
        t, key = self.nw()
        dst = t[:, 0:a * b].rearrange("p (a b) -> p a b", a=a)
        self.dma("gpsimd", dst, src, w=[key])
        return dst, key

    def load_w_ind(self, Wrows, idx, ikey, a, b, elem_off):
        t, key = self.nw()
        dst = t[:, 0:a * b].rearrange("p (a b) -> p a b", a=a)
        for kc in range(a):
            self.op("gpsimd", lambda e, kc=kc: e.indirect_dma_start(out=dst[:, kc, :], out_offset=None, in_=Wrows, in_offset=bass.IndirectOffsetOnAxis(ap=idx[:, kc:kc + 1], axis=0), element_offset=elem_off),
                    r=[ikey], w=[key], dma=True)
        return dst, key

    def ffn_sp(self, nt, c, b, wg_l, wu_l, wd_l, IH):
        ntok = nt * 128
        hT, Y2 = c["hT"], c["Y2"]
        WS = c["WS"]
        first = True
        for q in range(4):
            wts = []
            for Wl in (wg_l, wu_l, wd_l):
                i = c["wsi"][0] % len(WS)
                c["wsi"][0] += 1
                wt, wk = WS[i], "WS%d" % i
                self.op("gpsimd", lambda e, wt=wt, Wl=Wl, q=q: e.indirect_dma_start(out=wt, out_offset=None, in_=Wl[0],
                                                                               in_offset=bass.IndirectOffsetOnAxis(ap=IH[:, b, q:q + 1], axis=0)),
                        r=["IH", "SLI", "EIDI", Wl[1]], w=[wk], dma=True)
                wts.append((wt, wk))
            (wtg, wgk), (wtu, wuk), (wtd, wdk) = wts
            vg = wtg.rearrange("p (k f) -> p k f", k=8)
            vu = wtu.rearrange("p (k f) -> p k f", k=8)
            vd = wtd.rearrange("p (k n) -> p k n", k=7)
            for fbh in range(2):
                nch = (4, 3)[fbh]
                j0 = fbh * 4
                sl = c["fbi"][0] % 2
                c["fbi"][0] += 1
                sg, sgk = c["sg"][sl], "sg%d" % sl
                gT, gk = c["gT"][sl], "gT%d" % sl
                for j in range(nch):
                    pa, pk = self.nA()
                    for kc in range(8):
                        self.op("tensor", lambda e, kc=kc, j=j, pa=pa, vg=vg, j0=j0: e.matmul(pa[:, 0:ntok], lhsT=vg[:, kc, (j0 + j) * 128:(j0 + j + 1) * 128], rhs=hT[:, kc, 0:ntok], start=(kc == 0), stop=(kc == 7)),
                                r=[wgk, "hT"], w=[pk])
                    self.op("scalar", lambda e, j=j, pa=pa, sg=sg: e.activation(out=sg[:, j, 0:ntok], in_=pa[:, 0:ntok], func=AF.Silu), r=[pk], w=[sgk])
                for j in range(nch):
                    pa, pk = self.nA()
                    for kc in range(8):
                        self.op("tensor", lambda e, kc=kc, j=j, pa=pa, vu=vu, j0=j0: e.matmul(pa[:, 0:ntok], lhsT=vu[:, kc, (j0 + j) * 128:(j0 + j + 1) * 128], rhs=hT[:, kc, 0:ntok], start=(kc == 0), stop=(kc == 7)),
                                r=[wuk, "hT"], w=[pk])
                    self.op("vector", lambda e, j=j, pa=pa, sg=sg, gT=gT: e.tensor_tensor(out=gT[:, j, 0:ntok], in0=sg[:, j, 0:ntok], in1=pa[:, 0:ntok], op=ALU.mult), r=[pk, sgk], w=[gk])
                for ti in range(nt):
                    py, pyk = self.nY()
                    for hf in range(2):
                        for j in range(nch):
                            self.op("tensor", lambda e, j=j, hf=hf, ti=ti, py=py, gT=gT, vd=vd, j0=j0, nch=nch: e.matmul(py[:, hf * 512:(hf + 1) * 512], lhsT=gT[:, j, ti * 128:(ti + 1) * 128], rhs=vd[:, j0 + j, hf * 512:(hf + 1) * 512], start=(j == 0), stop=(j == nch - 1)),
                                    r=[wdk, gk], w=[pyk])
                    yk = "Y2_%d" % ti
                    if first:
                        self.op("scalar", lambda e, ti=ti, py=py: e.copy(out=Y2[:, ti, :], in_=py[:, :]), r=[pyk], w=[yk])
                    else:
                        self.op("vector", lambda e, ti=ti, py=py: e.tensor_tensor(out=Y2[:, ti, :], in0=py[:, :], in1=Y2[:, ti, :], op=ALU.add), r=[pyk, yk], w=[yk])
                first = False

    def bc_dram(self, dst, dkey, src1d, off, n, q="sync"):
        ap = bass.AP(tensor=src1d.tensor, offset=off, ap=[[0, 128], [1, n]])
        self.dma(q, dst, ap, w=[dkey])

    def ada_mod(self, pref, cvec, ada_w, ada_b, normg, segs):
        self.dma("sync", self.csb[:], cvec, w=["csb"])
        self.op("scalar", lambda e: e.activation(out=self.csl[:], in_=self.csb[:], func=AF.Silu), r=["csb"], w=["csl"])
        wv = ada_w.rearrange("(kc p) n -> p kc n", p=128)
        bv = ada_b.rearrange("(o n) -> o n", o=1)
        rb = self.rowbuf
        out = {}
        for (seg, name, kind, gidx) in segs:
            t = self.get_tile(pref + name)
            tk = pref + name
            for hb in range(2):
                nb = seg * 2 + hb
                sl = self.si2 % self.nrow
                self.si2 += 1
                rk = "row%d" % sl
                self.dma("sync", rb[0:1, sl * 1024 + 512:sl * 1024 + 1024], bv[0:1, nb * 512:(nb + 1) * 512], w=[rk + "b"])
                wt, wk = self.load_w(wv[:, :, nb * 512:(nb + 1) * 512], 8, 512)
                pa, pk = self.nA()
                for kc in range(8):
                    self.op("tensor", lambda e, kc=kc, wt=wt, pa=pa: e.matmul(pa[0:1, :], lhsT=self.csl[:, kc:kc + 1], rhs=wt[:, kc, :], start=(kc == 0), stop=(kc == 7)),
                            r=[wk, "csl"], w=[pk])
                self.op("vector", lambda e, sl=sl, pa=pa: e.tensor_tensor(out=rb[0:1, sl * 1024:sl * 1024 + 512], in0=pa[0:1, :], in1=rb[0:1, sl * 1024 + 512:sl * 1024 + 1024], op=ALU.add),
                        r=[pk, rk + "b"], w=[rk])
                pb, pbk = self.nA()
                self.op("tensor", lambda e, sl=sl, pb=pb: e.matmul(pb[:, :], lhsT=self.ones[0:1, :], rhs=rb[0:1, sl * 1024:sl * 1024 + 512], start=True, stop=True),
                        r=[rk, "ones"], w=[pbk])
                self.op("scalar", lambda e, hb=hb, pb=pb, t=t: e.copy(out=t[:, hb * 512:(hb + 1) * 512], in_=pb[:, :]), r=[pbk], w=[tk])
            if kind != "raw":
                tmp, tmk = self.ntmp()
                self.bc_dram(tmp[:], tmk, normg, gidx * D, D)
                if kind == "mul1p":
                    self.op("vector", lambda e, t=t, tmp=tmp: e.scalar_tensor_tensor(out=t[:], in0=t[:], scalar=1.0, in1=tmp[:], op0=ALU.add, op1=ALU.mult), r=[tk, tmk], w=[tk])
                else:
                    self.op("vector", lambda e, t=t, tmp=tmp: e.tensor_tensor(out=t[:], in0=t[:], in1=tmp[:], op=ALU.mult), r=[tk, tmk], w=[tk])
            out[name] = (t, tk)
        return out

    def get_tile(self, name, shape=None, dt=F32):
        if not hasattr(self, "_tiles"):
            self._tiles = {}
        if name not in self._tiles:
            self._tiles[name] = self.sb(name, shape or [128, D], dt)
        return self._tiles[name]

    def rstd_of(self, srcs, skeys):
        n = len(srcs)
        c = self.nstat(n + 2)
        st = self.stat
        self.op("vector", lambda e: e.memset(st[:, c:c + n], 0.0), w=["stat"])
        for i, s in enumerate(srcs):
            self.op("scalar", lambda e, s=s, i=i: e.activation(out=self.junk[:, 0:s.shape[-1]], in_=s, func=AF.Square, accum_out=st[:, c + i:c + i + 1]),
                    r=list(skeys), w=["junk", "stat"])
        if n == 2:
            self.op("vector", lambda e: e.tensor_tensor(out=st[:, c:c + 1], in0=st[:, c:c + 1], in1=st[:, c + 1:c + 2], op=ALU.add), r=["stat"], w=["stat"])
        self.op("scalar", lambda e: e.activation(out=st[:, c + n:c + n + 1], in_=st[:, c:c + 1], func=AF.Sqrt, scale=1.0 / D, bias=EPS), r=["stat"], w=["stat"])
        self.op("vector", lambda e: e.reciprocal(out=st[:, c + n + 1:c + n + 2], in_=st[:, c + n:c + n + 1]), r=["stat"], w=["stat"])
        return st[:, c + n + 1:c + n + 2]

    def norm_mod(self, xt, xkey, Gt, SHt, out_bf, okey):
        rs = self.rstd_of([xt], [xkey])
        tmp, tk = self.ntmp()
        self.op("vector", lambda e: e.scalar_tensor_tensor(out=tmp[:], in0=xt, scalar=rs, in1=Gt[0][:], op0=ALU.mult, op1=ALU.mult),
                r=[xkey, "stat", Gt[1]], w=[tk])
        self.op("gpsimd", lambda e: e.tensor_tensor(out=out_bf, in0=tmp[:], in1=SHt[0][:], op=ALU.add), r=[tk, SHt[1]], w=[okey])

    def transpose_to(self, src_bf, skey, dstT, dkey, tcol, nk=8):
        pv = self.psT[:, 0:nk * 128].rearrange("p (a b) -> p a b", a=nk)
        for kc in range(nk):
            self.op("tensor", lambda e, kc=kc: e.transpose(out=pv[:, kc, :], in_=src_bf[:, kc * 128:(kc + 1) * 128], identity=self.ident[:]),
                    r=[skey, "ident"], w=["psT"])
        self.op("vector", lambda e: e.tensor_copy(out=dstT[:, 0:nk, tcol:tcol + 128], in_=pv), r=["psT"], w=[dkey])

    def post_res(self, ypsum, ykey, xt, xkey, GTt):
        rs = self.rstd_of([ypsum[:, 0:512], ypsum[:, 512:1024]], [ykey])
        tmp, tk = self.ntmp()
        self.op("vector", lambda e: e.scalar_tensor_tensor(out=tmp[:], in0=ypsum[:, :], scalar=rs, in1=GTt[0][:], op0=ALU.mult, op1=ALU.mult),
                r=[ykey, "stat", GTt[1]], w=[tk])
        self.op("gpsimd", lambda e: e.tensor_tensor(out=xt, in0=xt, in1=tmp[:], op=ALU.add), r=[tk, xkey], w=[xkey])

    def post_res_sb(self, ysb, ykey, xt, xkey, GTt):
        rs = self.rstd_of([ysb], [ykey, "uT"])
        tmp, tk = self.ntmp()
        self.op("vector", lambda e: e.scalar_tensor_tensor(out=tmp[:], in0=ysb, scalar=rs, in1=GTt[0][:], op0=ALU.mult, op1=ALU.mult),
                r=[ykey, "stat", GTt[1]], w=[tk])
        self.op("gpsimd", lambda e: e.tensor_tensor(out=xt, in0=xt, in1=tmp[:], op=ALU.add), r=[tk, xkey], w=[xkey])

    def lin_fm(self, W, KCn, hT, hkey, t0, ntok, nchunks, evac, group=4):
        def wsrc(a, b_):
            if callable(W):
                return lambda: W().rearrange("(kc p) n -> p kc n", p=128)[:, :, a:b_]
            return W.rearrange("(kc p) n -> p kc n", p=128)[:, :, a:b_]
        cols_per_load = 8192 // KCn // 128 * 128
        cols_per_load = min(cols_per_load, 512)
        i = 0
        while i < len(nchunks):
            n0 = nchunks[i]
            run = [n0]
            while len(run) * 128 < cols_per_load and i + len(run) < len(nchunks) and nchunks[i + len(run)] == run[-1] + 1:
                run.append(run[-1] + 1)
            if isinstance(W, dict):
                wt, wk = self.load_w_ind(W["rows"], W["idx"], W["ikey"], KCn, 512, (n0 // 4) * 1024 * 512)
            else:
                wt, wk = self.load_w(wsrc(n0 * 128, (n0 + len(run)) * 128), KCn, len(run) * 128)
            for j, ncx in enumerate(run):
                pa, pk = self.nA()
                for kc in range(KCn):
                    self.op("tensor", lambda e, kc=kc, j=j, wt=wt, pa=pa: e.matmul(pa[:, 0:ntok], lhsT=wt[:, kc, j * 128:(j + 1) * 128], rhs=hT[:, kc, t0:t0 + ntok], start=(kc == 0), stop=(kc == KCn - 1)),
                            r=[wk, hkey], w=[pk])
                evac(ncx, pa[:, 0:ntok], pk)
            i += len(run)

    def gelu_evac(self, ps, pk, out, okey):
        n = ps.shape[-1]
        tmp, tk = self.ntmp()
        t = tmp[:, 0:n]
        self.op("scalar", lambda e: e.activation(out=t, in_=ps, func=AF.Square), r=[pk], w=[tk])
        self.op("vector", lambda e: e.tensor_scalar(out=t, in0=t, scalar1=0.044715, scalar2=1.0, op0=ALU.mult, op1=ALU.add), r=[tk], w=[tk])
        self.op("vector", lambda e: e.tensor_tensor(out=t, in0=t, in1=ps, op=ALU.mult), r=[tk, pk], w=[tk])
        self.op("scalar", lambda e: e.activation(out=t, in_=t, func=AF.Sigmoid, scale=1.5957691216057308), r=[tk], w=[tk])
        self.op("vector", lambda e: e.tensor_tensor(out=out, in0=t, in1=ps, op=ALU.mult), r=[tk, pk], w=[okey])

    def load_block(self, X, srcs, pos=None):
        for ti, s in enumerate(srcs):
            self.dma("sync", X[:, ti, :], s, w=["X%d" % ti])
            if pos is not None:
                tmp, tk = self.ntmp()
                self.dma("sync", tmp[:], pos[ti], w=[tk])
                self.op("gpsimd", lambda e, ti=ti, tmp=tmp: e.tensor_tensor(out=X[:, ti, :], in0=X[:, ti, :], in1=tmp[:], op=ALU.add), r=[tk, "X%d" % ti], w=["X%d" % ti])

    def pre_T(self, X, nt, Gt, SHt, hT, want32=None):
        for ti in range(nt):
            hb, hk = self.nh()
            if want32 is None:
                self.norm_mod(X[:, ti, :], "X%d" % ti, Gt, SHt, hb[:], hk)
            else:
                h32, h32k = want32
                self.norm_mod(X[:, ti, :], "X%d" % ti, Gt, SHt, h32[:, ti, :], h32k + str(ti))
                self.op("scalar", lambda e, ti=ti, hb=hb: e.copy(out=hb[:], in_=h32[:, ti, :]), r=[h32k + str(ti)], w=[hk])
            self.transpose_to(hb, hk, hT, "hT", ti * 128)

    def gmlp(self, X, nt, c, mods):
        ntok = nt * 128
        hT, uT, vg = c["hT"], c["uT"], c["vg"]
        w_in, w_out = c["w_in"], c["w_out"]
        def ev_u(ncx, ps, pk):
            self.gelu_evac(ps, pk, uT[:, ncx, 0:ntok], "uT")
        self.lin_fm(w_in, 8, hT, "hT", 0, ntok, list(range(16)), ev_u)
        wv = w_in.rearrange("(kc p) n -> p kc n", p=128)
        for cb in range(4):
            wt, wk = self.load_w(wv[:, :, 2048 + cb * 512:2048 + (cb + 1) * 512], 8, 512)
            for ti in range(nt):
                pa, pk = self.nA()
                for kc in range(8):
                    self.op("tensor", lambda e, kc=kc, ti=ti, wt=wt, pa=pa: e.matmul(pa[:, :], lhsT=hT[:, kc, ti * 128:(ti + 1) * 128], rhs=wt[:, kc, :], start=(kc == 0), stop=(kc == 7)),
                            r=[wk, "hT"], w=[pk])
                self.gelu_evac(pa[:, :], pk, vg[:, ti, cb * 512:(cb + 1) * 512], "vg%d" % ti)
        bst = c["bst"]
        wsT = c["wsT"]
        for ti in range(nt):
            for cb in range(4):
                self.op("vector", lambda e, ti=ti, cb=cb: e.bn_stats(out=bst[:, cb * 6:(cb + 1) * 6], in_=vg[:, ti, cb * 512:(cb + 1) * 512]), r=["vg%d" % ti], w=["bst"])
            self.op("vector", lambda e: e.bn_aggr(out=bst[:, 24:26], in_=bst[:, 0:24]), r=["bst"], w=["bst"])
            self.op("scalar", lambda e: e.activation(out=bst[:, 26:27], in_=bst[:, 25:26], func=AF.Sqrt, scale=1.0, bias=EPS), r=["bst"], w=["bst"])
            self.op("vector", lambda e: e.reciprocal(out=bst[:, 27:28], in_=bst[:, 26:27]), r=["bst"], w=["bst"])
            vn, vk = c["vn"][ti % 2], "vn%d" % (ti % 2)
            for hf in range(2):
                tmp, tk = self.ntmp()
                self.op("vector", lambda e, ti=ti, hf=hf, tmp=tmp: e.tensor_scalar(out=tmp[:], in0=vg[:, ti, hf * 1024:(hf + 1) * 1024], scalar1=bst[:, 24:25], scalar2=bst[:, 27:28], op0=ALU.subtract, op1=ALU.mult),
                        r=["vg%d" % ti, "bst"], w=[tk])
                self.op("gpsimd", lambda e, hf=hf, tmp=tmp, vn=vn: e.tensor_tensor(out=vn[:, hf * 1024:(hf + 1) * 1024], in0=tmp[:], in1=c["GV"][:, hf * 1024:(hf + 1) * 1024], op=ALU.mult),
                        r=[tk, "GV"], w=[vk])
            for q4 in range(4):
                pa, pk = self.nA()
                for j in range(4):
                    cc = q4 * 4 + j
                    self.op("tensor", lambda e, j=j, cc=cc, pa=pa, vn=vn: e.matmul(pa[:, j * 128:(j + 1) * 128], lhsT=vn[:, cc * 128:(cc + 1) * 128], rhs=wsT[:, cc // 2, :], start=True, stop=True),
                            r=[vk, "wsT"], w=[pk])
                tmp, tk = self.ntmp()
                self.op("vector", lambda e, q4=q4, pa=pa, tmp=tmp: e.tensor_tensor(out=tmp[:, 0:512], in0=pa[:, :], in1=c["BS"][:, q4 * 512:(q4 + 1) * 512], op=ALU.add),
                        r=[pk, "BS"], w=[tk])
                self.op("gpsimd", lambda e, q4=q4, ti=ti, tmp=tmp: e.tensor_tensor(out=uT[:, q4 * 4:(q4 + 1) * 4, ti * 128:(ti + 1) * 128], in0=tmp[:, 0:512].rearrange("p (a b) -> p a b", a=4),
                                                                                    in1=uT[:, q4 * 4:(q4 + 1) * 4, ti * 128:(ti + 1) * 128], op=ALU.mult),
                        r=[tk, "uT"], w=["uT"])
        wo = w_out.rearrange("(kc p) n -> p kc n", p=128)
        wts = [self.load_w(wo[:, :, hf * 512:(hf + 1) * 512], 16, 512) for hf in range(2)]
        for ti in range(nt):
            py, pyk = self.nY()
            for hf in range(2):
                wt, wk = wts[hf]
                for kc in range(16):
                    self.op("tensor", lambda e, kc=kc, hf=hf, ti=ti, wt=wt, py=py: e.matmul(py[:, hf * 512:(hf + 1) * 512], lhsT=uT[:, kc, ti * 128:(ti + 1) * 128], rhs=wt[:, kc, :], start=(kc == 0), stop=(kc == 15)),
                            r=[wk, "uT"], w=[pyk])
            self.post_res(py, pyk, X[:, ti, :], "X%d" % ti, mods["GT1"])

    def mixer_out(self, X, nt, mTs, mkeys, w_out, KCn, GT):
        wo = w_out.rearrange("(kc p) n -> p kc n", p=128)
        wts = [self.load_w(wo[:, :, hf * 512:(hf + 1) * 512], KCn, 512) for hf in range(2)]
        nm = len(mTs)
        for ti in range(nt):
            py, pyk = self.nY()
            for hf in range(2):
                wt, wk = wts[hf]
                idx = 0
                for mi in range(nm):
                    for kc in range(KCn):
                        self.op("tensor", lambda e, kc=kc, hf=hf, ti=ti, wt=wt, py=py, mi=mi, idx=idx: e.matmul(py[:, hf * 512:(hf + 1) * 512], lhsT=mTs[mi][:, kc, ti * 128:(ti + 1) * 128], rhs=wt[:, kc, :], start=(idx == 0), stop=(idx == nm * KCn - 1)),
                                r=[wk, mkeys[mi]], w=[pyk])
                        idx += 1
            self.post_res(py, pyk, X[:, ti, :], "X%d" % ti, GT)

    def ffn(self, X, nt, c, GT, wg, wu, wd, FF, comb=None, first=True):
        ntok = nt * 128
        hT, Y2 = c["hT"], c["Y2"]
        nfb = (FF + 511) // 512
        def wdsrc(a, b_):
            if callable(wd):
                return lambda: wd().rearrange("(kc p) n -> p kc n", p=128)[:, a:b_, :]
            return wd.rearrange("(kc p) n -> p kc n", p=128)[:, a:b_, :]
        for fb in range(nfb):
            nch = min(4, FF // 128 - fb * 4)
            sg, sgk = c["sg"][fb % 2], "sg%d" % (fb % 2)
            gT, gk = c["gT"][fb % 2], "gT%d" % (fb % 2)
            chunks = list(range(fb * 4, fb * 4 + nch))
            def ev_g(ncx, ps, pk, sg=sg, sgk=sgk, fb=fb):
                self.op("scalar", lambda e: e.activation(out=sg[:, ncx - fb * 4, 0:ntok], in_=ps, func=AF.Silu), r=[pk], w=[sgk])
            self.lin_fm(wg, 8, hT, "hT", 0, ntok, chunks, ev_g)
            def ev_u(ncx, ps, pk, sg=sg, sgk=sgk, gT=gT, gk=gk, fb=fb):
                self.op("vector", lambda e: e.tensor_tensor(out=gT[:, ncx - fb * 4, 0:ntok], in0=sg[:, ncx - fb * 4, 0:ntok], in1=ps, op=ALU.mult), r=[pk, sgk], w=[gk])
            self.lin_fm(wu, 8, hT, "hT", 0, ntok, chunks, ev_u)
            if isinstance(wd, dict):
                wt, wk = self.load_w_ind(wd["rows"], wd["idx"], wd["ikey"], nch, 1024, fb * 512 * 1024)
            else:
                wt, wk = self.load_w(wdsrc(fb * 4, fb * 4 + nch), nch, 1024)
            for ti in range(nt):
                py, pyk = self.nY()
                for hf in range(2):
                    for kc in range(nch):
                        self.op("tensor", lambda e, kc=kc, hf=hf, ti=ti, wt=wt, py=py, gT=gT: e.matmul(py[:, hf * 512:(hf + 1) * 512], lhsT=gT[:, kc, ti * 128:(ti + 1) * 128], rhs=wt[:, kc, hf * 512:(hf + 1) * 512], start=(kc == 0), stop=(kc == nch - 1)),
                                r=[wk, gk], w=[pyk])
                yk = "Y2_%d" % ti
                if comb is None:
                    if first and fb == 0:
                        self.op("scalar", lambda e, ti=ti, py=py: e.copy(out=Y2[:, ti, :], in_=py[:, :]), r=[pyk], w=[yk, "uT"])
                    else:
                        self.op("vector", lambda e, ti=ti, py=py: e.tensor_tensor(out=Y2[:, ti, :], in0=py[:, :], in1=Y2[:, ti, :], op=ALU.add), r=[pyk, yk], w=[yk])
                else:
                    cap, ck = comb
                    if first and fb == 0:
                        self.op("vector", lambda e, ti=ti, py=py: e.tensor_scalar(out=Y2[:, ti, :], in0=py[:, :], scalar1=cap(ti), scalar2=None, op0=ALU.mult), r=[pyk, ck], w=[yk, "uT"])
                    else:
                        self.op("vector", lambda e, ti=ti, py=py: e.scalar_tensor_tensor(out=Y2[:, ti, :], in0=py[:, :], scalar=cap(ti), in1=Y2[:, ti, :], op0=ALU.mult, op1=ALU.add), r=[pyk, yk, ck], w=[yk])

    def ffn_post(self, X, nt, c, GT):
        for ti in range(nt):
            self.post_res_sb(c["Y2"][:, ti, :], "Y2_%d" % ti, X[:, ti, :], "X%d" % ti, GT)

    def out_block(self, X, nt, xo_rows, ho_rows=None, Gn=None, SHn=None):
        for ti in range(nt):
            if ho_rows is not None:
                hb, hk = self.nh()
                self.norm_mod(X[:, ti, :], "X%d" % ti, Gn, SHn, hb[:], hk)
                self.dma("sync", ho_rows[ti], hb[:], r=[hk])
            if xo_rows is not None:
                self.dma("sync", xo_rows[ti], X[:, ti, :], r=["X%d" % ti])

def build_S0(n_lat_tiles=32, TB=512):
    P = Tok(TB)
    NT = P.NT
    NTOK = n_lat_tiles * 128
    dI = lambda n, s, dt=F32: P.dram(n, s, dt, "ExternalInput")
    dO = lambda n, s, dt=F32: P.dram(n, s, dt, "ExternalOutput")
    x_d, pos_d, ctx_d = dI("x", [NTOK, D]), dI("pos", [NTOK, D]), dI("ctx", [128, D])
    c_d, cc_d = dI("c", [128, 8]), dI("c_ctx", [128, 8])
    aw0, ab0, ng0 = dI("ada_w0", [D, 6 * D]), dI("ada_b0", [6 * D]), dI("ng0", [4 * D])
    aw1, ab1, ng1 = dI("ada_w1", [D, 6 * D]), dI("ada_b1", [6 * D]), dI("ng1", [4 * D])
    g_win, g_gv, g_wsT, g_bs, g_wout = dI("g_win", [D, 4096]), dI("g_gv", [2048]), dI("g_wsT", [128, 1024]), dI("g_bs", [2048]), dI("g_wout", [2048, D])
    f_wg, f_wu, f_wd = dI("f_wg", [D, 2816]), dI("f_wu", [D, 2816]), dI("f_wd", [2816, D])
    xo, ho = dO("xo", [NTOK, D]), dO("ho", [NTOK, D], BF16)
    xco, hco = dO("xco", [128, D]), dO("hco", [128, D], BF16)
    X = P.sb("X", [128, NT, D], F32)
    c = dict(hT=P.sb("hT", [128, 8, TB], BF16), uT=None, vg=P.sb("vg", [128, NT, 2048], BF16),
             vn=[P.sb("vn%d" % i, [128, 2048], BF16) for i in range(2)], bst=P.sb("bst", [128, 32], F32),
             Y2=None, sg=[P.sb("sg%d" % i, [128, 4, TB], BF16) for i in range(2)],
             gT=[P.sb("gT%d" % i, [128, 4, TB], BF16) for i in range(2)],
             w_in=g_win, w_out=g_wout)
    uY = P.sb("uY", [128, NT * D], F32)
    c["Y2"] = uY[:].rearrange("p (a b) -> p a b", a=NT)
    c["uT"] = uY[:].bitcast(BF16).rearrange("p (a b) -> p a b", a=16)
    c["GV"] = P.sb("GV", [128, 2048], F32)
    c["BS"] = P.sb("BS", [128, 2048], F32)
    P.bc_dram(c["GV"][:], "GV", g_gv, 0, 2048)
    P.bc_dram(c["BS"][:], "BS", g_bs, 0, 2048)
    wsT = P.sb("wsT", [128, 8, 128], BF16)
    P.dma("gpsimd", wsT[:].rearrange("p a b -> p (a b)"), g_wsT, w=["wsT"])
    c["wsT"] = wsT

    def run_pass(pref, cvec, blocks):
        m = P.ada_mod("m", cvec, aw0, ab0, ng0, [(0, "SH1", "raw", 0), (1, "G1", "mul1p", 0), (2, "GT1", "mul", 1),
                                                  (3, "SH2", "raw", 0), (4, "G2", "mul1p", 2), (5, "GT2", "mul", 3)])
        mn = P.ada_mod("n", cvec, aw1, ab1, ng1, [(0, "SH1", "raw", 0), (1, "G1", "mul1p", 0)])
        for (srcs, poss, xos, hos) in blocks:
            nt = len(srcs)
            P.load_block(X, srcs, poss)
            P.pre_T(X, nt, m["G1"], m["SH1"], c["hT"])
            P.gmlp(X, nt, c, m)
            P.pre_T(X, nt, m["G2"], m["SH2"], c["hT"])
            P.ffn(X, nt, c, m["GT2"], f_wg, f_wu, f_wd, 2816)
            P.ffn_post(X, nt, c, m["GT2"])
            P.out_block(X, nt, xos, hos, mn["G1"], mn["SH1"])

    rows = lambda d, t: d[t * 128:(t + 1) * 128, :]
    blocks = []
    for b in range(n_lat_tiles // NT):
        ts = list(range(b * NT, (b + 1) * NT))
        blocks.append(([rows(x_d, t) for t in ts], [rows(pos_d, t) for t in ts], [rows(xo, t) for t in ts], [rows(ho, t) for t in ts]))
    run_pass("l", c_d, blocks)
    run_pass("c", cc_d, [([ctx_d], None, [xco], [hco])])
    return P

U32 = mybir.dt.uint32


def tok_router2(P, g, h32t, h32k, c):
    hT32, wr32, lg, mx8, identf, M1, M2, PW = c["hT32"], c["wr32"], c["lg"], c["mx8"], c["identf"], c["M1"], c["M2"], c["PW"]
    py, pyk = P.nY()
    pv = py[:, :].rearrange("p (a b) -> p a b", a=8)
    for kc in range(8):
        P.op("tensor", lambda e, kc=kc, pv=pv: e.transpose(out=pv[:, kc, :], in_=h32t[:, kc * 128:(kc + 1) * 128], identity=identf[:]), r=[h32k, "identf"], w=[pyk])
    P.op("vector", lambda e, pv=pv: e.tensor_copy(out=hT32[:], in_=pv), r=[pyk], w=["hT32"])
    pa, pk = P.nA()
    for kc in range(8):
        P.op("tensor", lambda e, kc=kc, pa=pa: e.matmul(pa[:, 0:8], lhsT=hT32[:, kc, :], rhs=wr32[:, kc, :], start=(kc == 0), stop=(kc == 7)), r=["hT32", "wr32"], w=[pk])
    P.op("vector", lambda e, pa=pa: e.tensor_copy(out=lg[:, 0:8], in_=pa[:, 0:8]), r=[pk], w=["lg"])
    P.op("vector", lambda e: e.max(out=mx8[:, 0:8], in_=lg[:, 0:8]), r=["lg"], w=["mx8"])
    P.op("vector", lambda e: e.tensor_tensor(out=mx8[:, 8:9], in0=mx8[:, 0:1], in1=mx8[:, 1:2], op=ALU.subtract), r=["mx8"], w=["mx8"])
    P.op("scalar", lambda e: e.activation(out=PW[:, g, 0:1], in_=mx8[:, 8:9], func=AF.Sigmoid, scale=1.0), r=["mx8"], w=["PW"])
    P.op("scalar", lambda e: e.activation(out=PW[:, g, 1:2], in_=mx8[:, 8:9], func=AF.Sigmoid, scale=-1.0), r=["mx8"], w=["PW"])
    P.op("vector", lambda e: e.tensor_scalar(out=M1[:, g, :], in0=lg[:, 0:8], scalar1=mx8[:, 0:1], scalar2=None, op0=ALU.is_equal), r=["lg", "mx8"], w=["M1"])
    P.op("vector", lambda e: e.tensor_scalar(out=M2[:, g, :], in0=lg[:, 0:8], scalar1=mx8[:, 1:2], scalar2=None, op0=ALU.is_equal), r=["lg", "mx8"], w=["M2"])


def moe_slots(P, NG, NBLK, c):
    M1, M2 = c["M1"], c["M2"]
    W = NG * 8
    fl = lambda T: T[:].rearrange("p g e -> p (g e)")
    ao = [3 * 7168]
    def sbf(n, w_, dt=F32):
        sl = c["arena"][:, ao[0]:ao[0] + 2 * w_]
        ao[0] += 2 * w_
        assert ao[0] <= 4 * 7168
        return sl.bitcast(dt)
    maskv, RK, TT, INC, EXC, SLOT, TMP = [sbf(n, W) for n in ("maskv", "RK", "TT", "INC", "EXC", "SLOT", "TMPs")]
    onesM, onesr = sbf("onesM", 128), sbf("onesr", NG)
    PADF, ENDF, BASEF = sbf("PADF", 8), sbf("ENDF", 8), sbf("BASEF", 8)
    PADI = sbf("PADI", 8, I32)
    SLF = sbf("SLF", 2 * NG)
    eidf = sbf("eidf", NBLK)
    U, blk = c["U"], c["blk"]
    SLI, EIDI = c["SLI"], c["EIDI"]
    P.op("vector", lambda e: e.memset(onesM[:], 1.0), w=["onesM"])
    P.op("vector", lambda e: e.memset(onesr[:], 1.0), w=["onesr"])
    P.op("vector", lambda e: e.tensor_tensor(out=maskv[:], in0=fl(M1), in1=fl(M2), op=ALU.add), r=["M1", "M2"], w=["maskv"])
    pa, pk = P.nA()
    P.op("tensor", lambda e: e.matmul(pa[:, 0:W], lhsT=U[:], rhs=maskv[:], start=True, stop=True), r=["U", "maskv"], w=[pk])
    P.op("vector", lambda e: e.tensor_copy(out=RK[:], in_=pa[:, 0:W]), r=[pk], w=["RK"])
    pb, pbk = P.nA()
    P.op("tensor", lambda e: e.matmul(pb[:, 0:W], lhsT=onesM[:], rhs=maskv[:], start=True, stop=True), r=["onesM", "maskv"], w=[pbk])
    P.op("vector", lambda e: e.tensor_copy(out=TT[:], in_=pb[:, 0:W]), r=[pbk], w=["TT"])
    ev = lambda T, e_: T[:].rearrange("p (g e) -> p e g", e=8)[:, e_, :]
    for e_ in range(8):
        P.op("vector", lambda e, e_=e_: e.tensor_tensor_scan(out=ev(INC, e_), data0=onesr[:], data1=ev(TT, e_), initial=0.0, op0=ALU.mult, op1=ALU.add), r=["TT", "onesr"], w=["INC"])
    P.op("vector", lambda e: e.tensor_tensor(out=EXC[:], in0=INC[:], in1=TT[:], op=ALU.subtract), r=["INC", "TT"], w=["EXC"])
    P.op("vector", lambda e: e.tensor_scalar(out=PADF[:], in0=INC[:, (NG - 1) * 8:NG * 8], scalar1=511.0, scalar2=None, op0=ALU.add), r=["INC"], w=["PADF"])
    P.op("vector", lambda e: e.tensor_copy(out=PADI[:], in_=PADF[:]), r=["PADF"], w=["PADI"])
    P.op("vector", lambda e: e.tensor_scalar(out=PADI[:], in0=PADI[:], scalar1=9, scalar2=9, op0=ALU.arith_shift_right, op1=ALU.logical_shift_left), r=["PADI"], w=["PADI"])
    P.op("vector", lambda e: e.tensor_copy(out=PADF[:], in_=PADI[:]), r=["PADI"], w=["PADF"])
    P.op("vector", lambda e: e.tensor_tensor_scan(out=ENDF[:], data0=onesr[:, 0:8], data1=PADF[:], initial=0.0, op0=ALU.mult, op1=ALU.add), r=["PADF", "onesr"], w=["ENDF"])
    P.op("vector", lambda e: e.tensor_tensor(out=BASEF[:], in0=ENDF[:], in1=PADF[:], op=ALU.subtract), r=["ENDF", "PADF"], w=["BASEF"])
    for e_ in range(8):
        P.op("vector", lambda e, e_=e_: e.scalar_tensor_tensor(out=ev(SLOT, e_), in0=ev(RK, e_), scalar=BASEF[:, e_:e_ + 1], in1=ev(EXC, e_), op0=ALU.add, op1=ALU.add), r=["RK", "EXC", "BASEF"], w=["SLOT"])
    for k, M in enumerate((M1, M2)):
        P.op("vector", lambda e, M=M: e.tensor_tensor(out=TMP[:], in0=fl(M), in1=SLOT[:], op=ALU.mult), r=["M1", "M2", "SLOT"], w=["TMPs"])
        P.op("vector", lambda e, k=k: e.tensor_reduce(out=SLF[:, k * NG:(k + 1) * NG], in_=TMP[:].rearrange("p (g e) -> p g e", e=8), axis=AX.X, op=ALU.add), r=["TMPs"], w=["SLF"])
    P.op("vector", lambda e: e.tensor_copy(out=SLI[:].rearrange("p k g -> p (k g)"), in_=SLF[:]), r=["SLF"], w=["SLI"])
    P.op("vector", lambda e: e.memset(eidf[:], 0.0), w=["eidf"])
    for e_ in range(8):
        P.op("vector", lambda e, e_=e_: e.scalar_tensor_tensor(out=eidf[:], in0=blk[:], scalar=ENDF[:, e_:e_ + 1], in1=eidf[:], op0=ALU.is_ge, op1=ALU.add), r=["blk", "ENDF", "eidf"], w=["eidf"])
    P.op("vector", lambda e: e.tensor_scalar(out=eidf[:], in0=eidf[:], scalar1=7.0, scalar2=None, op0=ALU.min), r=["eidf"], w=["eidf"])
    IH, rowc = c["IH"], c["rowc"]
    IHf = sbf("IHf", NBLK * 4).rearrange("p (a b) -> p a b", b=4)
    eg = sbf("eg", NBLK)
    P.op("vector", lambda e: e.tensor_scalar(out=eg[:], in0=eidf[:], scalar1=512.0, scalar2=None, op0=ALU.mult), r=["eidf"], w=["eg"])
    for b in range(NBLK):
        P.op("vector", lambda e, b=b: e.tensor_scalar(out=IHf[:, b, :], in0=rowc[:, 0:4], scalar1=eg[:, b:b + 1], scalar2=None, op0=ALU.add), r=["eg", "rowc"], w=["IHf"])
    P.op("vector", lambda e: e.tensor_copy(out=IH[:].rearrange("p a b -> p (a b)"), in_=IHf[:].rearrange("p a b -> p (a b)")), r=["IHf"], w=["IH"])


def moe_sparse(P, X, n_tiles, c, m, wg_d, wu_d, wd_d, FF, Xd, Hd, finish_tile):
    NG = n_tiles
    NTOK = NG * 128
    NBLK = (2 * NTOK + 8 * 511) // 512
    NS = NBLK * 512
    Hs = P.dram("Hs", [NS, D], BF16, "Internal")
    Ys = P.dram("Ys", [NS, D], F32, "Internal")
    SLI, EIDI = c["SLI"], c["EIDI"]
    moe_slots(P, NG, NBLK, c)
    for g in range(NG):
        hb, hk = P.nh()
        P.dma("sync", hb[:], Hd[g * 128:(g + 1) * 128, :], r=["Hd"], w=[hk])
        for k in range(2):
            P.op("gpsimd", lambda e, g=g, k=k, hb=hb: e.indirect_dma_start(out=Hs, out_offset=bass.IndirectOffsetOnAxis(ap=SLI[:, k, g:g + 1], axis=0), in_=hb[:], in_offset=None),
                 r=[hk, "SLI"], w=["Hs"], dma=True)
    NT = P.NT
    for b in range(NBLK):
        for ti in range(NT):
            hb, hk = P.nh()
            P.dma("sync", hb[:], Hs[(b * NT + ti) * 128:(b * NT + ti + 1) * 128, :], r=["Hs"], w=[hk])
            P.transpose_to(hb, hk, c["hT"], "hT", ti * 128)
        P.ffn_sp(NT, c, b, c["Wb"][0], c["Wb"][1], c["Wb"][2], c["IH"])
        for ti in range(NT):
            P.dma("sync", Ys[(b * NT + ti) * 128:(b * NT + ti + 1) * 128, :], c["Y2"][:, ti, :], r=["Y2_%d" % ti], w=["Ys"])
    YG = c["YG"]
    for g in range(NG):
        ti = g % NT
        P.dma("sync", X[:, ti, :], Xd[g * 128:(g + 1) * 128, :], r=["Xd"], w=["X%d" % ti])
        ys = []
        for k in range(2):
            yt, yk = YG[k], "YG%d" % k
            P.op("gpsimd", lambda e, g=g, k=k, yt=yt: e.indirect_dma_start(out=yt[:], out_offset=None, in_=Ys, in_offset=bass.IndirectOffsetOnAxis(ap=SLI[:, k, g:g + 1], axis=0)),
                 r=["Ys", "SLI"], w=[yk], dma=True)
            ys.append((yt, yk))
        (y1, y1k), (y2, y2k) = ys
        P.op("vector", lambda e, g=g, y1=y1: e.tensor_scalar(out=y1[:], in0=y1[:], scalar1=c["PW"][:, g, 0:1], scalar2=None, op0=ALU.mult), r=[y1k, "PW"], w=[y1k])
        P.op("vector", lambda e, g=g, y1=y1, y2=y2: e.scalar_tensor_tensor(out=y1[:], in0=y2[:], scalar=c["PW"][:, g, 1:2], in1=y1[:], op0=ALU.mult, op1=ALU.add), r=[y1k, y2k, "PW"], w=[y1k])
        P.post_res_sb(y1[:], y1k, X[:, ti, :], "X%d" % ti, m["GT2"])
        finish_tile(g, ti)

def tok_router(P, nt, c):
    h32, hT32, wr32, lg, mx8, comb, identf = c["h32"], c["hT32"], c["wr32"], c["lg"], c["mx8"], c["comb"], c["identf"]
    for ti in range(nt):
        py, pyk = P.nY()
        pv = py[:, :].rearrange("p (a b) -> p a b", a=8)
        for kc in range(8):
            P.op("tensor", lambda e, kc=kc, ti=ti, pv=pv: e.transpose(out=pv[:, kc, :], in_=h32[:, ti, kc * 128:(kc + 1) * 128], identity=identf[:]),
                 r=["h32_%d" % ti, "identf"], w=[pyk])
        P.op("vector", lambda e, pv=pv: e.tensor_copy(out=hT32[:], in_=pv), r=[pyk], w=["hT32"])
        pa, pk = P.nA()
        for kc in range(8):
            P.op("tensor", lambda e, kc=kc, pa=pa: e.matmul(pa[:, 0:8], lhsT=hT32[:, kc, :], rhs=wr32[:, kc, :], start=(kc == 0), stop=(kc == 7)),
                 r=["hT32", "wr32"], w=[pk])
        P.op("vector", lambda e, pa=pa: e.tensor_copy(out=lg[:, 0:8], in_=pa[:, 0:8]), r=[pk], w=["lg"])
        P.op("vector", lambda e: e.max(out=mx8[:, 0:8], in_=lg[:, 0:8]), r=["lg"], w=["mx8"])
        P.op("vector", lambda e: e.tensor_tensor(out=mx8[:, 8:9], in0=mx8[:, 0:1], in1=mx8[:, 1:2], op=ALU.subtract), r=["mx8"], w=["mx8"])
        P.op("scalar", lambda e: e.activation(out=mx8[:, 9:10], in_=mx8[:, 8:9], func=AF.Sigmoid, scale=1.0), r=["mx8"], w=["mx8"])
        P.op("scalar", lambda e: e.activation(out=mx8[:, 10:11], in_=mx8[:, 8:9], func=AF.Sigmoid, scale=-1.0), r=["mx8"], w=["mx8"])
        P.op("vector", lambda e: e.tensor_scalar(out=lg[:, 8:16], in0=lg[:, 0:8], scalar1=mx8[:, 0:1], scalar2=mx8[:, 9:10], op0=ALU.is_equal, op1=ALU.mult), r=["lg", "mx8"], w=["lg"])
        P.op("vector", lambda e: e.tensor_scalar(out=lg[:, 16:24], in0=lg[:, 0:8], scalar1=mx8[:, 1:2], scalar2=mx8[:, 10:11], op0=ALU.is_equal, op1=ALU.mult), r=["lg", "mx8"], w=["lg"])
        P.op("vector", lambda e, ti=ti: e.tensor_tensor(out=comb[:, ti, :], in0=lg[:, 8:16], in1=lg[:, 16:24], op=ALU.add), r=["lg"], w=["comb"])


def build_TOK2(kind, n_tiles=32, TB=512, sparse=True):
    P = Tok(TB, nwsl=(2 if (sparse and kind in ("S2", "S5")) else 3))
    NT = P.NT
    NTOK = n_tiles * 128
    dI = lambda n, s, dt=F32: P.dram(n, s, dt, "ExternalInput")
    dO = lambda n, s, dt=F32: P.dram(n, s, dt, "ExternalOutput")
    moe = kind in ("S2", "S5")
    sparse = sparse and moe
    has_next = kind in ("S2", "S4")
    x_d = dI("x", [NTOK, D])
    c_d = dI("c", [128, 8])
    aw, ab, ng = dI("ada_w", [D, 6 * D]), dI("ada_b", [6 * D]), dI("ng", [4 * D])
    if has_next:
        awn, abn, ngn = dI("ada_wn", [D, 6 * D]), dI("ada_bn", [6 * D]), dI("ngn", [4 * D])
        ho = dO("ho", [NTOK, D], BF16)
    xo = dO("xo", [NTOK, D])
    w_out = dI("w_out", [D, D])
    FF = 3584 if moe else 2816
    if sparse:
        wr_d = dI("w_router", [128, 64])
        wg_d, wu_d, wd_d = dI("wg", [4096, 4, 1792]), dI("wu", [4096, 4, 1792]), dI("wd", [4096, 4, 1792])
    elif moe:
        wr_d = dI("w_router", [128, 64])
        wg_d, wu_d, wd_d = dI("wg", [8, D, FF]), dI("wu", [8, D, FF]), dI("wd", [8, FF, D])
    else:
        wg_d, wu_d, wd_d = dI("wg", [D, FF]), dI("wu", [D, FF]), dI("wd", [FF, D])
    X = P.sb("X", [128, NT, D], F32)
    c = dict(hT=P.sb("hT", [128, 8, TB], BF16), Y2=P.sb("Y2", [128, NT, D], F32),
             sg=[P.sb("sg%d" % i, [128, 4, TB], BF16) for i in range(2)],
             gT=[P.sb("gT%d" % i, [128, 4, TB], BF16) for i in range(2)])
    if sparse:
        NBLKS = (2 * NTOK + 8 * 511) // 512
        U_d, blk_d = dI("Utri", [128, 128]), dI("blkrow", [128, NBLKS])
        Xd, Hd = P.dram("Xd", [NTOK, D], F32, "Internal"), P.dram("Hd", [NTOK, D], BF16, "Internal")
        c["M1"], c["M2"] = P.sb("M1", [128, n_tiles, 8], F32), P.sb("M2", [128, n_tiles, 8], F32)
        c["PW"] = P.sb("PW", [128, n_tiles, 2], F32)
        c["SLI"], c["EIDI"] = P.sb("SLI", [128, 2, n_tiles], U32), P.sb("EIDI", [128, NBLKS], I32)
        c["IH"] = P.sb("IH", [128, NBLKS, 4], U32)
        arena = P.sb("arena", [128, 4 * 7168], BF16)
        c["WS"] = [arena[:, i * 7168:(i + 1) * 7168] for i in range(4)]
        c["arena"] = arena
        c["Wb"] = []
        for wi_, Wsrc in enumerate((wg_d, wu_d, wd_d)):
            Wb = P.dram("Wb%d" % wi_, [4096, 7168], BF16, "Internal")
            for r_ in range(32):
                P.dma("gpsimd", Wb[r_ * 128:(r_ + 1) * 128, :].rearrange("r (a b) -> r a b", a=4), Wsrc[r_ * 128:(r_ + 1) * 128], w=["Wb%d" % wi_])
            c["Wb"].append((Wb, "Wb%d" % wi_))
        c["wsi"], c["fbi"] = [0], [0]
        c["rowc"] = P.sb("rowc_s", [128, 8], F32)
        rowc_d = dI("rowc", [128, 8])
        P.dma("sync", c["rowc"][:], rowc_d, w=["rowc"])
        c["U"], c["blk"] = P.sb("U_s", [128, 128], F32), P.sb("blk_s", [128, NBLKS], F32)
        P.dma("sync", c["U"][:], U_d, w=["U"])
        P.dma("sync", c["blk"][:], blk_d, w=["blk"])
        c["YG"] = [P.sb("YG%d" % i, [128, D], F32) for i in range(2)]
        h32r = [P.sb("h32r%d" % i, [128, D], F32) for i in range(1)]
    if moe:
        c["h32"] = P.sb("h32", [128, NT, D], F32) if not sparse else None
        c["hT32"] = P.sb("hT32", [128, 8, 128], F32)
        c["wr32"] = P.sb("wr32", [128, 8, 8], F32)
        c["lg"] = P.sb("lg", [128, 24], F32)
        c["mx8"] = P.sb("mx8", [128, 16], F32)
        c["comb"] = P.sb("comb", [128, NT, 8], F32)
        c["identf"] = P.sb("identf", [128, 128], F32)
        P.dma("sync", c["identf"][:], P.ident_d, w=["identf"])
        P.dma("sync", c["wr32"][:].rearrange("p a b -> p (a b)"), wr_d, w=["wr32"])
    if kind == "S2":
        mf_d, mb_d = dI("mf", [D, NTOK], BF16), dI("mb", [D, NTOK], BF16)
        if sparse:
            mTs = [arena[:, i * 8 * TB:(i + 1) * 8 * TB].rearrange("p (a b) -> p a b", a=8) for i in range(2)]
        else:
            mTs = [P.sb("mT0", [128, 8, TB], BF16), P.sb("mT1", [128, 8, TB], BF16)]
        msrc = [mf_d, mb_d]
    elif kind == "S4":
        mf_d = dI("mf", [D, NTOK], BF16)
        mTs = [P.sb("mT0", [128, 8, TB], BF16)]
        msrc = [mf_d]
    else:
        HW = 8
        h3_d = dI("h3T", [D, NTOK + 2 * HW], BF16)
        pw_in = dI("p_win", [D, D])
        pwg_d = dI("p_wg", [4, 256, 256])
        pbg_d, psc_d = dI("p_bg", [128, 8]), dI("p_scale", [128, 8])
        pfix_d = dI("p_fix", [128, 64])
        if sparse:
            NHp = TB + 2 * HW
            o = [0]
            def carve(n_el, a=None, f32=False):
                sl = arena[:, o[0]:o[0] + n_el]
                o[0] += n_el
                if f32:
                    return sl.bitcast(F32)
                return sl.rearrange("p (a b) -> p a b", a=a) if a else sl
            hTh, qT, yT = carve(8 * NHp, 8), carve(8 * TB, 8), carve(8 * TB, 8)
            pT, sA, sB = carve(2 * NHp, f32=True), carve(2 * NHp, f32=True), carve(2 * NHp, f32=True)
        else:
            hTh = P.sb("hTh", [128, 8, TB + 2 * HW], BF16)
            pT = P.sb("pT", [128, TB + 2 * HW], F32)
            sA = P.sb("sA", [128, TB + 2 * HW], F32)
            sB = P.sb("sB", [128, TB + 2 * HW], F32)
            qT = P.sb("qT", [128, 8, TB], BF16)
            yT = P.sb("yT", [128, 8, TB], BF16)
        wgs = P.sb("wgs", [128, 4, 2, 256], BF16)
        P.dma("gpsimd", wgs[:].rearrange("p g c d -> p (g c) d"), pwg_d.rearrange("g (cc p) d -> p (g cc) d", p=128), w=["wgs"])
        pbg, psc, pfix = P.sb("pbg", [128, 8], F32), P.sb("psc", [128, 8], F32), P.sb("pfix", [128, 4, 16], F32)
        P.dma("sync", pbg[:], pbg_d, w=["pbg"])
        P.dma("sync", psc[:], psc_d, w=["psc"])
        P.dma("sync", pfix[:].rearrange("p a b -> p (a b)"), pfix_d, w=["pfix"])

    segs = [(2, "GT1", "mul", 1), (3, "SH2", "raw", 0), (4, "G2", "mul1p", 2), (5, "GT2", "mul", 3)]
    m = P.ada_mod("m", c_d, aw, ab, ng, segs)
    if has_next:
        mn = P.ada_mod("n", c_d, awn, abn, ngn, [(0, "SH1", "raw", 0), (1, "G1", "mul1p", 0)])
    rows = lambda d, t: d[t * 128:(t + 1) * 128, :]
    nblk = n_tiles // NT
    for b in range(nblk):
        ts = list(range(b * NT, (b + 1) * NT))
        nt = NT
        t0 = b * TB
        P.load_block(X, [rows(x_d, t) for t in ts], None)
        if kind in ("S2", "S4"):
            for mi, src in enumerate(msrc):
                P.dma("sync", mTs[mi][:], src.rearrange("(kc p) t -> p kc t", p=128)[:, :, t0:t0 + TB], w=["mT%d" % mi])
            P.mixer_out(X, nt, mTs, ["mT%d" % i for i in range(len(mTs))], w_out, 8, m["GT1"])
        else:
            NH = TB + 2 * HW
            P.dma("sync", hTh[:], h3_d.rearrange("(kc p) t -> p kc t", p=128)[:, :, t0:t0 + NH], w=["hTh"])
            wv = pw_in.rearrange("(kc p) n -> p kc n", p=128)
            for cg in range(2):
                wt, wk = P.load_w(wv[:, :, cg * 512:(cg + 1) * 512], 8, 512)
                for j in range(4):
                    cc = cg * 4 + j
                    win = (2, 4, 8, 16)[cc // 2]
                    for hf in range(2):
                        pa, pk = P.nA()
                        c0 = hf * (NH // 2)
                        for kc in range(8):
                            P.op("tensor", lambda e, kc=kc, j=j, wt=wt, pa=pa, c0=c0: e.matmul(pa[:, 0:NH // 2], lhsT=wt[:, kc, j * 128:(j + 1) * 128], rhs=hTh[:, kc, c0:c0 + NH // 2], start=(kc == 0), stop=(kc == 7)),
                                 r=[wk, "hTh"], w=[pk])
                        P.op("scalar", lambda e, pa=pa, c0=c0: e.copy(out=pT[:, c0:c0 + NH // 2], in_=pa[:, 0:NH // 2]), r=[pk], w=["pT"])
                    P.op("vector", lambda e: e.tensor_tensor(out=sA[:, 1:NH], in0=pT[:, 0:NH - 1], in1=pT[:, 1:NH], op=ALU.add), r=["pT", "sB"], w=["sA"])
                    cur, ck, oth, ok = sA, "sA", sB, "sB"
                    sh = 1
                    lo, hi = 1, NH
                    while sh * 2 < win:
                        l2, h2 = lo + sh, hi - sh
                        P.op("vector", lambda e, cur=cur, oth=oth, sh=sh, l2=l2, h2=h2: e.tensor_tensor(out=oth[:, l2:h2], in0=cur[:, l2 - sh:h2 - sh], in1=cur[:, l2 + sh:h2 + sh], op=ALU.add),
                             r=[ck], w=[ok])
                        cur, ck, oth, ok = oth, ok, cur, ck
                        lo, hi = l2, h2
                        sh *= 2
                    wi = cc // 2
                    P.op("vector", lambda e, cur=cur, win=win: e.tensor_scalar(out=cur[:, HW:HW + TB], in0=cur[:, HW:HW + TB], scalar1=1.0 / win, scalar2=None, op0=ALU.mult), r=[ck], w=[ck])
                    if b == 0:
                        P.op("vector", lambda e, cur=cur, wi=wi: e.tensor_tensor(out=cur[:, HW:HW + 8], in0=cur[:, HW:HW + 8], in1=pfix[:, wi, 0:8], op=ALU.mult), r=[ck, "pfix"], w=[ck])
                    if b == nblk - 1:
                        P.op("vector", lambda e, cur=cur, wi=wi: e.tensor_tensor(out=cur[:, HW + TB - 8:HW + TB], in0=cur[:, HW + TB - 8:HW + TB], in1=pfix[:, wi, 8:16], op=ALU.mult), r=[ck, "pfix"], w=[ck])
                    P.op("vector", lambda e, cur=cur, cc=cc: e.tensor_tensor(out=qT[:, cc, :], in0=cur[:, HW:HW + TB], in1=pT[:, HW:HW + TB], op=ALU.subtract), r=[ck, "pT"], w=["qT", "sA", "sB"])
            for cc in range(8):
                g = cc // 2
                pa, pk = P.nA()
                for ci in range(2):
                    P.op("tensor", lambda e, ci=ci, g=g, cc=cc, pa=pa: e.matmul(pa[:, :], lhsT=wgs[:, g, ci, (cc % 2) * 128:(cc % 2 + 1) * 128], rhs=qT[:, 2 * g + ci, :], start=(ci == 0), stop=(ci == 1)),
                         r=["wgs", "qT"], w=[pk])
                P.op("vector", lambda e, cc=cc, pa=pa: e.tensor_scalar(out=yT[:, cc, :], in0=pa[:, :], scalar1=pbg[:, cc:cc + 1], scalar2=psc[:, cc:cc + 1], op0=ALU.add, op1=ALU.mult), r=[pk, "pbg", "psc"], w=["yT"])
            P.mixer_out(X, nt, [yT], ["yT"], w_out, 8, m["GT1"])
        if sparse:
            for ti in range(nt):
                g = b * NT + ti
                h32t, h32k = h32r[0], "h32r0"
                P.norm_mod(X[:, ti, :], "X%d" % ti, m["G2"], m["SH2"], h32t[:], h32k)
                hb, hk = P.nh()
                P.op("scalar", lambda e, hb=hb, h32t=h32t: e.copy(out=hb[:], in_=h32t[:]), r=[h32k], w=[hk])
                P.dma("sync", Hd[g * 128:(g + 1) * 128, :], hb[:], r=[hk], w=["Hd"])
                tok_router2(P, g, h32t, h32k, c)
                P.dma("sync", Xd[g * 128:(g + 1) * 128, :], X[:, ti, :], r=["X%d" % ti], w=["Xd"])
            continue
        if moe:
            P.pre_T(X, nt, m["G2"], m["SH2"], c["hT"], want32=(c["h32"], "h32_"))
            tok_router(P, nt, c)
            for ex in range(8):
                comb = (lambda ti, ex=ex: c["comb"][:, ti, ex:ex + 1], "comb")
                P.ffn(X, nt, c, m["GT2"], wg_d[ex], wu_d[ex], wd_d[ex], FF, comb=comb, first=(ex == 0))
        else:
            P.pre_T(X, nt, m["G2"], m["SH2"], c["hT"])
            P.ffn(X, nt, c, m["GT2"], wg_d, wu_d, wd_d, FF)
        P.ffn_post(X, nt, c, m["GT2"])
        if has_next:
            P.out_block(X, nt, [rows(xo, t) for t in ts], [rows(ho, t) for t in ts], mn["G1"], mn["SH1"])
        else:
            P.out_block(X, nt, [rows(xo, t) for t in ts])
    if sparse:
        def finish_tile(g, ti):
            if has_next:
                hb, hk = P.nh()
                P.norm_mod(X[:, ti, :], "X%d" % ti, mn["G1"], mn["SH1"], hb[:], hk)
                P.dma("sync", rows(ho, g), hb[:], r=[hk])
            P.dma("sync", rows(xo, g), X[:, ti, :], r=["X%d" % ti])
        moe_sparse(P, X, n_tiles, c, m, wg_d, wu_d, wd_d, FF, Xd, Hd, finish_tile)
    return P

def build_S1(L=16384, CTX=256):
    P = Prog()
    SEQP = 2 + CTX + 4 + L + 2
    dI = lambda n, s, dt=F32: P.dram(n, s, dt, "ExternalInput")
    hseq = dI("hseq", [4, D, SEQP], BF16)
    wg_d, wx_d = dI("w_gate", [D, 128]), dI("w_xb", [D, 128])
    cw_d, cb_d = dI("conv_w", [4 * 128]), dI("conv_b", [128, 1])
    wa_d, wxx_d = dI("w_a", [2, 128, 128]), dI("w_x", [2, 128, 128])
    ba_d, bx_d, lam_d = dI("b_a", [128, 2]), dI("b_x", [128, 2]), dI("lam", [128, 2])
    m_d = P.dram("m", [4, 128, L], BF16, "ExternalOutput")
    Wg = P.sb("Wg", [128, 8, 128], BF16)
    Wx = P.sb("Wx", [128, 8, 128], BF16)
    Wxk = P.sb("Wxk", [128, 8, 4, 128], BF16)
    cw = P.sb("cw", [128, 4, 128], F32)
    cb = P.sb("cb", [128, 1], F32)
    wa = P.sb("wa", [128, 2, 128], BF16)
    wxx = P.sb("wxx", [128, 2, 128], BF16)
    ba, bx, lam, cl = P.sb("ba", [128, 2], F32), P.sb("bx", [128, 2], F32), P.sb("lam_s", [128, 2], F32), P.sb("cl", [128, 2], F32)
    st = P.sb("st", [128, 1], F32)
    P.dma("gpsimd", Wg[:], wg_d.rearrange("(kc p) n -> p kc n", p=128), w=["Wg"])
    P.dma("gpsimd", Wx[:], wx_d.rearrange("(kc p) n -> p kc n", p=128), w=["Wx"])
    P.dma("sync", cw[:].rearrange("p a b -> p (a b)"), bass.AP(tensor=cw_d.tensor, offset=0, ap=[[0, 128], [1, 512]]), w=["cw"])
    P.dma("sync", cb[:], cb_d, w=["cb"])
    P.dma("gpsimd", wa[:], wa_d.rearrange("d i j -> i d j"), w=["wa"])
    P.dma("gpsimd", wxx[:], wxx_d.rearrange("d i j -> i d j"), w=["wxx"])
    P.dma("sync", ba[:], ba_d, w=["ba"])
    P.dma("sync", bx[:], bx_d, w=["bx"])
    P.dma("sync", lam[:], lam_d, w=["lam"])
    for k in range(4):
        for kc in range(8):
            P.op("vector", lambda e, k=k, kc=kc: e.tensor_tensor(out=Wxk[:, kc, k, :], in0=Wx[:, kc, :], in1=cw[:, k, :], op=ALU.mult), r=["Wx", "cw"], w=["Wxk"])
    P.op("scalar", lambda e: e.activation(out=cl[:], in_=lam[:], func=AF.Exp, scale=-1.0), r=["lam"], w=["cl"])
    P.op("scalar", lambda e: e.activation(out=cl[:], in_=cl[:], func=AF.Ln, scale=1.0, bias=1.0), r=["cl"], w=["cl"])
    P.op("vector", lambda e: e.tensor_scalar(out=cl[:], in0=cl[:], scalar1=-8.0, scalar2=None, op0=ALU.mult), r=["cl"], w=["cl"])
    ps = [P.ps("ps%d" % i, [128, 512], F32) for i in range(6)]
    pi = [0]
    def nps():
        i = pi[0] % 6
        pi[0] += 1
        return ps[i], "ps%d" % i
    NB = 2
    hc = [P.sb("hc%d" % i, [128, 8, 516], BF16) for i in range(NB)]
    bufs = {}
    for nm, dt in (("xc32", F32), ("xcb", BF16), ("r", F32), ("ii", F32), ("a", F32), ("sq", F32), ("bb", F32), ("hs", F32), ("t1", F32), ("mo", BF16)):
        bufs[nm] = [P.sb("%s%d" % (nm, i), [128, 512], dt) for i in range(NB)]
    ci = 0
    for s in range(4):
        d = s // 2
        P.op("vector", lambda e: e.memset(st[:], 0.0), r=[], w=["st"])
        chunks = [(2, CTX, False, 0)] + [(2 + CTX + 4 + t0, min(512, L - t0), True, t0) for t0 in range(0, L, 512)]
        for (c0, n, is_lat, t0) in chunks:
            sl = ci % NB
            ci += 1
            B = {k: (v[sl], "%s%d" % (k, sl)) for k, v in bufs.items()}
            h, hk = hc[sl], "hc%d" % sl
            P.dma("sync", h[:, :, 0:n + 4], hseq[s].rearrange("(kc p) t -> p kc t", p=128)[:, :, c0 - 2:c0 + n + 2], w=[hk])
            px, pxk = nps()
            idx = 0
            for k in range(4):
                off = (k - 2) if d == 0 else (2 - k)
                for kc in range(8):
                    P.op("tensor", lambda e, k=k, kc=kc, off=off, px=px, h=h, n=n, idx=idx: e.matmul(px[:, 0:n], lhsT=Wxk[:, kc, k, :], rhs=h[:, kc, 2 + off:2 + off + n], start=(idx == 0), stop=(idx == 31)),
                         r=["Wxk", hk], w=[pxk])
                    idx += 1
            xc32, xk = B["xc32"]
            xcb, xbk = B["xcb"]
            P.op("scalar", lambda e, px=px, xc32=xc32, n=n: e.activation(out=xc32[:, 0:n], in_=px[:, 0:n], func=AF.Identity, bias=cb[:, 0:1], scale=1.0), r=[pxk, "cb"], w=[xk])
            P.op("vector", lambda e, xc32=xc32, xcb=xcb, n=n: e.tensor_copy(out=xcb[:, 0:n], in_=xc32[:, 0:n]), r=[xk], w=[xbk])
            pr, prk = nps()
            P.op("tensor", lambda e, pr=pr, xcb=xcb, n=n, d=d: e.matmul(pr[:, 0:n], lhsT=wa[:, d, :], rhs=xcb[:, 0:n], start=True, stop=True), r=["wa", xbk], w=[prk])
            pI, pik = nps()
            P.op("tensor", lambda e, pI=pI, xcb=xcb, n=n, d=d: e.matmul(pI[:, 0:n], lhsT=wxx[:, d, :], rhs=xcb[:, 0:n], start=True, stop=True), r=["wxx", xbk], w=[pik])
            r_, rk = B["r"]
            i_, ik = B["ii"]
            a_, ak = B["a"]
            sq, sqk = B["sq"]
            bb, bbk = B["bb"]
            hs, hsk = B["hs"]
            P.op("scalar", lambda e, pr=pr, r_=r_, n=n, d=d: e.activation(out=r_[:, 0:n], in_=pr[:, 0:n], func=AF.Sigmoid, bias=ba[:, d:d + 1], scale=1.0), r=[prk, "ba"], w=[rk])
            P.op("scalar", lambda e, pI=pI, i_=i_, n=n, d=d: e.activation(out=i_[:, 0:n], in_=pI[:, 0:n], func=AF.Sigmoid, bias=bx[:, d:d + 1], scale=1.0), r=[pik, "bx"], w=[ik])
            P.op("scalar", lambda e, r_=r_, a_=a_, n=n, d=d: e.activation(out=a_[:, 0:n], in_=r_[:, 0:n], func=AF.Exp, scale=cl[:, d:d + 1]), r=[rk, "cl"], w=[ak])
            P.op("scalar", lambda e, a_=a_, sq=sq, n=n: e.activation(out=sq[:, 0:n], in_=a_[:, 0:n], func=AF.Square), r=[ak], w=[sqk])
            P.op("scalar", lambda e, sq=sq, n=n: e.activation(out=sq[:, 0:n], in_=sq[:, 0:n], func=AF.Sqrt, scale=-1.0, bias=1.0), r=[sqk], w=[sqk])
            P.op("vector", lambda e, i_=i_, xc32=xc32, bb=bb, n=n: e.tensor_tensor(out=bb[:, 0:n], in0=i_[:, 0:n], in1=xc32[:, 0:n], op=ALU.mult), r=[ik, xk], w=[bbk])
            P.op("vector", lambda e, sq=sq, bb=bb, n=n: e.tensor_tensor(out=bb[:, 0:n], in0=bb[:, 0:n], in1=sq[:, 0:n], op=ALU.mult), r=[sqk, bbk], w=[bbk])
            P.op("vector", lambda e, a_=a_, bb=bb, hs=hs, n=n: e.tensor_tensor_scan(out=hs[:, 0:n], data0=a_[:, 0:n], data1=bb[:, 0:n], initial=st[:, 0:1], op0=ALU.mult, op1=ALU.add), r=[ak, bbk, "st"], w=[hsk])
            P.op("vector", lambda e, hs=hs, n=n: e.tensor_copy(out=st[:, 0:1], in_=hs[:, n - 1:n]), r=[hsk], w=["st"])
            if is_lat:
                pg, pgk = nps()
                for kc in range(8):
                    P.op("tensor", lambda e, kc=kc, pg=pg, h=h, n=n: e.matmul(pg[:, 0:n], lhsT=Wg[:, kc, :], rhs=h[:, kc, 2:2 + n], start=(kc == 0), stop=(kc == 7)), r=["Wg", hk], w=[pgk])
                t1, t1k = B["t1"]
                mo, mok = B["mo"]
                g = pg[:, 0:n]
                t = t1[:, 0:n]
                P.op("scalar", lambda e, t=t, g=g: e.activation(out=t, in_=g, func=AF.Square), r=[pgk], w=[t1k])
                P.op("vector", lambda e, t=t: e.tensor_scalar(out=t, in0=t, scalar1=0.044715, scalar2=1.0, op0=ALU.mult, op1=ALU.add), r=[t1k], w=[t1k])
                P.op("vector", lambda e, t=t, g=g: e.tensor_tensor(out=t, in0=t, in1=g, op=ALU.mult), r=[t1k, pgk], w=[t1k])
                P.op("scalar", lambda e, t=t: e.activation(out=t, in_=t, func=AF.Sigmoid, scale=1.5957691216057308), r=[t1k], w=[t1k])
                P.op("vector", lambda e, t=t, g=g: e.tensor_tensor(out=t, in0=t, in1=g, op=ALU.mult), r=[t1k, pgk], w=[t1k])
                P.op("vector", lambda e, t=t, hs=hs, mo=mo, n=n: e.tensor_tensor(out=mo[:, 0:n], in0=t, in1=hs[:, 0:n], op=ALU.mult), r=[t1k, hsk], w=[mok])
                P.dma("sync", m_d[s][:, t0:t0 + n], mo[:, 0:n], r=[mok])
    return P

def build_S3a(n_tiles=32):
    P = Prog()
    NTOK = n_tiles * 128
    dI = lambda n, s, dt=F32: P.dram(n, s, dt, "ExternalInput")
    hT_d = dI("hT", [D, NTOK + 2], BF16)
    w_d, cw_d, cb_d = dI("w_in", [D, 3072]), dI("conv_w", [3 * 3072]), dI("conv_b", [3072])
    z_d = P.dram("z", [NTOK, 3072], F32, "ExternalOutput")
    hT = P.sb("hT_s", [128, 8, NTOK + 2], BF16)
    P.dma("sync", hT[:], hT_d.rearrange("(kc p) t -> p kc t", p=128), w=["hT"])
    W = [P.sb("W%d" % i, [128, 8, 512], BF16) for i in range(2)]
    Wk = [P.sb("Wk%d" % i, [128, 3, 8, 512], BF16) for i in range(2)]
    cwb = [P.sb("cwb%d" % i, [128, 3, 512], F32) for i in range(2)]
    bb = [P.sb("bb%d" % i, [128, 512], F32) for i in range(2)]
    zo = [P.sb("zo%d" % i, [128, 512], F32) for i in range(3)]
    ps = [P.ps("ps%d" % i, [128, 512], F32) for i in range(4)]
    wv = w_d.rearrange("(kc p) n -> p kc n", p=128)
    n = 0
    for cb in range(6):
        s = cb % 2
        P.dma("gpsimd", W[s][:], wv[:, :, cb * 512:(cb + 1) * 512], w=["W%d" % s])
        for k in range(3):
            P.dma("sync", cwb[s][:, k, :], bass.AP(tensor=cw_d.tensor, offset=k * 3072 + cb * 512, ap=[[0, 128], [1, 512]]), w=["cwb%d_%d" % (s, k)])
        P.dma("sync", bb[s][:], bass.AP(tensor=cb_d.tensor, offset=cb * 512, ap=[[0, 128], [1, 512]]), w=["bb%d" % s])
        for k in range(3):
            for kc in range(8):
                eng = "vector" if (kc % 2 == 0) else "gpsimd"
                P.op(eng, lambda e, s=s, k=k, kc=kc: e.tensor_tensor(out=Wk[s][:, k, kc, :], in0=W[s][:, kc, :], in1=cwb[s][:, k, :], op=ALU.mult),
                     r=["W%d" % s, "cwb%d_%d" % (s, k)], w=["Wk%d_%d" % (s, k)])
        for ti in range(n_tiles):
            pa, pk = ps[n % 4], "ps%d" % (n % 4)
            zt, zk = zo[n % 3], "zo%d" % (n % 3)
            n += 1
            idx = 0
            for k in range(3):
                for kc in range(8):
                    P.op("tensor", lambda e, s=s, k=k, kc=kc, ti=ti, pa=pa, idx=idx: e.matmul(pa[:, :], lhsT=hT[:, kc, ti * 128 + k:ti * 128 + k + 128], rhs=Wk[s][:, k, kc, :], start=(idx == 0), stop=(idx == 23)),
                         r=["hT", "Wk%d_%d" % (s, k)], w=[pk])
                    idx += 1
            P.op("vector", lambda e, pa=pa, zt=zt, s=s: e.tensor_tensor(out=zt[:], in0=pa[:, :], in1=bb[s][:], op=ALU.add), r=[pk, "bb%d" % s], w=[zk])
            P.dma("sync", z_d[ti * 128:(ti + 1) * 128, cb * 512:(cb + 1) * 512], zt[:], r=[zk])
    return P


def build_S3b(L=16384, NCH=128):
    P = Prog()
    NB = L // 128
    NC2 = 2 * NB
    L2 = 2 * L
    NBLK = L2 // 512
    dI = lambda n, s, dt=F32: P.dram(n, s, dt, "ExternalInput")
    vrev_d, vnat_d = dI("vrev", [NCH, 128, NC2]), dI("vnat", [NCH, 128, NC2])
    x1_d, x2_d = dI("x1nat", [NCH, 128, NC2]), dI("x2rev", [NCH, 128, NC2])
    zT_d = [dI("zT1", [33, L2]), dI("zT2", [33, L2])]
    win_d = [dI("win1", [128, L2]), dI("win2", [128, L2])]
    w1_d, w2_d, w3_d = dI("f_w1", [33, 64]), dI("f_w2", [64, 64]), dI("f_w3", [64, 64])
    fb_d, fr_d = dI("f_b", [64, 3]), dI("f_freq", [64, 3])
    wo_d = dI("f_wout", [64, 4 * 128])
    hb_d = dI("hbias", [2 * 128])
    J_d = dI("J", [128, 128])
    y2_d = P.dram("y2rev", [NCH, 128, NC2], BF16, "ExternalOutput")
    Ktab = [P.dram("Ktab%d" % i, [128, L2], BF16, "Internal") for i in range(2)]
    Y1d = P.dram("Y1d", [NCH, 128, NC2], BF16, "Internal")
    w1, w2, w3 = P.sb("w1", [33, 64], F32), P.sb("w2", [64, 64], F32), P.sb("w3", [64, 64], F32)
    fb, fr, wo = P.sb("fb", [64, 3], F32), P.sb("fr", [64, 3], F32), P.sb("wo", [64, 4, 128], F32)
    hbias = P.sb("hbias_s", [128, 2, 128], F32)
    J = P.sb("J_s", [128, 128], BF16)
    for t, d_, k in ((w1, w1_d, "w1"), (w2, w2_d, "w2"), (w3, w3_d, "w3"), (fb, fb_d, "fb"), (fr, fr_d, "fr")):
        P.dma("sync", t[:], d_, w=[k])
    P.dma("sync", wo[:].rearrange("p a b -> p (a b)"), wo_d, w=["wo"])
    P.dma("sync", hbias[:].rearrange("p a b -> p (a b)"), bass.AP(tensor=hb_d.tensor, offset=0, ap=[[0, 128], [1, 256]]), w=["hbias"])
    P.dma("gpsimd", J[:], J_d, w=["J"])
    ps = [P.ps("ps%d" % i, [128, 512], F32) for i in range(8)]
    pi = [0]
    def nps():
        i = pi[0] % 8
        pi[0] += 1
        return ps[i], "ps%d" % i
    zt = [P.sb("zt%d" % i, [33, 512], F32) for i in range(2)]
    wn = [P.sb("wn%d" % i, [128, 512], F32) for i in range(2)]
    hd = [P.sb("hd%d" % i, [64, 512], F32) for i in range(2)]
    wi = [P.sb("wi%d" % i, [64, 512], I32) for i in range(2)]
    g1 = [P.sb("g1_%d" % i, [64, 512], F32) for i in range(2)]
    kb = [P.sb("kb%d" % i, [128, 512], BF16) for i in range(2)]
    TWO_PI = 6.283185307179586
    n = 0
    for tab in range(2):
        for blk in range(NBLK):
            s = n % 2
            n += 1
            m0 = blk * 512
            P.dma("sync", zt[s][:], zT_d[tab][:, m0:m0 + 512], w=["zt%d" % s])
            P.dma("sync", wn[s][:], win_d[tab][:, m0:m0 + 512], w=["wn%d" % s])
            src, skey, W_ = zt[s], "zt%d" % s, [w1, w2, w3]
            for ly in range(3):
                pa, pk = nps()
                nk = 33 if ly == 0 else 64
                P.op("tensor", lambda e, pa=pa, src=src, ly=ly, W_=W_, nk=nk: e.matmul(pa[0:64, :], lhsT=W_[ly][0:nk, :], rhs=src[0:nk, :], start=True, stop=True),
                     r=[skey, "w%d" % (ly + 1)], w=[pk])
                h, hk = hd[s], "hd%d" % s
                P.op("vector", lambda e, pa=pa, h=h, ly=ly: e.tensor_scalar(out=h[:], in0=pa[0:64, :], scalar1=fb[:, ly:ly + 1], scalar2=fr[:, ly:ly + 1], op0=ALU.add, op1=ALU.mult), r=[pk, "fb", "fr"], w=[hk])
                P.op("vector", lambda e, h=h: e.tensor_scalar(out=h[:], in0=h[:], scalar1=1.0 / TWO_PI, scalar2=None, op0=ALU.mult), r=[hk], w=[hk])
                P.op("vector", lambda e, h=h, s=s: e.tensor_copy(out=wi[s][:], in_=h[:]), r=[hk], w=["wi%d" % s])
                P.op("vector", lambda e, s=s: e.tensor_copy(out=g1[s][:], in_=wi[s][:]), r=["wi%d" % s], w=["g1_%d" % s])
                P.op("vector", lambda e, h=h, s=s: e.tensor_tensor(out=h[:], in0=h[:], in1=g1[s][:], op=ALU.subtract), r=[hk, "g1_%d" % s], w=[hk])
                P.op("vector", lambda e, h=h, s=s: e.tensor_scalar(out=g1[s][:], in0=h[:], scalar1=0.5, scalar2=None, op0=ALU.is_gt), r=[hk], w=["g1_%d" % s])
                P.op("vector", lambda e, h=h, s=s: e.tensor_tensor(out=h[:], in0=h[:], in1=g1[s][:], op=ALU.subtract), r=[hk, "g1_%d" % s], w=[hk])
                P.op("vector", lambda e, h=h, s=s: e.tensor_scalar(out=g1[s][:], in0=h[:], scalar1=-0.5, scalar2=None, op0=ALU.is_lt), r=[hk], w=["g1_%d" % s])
                P.op("vector", lambda e, h=h, s=s: e.tensor_tensor(out=h[:], in0=h[:], in1=g1[s][:], op=ALU.add), r=[hk, "g1_%d" % s], w=[hk])
                P.op("scalar", lambda e, h=h: e.activation(out=h[:], in_=h[:], func=AF.Sin, scale=TWO_PI), r=[hk], w=[hk])
                src, skey = h, hk
            pa, pk = nps()
            o = tab
            if tab == 0:
                dirs = [(1, 0, 512)] if blk < NBLK // 2 - 1 else ([(1, 0, 511), (0, 511, 512)] if blk == NBLK // 2 - 1 else [(0, 0, 512)])
            else:
                dirs = [(0, 0, 512)] if blk < NBLK // 2 else [(1, 0, 512)]
            for (dr, ca, cb_) in dirs:
                P.op("tensor", lambda e, pa=pa, src=src, o=o, dr=dr, ca=ca, cb_=cb_: e.matmul(pa[:, ca:cb_], lhsT=wo[:, o * 2 + dr, :], rhs=src[:, ca:cb_], start=True, stop=True),
                     r=[skey, "wo"], w=[pk])
            P.op("vector", lambda e, pa=pa, s=s: e.tensor_tensor(out=kb[s][:], in0=pa[:, :], in1=wn[s][:], op=ALU.mult), r=[pk, "wn%d" % s], w=["kb%d" % s])
            P.dma("sync", Ktab[tab][:, m0:m0 + 512], kb[s][:], r=["kb%d" % s], w=["Ktab%d" % tab])
    HALF = NB * 128
    T = [P.sb("T%d" % i, [128, HALF], BF16) for i in range(4)]
    vb = [P.sb("vb%d" % i, [128, NC2], BF16) for i in range(2)]
    gA = [P.sb("gA%d" % i, [128, NC2], F32) for i in range(2)]
    gB = [P.sb("gB%d" % i, [128, NC2], F32) for i in range(2)]
    tt = [P.sb("tt%d" % i, [128, NC2], F32) for i in range(2)]
    yo = [P.sb("yo%d" % i, [128, NC2], BF16) for i in range(2)]
    tn = 0
    n = 0
    for stage in range(2):
        for c in range(NCH):
            s = n % 2
            n += 1
            if stage == 0:
                P.dma("gpsimd", vb[s][:], vrev_d[c], w=["vb%d" % s])
                P.dma("sync", gA[s][:], vnat_d[c], w=["gA%d" % s])
                P.dma("sync", gB[s][:], x1_d[c], w=["gB%d" % s])
            else:
                P.dma("sync", vb[s][:], Y1d[c], r=["Y1d"], w=["vb%d" % s])
                P.dma("sync", gB[s][:], x2_d[c], w=["gB%d" % s])
                pj, pjk = nps()
                P.op("tensor", lambda e, pj=pj, s=s: e.matmul(pj[:, 0:NC2], lhsT=J[:], rhs=vb[s][:], start=True, stop=True), r=["J", "vb%d" % s], w=[pjk])
                P.op("scalar", lambda e, pj=pj, s=s: e.copy(out=gA[s][:], in_=pj[:, 0:NC2]), r=[pjk], w=["gA%d" % s])
            pa, pk = nps()
            pav = pa[:, 0:NC2].rearrange("p (b i) -> p b i", b=2)
            vv = vb[s][:].rearrange("p (b i) -> p b i", b=2)
            order = [NB - 1] + [k for k in range(2 * NB - 1) if k != NB - 1]
            loaded = {}
            cnt = 0
            for k in order:
                hf = 0 if k < NB else 1
                if hf not in loaded:
                    ts_ = tn % 4
                    tn += 1
                    nel = HALF if hf == 0 else HALF - 128
                    P.dma(("sync", "scalar")[tn % 2], T[ts_][:, 0:nel], bass.AP(tensor=Ktab[stage].tensor, offset=c * L2 + hf * HALF, ap=[[1, 128], [1, nel]]),
                          r=["Ktab%d" % stage], w=["T%d" % ts_])
                    loaded[hf] = ts_
                ts_ = loaded[hf]
                delta = (k - (NB - 1)) if stage == 0 else ((NB - 1) - k)
                i0, i1 = max(0, delta), min(NB, NB + delta)
                kk = k - hf * NB
                P.op("tensor", lambda e, pav=pav, vv=vv, ts_=ts_, kk=kk, i0=i0, i1=i1, delta=delta, cnt=cnt: e.matmul(pav[:, :, i0:i1], lhsT=T[ts_][:, kk * 128:(kk + 1) * 128], rhs=vv[:, :, i0 - delta:i1 - delta], start=(cnt == 0), stop=(cnt == 2 * NB - 2)),
                     r=["T%d" % ts_, "vb%d" % s], w=[pk])
                cnt += 1
            P.op("vector", lambda e, pa=pa, s=s, stage=stage, c=c: e.scalar_tensor_tensor(out=tt[s][:], in0=gA[s][:], scalar=hbias[:, stage, c:c + 1], in1=pa[:, 0:NC2], op0=ALU.mult, op1=ALU.add),
                 r=[pk, "gA%d" % s, "hbias"], w=["tt%d" % s])
            P.op("gpsimd", lambda e, s=s: e.tensor_tensor(out=yo[s][:], in0=tt[s][:], in1=gB[s][:], op=ALU.mult), r=["tt%d" % s, "gB%d" % s], w=["yo%d" % s])
            if stage == 0:
                P.dma("sync", Y1d[c], yo[s][:], r=["yo%d" % s], w=["Y1d"])
            else:
                P.dma("sync", y2_d[c], yo[s][:], r=["yo%d" % s])
    return P

def hyena_consts(L, ch0, nch):
    f32 = np.float32
    t = np.linspace(0.0, 1.0, L, dtype=f32)[:, None]
    w = (2.0 * np.pi * np.arange(L, dtype=f32)[:, None] / L).astype(f32)
    bands = np.linspace(1e-4, 15, 16, dtype=f32)[None, :]
    z = np.concatenate([t, np.cos(bands * w), np.sin(-bands * w)], axis=-1).astype(f32)
    max_decay = math.log(1e-2) / 0.3
    min_decay = math.log(1e-2) / 1.5
    deltas = np.abs(np.linspace(min_decay, max_decay, 1024, dtype=f32))
    window = np.exp(-t * deltas[None, ch0:ch0 + nch]).astype(f32)
    m = np.arange(2 * L)
    d1 = np.clip(np.abs(m - (L - 1)), 0, L - 1)
    d2 = np.clip(np.abs((L - 1) - m), 0, L - 1)
    zT1 = np.ascontiguousarray(z[d1].T)
    zT2 = np.ascontiguousarray(z[d2].T)
    win1 = np.zeros((128, 2 * L), f32)
    win2 = np.zeros((128, 2 * L), f32)
    win1[:nch] = window[d1].T
    win2[:nch] = window[d2].T
    return zT1, zT2, win1, win2


def moe_lay_gu(w):
    return np.ascontiguousarray(w.reshape(8, 8, 128, 4, 896).transpose(0, 3, 2, 1, 4)).reshape(4096, 4, 1792)


def moe_lay_d(w):
    return np.ascontiguousarray(w.reshape(8, 4, 7, 128, 1024).transpose(0, 1, 3, 2, 4)).reshape(4096, 4, 1792)

def _pos_embed(rows):
    f32 = np.float32
    r, col = np.meshgrid(np.arange(rows, dtype=f32), np.arange(64, dtype=f32), indexing='ij')
    quarter = D // 4
    omega = (1.0 / (f32(10000.0) ** (np.arange(quarter, dtype=f32) / f32(quarter)))).astype(f32)
    def sincos(p):
        ang = (p.reshape(-1, 1) * omega[None, :]).astype(f32)
        return np.concatenate([np.sin(ang), np.cos(ang)], axis=-1)
    return np.concatenate([sincos(r), sincos(col)], axis=-1).astype(f32)


_LAST_DBG = {}


def _run(P, in_maps):
    nc = P.emit()
    res = run_bass_kernel_spmd(nc, in_maps, core_ids=list(range(8)))
    return res.results


def _col8(v):
    return np.ascontiguousarray(np.asarray(v, np.float32).reshape(8, 128).T)


def kernel(**inp):
    f32 = np.float32
    bf = ml_dtypes.bfloat16
    inp = {k: np.asarray(v) for k, v in inp.items()}
    B, L = 2, 16384
    TPC = 4096
    ident = np.eye(128, dtype=f32)
    pos = _pos_embed(L // 64)
    dbg = _LAST_DBG
    dbg.clear()
    cb = lambda k: (k // 4, (k % 4) * TPC)
    wsT = np.ascontiguousarray(inp['gmlp_w_s'][0].transpose(2, 0, 1)).reshape(128, 1024)
    bs16 = np.ascontiguousarray(np.repeat(inp['gmlp_b_s'][0], 2, axis=0).reshape(-1))
    maps = []
    for k in range(8):
        b, t0 = cb(k)
        bc_, half = (k % 4) // 2, (k % 4) % 2
        maps.append(dict(ident=ident, x=inp['x'][b, t0:t0 + TPC], pos=pos[t0:t0 + TPC], ctx=inp['ctx'][bc_, half * 128:(half + 1) * 128],
                         c=_col8(inp['c'][b]), c_ctx=_col8(inp['c_ctx']),
                         ada_w0=inp['ada_w'][0], ada_b0=inp['ada_b'][0], ng0=inp['norm_g'][0].reshape(-1),
                         ada_w1=inp['ada_w'][1], ada_b1=inp['ada_b'][1], ng1=inp['norm_g'][1].reshape(-1),
                         g_win=inp['gmlp_w_in'][0], g_gv=inp['gmlp_g_v'][0], g_wsT=wsT, g_bs=bs16, g_wout=inp['gmlp_w_out'][0],
                         f_wg=inp['ffn_w_gate'][0], f_wu=inp['ffn_w_up'][0], f_wd=inp['ffn_w_down'][0]))
    r = _run(build_S0(), maps)
    x1 = np.stack([np.concatenate([r[b * 4 + j]['xo'] for j in range(4)], 0) for b in range(2)])
    h1 = np.stack([np.concatenate([r[b * 4 + j]['ho'] for j in range(4)], 0) for b in range(2)])
    h1c = np.stack([np.concatenate([r[bc_ * 2 + half]['hco'] for half in range(2)], 0) for bc_ in range(2)])
    dbg['x0'] = x1
    del r
    CTX = 256
    SEQP = 2 + CTX + 4 + L + 2
    hseq = np.zeros((4, D, SEQP), dtype=bf)
    for d in range(2):
        for b in range(2):
            cs, ls = h1c[b], h1[b]
            if d == 1:
                cs, ls = cs[::-1], ls[::-1]
            hseq[d * 2 + b, :, 2:2 + CTX] = cs.T
            hseq[d * 2 + b, :, 2 + CTX + 4:2 + CTX + 4 + L] = ls.T
    maps = []
    for k in range(8):
        sl = slice(k * 128, (k + 1) * 128)
        maps.append(dict(hseq=hseq, w_gate=np.ascontiguousarray(inp['lru_w_in'][0][:, sl]), w_xb=np.ascontiguousarray(inp['lru_w_in'][0][:, 1024 + k * 128:1024 + (k + 1) * 128]),
                         conv_w=np.ascontiguousarray(inp['lru_conv_w'][0][:, sl]).reshape(-1), conv_b=np.ascontiguousarray(inp['lru_conv_b'][0][sl].reshape(128, 1)),
                         w_a=np.ascontiguousarray(inp['lru_w_a'][0][:, k]), w_x=np.ascontiguousarray(inp['lru_w_x'][0][:, k]),
                         b_a=np.ascontiguousarray(inp['lru_b_a'][0][:, sl].T), b_x=np.ascontiguousarray(inp['lru_b_x'][0][:, sl].T),
                         lam=np.ascontiguousarray(inp['lru_lam'][0][:, sl].T)))
    r = _run(build_S1(L=L, CTX=CTX), maps)
    del hseq
    mf = [np.concatenate([r[k]['m'][b] for k in range(8)], 0) for b in range(2)]
    mb = [np.concatenate([r[k]['m'][2 + b][:, ::-1] for k in range(8)], 0) for b in range(2)]
    del r
    NBLK = (2 * TPC + 8 * 511) // 512
    Utri = np.triu(np.ones((128, 128), f32), 1)
    blkrow = np.ascontiguousarray(np.broadcast_to((512.0 * np.arange(NBLK, dtype=f32))[None], (128, NBLK)))
    rowc = np.ascontiguousarray(np.concatenate([np.arange(128, dtype=f32)[:, None] + 128.0 * np.arange(4, dtype=f32)[None, :], np.zeros((128, 4), f32)], 1))
    moe_w = [(moe_lay_gu(inp['moe_w_gate'][k_]), moe_lay_gu(inp['moe_w_up'][k_]), moe_lay_d(inp['moe_w_down'][k_])) for k_ in range(2)]
    def wr_lay(w):
        return np.ascontiguousarray(w.reshape(8, 128, 8).transpose(1, 0, 2).reshape(128, 64))
    maps = []
    for k in range(8):
        b, t0 = cb(k)
        maps.append(dict(ident=ident, x=x1[b, t0:t0 + TPC], c=_col8(inp['c'][b]),
                         ada_w=inp['ada_w'][1], ada_b=inp['ada_b'][1], ng=inp['norm_g'][1].reshape(-1),
                         ada_wn=inp['ada_w'][2], ada_bn=inp['ada_b'][2], ngn=inp['norm_g'][2].reshape(-1),
                         w_out=inp['lru_w_out'][0], w_router=wr_lay(inp['moe_w_router'][0]),
                         wg=moe_w[0][0], wu=moe_w[0][1], wd=moe_w[0][2], Utri=Utri, blkrow=blkrow, rowc=rowc,
                         mf=np.ascontiguousarray(mf[b][:, t0:t0 + TPC]), mb=np.ascontiguousarray(mb[b][:, t0:t0 + TPC])))
    r = _run(build_TOK2("S2"), maps)
    x2 = np.stack([np.concatenate([r[b * 4 + j]['xo'] for j in range(4)], 0) for b in range(2)])
    h2 = np.stack([np.concatenate([r[b * 4 + j]['ho'] for j in range(4)], 0) for b in range(2)])
    dbg['x1'] = x2
    del r, mf, mb, x1, h1
    maps = []
    for k in range(8):
        b, t0 = cb(k)
        hp = np.zeros((D, TPC + 2), dtype=bf)
        lo, hi = max(t0 - 1, 0), min(t0 + TPC + 1, L)
        hp[:, lo - (t0 - 1):hi - (t0 - 1)] = h2[b, lo:hi].T
        maps.append(dict(hT=hp, w_in=inp['hyena_w_in'][0], conv_w=inp['hyena_conv_w'][0].reshape(-1), conv_b=inp['hyena_conv_b'][0]))
    r = _run(build_S3a(), maps)
    z = np.stack([np.concatenate([r[b * 4 + j]['z'] for j in range(4)], 0) for b in range(2)])
    del r, h2
    NB = L // 128
    def lay(a, rev):
        t = a.reshape(2, NB, 128, 128).transpose(3, 2, 0, 1)
        if rev:
            t = t[:, ::-1]
        return np.ascontiguousarray(t.reshape(128, 128, 2 * NB))
    fbias = np.ascontiguousarray(np.stack([inp['hyena_f_b1'][0], inp['hyena_f_b2'][0], inp['hyena_f_b3'][0]], 1))
    ffreq = np.ascontiguousarray(inp['hyena_f_freq'][0].T)
    Jm = np.ascontiguousarray(np.eye(128, dtype=f32)[::-1])
    maps = []
    for k in range(8):
        ch0 = k * 128
        zT1, zT2, win1, win2 = hyena_consts(L, ch0, 128)
        wo = np.ascontiguousarray(inp['hyena_f_wout'][0].reshape(64, 4, 1024)[:, :, ch0:ch0 + 128]).reshape(64, 512)
        maps.append(dict(vrev=lay(z[:, :, 2048 + ch0:2048 + ch0 + 128], True), vnat=lay(z[:, :, 2048 + ch0:2048 + ch0 + 128], False),
                         x1nat=lay(z[:, :, ch0:ch0 + 128], False), x2rev=lay(z[:, :, 1024 + ch0:1024 + ch0 + 128], True),
                         zT1=zT1, zT2=zT2, win1=win1, win2=win2, f_w1=inp['hyena_f_w1'][0], f_w2=inp['hyena_f_w2'][0], f_w3=inp['hyena_f_w3'][0],
                         f_b=fbias, f_freq=ffreq, f_wout=wo, hbias=np.ascontiguousarray(inp['hyena_bias'][0][:, ch0:ch0 + 128]).reshape(-1), J=Jm))
    r = _run(build_S3b(L=L, NCH=128), maps)
    del z
    m2 = np.zeros((2, D, L), dtype=bf)
    for k in range(8):
        y = r[k]['y2rev'][:, ::-1]
        m2[:, k * 128:(k + 1) * 128, :] = y.reshape(128, 128, 2, NB).transpose(2, 0, 3, 1).reshape(2, 128, L)
    del r
    maps = []
    for k in range(8):
        b, t0 = cb(k)
        maps.append(dict(ident=ident, x=x2[b, t0:t0 + TPC], c=_col8(inp['c'][b]),
                         ada_w=inp['ada_w'][2], ada_b=inp['ada_b'][2], ng=inp['norm_g'][2].reshape(-1),
                         ada_wn=inp['ada_w'][3], ada_bn=inp['ada_b'][3], ngn=inp['norm_g'][3].reshape(-1),
                         w_out=inp['hyena_w_out'][0], wg=inp['ffn_w_gate'][1], wu=inp['ffn_w_up'][1], wd=inp['ffn_w_down'][1],
                         mf=np.ascontiguousarray(m2[b][:, t0:t0 + TPC])))
    r = _run(build_TOK2("S4"), maps)
    x3 = np.stack([np.concatenate([r[b * 4 + j]['xo'] for j in range(4)], 0) for b in range(2)])
    h3 = np.stack([np.concatenate([r[b * 4 + j]['ho'] for j in range(4)], 0) for b in range(2)])
    dbg['x2'] = x3
    del r, m2, x2
    HW = 8
    maps = []
    for k in range(8):
        b, t0 = cb(k)
        hp = np.zeros((D, TPC + 2 * HW), dtype=bf)
        lo, hi = max(t0 - HW, 0), min(t0 + TPC + HW, L)
        hp[:, lo - (t0 - HW):hi - (t0 - HW)] = h3[b, lo:hi].T
        fix = np.ones((4, 16), f32)
        for wi_, win in enumerate((2, 4, 8, 16)):
            for j in range(8):
                if k % 4 == 0:
                    t = j
                    fix[wi_, j] = win / float(min(t + win // 2, L) - max(t - win // 2, 0))
                if k % 4 == 3:
                    t = L - 8 + j
                    fix[wi_, 8 + j] = win / float(min(t + win // 2, L) - max(t - win // 2, 0))
        maps.append(dict(ident=ident, x=x3[b, t0:t0 + TPC], c=_col8(inp['c'][b]),
                         ada_w=inp['ada_w'][3], ada_b=inp['ada_b'][3], ng=inp['norm_g'][3].reshape(-1),
                         w_out=inp['pool_w_out'][0], w_router=wr_lay(inp['moe_w_router'][1]),
                         wg=moe_w[1][0], wu=moe_w[1][1], wd=moe_w[1][2], Utri=Utri, blkrow=blkrow, rowc=rowc,
                         h3T=hp, p_win=inp['pool_w_in'][0], p_wg=inp['pool_w_g'][0], p_bg=_col8(inp['pool_b_g'][0].reshape(-1)),
                         p_scale=_col8(inp['pool_scale'][0]), p_fix=np.ascontiguousarray(np.broadcast_to(fix.reshape(1, 64), (128, 64)))))
    r = _run(build_TOK2("S5"), maps)
    out = np.stack([np.concatenate([r[b * 4 + j]['xo'] for j in range(4)], 0) for b in range(2)]).astype(f32)
    return out
```

```python
import os, math
import ml_dtypes
import numpy as np
from contextlib import ExitStack
import concourse.bass as bass
import concourse.mybir as mybir
from concourse.bass_utils import run_bass_kernel_spmd

F32 = mybir.dt.float32
BF16 = mybir.dt.bfloat16
I32 = mybir.dt.int32
AF = mybir.ActivationFunctionType
ALU = mybir.AluOpType
AX = mybir.AxisListType

COMPUTE = ("tensor", "vector", "scalar", "gpsimd")
DMAQ = ("sync", "gpsimd")
SAME_ENGINE_SYNC = True


class Prog:
    def __init__(self, name="k"):
        self.nc = bass.Bass("TRN2", target_bir_lowering=False)
        self.es = ExitStack()
        self.ops = []
        self.last_w = {}
        self.readers = {}
        self.n_dma_sems = {"sync": 24, "gpsimd": 24, "scalar": 16}
        self.names = set()

    def dram(self, name, shape, dt, kind):
        return self.nc.dram_tensor(name, list(shape), dt, kind=kind).ap()

    def sb(self, name, shape, dt):
        assert name not in self.names, name
        self.names.add(name)
        return self.es.enter_context(self.nc.sbuf_tensor(name, list(shape), dt))

    def ps(self, name, shape, dt):
        assert name not in self.names, name
        self.names.add(name)
        return self.es.enter_context(self.nc.psum_tensor(name, list(shape), dt))

    def op(self, eng, fn, r=(), w=(), dma=False):
        i = len(self.ops)
        deps = set()
        for k in r:
            if k in self.last_w:
                deps.add(self.last_w[k])
        for k in w:
            if k in self.last_w:
                deps.add(self.last_w[k])
            for j in self.readers.get(k, ()):
                deps.add(j)
        deps.discard(i)
        self.ops.append(dict(eng=eng, fn=fn, deps=sorted(deps), dma=dma))
        for k in r:
            self.readers.setdefault(k, []).append(i)
        for k in w:
            self.last_w[k] = i
            self.readers[k] = []
        return i

    def dma(self, q, out, in_, r=(), w=(), **kw):
        return self.op(q, lambda e: e.dma_start(out=out, in_=(in_() if callable(in_) else in_), **kw), r, w, dma=True)

    def emit(self):
        nc = self.nc
        ops = self.ops
        es = self.es
        eng_ops = {}
        for i, o in enumerate(ops):
            lst = eng_ops.setdefault(o["eng"], [])
            o["pos"] = len(lst)
            lst.append(i)
        dcount = {}
        for i, o in enumerate(ops):
            if o["dma"]:
                q = o["eng"]
                o["dnum"] = dcount.get(q, 0)
                dcount[q] = o["dnum"] + 1
        seen = {e: {} for e in eng_ops}
        seen_dma = {e: set() for e in eng_ops}
        for i, o in enumerate(ops):
            X = o["eng"]
            waits = []
            best = {}
            for j in o["deps"]:
                p = ops[j]
                if p["dma"]:
                    if j in seen_dma[X]:
                        continue
                    seen_dma[X].add(j)
                    waits.append(j)
                else:
                    E = p["eng"]
                    if E == X and (E == "tensor" or not SAME_ENGINE_SYNC):
                        continue
                    if E not in best or ops[best[E]]["pos"] < p["pos"]:
                        best[E] = j
            for E, j in best.items():
                p = ops[j]
                if seen[X].get(E, -1) >= p["pos"]:
                    continue
                seen[X][E] = p["pos"]
                waits.append(j)
            o["waits"] = waits
            for j in waits:
                ops[j]["sig"] = True
        sigcount = {}
        for i, o in enumerate(ops):
            if o["dma"]:
                continue
            if o.get("sig"):
                E = o["eng"]
                sigcount[E] = sigcount.get(E, 0) + 1
                o["signum"] = sigcount[E]
        csem = {e: es.enter_context(nc.semaphore("c_" + e)) for e in COMPUTE}
        dsem = {q: [es.enter_context(nc.semaphore("d_%s_%d" % (q, k))) for k in range(self.n_dma_sems[q])]
                for q in dcount}
        self.stats = {e: len(v) for e, v in eng_ops.items()}
        self.stats["sig"] = dict(sigcount)

        def waitspec(j):
            p = ops[j]
            if p["dma"]:
                K = self.n_dma_sems[p["eng"]]
                return dsem[p["eng"]][p["dnum"] % K], 16 * (p["dnum"] // K + 1)
            return csem[p["eng"]], p["signum"]

        def run_engine(ename, e):
            for i in eng_ops.get(ename, []):
                o = ops[i]
                for j in o["waits"]:
                    s, v = waitspec(j)
                    e.wait_ge(s, v)
                if o["dma"]:
                    K = self.n_dma_sems[ename]
                    m = o["dnum"]
                    if m >= K:
                        e.wait_ge(dsem[ename][m % K], 16 * (m // K))
                    ins = o["fn"](e)
                    ins.then_inc(dsem[ename][m % K], 16)
                else:
                    ins = o["fn"](e)
                    if o.get("sig"):
                        ins.then_inc(csem[ename], 1)
            if ename in dsem:
                K = self.n_dma_sems[ename]
                n = dcount[ename]
                for k in range(min(K, n)):
                    cnt = (n - 1 - k) // K + 1
                    e.wait_ge(dsem[ename][k], 16 * cnt)

        with nc.Block() as block:
            @block.sync
            def _(e):
                run_engine("sync", e)

            @block.tensor
            def _(e):
                run_engine("tensor", e)

            @block.vector
            def _(e):
                run_engine("vector", e)

            @block.scalar
            def _(e):
                run_engine("scalar", e)

            @block.gpsimd
            def _(e):
                run_engine("gpsimd", e)
        self.es.close()
        return nc
D = 1024
EPS = 1e-6


class Tok(Prog):
    def __init__(self, TB, nwsl=3):
        super().__init__()
        self.nwsl = nwsl
        self.TB = TB
        self.NT = TB // 128
        self.ident_d = self.dram("ident", [128, 128], F32, "ExternalInput")
        self.ident = self.sb("ident_sb", [128, 128], BF16)
        self.dma("gpsimd", self.ident[:], self.ident_d, w=["ident"])
        self.ones = self.sb("ones", [1, 128], F32)
        self.op("vector", lambda e: e.memset(self.ones[:], 1.0), w=["ones"])
        self.psA = [self.ps("psA%d" % i, [128, 512], F32) for i in range(3)]
        self.psY = [self.ps("psY%d" % i, [128, 1024], F32) for i in range(2)]
        self.psT = self.ps("psT", [128, 1024], BF16)
        self.wsl = [self.sb("wsl%d" % i, [128, 8192], BF16) for i in range(nwsl)]
        self.wi = 0
        self.ai = 0
        self.yi = 0
        self.tmpf = [self.sb("tmpf%d" % i, [128, 1024], F32) for i in range(2)]
        self.ti = 0
        self.hbf = [self.sb("hbf%d" % i, [128, 1024], BF16) for i in range(2)]
        self.hi = 0
        self.stat = self.sb("stat", [128, 64], F32)
        self.si = 0
        self.nrow = 1 if nwsl == 2 else 2
        self.rowbuf = self.sb("rowbuf", [1, 1024 * self.nrow], F32)
        self.si2 = 0
        self.csb = self.sb("csb", [128, 8], F32)
        self.csl = self.sb("csl", [128, 8], BF16)
        self.junk = self.sb("junk", [128, 1024], BF16)

    def nw(self):
        i = self.wi % self.nwsl
        self.wi += 1
        return self.wsl[i], "wsl%d" % i

    def nA(self):
        i = self.ai % 3
        self.ai += 1
        return self.psA[i], "psA%d" % i

    def nY(self):
        i = self.yi % 2
        self.yi += 1
        return self.psY[i], "psY%d" % i

    def ntmp(self):
        i = self.ti % 2
        self.ti += 1
        return self.tmpf[i], "tmpf%d" % i

    def nh(self):
        i = self.hi % 2
        self.hi += 1
        return self.hbf[i], "hbf%d" % i

    def nstat(self, n=1):
        if self.si + n > 64:
            self.si = 0
        c = self.si
        self.si += n
        return c

    def load_w(self, src, a, b):
        t, key = self.nw()
        dst = t[:, 0:a * b].rearrange("p (a b) -> p a b", a=a)
        self.dma("gpsimd", dst, src, w=[key])
        return dst, key

    def load_w_ind(self, Wrows, idx, ikey, a, b, elem_off):
        t, key = self.nw()
        dst = t[:, 0:a * b].rearrange("p (a b) -> p a b", a=a)
        for kc in range(a):
            self.op("gpsimd", lambda e, kc=kc: e.indirect_dma_start(out=dst[:, kc, :], out_offset=None, in_=Wrows, in_offset=bass.IndirectOffsetOnAxis(ap=idx[:, kc:kc + 1], axis=0), element_offset=elem_off),
                    r=[ikey], w=[key], dma=True)
        return dst, key

    def ffn_sp(self, nt, c, b, wg_l, wu_l, wd_l, IH):
        ntok = nt * 128
        hT, Y2 = c["hT"], c["Y2"]
        WS = c["WS"]
        first = True
        for q in range(4):
            wts = []
            for Wl in (wg_l, wu_l, wd_l):
                i = c["wsi"][0] % len(WS)
                c["wsi"][0] += 1
                wt, wk = WS[i], "WS%d" % i
                self.op("gpsimd", lambda e, wt=wt, Wl=Wl, q=q: e.indirect_dma_start(out=wt, out_offset=None, in_=Wl[0],
                                                                               in_offset=bass.IndirectOffsetOnAxis(ap=IH[:, b, q:q + 1], axis=0)),
                        r=["IH", "SLI", "EIDI", Wl[1]], w=[wk], dma=True)
                wts.append((wt, wk))
            (wtg, wgk), (wtu, wuk), (wtd, wdk) = wts
            vg = wtg.rearrange("p (k f) -> p k f", k=8)
            vu = wtu.rearrange("p (k f) -> p k f", k=8)
            vd = wtd.rearrange("p (k n) -> p k n", k=7)
            for fbh in range(2):
                nch = (4, 3)[fbh]
                j0 = fbh * 4
                sl = c["fbi"][0] % 2
                c["fbi"][0] += 1
                sg, sgk = c["sg"][sl], "sg%d" % sl
                gT, gk = c["gT"][sl], "gT%d" % sl
                for j in range(nch):
                    pa, pk = self.nA()
                    for kc in range(8):
                        self.op("tensor", lambda e, kc=kc, j=j, pa=pa, vg=vg, j0=j0: e.matmul(pa[:, 0:ntok], lhsT=vg[:, kc, (j0 + j) * 128:(j0 + j + 1) * 128], rhs=hT[:, kc, 0:ntok], start=(kc == 0), stop=(kc == 7)),
                                r=[wgk, "hT"], w=[pk])
                    self.op("scalar", lambda e, j=j, pa=pa, sg=sg: e.activation(out=sg[:, j, 0:ntok], in_=pa[:, 0:ntok], func=AF.Silu), r=[pk], w=[sgk])
                for j in range(nch):
                    pa, pk = self.nA()
                    for kc in range(8):
                        self.op("tensor", lambda e, kc=kc, j=j, pa=pa, vu=vu, j0=j0: e.matmul(pa[:, 0:ntok], lhsT=vu[:, kc, (j0 + j) * 128:(j0 + j + 1) * 128], rhs=hT[:, kc, 0:ntok], start=(kc == 0), stop=(kc == 7)),
                                r=[wuk, "hT"], w=[pk])
                    self.op("vector", lambda e, j=j, pa=pa, sg=sg, gT=gT: e.tensor_tensor(out=gT[:, j, 0:ntok], in0=sg[:, j, 0:ntok], in1=pa[:, 0:ntok], op=ALU.mult), r=[pk, sgk], w=[gk])
                for ti in range(nt):
                    py, pyk = self.nY()
                    for hf in range(2):
                        for j in range(nch):
                            self.op("tensor", lambda e, j=j, hf=hf, ti=ti, py=py, gT=gT, vd=vd, j0=j0, nch=nch: e.matmul(py[:, hf * 512:(hf + 1) * 512], lhsT=gT[:, j, ti * 128:(ti + 1) * 128], rhs=vd[:, j0 + j, hf * 512:(hf + 1) * 512], start=(j == 0), stop=(j == nch - 1)),
                                    r=[wdk, gk], w=[pyk])
                    yk = "Y2_%d" % ti
                    if first:
                        self.op("scalar", lambda e, ti=ti, py=py: e.copy(out=Y2[:, ti, :], in_=py[:, :]), r=[pyk], w=[yk])
                    else:
                        self.op("vector", lambda e, ti=ti, py=py: e.tensor_tensor(out=Y2[:, ti, :], in0=py[:, :], in1=Y2[:, ti, :], op=ALU.add), r=[pyk, yk], w=[yk])
                first = False

    def bc_dram(self, dst, dkey, src1d, off, n, q="sync"):
        ap = bass.AP(tensor=src1d.tensor, offset=off, ap=[[0, 128], [1, n]])
        self.dma(q, dst, ap, w=[dkey])

    def ada_mod(self, pref, cvec, ada_w, ada_b, normg, segs):
        self.dma("sync", self.csb[:], cvec, w=["csb"])
        self.op("scalar", lambda e: e.activation(out=self.csl[:], in_=self.csb[:], func=AF.Silu), r=["csb"], w=["csl"])
        wv = ada_w.rearrange("(kc p) n -> p kc n", p=128)
        bv = ada_b.rearrange("(o n) -> o n", o=1)
        rb = self.rowbuf
        out = {}
        for (seg, name, kind, gidx) in segs:
            t = self.get_tile(pref + name)
            tk = pref + name
            for hb in range(2):
                nb = seg * 2 + hb
                sl = self.si2 % self.nrow
                self.si2 += 1
                rk = "row%d" % sl
                self.dma("sync", rb[0:1, sl * 1024 + 512:sl * 1024 + 1024], bv[0:1, nb * 512:(nb + 1) * 512], w=[rk + "b"])
                wt, wk = self.load_w(wv[:, :, nb * 512:(nb + 1) * 512], 8, 512)
                pa, pk = self.nA()
                for kc in range(8):
                    self.op("tensor", lambda e, kc=kc, wt=wt, pa=pa: e.matmul(pa[0:1, :], lhsT=self.csl[:, kc:kc + 1], rhs=wt[:, kc, :], start=(kc == 0), stop=(kc == 7)),
                            r=[wk, "csl"], w=[pk])
                self.op("vector", lambda e, sl=sl, pa=pa: e.tensor_tensor(out=rb[0:1, sl * 1024:sl * 1024 + 512], in0=pa[0:1, :], in1=rb[0:1, sl * 1024 + 512:sl * 1024 + 1024], op=ALU.add),
                        r=[pk, rk + "b"], w=[rk])
                pb, pbk = self.nA()
                self.op("tensor", lambda e, sl=sl, pb=pb: e.matmul(pb[:, :], lhsT=self.ones[0:1, :], rhs=rb[0:1, sl * 1024:sl * 1024 + 512], start=True, stop=True),
                        r=[rk, "ones"], w=[pbk])
                self.op("scalar", lambda e, hb=hb, pb=pb, t=t: e.copy(out=t[:, hb * 512:(hb + 1) * 512], in_=pb[:, :]), r=[pbk], w=[tk])
            if kind != "raw":
                tmp, tmk = self.ntmp()
                self.bc_dram(tmp[:], tmk, normg, gidx * D, D)
                if kind == "mul1p":
                    self.op("vector", lambda e, t=t, tmp=tmp: e.scalar_tensor_tensor(out=t[:], in0=t[:], scalar=1.0, in1=tmp[:], op0=ALU.add, op1=ALU.mult), r=[tk, tmk], w=[tk])
                else:
                    self.op("vector", lambda e, t=t, tmp=tmp: e.tensor_tensor(out=t[:], in0=t[:], in1=tmp[:], op=ALU.mult), r=[tk, tmk], w=[tk])
            out[name] = (t, tk)
        return out

    def get_tile(self, name, shape=None, dt=F32):
        if not hasattr(self, "_tiles"):
            self._tiles = {}
        if name not in self._tiles:
            self._tiles[name] = self.sb(name, shape or [128, D], dt)
        return self._tiles[name]

    def rstd_of(self, srcs, skeys):
        n = len(srcs)
        c = self.nstat(n + 2)
        st = self.stat
        self.op("vector", lambda e: e.memset(st[:, c:c + n], 0.0), w=["stat"])
        for i, s in enumerate(srcs):
            self.op("scalar", lambda e, s=s, i=i: e.activation(out=self.junk[:, 0:s.shape[-1]], in_=s, func=AF.Square, accum_out=st[:, c + i:c + i + 1]),
                    r=list(skeys), w=["junk", "stat"])
        if n == 2:
            self.op("vector", lambda e: e.tensor_tensor(out=st[:, c:c + 1], in0=st[:, c:c + 1], in1=st[:, c + 1:c + 2], op=ALU.add), r=["stat"], w=["stat"])
        self.op("scalar", lambda e: e.activation(out=st[:, c + n:c + n + 1], in_=st[:, c:c + 1], func=AF.Sqrt, scale=1.0 / D, bias=EPS), r=["stat"], w=["stat"])
        self.op("vector", lambda e: e.reciprocal(out=st[:, c + n + 1:c + n + 2], in_=st[:, c + n:c + n + 1]), r=["stat"], w=["stat"])
        return st[:, c + n + 1:c + n + 2]

    def norm_mod(self, xt, xkey, Gt, SHt, out_bf, okey):
        rs = self.rstd_of([xt], [xkey])
        tmp, tk = self.ntmp()
        self.op("vector", lambda e: e.scalar_tensor_tensor(out=tmp[:], in0=xt, scalar=rs, in1=Gt[0][:], op0=ALU.mult, op1=ALU.mult),
                r=[xkey, "stat", Gt[1]], w=[tk])
        self.op("gpsimd", lambda e: e.tensor_tensor(out=out_bf, in0=tmp[:], in1=SHt[0][:], op=ALU.add), r=[tk, SHt[1]], w=[okey])

    def transpose_to(self, src_bf, skey, dstT, dkey, tcol, nk=8):
        pv = self.psT[:, 0:nk * 128].rearrange("p (a b) -> p a b", a=nk)
        for kc in range(nk):
            self.op("tensor", lambda e, kc=kc: e.transpose(out=pv[:, kc, :], in_=src_bf[:, kc * 128:(kc + 1) * 128], identity=self.ident[:]),
                    r=[skey, "ident"], w=["psT"])
        self.op("vector", lambda e: e.tensor_copy(out=dstT[:, 0:nk, tcol:tcol + 128], in_=pv), r=["psT"], w=[dkey])

    def post_res(self, ypsum, ykey, xt, xkey, GTt):
        rs = self.rstd_of([ypsum[:, 0:512], ypsum[:, 512:1024]], [ykey])
        tmp, tk = self.ntmp()
        self.op("vector", lambda e: e.scalar_tensor_tensor(out=tmp[:], in0=ypsum[:, :], scalar=rs, in1=GTt[0][:], op0=ALU.mult, op1=ALU.mult),
                r=[ykey, "stat", GTt[1]], w=[tk])
        self.op("gpsimd", lambda e: e.tensor_tensor(out=xt, in0=xt, in1=tmp[:], op=ALU.add), r=[tk, xkey], w=[xkey])

    def post_res_sb(self, ysb, ykey, xt, xkey, GTt):
        rs = self.rstd_of([ysb], [ykey, "uT"])
        tmp, tk = self.ntmp()
        self.op("vector", lambda e: e.scalar_tensor_tensor(out=tmp[:], in0=ysb, scalar=rs, in1=GTt[0][:], op0=ALU.mult, op1=ALU.mult),
                r=[ykey, "stat", GTt[1]], w=[tk])
        self.op("gpsimd", lambda e: e.tensor_tensor(out=xt, in0=xt, in1=tmp[:], op=ALU.add), r=[tk, xkey], w=[xkey])

    def lin_fm(self, W, KCn, hT, hkey, t0, ntok, nchunks, evac, group=4):
        def wsrc(a, b_):
            if callable(W):
                return lambda: W().rearrange("(kc p) n -> p kc n", p=128)[:, :, a:b_]
            return W.rearrange("(kc p) n -> p kc n", p=128)[:, :, a:b_]
        cols_per_load = 8192 // KCn // 128 * 128
        cols_per_load = min(cols_per_load, 512)
        i = 0
        while i < len(nchunks):
            n0 = nchunks[i]
            run = [n0]
            while len(run) * 128 < cols_per_load and i + len(run) < len(nchunks) and nchunks[i + len(run)] == run[-1] + 1:
                run.append(run[-1] + 1)
            if isinstance(W, dict):
                wt, wk = self.load_w_ind(W["rows"], W["idx"], W["ikey"], KCn, 512, (n0 // 4) * 1024 * 512)
            else:
                wt, wk = self.load_w(wsrc(n0 * 128, (n0 + len(run)) * 128), KCn, len(run) * 128)
            for j, ncx in enumerate(run):
                pa, pk = self.nA()
                for kc in range(KCn):
                    self.op("tensor", lambda e, kc=kc, j=j, wt=wt, pa=pa: e.matmul(pa[:, 0:ntok], lhsT=wt[:, kc, j * 128:(j + 1) * 128], rhs=hT[:, kc, t0:t0 + ntok], start=(kc == 0), stop=(kc == KCn - 1)),
                            r=[wk, hkey], w=[pk])
                evac(ncx, pa[:, 0:ntok], pk)
            i += len(run)

    def gelu_evac(self, ps, pk, out, okey):
        n = ps.shape[-1]
        tmp, tk = self.ntmp()
        t = tmp[:, 0:n]
        self.op("scalar", lambda e: e.activation(out=t, in_=ps, func=AF.Square), r=[pk], w=[tk])
        self.op("vector", lambda e: e.tensor_scalar(out=t, in0=t, scalar1=0.044715, scalar2=1.0, op0=ALU.mult, op1=ALU.add), r=[tk], w=[tk])
        self.op("vector", lambda e: e.tensor_tensor(out=t, in0=t, in1=ps, op=ALU.mult), r=[tk, pk], w=[tk])
        self.op("scalar", lambda e: e.activation(out=t, in_=t, func=AF.Sigmoid, scale=1.5957691216057308), r=[tk], w=[tk])
        self.op("vector", lambda e: e.tensor_tensor(out=out, in0=t, in1=ps, op=ALU.mult), r=[tk, pk], w=[okey])

    def load_block(self, X, srcs, pos=None):
        for ti, s in enumerate(srcs):
            self.dma("sync", X[:, ti, :], s, w=["X%d" % ti])
            if pos is not None:
                tmp, tk = self.ntmp()
                self.dma("sync", tmp[:], pos[ti], w=[tk])
                self.op("gpsimd", lambda e, ti=ti, tmp=tmp: e.tensor_tensor(out=X[:, ti, :], in0=X[:, ti, :], in1=tmp[:], op=ALU.add), r=[tk, "X%d" % ti], w=["X%d" % ti])

    def pre_T(self, X, nt, Gt, SHt, hT, want32=None):
        for ti in range(nt):
            hb, hk = self.nh()
            if want32 is None:
                self.norm_mod(X[:, ti, :], "X%d" % ti, Gt, SHt, hb[:], hk)
            else:
                h32, h32k = want32
                self.norm_mod(X[:, ti, :], "X%d" % ti, Gt, SHt, h32[:, ti, :], h32k + str(ti))
                self.op("scalar", lambda e, ti=ti, hb=hb: e.copy(out=hb[:], in_=h32[:, ti, :]), r=[h32k + str(ti)], w=[hk])
            self.transpose_to(hb, hk, hT, "hT", ti * 128)

    def gmlp(self, X, nt, c, mods):
        ntok = nt * 128
        hT, uT, vg = c["hT"], c["uT"], c["vg"]
        w_in, w_out = c["w_in"], c["w_out"]
        def ev_u(ncx, ps, pk):
            self.gelu_evac(ps, pk, uT[:, ncx, 0:ntok], "uT")
        self.lin_fm(w_in, 8, hT, "hT", 0, ntok, list(range(16)), ev_u)
        wv = w_in.rearrange("(kc p) n -> p kc n", p=128)
        for cb in range(4):
            wt, wk = self.load_w(wv[:, :, 2048 + cb * 512:2048 + (cb + 1) * 512], 8, 512)
            for ti in range(nt):
                pa, pk = self.nA()
                for kc in range(8):
                    self.op("tensor", lambda e, kc=kc, ti=ti, wt=wt, pa=pa: e.matmul(pa[:, :], lhsT=hT[:, kc, ti * 128:(ti + 1) * 128], rhs=wt[:, kc, :], start=(kc == 0), stop=(kc == 7)),
                            r=[wk, "hT"], w=[pk])
                self.gelu_evac(pa[:, :], pk, vg[:, ti, cb * 512:(cb + 1) * 512], "vg%d" % ti)
        bst = c["bst"]
        wsT = c["wsT"]
        for ti in range(nt):
            for cb in range(4):
                self.op("vector", lambda e, ti=ti, cb=cb: e.bn_stats(out=bst[:, cb * 6:(cb + 1) * 6], in_=vg[:, ti, cb * 512:(cb + 1) * 512]), r=["vg%d" % ti], w=["bst"])
            self.op("vector", lambda e: e.bn_aggr(out=bst[:, 24:26], in_=bst[:, 0:24]), r=["bst"], w=["bst"])
            self.op("scalar", lambda e: e.activation(out=bst[:, 26:27], in_=bst[:, 25:26], func=AF.Sqrt, scale=1.0, bias=EPS), r=["bst"], w=["bst"])
            self.op("vector", lambda e: e.reciprocal(out=bst[:, 27:28], in_=bst[:, 26:27]), r=["bst"], w=["bst"])
            vn, vk = c["vn"][ti % 2], "vn%d" % (ti % 2)
            for hf in range(2):
                tmp, tk = self.ntmp()
                self.op("vector", lambda e, ti=ti, hf=hf, tmp=tmp: e.tensor_scalar(out=tmp[:], in0=vg[:, ti, hf * 1024:(hf + 1) * 1024], scalar1=bst[:, 24:25], scalar2=bst[:, 27:28], op0=ALU.subtract, op1=ALU.mult),
                        r=["vg%d" % ti, "bst"], w=[tk])
                self.op("gpsimd", lambda e, hf=hf, tmp=tmp, vn=vn: e.tensor_tensor(out=vn[:, hf * 1024:(hf + 1) * 1024], in0=tmp[:], in1=c["GV"][:, hf * 1024:(hf + 1) * 1024], op=ALU.mult),
                        r=[tk, "GV"], w=[vk])
            for q4 in range(4):
                pa, pk = self.nA()
                for j in range(4):
                    cc = q4 * 4 + j
                    self.op("tensor", lambda e, j=j, cc=cc, pa=pa, vn=vn: e.matmul(pa[:, j * 128:(j + 1) * 128], lhsT=vn[:, cc * 128:(cc + 1) * 128], rhs=wsT[:, cc // 2, :], start=True, stop=True),
                            r=[vk, "wsT"], w=[pk])
                tmp, tk = self.ntmp()
                self.op("vector", lambda e, q4=q4, pa=pa, tmp=tmp: e.tensor_tensor(out=tmp[:, 0:512], in0=pa[:, :], in1=c["BS"][:, q4 * 512:(q4 + 1) * 512], op=ALU.add),
                        r=[pk, "BS"], w=[tk])
                self.op("gpsimd", lambda e, q4=q4, ti=ti, tmp=tmp: e.tensor_tensor(out=uT[:, q4 * 4:(q4 + 1) * 4, ti * 128:(ti + 1) * 128], in0=tmp[:, 0:512].rearrange("p (a b) -> p a b", a=4),
                                                                                    in1=uT[:, q4 * 4:(q4 + 1) * 4, ti * 128:(ti + 1) * 128], op=ALU.mult),
                        r=[tk, "uT"], w=["uT"])
        wo = w_out.rearrange("(kc p) n -> p kc n", p=128)
        wts = [self.load_w(wo[:, :, hf * 512:(hf + 1) * 512], 16, 512) for hf in range(2)]
        for ti in range(nt):
            py, pyk = self.nY()
            for hf in range(2):
                wt, wk = wts[hf]
                for kc in range(16):
                    self.op("tensor", lambda e, kc=kc, hf=hf, ti=ti, wt=wt, py=py: e.matmul(py[:, hf * 512:(hf + 1) * 512], lhsT=uT[:, kc, ti * 128:(ti + 1) * 128], rhs=wt[:, kc, :], start=(kc == 0), stop=(kc == 15)),
                            r=[wk, "uT"], w=[pyk])
            self.post_res(py, pyk, X[:, ti, :], "X%d" % ti, mods["GT1"])

    def mixer_out(self, X, nt, mTs, mkeys, w_out, KCn, GT):
        wo = w_out.rearrange("(kc p) n -> p kc n", p=128)
        wts = [self.load_w(wo[:, :, hf * 512:(hf + 1) * 512], KCn, 512) for hf in range(2)]
        nm = len(mTs)
        for ti in range(nt):
            py, pyk = self.nY()
            for hf in range(2):
                wt, wk = wts[hf]
                idx = 0
                for mi in range(nm):
                    for kc in range(KCn):
                        self.op("tensor", lambda e, kc=kc, hf=hf, ti=ti, wt=wt, py=py, mi=mi, idx=idx: e.matmul(py[:, hf * 512:(hf + 1) * 512], lhsT=mTs[mi][:, kc, ti * 128:(ti + 1) * 128], rhs=wt[:, kc, :], start=(idx == 0), stop=(idx == nm * KCn - 1)),
                                r=[wk, mkeys[mi]], w=[pyk])
                        idx += 1
            self.post_res(py, pyk, X[:, ti, :], "X%d" % ti, GT)

    def ffn(self, X, nt, c, GT, wg, wu, wd, FF, comb=None, first=True):
        ntok = nt * 128
        hT, Y2 = c["hT"], c["Y2"]
        nfb = (FF + 511) // 512
        def wdsrc(a, b_):
            if callable(wd):
                return lambda: wd().rearrange("(kc p) n -> p kc n", p=128)[:, a:b_, :]
            return wd.rearrange("(kc p) n -> p kc n", p=128)[:, a:b_, :]
        for fb in range(nfb):
            nch = min(4, FF // 128 - fb * 4)
            sg, sgk = c["sg"][fb % 2], "sg%d" % (fb % 2)
            gT, gk = c["gT"][fb % 2], "gT%d" % (fb % 2)
            chunks = list(range(fb * 4, fb * 4 + nch))
            def ev_g(ncx, ps, pk, sg=sg, sgk=sgk, fb=fb):
                self.op("scalar", lambda e: e.activation(out=sg[:, ncx - fb * 4, 0:ntok], in_=ps, func=AF.Silu), r=[pk], w=[sgk])
            self.lin_fm(wg, 8, hT, "hT", 0, ntok, chunks, ev_g)
            def ev_u(ncx, ps, pk, sg=sg, sgk=sgk, gT=gT, gk=gk, fb=fb):
                self.op("vector", lambda e: e.tensor_tensor(out=gT[:, ncx - fb * 4, 0:ntok], in0=sg[:, ncx - fb * 4, 0:ntok], in1=ps, op=ALU.mult), r=[pk, sgk], w=[gk])
            self.lin_fm(wu, 8, hT, "hT", 0, ntok, chunks, ev_u)
            if isinstance(wd, dict):
                wt, wk = self.load_w_ind(wd["rows"], wd["idx"], wd["ikey"], nch, 1024, fb * 512 * 1024)
            else:
                wt, wk = self.load_w(wdsrc(fb * 4, fb * 4 + nch), nch, 1024)
            for ti in range(nt):
                py, pyk = self.nY()
                for hf in range(2):
                    for kc in range(nch):
                        self.op("tensor", lambda e, kc=kc, hf=hf, ti=ti, wt=wt, py=py, gT=gT: e.matmul(py[:, hf * 512:(hf + 1) * 512], lhsT=gT[:, kc, ti * 128:(ti + 1) * 128], rhs=wt[:, kc, hf * 512:(hf + 1) * 512], start=(kc == 0), stop=(kc == nch - 1)),
                                r=[wk, gk], w=[pyk])
                yk = "Y2_%d" % ti
                if comb is None:
                    if first and fb == 0:
                        self.op("scalar", lambda e, ti=ti, py=py: e.copy(out=Y2[:, ti, :], in_=py[:, :]), r=[pyk], w=[yk, "uT"])
                    else:
                        self.op("vector", lambda e, ti=ti, py=py: e.tensor_tensor(out=Y2[:, ti, :], in0=py[:, :], in1=Y2[:, ti, :], op=ALU.add), r=[pyk, yk], w=[yk])
                else:
                    cap, ck = comb
                    if first and fb == 0:
                        self.op("vector", lambda e, ti=ti, py=py: e.tensor_scalar(out=Y2[:, ti, :], in0=py[:, :], scalar1=cap(ti), scalar2=None, op0=ALU.mult), r=[pyk, ck], w=[yk, "uT"])
                    else:
                        self.op("vector", lambda e, ti=ti, py=py: e.scalar_tensor_tensor(out=Y2[:, ti, :], in0=py[:, :], scalar=cap(ti), in1=Y2[:, ti, :], op0=ALU.mult, op1=ALU.add), r=[pyk, yk, ck], w=[yk])

    def ffn_post(self, X, nt, c, GT):
        for ti in range(nt):
            self.post_res_sb(c["Y2"][:, ti, :], "Y2_%d" % ti, X[:, ti, :], "X%d" % ti, GT)

    def out_block(self, X, nt, xo_rows, ho_rows=None, Gn=None, SHn=None):
        for ti in range(nt):
            if ho_rows is not None:
                hb, hk = self.nh()
                self.norm_mod(X[:, ti, :], "X%d" % ti, Gn, SHn, hb[:], hk)
                self.dma("sync", ho_rows[ti], hb[:], r=[hk])
            if xo_rows is not None:
                self.dma("sync", xo_rows[ti], X[:, ti, :], r=["X%d" % ti])

def build_S0(n_lat_tiles=32, TB=512):
    P = Tok(TB)
    NT = P.NT
    NTOK = n_lat_tiles * 128
    dI = lambda n, s, dt=F32: P.dram(n, s, dt, "ExternalInput")
    dO = lambda n, s, dt=F32: P.dram(n, s, dt, "ExternalOutput")
    x_d, pos_d, ctx_d = dI("x", [NTOK, D]), dI("pos", [NTOK, D]), dI("ctx", [128, D])
    c_d, cc_d = dI("c", [128, 8]), dI("c_ctx", [128, 8])
    aw0, ab0, ng0 = dI("ada_w0", [D, 6 * D]), dI("ada_b0", [6 * D]), dI("ng0", [4 * D])
    aw1, ab1, ng1 = dI("ada_w1", [D, 6 * D]), dI("ada_b1", [6 * D]), dI("ng1", [4 * D])
    g_win, g_gv, g_wsT, g_bs, g_wout = dI("g_win", [D, 4096]), dI("g_gv", [2048]), dI("g_wsT", [128, 1024]), dI("g_bs", [2048]), dI("g_wout", [2048, D])
    f_wg, f_wu, f_wd = dI("f_wg", [D, 2816]), dI("f_wu", [D, 2816]), dI("f_wd", [2816, D])
    xo, ho = dO("xo", [NTOK, D]), dO("ho", [NTOK, D], BF16)
    xco, hco = dO("xco", [128, D]), dO("hco", [128, D], BF16)
    X = P.sb("X", [128, NT, D], F32)
    c = dict(hT=P.sb("hT", [128, 8, TB], BF16), uT=None, vg=P.sb("vg", [128, NT, 2048], BF16),
             vn=[P.sb("vn%d" % i, [128, 2048], BF16) for i in range(2)], bst=P.sb("bst", [128, 32], F32),
             Y2=None, sg=[P.sb("sg%d" % i, [128, 4, TB], BF16) for i in range(2)],
             gT=[P.sb("gT%d" % i, [128, 4, TB], BF16) for i in range(2)],
             w_in=g_win, w_out=g_wout)
    uY = P.sb("uY", [128, NT * D], F32)
    c["Y2"] = uY[:].rearrange("p (a b) -> p a b", a=NT)
    c["uT"] = uY[:].bitcast(BF16).rearrange("p (a b) -> p a b", a=16)
    c["GV"] = P.sb("GV", [128, 2048], F32)
    c["BS"] = P.sb("BS", [128, 2048], F32)
    P.bc_dram(c["GV"][:], "GV", g_gv, 0, 2048)
    P.bc_dram(c["BS"][:], "BS", g_bs, 0, 2048)
    wsT = P.sb("wsT", [128, 8, 128], BF16)
    P.dma("gpsimd", wsT[:].rearrange("p a b -> p (a b)"), g_wsT, w=["wsT"])
    c["wsT"] = wsT

    def run_pass(pref, cvec, blocks):
        m = P.ada_mod("m", cvec, aw0, ab0, ng0, [(0, "SH1", "raw", 0), (1, "G1", "mul1p", 0), (2, "GT1", "mul", 1),
                                                  (3, "SH2", "raw", 0), (4, "G2", "mul1p", 2), (5, "GT2", "mul", 3)])
        mn = P.ada_mod("n", cvec, aw1, ab1, ng1, [(0, "SH1", "raw", 0), (1, "G1", "mul1p", 0)])
        for (srcs, poss, xos, hos) in blocks:
            nt = len(srcs)
            P.load_block(X, srcs, poss)
            P.pre_T(X, nt, m["G1"], m["SH1"], c["hT"])
            P.gmlp(X, nt, c, m)
            P.pre_T(X, nt, m["G2"], m["SH2"], c["hT"])
            P.ffn(X, nt, c, m["GT2"], f_wg, f_wu, f_wd, 2816)
            P.ffn_post(X, nt, c, m["GT2"])
            P.out_block(X, nt, xos, hos, mn["G1"], mn["SH1"])

    rows = lambda d, t: d[t * 128:(t + 1) * 128, :]
    blocks = []
    for b in range(n_lat_tiles // NT):
        ts = list(range(b * NT, (b + 1) * NT))
        blocks.append(([rows(x_d, t) for t in ts], [rows(pos_d, t) for t in ts], [rows(xo, t) for t in ts], [rows(ho, t) for t in ts]))
    run_pass("l", c_d, blocks)
    run_pass("c", cc_d, [([ctx_d], None, [xco], [hco])])
    return P

U32 = mybir.dt.uint32


def tok_router2(P, g, h32t, h32k, c):
    hT32, wr32, lg, mx8, identf, M1, M2, PW = c["hT32"], c["wr32"], c["lg"], c["mx8"], c["identf"], c["M1"], c["M2"], c["PW"]
    py, pyk = P.nY()
    pv = py[:, :].rearrange("p (a b) -> p a b", a=8)
    for kc in range(8):
        P.op("tensor", lambda e, kc=kc, pv=pv: e.transpose(out=pv[:, kc, :], in_=h32t[:, kc * 128:(kc + 1) * 128], identity=identf[:]), r=[h32k, "identf"], w=[pyk])
    P.op("vector", lambda e, pv=pv: e.tensor_copy(out=hT32[:], in_=pv), r=[pyk], w=["hT32"])
    pa, pk = P.nA()
    for kc in range(8):
        P.op("tensor", lambda e, kc=kc, pa=pa: e.matmul(pa[:, 0:8], lhsT=hT32[:, kc, :], rhs=wr32[:, kc, :], start=(kc == 0), stop=(kc == 7)), r=["hT32", "wr32"], w=[pk])
    P.op("vector", lambda e, pa=pa: e.tensor_copy(out=lg[:, 0:8], in_=pa[:, 0:8]), r=[pk], w=["lg"])
    P.op("vector", lambda e: e.max(out=mx8[:, 0:8], in_=lg[:, 0:8]), r=["lg"], w=["mx8"])
    P.op("vector", lambda e: e.tensor_tensor(out=mx8[:, 8:9], in0=mx8[:, 0:1], in1=mx8[:, 1:2], op=ALU.subtract), r=["mx8"], w=["mx8"])
    P.op("scalar", lambda e: e.activation(out=PW[:, g, 0:1], in_=mx8[:, 8:9], func=AF.Sigmoid, scale=1.0), r=["mx8"], w=["PW"])
    P.op("scalar", lambda e: e.activation(out=PW[:, g, 1:2], in_=mx8[:, 8:9], func=AF.Sigmoid, scale=-1.0), r=["mx8"], w=["PW"])
    P.op("vector", lambda e: e.tensor_scalar(out=M1[:, g, :], in0=lg[:, 0:8], scalar1=mx8[:, 0:1], scalar2=None, op0=ALU.is_equal), r=["lg", "mx8"], w=["M1"])
    P.op("vector", lambda e: e.tensor_scalar(out=M2[:, g, :], in0=lg[:, 0:8], scalar1=mx8[:, 1:2], scalar2=None, op0=ALU.is_equal), r=["lg", "mx8"], w=["M2"])


def moe_slots(P, NG, NBLK, c):
    M1, M2 = c["M1"], c["M2"]
    W = NG * 8
    fl = lambda T: T[:].rearrange("p g e -> p (g e)")
    ao = [3 * 7168]
    def sbf(n, w_, dt=F32):
        sl = c["arena"][:, ao[0]:ao[0] + 2 * w_]
        ao[0] += 2 * w_
        assert ao[0] <= 4 * 7168
        return sl.bitcast(dt)
    maskv, RK, TT, INC, EXC, SLOT, TMP = [sbf(n, W) for n in ("maskv", "RK", "TT", "INC", "EXC", "SLOT", "TMPs")]
    onesM, onesr = sbf("onesM", 128), sbf("onesr", NG)
    PADF, ENDF, BASEF = sbf("PADF", 8), sbf("ENDF", 8), sbf("BASEF", 8)
    PADI = sbf("PADI", 8, I32)
    SLF = sbf("SLF", 2 * NG)
    eidf = sbf("eidf", NBLK)
    U, blk = c["U"], c["blk"]
    SLI, EIDI = c["SLI"], c["EIDI"]
    P.op("vector", lambda e: e.memset(onesM[:], 1.0), w=["onesM"])
    P.op("vector", lambda e: e.memset(onesr[:], 1.0), w=["onesr"])
    P.op("vector", lambda e: e.tensor_tensor(out=maskv[:], in0=fl(M1), in1=fl(M2), op=ALU.add), r=["M1", "M2"], w=["maskv"])
    pa, pk = P.nA()
    P.op("tensor", lambda e: e.matmul(pa[:, 0:W], lhsT=U[:], rhs=maskv[:], start=True, stop=True), r=["U", "maskv"], w=[pk])
    P.op("vector", lambda e: e.tensor_copy(out=RK[:], in_=pa[:, 0:W]), r=[pk], w=["RK"])
    pb, pbk = P.nA()
    P.op("tensor", lambda e: e.matmul(pb[:, 0:W], lhsT=onesM[:], rhs=maskv[:], start=True, stop=True), r=["onesM", "maskv"], w=[pbk])
    P.op("vector", lambda e: e.tensor_copy(out=TT[:], in_=pb[:, 0:W]), r=[pbk], w=["TT"])
    ev = lambda T, e_: T[:].rearrange("p (g e) -> p e g", e=8)[:, e_, :]
    for e_ in range(8):
        P.op("vector", lambda e, e_=e_: e.tensor_tensor_scan(out=ev(INC, e_), data0=onesr[:], data1=ev(TT, e_), initial=0.0, op0=ALU.mult, op1=ALU.add), r=["TT", "onesr"], w=["INC"])
    P.op("vector", lambda e: e.tensor_tensor(out=EXC[:], in0=INC[:], in1=TT[:], op=ALU.subtract), r=["INC", "TT"], w=["EXC"])
    P.op("vector", lambda e: e.tensor_scalar(out=PADF[:], in0=INC[:, (NG - 1) * 8:NG * 8], scalar1=511.0, scalar2=None, op0=ALU.add), r=["INC"], w=["PADF"])
    P.op("vector", lambda e: e.tensor_copy(out=PADI[:], in_=PADF[:]), r=["PADF"], w=["PADI"])
    P.op("vector", lambda e: e.tensor_scalar(out=PADI[:], in0=PADI[:], scalar1=9, scalar2=9, op0=ALU.arith_shift_right, op1=ALU.logical_shift_left), r=["PADI"], w=["PADI"])
    P.op("vector", lambda e: e.tensor_copy(out=PADF[:], in_=PADI[:]), r=["PADI"], w=["PADF"])
    P.op("vector", lambda e: e.tensor_tensor_scan(out=ENDF[:], data0=onesr[:, 0:8], data1=PADF[:], initial=0.0, op0=ALU.mult, op1=ALU.add), r=["PADF", "onesr"], w=["ENDF"])
    P.op("vector", lambda e: e.tensor_tensor(out=BASEF[:], in0=ENDF[:], in1=PADF[:], op=ALU.subtract), r=["ENDF", "PADF"], w=["BASEF"])
    for e_ in range(8):
        P.op("vector", lambda e, e_=e_: e.scalar_tensor_tensor(out=ev(SLOT, e_), in0=ev(RK, e_), scalar=BASEF[:, e_:e_ + 1], in1=ev(EXC, e_), op0=ALU.add, op1=ALU.add), r=["RK", "EXC", "BASEF"], w=["SLOT"])
    for k, M in enumerate((M1, M2)):
        P.op("vector", lambda e, M=M: e.tensor_tensor(out=TMP[:], in0=fl(M), in1=SLOT[:], op=ALU.mult), r=["M1", "M2", "SLOT"], w=["TMPs"])
        P.op("vector", lambda e, k=k: e.tensor_reduce(out=SLF[:, k * NG:(k + 1) * NG], in_=TMP[:].rearrange("p (g e) -> p g e", e=8), axis=AX.X, op=ALU.add), r=["TMPs"], w=["SLF"])
    P.op("vector", lambda e: e.tensor_copy(out=SLI[:].rearrange("p k g -> p (k g)"), in_=SLF[:]), r=["SLF"], w=["SLI"])
    P.op("vector", lambda e: e.memset(eidf[:], 0.0), w=["eidf"])
    for e_ in range(8):
        P.op("vector", lambda e, e_=e_: e.scalar_tensor_tensor(out=eidf[:], in0=blk[:], scalar=ENDF[:, e_:e_ + 1], in1=eidf[:], op0=ALU.is_ge, op1=ALU.add), r=["blk", "ENDF", "eidf"], w=["eidf"])
    P.op("vector", lambda e: e.tensor_scalar(out=eidf[:], in0=eidf[:], scalar1=7.0, scalar2=None, op0=ALU.min), r=["eidf"], w=["eidf"])
    IH, rowc = c["IH"], c["rowc"]
    IHf = sbf("IHf", NBLK * 4).rearrange("p (a b) -> p a b", b=4)
    eg = sbf("eg", NBLK)
    P.op("vector", lambda e: e.tensor_scalar(out=eg[:], in0=eidf[:], scalar1=512.0, scalar2=None, op0=ALU.mult), r=["eidf"], w=["eg"])
    for b in range(NBLK):
        P.op("vector", lambda e, b=b: e.tensor_scalar(out=IHf[:, b, :], in0=rowc[:, 0:4], scalar1=eg[:, b:b + 1], scalar2=None, op0=ALU.add), r=["eg", "rowc"], w=["IHf"])
    P.op("vector", lambda e: e.tensor_copy(out=IH[:].rearrange("p a b -> p (a b)"), in_=IHf[:].rearrange("p a b -> p (a b)")), r=["IHf"], w=["IH"])


def moe_sparse(P, X, n_tiles, c, m, wg_d, wu_d, wd_d, FF, Xd, Hd, finish_tile):
    NG = n_tiles
    NTOK = NG * 128
    NBLK = (2 * NTOK + 8 * 511) // 512
    NS = NBLK * 512
    Hs = P.dram("Hs", [NS, D], BF16, "Internal")
    Ys = P.dram("Ys", [NS, D], F32, "Internal")
    SLI, EIDI = c["SLI"], c["EIDI"]
    moe_slots(P, NG, NBLK, c)
    for g in range(NG):
        hb, hk = P.nh()
        P.dma("sync", hb[:], Hd[g * 128:(g + 1) * 128, :], r=["Hd"], w=[hk])
        for k in range(2):
            P.op("gpsimd", lambda e, g=g, k=k, hb=hb: e.indirect_dma_start(out=Hs, out_offset=bass.IndirectOffsetOnAxis(ap=SLI[:, k, g:g + 1], axis=0), in_=hb[:], in_offset=None),
                 r=[hk, "SLI"], w=["Hs"], dma=True)
    NT = P.NT
    for b in range(NBLK):
        for ti in range(NT):
            hb, hk = P.nh()
            P.dma("sync", hb[:], Hs[(b * NT + ti) * 128:(b * NT + ti + 1) * 128, :], r=["Hs"], w=[hk])
            P.transpose_to(hb, hk, c["hT"], "hT", ti * 128)
        P.ffn_sp(NT, c, b, c["Wb"][0], c["Wb"][1], c["Wb"][2], c["IH"])
        for ti in range(NT):
            P.dma("sync", Ys[(b * NT + ti) * 128:(b * NT + ti + 1) * 128, :], c["Y2"][:, ti, :], r=["Y2_%d" % ti], w=["Ys"])
    YG = c["YG"]
    for g in range(NG):
        ti = g % NT
        P.dma("sync", X[:, ti, :], Xd[g * 128:(g + 1) * 128, :], r=["Xd"], w=["X%d" % ti])
        ys = []
        for k in range(2):
            yt, yk = YG[k], "YG%d" % k
            P.op("gpsimd", lambda e, g=g, k=k, yt=yt: e.indirect_dma_start(out=yt[:], out_offset=None, in_=Ys, in_offset=bass.IndirectOffsetOnAxis(ap=SLI[:, k, g:g + 1], axis=0)),
                 r=["Ys", "SLI"], w=[yk], dma=True)
            ys.append((yt, yk))
        (y1, y1k), (y2, y2k) = ys
        P.op("vector", lambda e, g=g, y1=y1: e.tensor_scalar(out=y1[:], in0=y1[:], scalar1=c["PW"][:, g, 0:1], scalar2=None, op0=ALU.mult), r=[y1k, "PW"], w=[y1k])
        P.op("vector", lambda e, g=g, y1=y1, y2=y2: e.scalar_tensor_tensor(out=y1[:], in0=y2[:], scalar=c["PW"][:, g, 1:2], in1=y1[:], op0=ALU.mult, op1=ALU.add), r=[y1k, y2k, "PW"], w=[y1k])
        P.post_res_sb(y1[:], y1k, X[:, ti, :], "X%d" % ti, m["GT2"])
        finish_tile(g, ti)

def tok_router(P, nt, c):
    h32, hT32, wr32, lg, mx8, comb, identf = c["h32"], c["hT32"], c["wr32"], c["lg"], c["mx8"], c["comb"], c["identf"]
    for ti in range(nt):
        py, pyk = P.nY()
        pv = py[:, :].rearrange("p (a b) -> p a b", a=8)
        for kc in range(8):
            P.op("tensor", lambda e, kc=kc, ti=ti, pv=pv: e.transpose(out=pv[:, kc, :], in_=h32[:, ti, kc * 128:(kc + 1) * 128], identity=identf[:]),
                 r=["h32_%d" % ti, "identf"], w=[pyk])
        P.op("vector", lambda e, pv=pv: e.tensor_copy(out=hT32[:], in_=pv), r=[pyk], w=["hT32"])
        pa, pk = P.nA()
        for kc in range(8):
            P.op("tensor", lambda e, kc=kc, pa=pa: e.matmul(pa[:, 0:8], lhsT=hT32[:, kc, :], rhs=wr32[:, kc, :], start=(kc == 0), stop=(kc == 7)),
                 r=["hT32", "wr32"], w=[pk])
        P.op("vector", lambda e, pa=pa: e.tensor_copy(out=lg[:, 0:8], in_=pa[:, 0:8]), r=[pk], w=["lg"])
        P.op("vector", lambda e: e.max(out=mx8[:, 0:8], in_=lg[:, 0:8]), r=["lg"], w=["mx8"])
        P.op("vector", lambda e: e.tensor_tensor(out=mx8[:, 8:9], in0=mx8[:, 0:1], in1=mx8[:, 1:2], op=ALU.subtract), r=["mx8"], w=["mx8"])
        P.op("scalar", lambda e: e.activation(out=mx8[:, 9:10], in_=mx8[:, 8:9], func=AF.Sigmoid, scale=1.0), r=["mx8"], w=["mx8"])
        P.op("scalar", lambda e: e.activation(out=mx8[:, 10:11], in_=mx8[:, 8:9], func=AF.Sigmoid, scale=-1.0), r=["mx8"], w=["mx8"])
        P.op("vector", lambda e: e.tensor_scalar(out=lg[:, 8:16], in0=lg[:, 0:8], scalar1=mx8[:, 0:1], scalar2=mx8[:, 9:10], op0=ALU.is_equal, op1=ALU.mult), r=["lg", "mx8"], w=["lg"])
        P.op("vector", lambda e: e.tensor_scalar(out=lg[:, 16:24], in0=lg[:, 0:8], scalar1=mx8[:, 1:2], scalar2=mx8[:, 10:11], op0=ALU.is_equal, op1=ALU.mult), r=["lg", "mx8"], w=["lg"])
        P.op("vector", lambda e, ti=ti: e.tensor_tensor(out=comb[:, ti, :], in0=lg[:, 8:16], in1=lg[:, 16:24], op=ALU.add), r=["lg"], w=["comb"])


def build_TOK2(kind, n_tiles=32, TB=512, sparse=True):
    P = Tok(TB, nwsl=(2 if (sparse and kind in ("S2", "S5")) else 3))
    NT = P.NT
    NTOK = n_tiles * 128
    dI = lambda n, s, dt=F32: P.dram(n, s, dt, "ExternalInput")
    dO = lambda n, s, dt=F32: P.dram(n, s, dt, "ExternalOutput")
    moe = kind in ("S2", "S5")
    sparse = sparse and moe
    has_next = kind in ("S2", "S4")
    x_d = dI("x", [NTOK, D])
    c_d = dI("c", [128, 8])
    aw, ab, ng = dI("ada_w", [D, 6 * D]), dI("ada_b", [6 * D]), dI("ng", [4 * D])
    if has_next:
        awn, abn, ngn = dI("ada_wn", [D, 6 * D]), dI("ada_bn", [6 * D]), dI("ngn", [4 * D])
        ho = dO("ho", [NTOK, D], BF16)
    xo = dO("xo", [NTOK, D])
    w_out = dI("w_out", [D, D])
    FF = 3584 if moe else 2816
    if sparse:
        wr_d = dI("w_router", [128, 64])
        wg_d, wu_d, wd_d = dI("wg", [4096, 4, 1792]), dI("wu", [4096, 4, 1792]), dI("wd", [4096, 4, 1792])
    elif moe:
        wr_d = dI("w_router", [128, 64])
        wg_d, wu_d, wd_d = dI("wg", [8, D, FF]), dI("wu", [8, D, FF]), dI("wd", [8, FF, D])
    else:
        wg_d, wu_d, wd_d = dI("wg", [D, FF]), dI("wu", [D, FF]), dI("wd", [FF, D])
    X = P.sb("X", [128, NT, D], F32)
    c = dict(hT=P.sb("hT", [128, 8, TB], BF16), Y2=P.sb("Y2", [128, NT, D], F32),
             sg=[P.sb("sg%d" % i, [128, 4, TB], BF16) for i in range(2)],
             gT=[P.sb("gT%d" % i, [128, 4, TB], BF16) for i in range(2)])
    if sparse:
        NBLKS = (2 * NTOK + 8 * 511) // 512
        U_d, blk_d = dI("Utri", [128, 128]), dI("blkrow", [128, NBLKS])
        Xd, Hd = P.dram("Xd", [NTOK, D], F32, "Internal"), P.dram("Hd", [NTOK, D], BF16, "Internal")
        c["M1"], c["M2"] = P.sb("M1", [128, n_tiles, 8], F32), P.sb("M2", [128, n_tiles, 8], F32)
        c["PW"] = P.sb("PW", [128, n_tiles, 2], F32)
        c["SLI"], c["EIDI"] = P.sb("SLI", [128, 2, n_tiles], U32), P.sb("EIDI", [128, NBLKS], I32)
        c["IH"] = P.sb("IH", [128, NBLKS, 4], U32)
        arena = P.sb("arena", [128, 4 * 7168], BF16)
        c["WS"] = [arena[:, i * 7168:(i + 1) * 7168] for i in range(4)]
        c["arena"] = arena
        c["Wb"] = []
        for wi_, Wsrc in enumerate((wg_d, wu_d, wd_d)):
            Wb = P.dram("Wb%d" % wi_, [4096, 7168], BF16, "Internal")
            for r_ in range(32):
                P.dma("gpsimd", Wb[r_ * 128:(r_ + 1) * 128, :].rearrange("r (a b) -> r a b", a=4), Wsrc[r_ * 128:(r_ + 1) * 128], w=["Wb%d" % wi_])
            c["Wb"].append((Wb, "Wb%d" % wi_))
        c["wsi"], c["fbi"] = [0], [0]
        c["rowc"] = P.sb("rowc_s", [128, 8], F32)
        rowc_d = dI("rowc", [128, 8])
        P.dma("sync", c["rowc"][:], rowc_d, w=["rowc"])
        c["U"], c["blk"] = P.sb("U_s", [128, 128], F32), P.sb("blk_s", [128, NBLKS], F32)
        P.dma("sync", c["U"][:], U_d, w=["U"])
        P.dma("sync", c["blk"][:], blk_d, w=["blk"])
        c["YG"] = [P.sb("YG%d" % i, [128, D], F32) for i in range(2)]
        h32r = [P.sb("h32r%d" % i, [128, D], F32) for i in range(1)]
    if moe:
        c["h32"] = P.sb("h32", [128, NT, D], F32) if not sparse else None
        c["hT32"] = P.sb("hT32", [128, 8, 128], F32)
        c["wr32"] = P.sb("wr32", [128, 8, 8], F32)
        c["lg"] = P.sb("lg", [128, 24], F32)
        c["mx8"] = P.sb("mx8", [128, 16], F32)
        c["comb"] = P.sb("comb", [128, NT, 8], F32)
        c["identf"] = P.sb("identf", [128, 128], F32)
        P.dma("sync", c["identf"][:], P.ident_d, w=["identf"])
        P.dma("sync", c["wr32"][:].rearrange("p a b -> p (a b)"), wr_d, w=["wr32"])
    if kind == "S2":
        mf_d, mb_d = dI("mf", [D, NTOK], BF16), dI("mb", [D, NTOK], BF16)
        if sparse:
            mTs = [arena[:, i * 8 * TB:(i + 1) * 8 * TB].rearrange("p (a b) -> p a b", a=8) for i in range(2)]
        else:
            mTs = [P.sb("mT0", [128, 8, TB], BF16), P.sb("mT1", [128, 8, TB], BF16)]
        msrc = [mf_d, mb_d]
    elif kind == "S4":
        mf_d = dI("mf", [D, NTOK], BF16)
        mTs = [P.sb("mT0", [128, 8, TB], BF16)]
        msrc = [mf_d]
    else:
        HW = 8
        h3_d = dI("h3T", [D, NTOK + 2 * HW], BF16)
        pw_in = dI("p_win", [D, D])
        pwg_d = dI("p_wg", [4, 256, 256])
        pbg_d, psc_d = dI("p_bg", [128, 8]), dI("p_scale", [128, 8])
        pfix_d = dI("p_fix", [128, 64])
        if sparse:
            NHp = TB + 2 * HW
            o = [0]
            def carve(n_el, a=None, f32=False):
                sl = arena[:, o[0]:o[0] + n_el]
                o[0] += n_el
                if f32:
                    return sl.bitcast(F32)
                return sl.rearrange("p (a b) -> p a b", a=a) if a else sl
            hTh, qT, yT = carve(8 * NHp, 8), carve(8 * TB, 8), carve(8 * TB, 8)
            pT, sA, sB = carve(2 * NHp, f32=True), carve(2 * NHp, f32=True), carve(2 * NHp, f32=True)
        else:
            hTh = P.sb("hTh", [128, 8, TB + 2 * HW], BF16)
            pT = P.sb("pT", [128, TB + 2 * HW], F32)
            sA = P.sb("sA", [128, TB + 2 * HW], F32)
            sB = P.sb("sB", [128, TB + 2 * HW], F32)
            qT = P.sb("qT", [128, 8, TB], BF16)
            yT = P.sb("yT", [128, 8, TB], BF16)
        wgs = P.sb("wgs", [128, 4, 2, 256], BF16)
        P.dma("gpsimd", wgs[:].rearrange("p g c d -> p (g c) d"), pwg_d.rearrange("g (cc p) d -> p (g cc) d", p=128), w=["wgs"])
        pbg, psc, pfix = P.sb("pbg", [128, 8], F32), P.sb("psc", [128, 8], F32), P.sb("pfix", [128, 4, 16], F32)
        P.dma("sync", pbg[:], pbg_d, w=["pbg"])
        P.dma("sync", psc[:], psc_d, w=["psc"])
        P.dma("sync", pfix[:].rearrange("p a b -> p (a b)"), pfix_d, w=["pfix"])

    segs = [(2, "GT1", "mul", 1), (3, "SH2", "raw", 0), (4, "G2", "mul1p", 2), (5, "GT2", "mul", 3)]
    m = P.ada_mod("m", c_d, aw, ab, ng, segs)
    if has_next:
        mn = P.ada_mod("n", c_d, awn, abn, ngn, [(0, "SH1", "raw", 0), (1, "G1", "mul1p", 0)])
    rows = lambda d, t: d[t * 128:(t + 1) * 128, :]
    nblk = n_tiles // NT
    for b in range(nblk):
        ts = list(range(b * NT, (b + 1) * NT))
        nt = NT
        t0 = b * TB
        P.load_block(X, [rows(x_d, t) for t in ts], None)
        if kind in ("S2", "S4"):
            for mi, src in enumerate(msrc):
                P.dma("sync", mTs[mi][:], src.rearrange("(kc p) t -> p kc t", p=128)[:, :, t0:t0 + TB], w=["mT%d" % mi])
            P.mixer_out(X, nt, mTs, ["mT%d" % i for i in range(len(mTs))], w_out, 8, m["GT1"])
        else:
            NH = TB + 2 * HW
            P.dma("sync", hTh[:], h3_d.rearrange("(kc p) t -> p kc t", p=128)[:, :, t0:t0 + NH], w=["hTh"])
            wv = pw_in.rearrange("(kc p) n -> p kc n", p=128)
            for cg in range(2):
                wt, wk = P.load_w(wv[:, :, cg * 512:(cg + 1) * 512], 8, 512)
                for j in range(4):
                    cc = cg * 4 + j
                    win = (2, 4, 8, 16)[cc // 2]
                    for hf in range(2):
                        pa, pk = P.nA()
                        c0 = hf * (NH // 2)
                        for kc in range(8):
                            P.op("tensor", lambda e, kc=kc, j=j, wt=wt, pa=pa, c0=c0: e.matmul(pa[:, 0:NH // 2], lhsT=wt[:, kc, j * 128:(j + 1) * 128], rhs=hTh[:, kc, c0:c0 + NH // 2], start=(kc == 0), stop=(kc == 7)),
                                 r=[wk, "hTh"], w=[pk])
                        P.op("scalar", lambda e, pa=pa, c0=c0: e.copy(out=pT[:, c0:c0 + NH // 2], in_=pa[:, 0:NH // 2]), r=[pk], w=["pT"])
                    P.op("vector", lambda e: e.tensor_tensor(out=sA[:, 1:NH], in0=pT[:, 0:NH - 1], in1=pT[:, 1:NH], op=ALU.add), r=["pT", "sB"], w=["sA"])
                    cur, ck, oth, ok = sA, "sA", sB, "sB"
                    sh = 1
                    lo, hi = 1, NH
                    while sh * 2 < win:
                        l2, h2 = lo + sh, hi - sh
                        P.op("vector", lambda e, cur=cur, oth=oth, sh=sh, l2=l2, h2=h2: e.tensor_tensor(out=oth[:, l2:h2], in0=cur[:, l2 - sh:h2 - sh], in1=cur[:, l2 + sh:h2 + sh], op=ALU.add),
                             r=[ck], w=[ok])
                        cur, ck, oth, ok = oth, ok, cur, ck
                        lo, hi = l2, h2
                        sh *= 2
                    wi = cc // 2
                    P.op("vector", lambda e, cur=cur, win=win: e.tensor_scalar(out=cur[:, HW:HW + TB], in0=cur[:, HW:HW + TB], scalar1=1.0 / win, scalar2=None, op0=ALU.mult), r=[ck], w=[ck])
                    if b == 0:
                        P.op("vector", lambda e, cur=cur, wi=wi: e.tensor_tensor(out=cur[:, HW:HW + 8], in0=cur[:, HW:HW + 8], in1=pfix[:, wi, 0:8], op=ALU.mult), r=[ck, "pfix"], w=[ck])
                    if b == nblk - 1:
                        P.op("vector", lambda e, cur=cur, wi=wi: e.tensor_tensor(out=cur[:, HW + TB - 8:HW + TB], in0=cur[:, HW + TB - 8:HW + TB], in1=pfix[:, wi, 8:16], op=ALU.mult), r=[ck, "pfix"], w=[ck])
                    P.op("vector", lambda e, cur=cur, cc=cc: e.tensor_tensor(out=qT[:, cc, :], in0=cur[:, HW:HW + TB], in1=pT[:, HW:HW + TB], op=ALU.subtract), r=[ck, "pT"], w=["qT", "sA", "sB"])
            for cc in range(8):
                g = cc // 2
                pa, pk = P.nA()
                for ci in range(2):
                    P.op("tensor", lambda e, ci=ci, g=g, cc=cc, pa=pa: e.matmul(pa[:, :], lhsT=wgs[:, g, ci, (cc % 2) * 128:(cc % 2 + 1) * 128], rhs=qT[:, 2 * g + ci, :], start=(ci == 0), stop=(ci == 1)),
                         r=["wgs", "qT"], w=[pk])
                P.op("vector", lambda e, cc=cc, pa=pa: e.tensor_scalar(out=yT[:, cc, :], in0=pa[:, :], scalar1=pbg[:, cc:cc + 1], scalar2=psc[:, cc:cc + 1], op0=ALU.add, op1=ALU.mult), r=[pk, "pbg", "psc"], w=["yT"])
            P.mixer_out(X, nt, [yT], ["yT"], w_out, 8, m["GT1"])
        if sparse:
            for ti in range(nt):
                g = b * NT + ti
                h32t, h32k = h32r[0], "h32r0"
                P.norm_mod(X[:, ti, :], "X%d" % ti, m["G2"], m["SH2"], h32t[:], h32k)
                hb, hk = P.nh()
                P.op("scalar", lambda e, hb=hb, h32t=h32t: e.copy(out=hb[:], in_=h32t[:]), r=[h32k], w=[hk])
                P.dma("sync", Hd[g * 128:(g + 1) * 128, :], hb[:], r=[hk], w=["Hd"])
                tok_router2(P, g, h32t, h32k, c)
                P.dma("sync", Xd[g * 128:(g + 1) * 128, :], X[:, ti, :], r=["X%d" % ti], w=["Xd"])
            continue
        if moe:
            P.pre_T(X, nt, m["G2"], m["SH2"], c["hT"], want32=(c["h32"], "h32_"))
            tok_router(P, nt, c)
            for ex in range(8):
                comb = (lambda ti, ex=ex: c["comb"][:, ti, ex:ex + 1], "comb")
                P.ffn(X, nt, c, m["GT2"], wg_d[ex], wu_d[ex], wd_d[ex], FF, comb=comb, first=(ex == 0))
        else:
            P.pre_T(X, nt, m["G2"], m["SH2"], c["hT"])
            P.ffn(X, nt, c, m["GT2"], wg_d, wu_d, wd_d, FF)
        P.ffn_post(X, nt, c, m["GT2"])
        if has_next:
            P.out_block(X, nt, [rows(xo, t) for t in ts], [rows(ho, t) for t in ts], mn["G1"], mn["SH1"])
        else:
            P.out_block(X, nt, [rows(xo, t) for t in ts])
    if sparse:
        def finish_tile(g, ti):
            if has_next:
                hb, hk = P.nh()
                P.norm_mod(X[:, ti, :], "X%d" % ti, mn["G1"], mn["SH1"], hb[:], hk)
                P.dma("sync", rows(ho, g), hb[:], r=[hk])
            P.dma("sync", rows(xo, g), X[:, ti, :], r=["X%d" % ti])
        moe_sparse(P, X, n_tiles, c, m, wg_d, wu_d, wd_d, FF, Xd, Hd, finish_tile)
    return P

def build_S1(L=16384, CTX=256):
    P = Prog()
    SEQP = 2 + CTX + 4 + L + 2
    dI = lambda n, s, dt=F32: P.dram(n, s, dt, "ExternalInput")
    hseq = dI("hseq", [4, D, SEQP], BF16)
    wg_d, wx_d = dI("w_gate", [D, 128]), dI("w_xb", [D, 128])
    cw_d, cb_d = dI("conv_w", [4 * 128]), dI("conv_b", [128, 1])
    wa_d, wxx_d = dI("w_a", [2, 128, 128]), dI("w_x", [2, 128, 128])
    ba_d, bx_d, lam_d = dI("b_a", [128, 2]), dI("b_x", [128, 2]), dI("lam", [128, 2])
    m_d = P.dram("m", [4, 128, L], BF16, "ExternalOutput")
    Wg = P.sb("Wg", [128, 8, 128], BF16)
    Wx = P.sb("Wx", [128, 8, 128], BF16)
    Wxk = P.sb("Wxk", [128, 8, 4, 128], BF16)
    cw = P.sb("cw", [128, 4, 128], F32)
    cb = P.sb("cb", [128, 1], F32)
    wa = P.sb("wa", [128, 2, 128], BF16)
    wxx = P.sb("wxx", [128, 2, 128], BF16)
    ba, bx, lam, cl = P.sb("ba", [128, 2], F32), P.sb("bx", [128, 2], F32), P.sb("lam_s", [128, 2], F32), P.sb("cl", [128, 2], F32)
    st = P.sb("st", [128, 1], F32)
    P.dma("gpsimd", Wg[:], wg_d.rearrange("(kc p) n -> p kc n", p=128), w=["Wg"])
    P.dma("gpsimd", Wx[:], wx_d.rearrange("(kc p) n -> p kc n", p=128), w=["Wx"])
    P.dma("sync", cw[:].rearrange("p a b -> p (a b)"), bass.AP(tensor=cw_d.tensor, offset=0, ap=[[0, 128], [1, 512]]), w=["cw"])
    P.dma("sync", cb[:], cb_d, w=["cb"])
    P.dma("gpsimd", wa[:], wa_d.rearrange("d i j -> i d j"), w=["wa"])
    P.dma("gpsimd", wxx[:], wxx_d.rearrange("d i j -> i d j"), w=["wxx"])
    P.dma("sync", ba[:], ba_d, w=["ba"])
    P.dma("sync", bx[:], bx_d, w=["bx"])
    P.dma("sync", lam[:], lam_d, w=["lam"])
    for k in range(4):
        for kc in range(8):
            P.op("vector", lambda e, k=k, kc=kc: e.tensor_tensor(out=Wxk[:, kc, k, :], in0=Wx[:, kc, :], in1=cw[:, k, :], op=ALU.mult), r=["Wx", "cw"], w=["Wxk"])
    P.op("scalar", lambda e: e.activation(out=cl[:], in_=lam[:], func=AF.Exp, scale=-1.0), r=["lam"], w=["cl"])
    P.op("scalar", lambda e: e.activation(out=cl[:], in_=cl[:], func=AF.Ln, scale=1.0, bias=1.0), r=["cl"], w=["cl"])
    P.op("vector", lambda e: e.tensor_scalar(out=cl[:], in0=cl[:], scalar1=-8.0, scalar2=None, op0=ALU.mult), r=["cl"], w=["cl"])
    ps = [P.ps("ps%d" % i, [128, 512], F32) for i in range(6)]
    pi = [0]
    def nps():
        i = pi[0] % 6
        pi[0] += 1
        return ps[i], "ps%d" % i
    NB = 2
    hc = [P.sb("hc%d" % i, [128, 8, 516], BF16) for i in range(NB)]
    bufs = {}
    for nm, dt in (("xc32", F32), ("xcb", BF16), ("r", F32), ("ii", F32), ("a", F32), ("sq", F32), ("bb", F32), ("hs", F32), ("t1", F32), ("mo", BF16)):
        bufs[nm] = [P.sb("%s%d" % (nm, i), [128, 512], dt) for i in range(NB)]
    ci = 0
    for s in range(4):
        d = s // 2
        P.op("vector", lambda e: e.memset(st[:], 0.0), r=[], w=["st"])
        chunks = [(2, CTX, False, 0)] + [(2 + CTX + 4 + t0, min(512, L - t0), True, t0) for t0 in range(0, L, 512)]
        for (c0, n, is_lat, t0) in chunks:
            sl = ci % NB
            ci += 1
            B = {k: (v[sl], "%s%d" % (k, sl)) for k, v in bufs.items()}
            h, hk = hc[sl], "hc%d" % sl
            P.dma("sync", h[:, :, 0:n + 4], hseq[s].rearrange("(kc p) t -> p kc t", p=128)[:, :, c0 - 2:c0 + n + 2], w=[hk])
            px, pxk = nps()
            idx = 0
            for k in range(4):
                off = (k - 2) if d == 0 else (2 - k)
                for kc in range(8):
                    P.op("tensor", lambda e, k=k, kc=kc, off=off, px=px, h=h, n=n, idx=idx: e.matmul(px[:, 0:n], lhsT=Wxk[:, kc, k, :], rhs=h[:, kc, 2 + off:2 + off + n], start=(idx == 0), stop=(idx == 31)),
                         r=["Wxk", hk], w=[pxk])
                    idx += 1
            xc32, xk = B["xc32"]
            xcb, xbk = B["xcb"]
            P.op("scalar", lambda e, px=px, xc32=xc32, n=n: e.activation(out=xc32[:, 0:n], in_=px[:, 0:n], func=AF.Identity, bias=cb[:, 0:1], scale=1.0), r=[pxk, "cb"], w=[xk])
            P.op("vector", lambda e, xc32=xc32, xcb=xcb, n=n: e.tensor_copy(out=xcb[:, 0:n], in_=xc32[:, 0:n]), r=[xk], w=[xbk])
            pr, prk = nps()
            P.op("tensor", lambda e, pr=pr, xcb=xcb, n=n, d=d: e.matmul(pr[:, 0:n], lhsT=wa[:, d, :], rhs=xcb[:, 0:n], start=True, stop=True), r=["wa", xbk], w=[prk])
            pI, pik = nps()
            P.op("tensor", lambda e, pI=pI, xcb=xcb, n=n, d=d: e.matmul(pI[:, 0:n], lhsT=wxx[:, d, :], rhs=xcb[:, 0:n], start=True, stop=True), r=["wxx", xbk], w=[pik])
            r_, rk = B["r"]
            i_, ik = B["ii"]
            a_, ak = B["a"]
            sq, sqk = B["sq"]
            bb, bbk = B["bb"]
            hs, hsk = B["hs"]
            P.op("scalar", lambda e, pr=pr, r_=r_, n=n, d=d: e.activation(out=r_[:, 0:n], in_=pr[:, 0:n], func=AF.Sigmoid, bias=ba[:, d:d + 1], scale=1.0), r=[prk, "ba"], w=[rk])
            P.op("scalar", lambda e, pI=pI, i_=i_, n=n, d=d: e.activation(out=i_[:, 0:n], in_=pI[:, 0:n], func=AF.Sigmoid, bias=bx[:, d:d + 1], scale=1.0), r=[pik, "bx"], w=[ik])
            P.op("scalar", lambda e, r_=r_, a_=a_, n=n, d=d: e.activation(out=a_[:, 0:n], in_=r_[:, 0:n], func=AF.Exp, scale=cl[:, d:d + 1]), r=[rk, "cl"], w=[ak])
            P.op("scalar", lambda e, a_=a_, sq=sq, n=n: e.activation(out=sq[:, 0:n], in_=a_[:, 0:n], func=AF.Square), r=[ak], w=[sqk])
            P.op("scalar", lambda e, sq=sq, n=n: e.activation(out=sq[:, 0:n], in_=sq[:, 0:n], func=AF.Sqrt, scale=-1.0, bias=1.0), r=[sqk], w=[sqk])
            P.op("vector", lambda e, i_=i_, xc32=xc32, bb=bb, n=n: e.tensor_tensor(out=bb[:, 0:n], in0=i_[:, 0:n], in1=xc32[:, 0:n], op=ALU.mult), r=[ik, xk], w=[bbk])
            P.op("vector", lambda e, sq=sq, bb=bb, n=n: e.tensor_tensor(out=bb[:, 0:n], in0=bb[:, 0:n], in1=sq[:, 0:n], op=ALU.mult), r=[sqk, bbk], w=[bbk])
            P.op("vector", lambda e, a_=a_, bb=bb, hs=hs, n=n: e.tensor_tensor_scan(out=hs[:, 0:n], data0=a_[:, 0:n], data1=bb[:, 0:n], initial=st[:, 0:1], op0=ALU.mult, op1=ALU.add), r=[ak, bbk, "st"], w=[hsk])
            P.op("vector", lambda e, hs=hs, n=n: e.tensor_copy(out=st[:, 0:1], in_=hs[:, n - 1:n]), r=[hsk], w=["st"])
            if is_lat:
                pg, pgk = nps()
                for kc in range(8):
                    P.op("tensor", lambda e, kc=kc, pg=pg, h=h, n=n: e.matmul(pg[:, 0:n], lhsT=Wg[:, kc, :], rhs=h[:, kc, 2:2 + n], start=(kc == 0), stop=(kc == 7)), r=["Wg", hk], w=[pgk])
                t1, t1k = B["t1"]
                mo, mok = B["mo"]
                g = pg[:, 0:n]
                t = t1[:, 0:n]
                P.op("scalar", lambda e, t=t, g=g: e.activation(out=t, in_=g, func=AF.Square), r=[pgk], w=[t1k])
                P.op("vector", lambda e, t=t: e.tensor_scalar(out=t, in0=t, scalar1=0.044715, scalar2=1.0, op0=ALU.mult, op1=ALU.add), r=[t1k], w=[t1k])
                P.op("vector", lambda e, t=t, g=g: e.tensor_tensor(out=t, in0=t, in1=g, op=ALU.mult), r=[t1k, pgk], w=[t1k])
                P.op("scalar", lambda e, t=t: e.activation(out=t, in_=t, func=AF.Sigmoid, scale=1.5957691216057308), r=[t1k], w=[t1k])
                P.op("vector", lambda e, t=t, g=g: e.tensor_tensor(out=t, in0=t, in1=g, op=ALU.mult), r=[t1k, pgk], w=[t1k])
                P.op("vector", lambda e, t=t, hs=hs, mo=mo, n=n: e.tensor_tensor(out=mo[:, 0:n], in0=t, in1=hs[:, 0:n], op=ALU.mult), r=[t1k, hsk], w=[mok])
                P.dma("sync", m_d[s][:, t0:t0 + n], mo[:, 0:n], r=[mok])
    return P

def build_S3a(n_tiles=32):
    P = Prog()
    NTOK = n_tiles * 128
    dI = lambda n, s, dt=F32: P.dram(n, s, dt, "ExternalInput")
    hT_d = dI("hT", [D, NTOK + 2], BF16)
    w_d, cw_d, cb_d = dI("w_in", [D, 3072]), dI("conv_w", [3 * 3072]), dI("conv_b", [3072])
    z_d = P.dram("z", [NTOK, 3072], F32, "ExternalOutput")
    hT = P.sb("hT_s", [128, 8, NTOK + 2], BF16)
    P.dma("sync", hT[:], hT_d.rearrange("(kc p) t -> p kc t", p=128), w=["hT"])
    W = [P.sb("W%d" % i, [128, 8, 512], BF16) for i in range(2)]
    Wk = [P.sb("Wk%d" % i, [128, 3, 8, 512], BF16) for i in range(2)]
    cwb = [P.sb("cwb%d" % i, [128, 3, 512], F32) for i in range(2)]
    bb = [P.sb("bb%d" % i, [128, 512], F32) for i in range(2)]
    zo = [P.sb("zo%d" % i, [128, 512], F32) for i in range(3)]
    ps = [P.ps("ps%d" % i, [128, 512], F32) for i in range(4)]
    wv = w_d.rearrange("(kc p) n -> p kc n", p=128)
    n = 0
    for cb in range(6):
        s = cb % 2
        P.dma("gpsimd", W[s][:], wv[:, :, cb * 512:(cb + 1) * 512], w=["W%d" % s])
        for k in range(3):
            P.dma("sync", cwb[s][:, k, :], bass.AP(tensor=cw_d.tensor, offset=k * 3072 + cb * 512, ap=[[0, 128], [1, 512]]), w=["cwb%d_%d" % (s, k)])
        P.dma("sync", bb[s][:], bass.AP(tensor=cb_d.tensor, offset=cb * 512, ap=[[0, 128], [1, 512]]), w=["bb%d" % s])
        for k in range(3):
            for kc in range(8):
                eng = "vector" if (kc % 2 == 0) else "gpsimd"
                P.op(eng, lambda e, s=s, k=k, kc=kc: e.tensor_tensor(out=Wk[s][:, k, kc, :], in0=W[s][:, kc, :], in1=cwb[s][:, k, :], op=ALU.mult),
                     r=["W%d" % s, "cwb%d_%d" % (s, k)], w=["Wk%d_%d" % (s, k)])
        for ti in range(n_tiles):
            pa, pk = ps[n % 4], "ps%d" % (n % 4)
            zt, zk = zo[n % 3], "zo%d" % (n % 3)
            n += 1
            idx = 0
            for k in range(3):
                for kc in range(8):
                    P.op("tensor", lambda e, s=s, k=k, kc=kc, ti=ti, pa=pa, idx=idx: e.matmul(pa[:, :], lhsT=hT[:, kc, ti * 128 + k:ti * 128 + k + 128], rhs=Wk[s][:, k, kc, :], start=(idx == 0), stop=(idx == 23)),
                         r=["hT", "Wk%d_%d" % (s, k)], w=[pk])
                    idx += 1
            P.op("vector", lambda e, pa=pa, zt=zt, s=s: e.tensor_tensor(out=zt[:], in0=pa[:, :], in1=bb[s][:], op=ALU.add), r=[pk, "bb%d" % s], w=[zk])
            P.dma("sync", z_d[ti * 128:(ti + 1) * 128, cb * 512:(cb + 1) * 512], zt[:], r=[zk])
    return P


def build_S3b(L=16384, NCH=128):
    P = Prog()
    NB = L // 128
    NC2 = 2 * NB
    L2 = 2 * L
    NBLK = L2 // 512
    dI = lambda n, s, dt=F32: P.dram(n, s, dt, "ExternalInput")
    vrev_d, vnat_d = dI("vrev", [NCH, 128, NC2]), dI("vnat", [NCH, 128, NC2])
    x1_d, x2_d = dI("x1nat", [NCH, 128, NC2]), dI("x2rev", [NCH, 128, NC2])
    zT_d = [dI("zT1", [33, L]), dI("zT2", [33, L])]
    win_d = [dI("win1", [128, L]), dI("win2", [128, L])]
    w1_d, w2_d, w3_d = dI("f_w1", [33, 64]), dI("f_w2", [64, 64]), dI("f_w3", [64, 64])
    fb_d, fr_d = dI("f_b", [64, 3]), dI("f_freq", [64, 3])
    wo_d = dI("f_wout", [64, 4 * 128])
    hb_d = dI("hbias", [2 * 128])
    J_d = dI("J", [128, 128])
    y2_d = P.dram("y2rev", [NCH, 128, NC2], BF16, "ExternalOutput")
    Ktab = [P.dram("Ktab%d" % i, [128, L2], BF16, "Internal") for i in range(2)]
    Y1d = P.dram("Y1d", [NCH, 128, NC2], BF16, "Internal")
    w1, w2, w3 = P.sb("w1", [33, 64], F32), P.sb("w2", [64, 64], F32), P.sb("w3", [64, 64], F32)
    fb, fr, wo = P.sb("fb", [64, 3], F32), P.sb("fr", [64, 3], F32), P.sb("wo", [64, 4, 128], F32)
    hbias = P.sb("hbias_s", [128, 2, 128], F32)
    J = P.sb("J_s", [128, 128], BF16)
    for t, d_, k in ((w1, w1_d, "w1"), (w2, w2_d, "w2"), (w3, w3_d, "w3"), (fb, fb_d, "fb"), (fr, fr_d, "fr")):
        P.dma("sync", t[:], d_, w=[k])
    P.dma("sync", wo[:].rearrange("p a b -> p (a b)"), wo_d, w=["wo"])
    P.dma("sync", hbias[:].rearrange("p a b -> p (a b)"), bass.AP(tensor=hb_d.tensor, offset=0, ap=[[0, 128], [1, 256]]), w=["hbias"])
    P.dma("gpsimd", J[:], J_d, w=["J"])
    ps = [P.ps("ps%d" % i, [128, 512], F32) for i in range(4)]
    pi = [0]
    def nps():
        i = pi[0] % 4
        pi[0] += 1
        return ps[i], "ps%d" % i
    GW = min(2048, L)
    NGR = L // GW
    NJ = GW // 512
    psF = P.ps("psF", [128, GW], F32)
    zt = P.sb("zt0", [33, GW], F32)
    wn = P.sb("wn0", [128, GW], F32)
    hd = P.sb("hd0", [64, GW], F32)
    wi = P.sb("wi0", [64, GW], I32)
    g1 = P.sb("g1_0", [64, GW], F32)
    kb = P.sb("kb0", [128, 2, GW], BF16)
    fr2 = P.sb("fr2", [64, 3], F32)
    TWO_PI = 6.283185307179586
    P.op("vector", lambda e: e.tensor_scalar(out=fr2[:], in0=fr[:], scalar1=1.0 / TWO_PI, scalar2=None, op0=ALU.mult), r=["fr"], w=["fr2"])
    for pas in range(2):
        for gr in range(NGR):
            m0 = gr * GW
            P.dma("sync", zt[:], zT_d[pas][:, m0:m0 + GW], w=["zt"])
            P.dma("sync", wn[:], win_d[pas][:, m0:m0 + GW], w=["wn"])
            src, skey, W_ = zt, "zt", [w1, w2, w3]
            for ly in range(3):
                nk = 33 if ly == 0 else 64
                for j in range(NJ):
                    P.op("tensor", lambda e, src=src, ly=ly, W_=W_, nk=nk, j=j: e.matmul(psF[0:64, j * 512:(j + 1) * 512], lhsT=W_[ly][0:nk, :], rhs=src[0:nk, j * 512:(j + 1) * 512], start=True, stop=True),
                         r=[skey, "w%d" % (ly + 1)], w=["psF"])
                h, hk = hd, "hd"
                P.op("vector", lambda e, h=h, ly=ly: e.tensor_scalar(out=h[:], in0=psF[0:64, :], scalar1=fb[:, ly:ly + 1], scalar2=fr2[:, ly:ly + 1], op0=ALU.add, op1=ALU.mult), r=["psF", "fb", "fr2"], w=[hk])
                P.op("vector", lambda e, h=h: e.tensor_copy(out=wi[:], in_=h[:]), r=[hk], w=["wi"])
                P.op("vector", lambda e: e.tensor_copy(out=g1[:], in_=wi[:]), r=["wi"], w=["g1"])
                P.op("vector", lambda e, h=h: e.tensor_tensor(out=h[:], in0=h[:], in1=g1[:], op=ALU.subtract), r=[hk, "g1"], w=[hk])
                P.op("vector", lambda e, h=h: e.tensor_scalar(out=g1[:], in0=h[:], scalar1=0.5, scalar2=None, op0=ALU.is_gt), r=[hk], w=["g1"])
                P.op("vector", lambda e, h=h: e.tensor_tensor(out=h[:], in0=h[:], in1=g1[:], op=ALU.subtract), r=[hk, "g1"], w=[hk])
                P.op("vector", lambda e, h=h: e.tensor_scalar(out=g1[:], in0=h[:], scalar1=-0.5, scalar2=None, op0=ALU.is_lt), r=[hk], w=["g1"])
                P.op("vector", lambda e, h=h: e.tensor_tensor(out=h[:], in0=h[:], in1=g1[:], op=ALU.add), r=[hk, "g1"], w=[hk])
                P.op("scalar", lambda e, h=h: e.activation(out=h[:], in_=h[:], func=AF.Sin, scale=TWO_PI), r=[hk], w=[hk])
                src, skey = h, hk
            for tab in range(2):
                dr = (1, 0)[tab] if pas == 0 else (0, 1)[tab]
                for j in range(NJ):
                    P.op("tensor", lambda e, src=src, tab=tab, dr=dr, j=j: e.matmul(psF[:, j * 512:(j + 1) * 512], lhsT=wo[:, tab * 2 + dr, :], rhs=src[:, j * 512:(j + 1) * 512], start=True, stop=True), r=[skey, "wo"], w=["psF"])
                P.op("vector", lambda e, tab=tab: e.tensor_tensor(out=kb[:, tab, :], in0=psF[:, :], in1=wn[:], op=ALU.mult), r=["psF", "wn"], w=["kb_%d" % tab])
                c0, c1 = 0, GW
                if pas == 0:
                    col0 = m0
                    if tab == 0 and gr == NGR - 1:
                        c1 = GW - 1
                else:
                    col0 = (L - 1) + m0
                    if tab == 1 and gr == 0:
                        c0 = 1
                P.dma("sync", Ktab[tab][:, col0 + c0:col0 + c1], kb[:, tab, c0:c1], r=["kb_%d" % tab], w=["Ktab%d" % tab])
    HALF = NB * 128
    T = [P.sb("T%d" % i, [128, HALF], BF16) for i in range(4)]
    vb = [P.sb("vb%d" % i, [128, NC2], BF16) for i in range(2)]
    gA = [P.sb("gA%d" % i, [128, NC2], F32) for i in range(2)]
    gB = [P.sb("gB%d" % i, [128, NC2], F32) for i in range(2)]
    tt = [P.sb("tt%d" % i, [128, NC2], F32) for i in range(2)]
    yo = [P.sb("yo%d" % i, [128, NC2], BF16) for i in range(2)]
    tn = 0
    n = 0
    for stage in range(2):
        for c in range(NCH):
            s = n % 2
            n += 1
            if stage == 0:
                P.dma("gpsimd", vb[s][:], vrev_d[c], w=["vb%d" % s])
                P.dma("sync", gA[s][:], vnat_d[c], w=["gA%d" % s])
                P.dma("sync", gB[s][:], x1_d[c], w=["gB%d" % s])
            else:
                P.dma("sync", vb[s][:], Y1d[c], r=["Y1d"], w=["vb%d" % s])
                P.dma("sync", gB[s][:], x2_d[c], w=["gB%d" % s])
                pj, pjk = nps()
                P.op("tensor", lambda e, pj=pj, s=s: e.matmul(pj[:, 0:NC2], lhsT=J[:], rhs=vb[s][:], start=True, stop=True), r=["J", "vb%d" % s], w=[pjk])
                P.op("scalar", lambda e, pj=pj, s=s: e.copy(out=gA[s][:], in_=pj[:, 0:NC2]), r=[pjk], w=["gA%d" % s])
            pa, pk = nps()
            pav = pa[:, 0:NC2].rearrange("p (b i) -> p b i", b=2)
            vv = vb[s][:].rearrange("p (b i) -> p b i", b=2)
            order = [NB - 1] + [k for k in range(2 * NB - 1) if k != NB - 1]
            loaded = {}
            cnt = 0
            for k in order:
                hf = 0 if k < NB else 1
                if hf not in loaded:
                    ts_ = tn % 4
                    tn += 1
                    nel = HALF if hf == 0 else HALF - 128
                    P.dma(("sync", "scalar")[tn % 2], T[ts_][:, 0:nel], bass.AP(tensor=Ktab[stage].tensor, offset=c * L2 + hf * HALF, ap=[[1, 128], [1, nel]]),
                          r=["Ktab%d" % stage], w=["T%d" % ts_])
                    loaded[hf] = ts_
                ts_ = loaded[hf]
                delta = (k - (NB - 1)) if stage == 0 else ((NB - 1) - k)
                i0, i1 = max(0, delta), min(NB, NB + delta)
                kk = k - hf * NB
                P.op("tensor", lambda e, pav=pav, vv=vv, ts_=ts_, kk=kk, i0=i0, i1=i1, delta=delta, cnt=cnt: e.matmul(pav[:, :, i0:i1], lhsT=T[ts_][:, kk * 128:(kk + 1) * 128], rhs=vv[:, :, i0 - delta:i1 - delta], start=(cnt == 0), stop=(cnt == 2 * NB - 2)),
                     r=["T%d" % ts_, "vb%d" % s], w=[pk])
                cnt += 1
            P.op("vector", lambda e, pa=pa, s=s, stage=stage, c=c: e.scalar_tensor_tensor(out=tt[s][:], in0=gA[s][:], scalar=hbias[:, stage, c:c + 1], in1=pa[:, 0:NC2], op0=ALU.mult, op1=ALU.add),
                 r=[pk, "gA%d" % s, "hbias"], w=["tt%d" % s])
            P.op("gpsimd", lambda e, s=s: e.tensor_tensor(out=yo[s][:], in0=tt[s][:], in1=gB[s][:], op=ALU.mult), r=["tt%d" % s, "gB%d" % s], w=["yo%d" % s])
            if stage == 0:
                P.dma("sync", Y1d[c], yo[s][:], r=["yo%d" % s], w=["Y1d"])
            else:
                P.dma("sync", y2_d[c], yo[s][:], r=["yo%d" % s])
    return P

def hyena_consts(L, ch0, nch):
    f32 = np.float32
    t = np.linspace(0.0, 1.0, L, dtype=f32)[:, None]
    w = (2.0 * np.pi * np.arange(L, dtype=f32)[:, None] / L).astype(f32)
    bands = np.linspace(1e-4, 15, 16, dtype=f32)[None, :]
    z = np.concatenate([t, np.cos(bands * w), np.sin(-bands * w)], axis=-1).astype(f32)
    max_decay = math.log(1e-2) / 0.3
    min_decay = math.log(1e-2) / 1.5
    deltas = np.abs(np.linspace(min_decay, max_decay, 1024, dtype=f32))
    window = np.exp(-t * deltas[None, ch0:ch0 + nch]).astype(f32)
    zT1 = np.ascontiguousarray(z[::-1].T)
    zT2 = np.ascontiguousarray(z.T)
    win1 = np.zeros((128, L), f32)
    win2 = np.zeros((128, L), f32)
    win1[:nch] = window[::-1].T
    win2[:nch] = window.T
    return zT1, zT2, win1, win2


def moe_lay_gu(w):
    return np.ascontiguousarray(w.reshape(8, 8, 128, 4, 896).transpose(0, 3, 2, 1, 4)).reshape(4096, 4, 1792)


def moe_lay_d(w):
    return np.ascontiguousarray(w.reshape(8, 4, 7, 128, 1024).transpose(0, 1, 3, 2, 4)).reshape(4096, 4, 1792)

def _pos_embed(rows):
    f32 = np.float32
    r, col = np.meshgrid(np.arange(rows, dtype=f32), np.arange(64, dtype=f32), indexing='ij')
    quarter = D // 4
    omega = (1.0 / (f32(10000.0) ** (np.arange(quarter, dtype=f32) / f32(quarter)))).astype(f32)
    def sincos(p):
        ang = (p.reshape(-1, 1) * omega[None, :]).astype(f32)
        return np.concatenate([np.sin(ang), np.cos(ang)], axis=-1)
    return np.concatenate([sincos(r), sincos(col)], axis=-1).astype(f32)


_LAST_DBG = {}


def _run(P, in_maps):
    nc = P.emit()
    res = run_bass_kernel_spmd(nc, in_maps, core_ids=list(range(8)))
    return res.results


def _col8(v):
    return np.ascontiguousarray(np.asarray(v, np.float32).reshape(8, 128).T)


def kernel(**inp):
    f32 = np.float32
    bf = ml_dtypes.bfloat16
    inp = {k: np.asarray(v) for k, v in inp.items()}
    B, L = 2, 16384
    TPC = 4096
    ident = np.eye(128, dtype=f32)
    pos = _pos_embed(L // 64)
    dbg = _LAST_DBG
    dbg.clear()
    cb = lambda k: (k // 4, (k % 4) * TPC)
    wsT = np.ascontiguousarray(inp['gmlp_w_s'][0].transpose(2, 0, 1)).reshape(128, 1024)
    bs16 = np.ascontiguousarray(np.repeat(inp['gmlp_b_s'][0], 2, axis=0).reshape(-1))
    maps = []
    for k in range(8):
        b, t0 = cb(k)
        bc_, half = (k % 4) // 2, (k % 4) % 2
        maps.append(dict(ident=ident, x=inp['x'][b, t0:t0 + TPC], pos=pos[t0:t0 + TPC], ctx=inp['ctx'][bc_, half * 128:(half + 1) * 128],
                         c=_col8(inp['c'][b]), c_ctx=_col8(inp['c_ctx']),
                         ada_w0=inp['ada_w'][0], ada_b0=inp['ada_b'][0], ng0=inp['norm_g'][0].reshape(-1),
                         ada_w1=inp['ada_w'][1], ada_b1=inp['ada_b'][1], ng1=inp['norm_g'][1].reshape(-1),
                         g_win=inp['gmlp_w_in'][0], g_gv=inp['gmlp_g_v'][0], g_wsT=wsT, g_bs=bs16, g_wout=inp['gmlp_w_out'][0],
                         f_wg=inp['ffn_w_gate'][0], f_wu=inp['ffn_w_up'][0], f_wd=inp['ffn_w_down'][0]))
    r = _run(build_S0(), maps)
    x1 = np.stack([np.concatenate([r[b * 4 + j]['xo'] for j in range(4)], 0) for b in range(2)])
    h1 = np.stack([np.concatenate([r[b * 4 + j]['ho'] for j in range(4)], 0) for b in range(2)])
    h1c = np.stack([np.concatenate([r[bc_ * 2 + half]['hco'] for half in range(2)], 0) for bc_ in range(2)])
    dbg['x0'] = x1
    del r
    CTX = 256
    SEQP = 2 + CTX + 4 + L + 2
    hseq = np.zeros((4, D, SEQP), dtype=bf)
    for d in range(2):
        for b in range(2):
            cs, ls = h1c[b], h1[b]
            if d == 1:
                cs, ls = cs[::-1], ls[::-1]
            hseq[d * 2 + b, :, 2:2 + CTX] = cs.T
            hseq[d * 2 + b, :, 2 + CTX + 4:2 + CTX + 4 + L] = ls.T
    maps = []
    for k in range(8):
        sl = slice(k * 128, (k + 1) * 128)
        maps.append(dict(hseq=hseq, w_gate=np.ascontiguousarray(inp['lru_w_in'][0][:, sl]), w_xb=np.ascontiguousarray(inp['lru_w_in'][0][:, 1024 + k * 128:1024 + (k + 1) * 128]),
                         conv_w=np.ascontiguousarray(inp['lru_conv_w'][0][:, sl]).reshape(-1), conv_b=np.ascontiguousarray(inp['lru_conv_b'][0][sl].reshape(128, 1)),
                         w_a=np.ascontiguousarray(inp['lru_w_a'][0][:, k]), w_x=np.ascontiguousarray(inp['lru_w_x'][0][:, k]),
                         b_a=np.ascontiguousarray(inp['lru_b_a'][0][:, sl].T), b_x=np.ascontiguousarray(inp['lru_b_x'][0][:, sl].T),
                         lam=np.ascontiguousarray(inp['lru_lam'][0][:, sl].T)))
    r = _run(build_S1(L=L, CTX=CTX), maps)
    del hseq
    mf = [np.concatenate([r[k]['m'][b] for k in range(8)], 0) for b in range(2)]
    mb = [np.concatenate([r[k]['m'][2 + b][:, ::-1] for k in range(8)], 0) for b in range(2)]
    del r
    NBLK = (2 * TPC + 8 * 511) // 512
    Utri = np.triu(np.ones((128, 128), f32), 1)
    blkrow = np.ascontiguousarray(np.broadcast_to((512.0 * np.arange(NBLK, dtype=f32))[None], (128, NBLK)))
    rowc = np.ascontiguousarray(np.concatenate([np.arange(128, dtype=f32)[:, None] + 128.0 * np.arange(4, dtype=f32)[None, :], np.zeros((128, 4), f32)], 1))
    moe_w = [(moe_lay_gu(inp['moe_w_gate'][k_]), moe_lay_gu(inp['moe_w_up'][k_]), moe_lay_d(inp['moe_w_down'][k_])) for k_ in range(2)]
    def wr_lay(w):
        return np.ascontiguousarray(w.reshape(8, 128, 8).transpose(1, 0, 2).reshape(128, 64))
    maps = []
    for k in range(8):
        b, t0 = cb(k)
        maps.append(dict(ident=ident, x=x1[b, t0:t0 + TPC], c=_col8(inp['c'][b]),
                         ada_w=inp['ada_w'][1], ada_b=inp['ada_b'][1], ng=inp['norm_g'][1].reshape(-1),
                         ada_wn=inp['ada_w'][2], ada_bn=inp['ada_b'][2], ngn=inp['norm_g'][2].reshape(-1),
                         w_out=inp['lru_w_out'][0], w_router=wr_lay(inp['moe_w_router'][0]),
                         wg=moe_w[0][0], wu=moe_w[0][1], wd=moe_w[0][2], Utri=Utri, blkrow=blkrow, rowc=rowc,
                         mf=np.ascontiguousarray(mf[b][:, t0:t0 + TPC]), mb=np.ascontiguousarray(mb[b][:, t0:t0 + TPC])))
    r = _run(build_TOK2("S2"), maps)
    x2 = np.stack([np.concatenate([r[b * 4 + j]['xo'] for j in range(4)], 0) for b in range(2)])
    h2 = np.stack([np.concatenate([r[b * 4 + j]['ho'] for j in range(4)], 0) for b in range(2)])
    dbg['x1'] = x2
    del r, mf, mb, x1, h1
    maps = []
    for k in range(8):
        b, t0 = cb(k)
        hp = np.zeros((D, TPC + 2), dtype=bf)
        lo, hi = max(t0 - 1, 0), min(t0 + TPC + 1, L)
        hp[:, lo - (t0 - 1):hi - (t0 - 1)] = h2[b, lo:hi].T
        maps.append(dict(hT=hp, w_in=inp['hyena_w_in'][0], conv_w=inp['hyena_conv_w'][0].reshape(-1), conv_b=inp['hyena_conv_b'][0]))
    r = _run(build_S3a(), maps)
    z = np.stack([np.concatenate([r[b * 4 + j]['z'] for j in range(4)], 0) for b in range(2)])
    del r, h2
    NB = L // 128
    def lay(a, rev):
        t = a.reshape(2, NB, 128, 128).transpose(3, 2, 0, 1)
        if rev:
            t = t[:, ::-1]
        return np.ascontiguousarray(t.reshape(128, 128, 2 * NB))
    fbias = np.ascontiguousarray(np.stack([inp['hyena_f_b1'][0], inp['hyena_f_b2'][0], inp['hyena_f_b3'][0]], 1))
    ffreq = np.ascontiguousarray(inp['hyena_f_freq'][0].T)
    Jm = np.ascontiguousarray(np.eye(128, dtype=f32)[::-1])
    maps = []
    for k in range(8):
        ch0 = k * 128
        zT1, zT2, win1, win2 = hyena_consts(L, ch0, 128)
        wo = np.ascontiguousarray(inp['hyena_f_wout'][0].reshape(64, 4, 1024)[:, :, ch0:ch0 + 128]).reshape(64, 512)
        maps.append(dict(vrev=lay(z[:, :, 2048 + ch0:2048 + ch0 + 128], True), vnat=lay(z[:, :, 2048 + ch0:2048 + ch0 + 128], False),
                         x1nat=lay(z[:, :, ch0:ch0 + 128], False), x2rev=lay(z[:, :, 1024 + ch0:1024 + ch0 + 128], True),
                         zT1=zT1, zT2=zT2, win1=win1, win2=win2, f_w1=inp['hyena_f_w1'][0], f_w2=inp['hyena_f_w2'][0], f_w3=inp['hyena_f_w3'][0],
                         f_b=fbias, f_freq=ffreq, f_wout=wo, hbias=np.ascontiguousarray(inp['hyena_bias'][0][:, ch0:ch0 + 128]).reshape(-1), J=Jm))
    r = _run(build_S3b(L=L, NCH=128), maps)
    del z
    m2 = np.zeros((2, D, L), dtype=bf)
    for k in range(8):
        y = r[k]['y2rev'][:, ::-1]
        m2[:, k * 128:(k + 1) * 128, :] = y.reshape(128, 128, 2, NB).transpose(2, 0, 3, 1).reshape(2, 128, L)
    del r
    maps = []
    for k in range(8):
        b, t0 = cb(k)
        maps.append(dict(ident=ident, x=x2[b, t0:t0 + TPC], c=_col8(inp['c'][b]),
                         ada_w=inp['ada_w'][2], ada_b=inp['ada_b'][2], ng=inp['norm_g'][2].reshape(-1),
                         ada_wn=inp['ada_w'][3], ada_bn=inp['ada_b'][3], ngn=inp['norm_g'][3].reshape(-1),
                         w_out=inp['hyena_w_out'][0], wg=inp['ffn_w_gate'][1], wu=inp['ffn_w_up'][1], wd=inp['ffn_w_down'][1],
                         mf=np.ascontiguousarray(m2[b][:, t0:t0 + TPC])))
    r = _run(build_TOK2("S4"), maps)
    x3 = np.stack([np.concatenate([r[b * 4 + j]['xo'] for j in range(4)], 0) for b in range(2)])
    h3 = np.stack([np.concatenate([r[b * 4 + j]['ho'] for j in range(4)], 0) for b in range(2)])
    dbg['x2'] = x3
    del r, m2, x2
    HW = 8
    maps = []
    for k in range(8):
        b, t0 = cb(k)
        hp = np.zeros((D, TPC + 2 * HW), dtype=bf)
        lo, hi = max(t0 - HW, 0), min(t0 + TPC + HW, L)
        hp[:, lo - (t0 - HW):hi - (t0 - HW)] = h3[b, lo:hi].T
        fix = np.ones((4, 16), f32)
        for wi_, win in enumerate((2, 4, 8, 16)):
            for j in range(8):
                if k % 4 == 0:
                    t = j
                    fix[wi_, j] = win / float(min(t + win // 2, L) - max(t - win // 2, 0))
                if k % 4 == 3:
                    t = L - 8 + j
                    fix[wi_, 8 + j] = win / float(min(t + win // 2, L) - max(t - win // 2, 0))
        maps.append(dict(ident=ident, x=x3[b, t0:t0 + TPC], c=_col8(inp['c'][b]),
                         ada_w=inp['ada_w'][3], ada_b=inp['ada_b'][3], ng=inp['norm_g'][3].reshape(-1),
                         w_out=inp['pool_w_out'][0], w_router=wr_lay(inp['moe_w_router'][1]),
                         wg=moe_w[1][0], wu=moe_w[1][1], wd=moe_w[1][2], Utri=Utri, blkrow=blkrow, rowc=rowc,
                         h3T=hp, p_win=inp['pool_w_in'][0], p_wg=inp['pool_w_g'][0], p_bg=_col8(inp['pool_b_g'][0].reshape(-1)),
                         p_scale=_col8(inp['pool_scale'][0]), p_fix=np.ascontiguousarray(np.broadcast_to(fix.reshape(1, 64), (128, 64)))))
    r = _run(build_TOK2("S5"), maps)
    out = np.stack([np.concatenate([r[b * 4 + j]['xo'] for j in range(4)], 0) for b in range(2)]).astype(f32)
    return out
```

```python
import os, math
import ml_dtypes
import numpy as np
from contextlib import ExitStack
import concourse.bass as bass
import concourse.mybir as mybir
from concourse.bass_utils import run_bass_kernel_spmd

F32 = mybir.dt.float32
BF16 = mybir.dt.bfloat16
I32 = mybir.dt.int32
AF = mybir.ActivationFunctionType
ALU = mybir.AluOpType
AX = mybir.AxisListType

COMPUTE = ("tensor", "vector", "scalar", "gpsimd")
DMAQ = ("sync", "gpsimd")
SAME_ENGINE_SYNC = True


class Prog:
    def __init__(self, name="k"):
        self.nc = bass.Bass("TRN2", target_bir_lowering=False)
        self.es = ExitStack()
        self.ops = []
        self.last_w = {}
        self.readers = {}
        self.n_dma_sems = {"sync": 24, "gpsimd": 24, "scalar": 16}
        self.names = set()

    def dram(self, name, shape, dt, kind):
        return self.nc.dram_tensor(name, list(shape), dt, kind=kind).ap()

    def sb(self, name, shape, dt):
        assert name not in self.names, name
        self.names.add(name)
        return self.es.enter_context(self.nc.sbuf_tensor(name, list(shape), dt))

    def ps(self, name, shape, dt):
        assert name not in self.names, name
        self.names.add(name)
        return self.es.enter_context(self.nc.psum_tensor(name, list(shape), dt))

    def op(self, eng, fn, r=(), w=(), dma=False):
        i = len(self.ops)
        deps = set()
        for k in r:
            if k in self.last_w:
                deps.add(self.last_w[k])
        for k in w:
            if k in self.last_w:
                deps.add(self.last_w[k])
            for j in self.readers.get(k, ()):
                deps.add(j)
        deps.discard(i)
        self.ops.append(dict(eng=eng, fn=fn, deps=sorted(deps), dma=dma))
        for k in r:
            self.readers.setdefault(k, []).append(i)
        for k in w:
            self.last_w[k] = i
            self.readers[k] = []
        return i

    def dma(self, q, out, in_, r=(), w=(), **kw):
        return self.op(q, lambda e: e.dma_start(out=out, in_=(in_() if callable(in_) else in_), **kw), r, w, dma=True)

    def emit(self):
        nc = self.nc
        ops = self.ops
        es = self.es
        eng_ops = {}
        for i, o in enumerate(ops):
            lst = eng_ops.setdefault(o["eng"], [])
            o["pos"] = len(lst)
            lst.append(i)
        dcount = {}
        for i, o in enumerate(ops):
            if o["dma"]:
                q = o["eng"]
                o["dnum"] = dcount.get(q, 0)
                dcount[q] = o["dnum"] + 1
        seen = {e: {} for e in eng_ops}
        seen_dma = {e: set() for e in eng_ops}
        for i, o in enumerate(ops):
            X = o["eng"]
            waits = []
            best = {}
            for j in o["deps"]:
                p = ops[j]
                if p["dma"]:
                    if j in seen_dma[X]:
                        continue
                    seen_dma[X].add(j)
                    waits.append(j)
                else:
                    E = p["eng"]
                    if E == X and (E == "tensor" or not SAME_ENGINE_SYNC):
                        continue
                    if E not in best or ops[best[E]]["pos"] < p["pos"]:
                        best[E] = j
            for E, j in best.items():
                p = ops[j]
                if seen[X].get(E, -1) >= p["pos"]:
                    continue
                seen[X][E] = p["pos"]
                waits.append(j)
            o["waits"] = waits
            for j in waits:
                ops[j]["sig"] = True
        sigcount = {}
        for i, o in enumerate(ops):
            if o["dma"]:
                continue
            if o.get("sig"):
                E = o["eng"]
                sigcount[E] = sigcount.get(E, 0) + 1
                o["signum"] = sigcount[E]
        csem = {e: es.enter_context(nc.semaphore("c_" + e)) for e in COMPUTE}
        dsem = {q: [es.enter_context(nc.semaphore("d_%s_%d" % (q, k))) for k in range(self.n_dma_sems[q])]
                for q in dcount}
        self.stats = {e: len(v) for e, v in eng_ops.items()}
        self.stats["sig"] = dict(sigcount)

        def waitspec(j):
            p = ops[j]
            if p["dma"]:
                K = self.n_dma_sems[p["eng"]]
                return dsem[p["eng"]][p["dnum"] % K], 16 * (p["dnum"] // K + 1)
            return csem[p["eng"]], p["signum"]

        def run_engine(ename, e):
            for i in eng_ops.get(ename, []):
                o = ops[i]
                for j in o["waits"]:
                    s, v = waitspec(j)
                    e.wait_ge(s, v)
                if o["dma"]:
                    K = self.n_dma_sems[ename]
                    m = o["dnum"]
                    if m >= K:
                        e.wait_ge(dsem[ename][m % K], 16 * (m // K))
                    ins = o["fn"](e)
                    ins.then_inc(dsem[ename][m % K], 16)
                else:
                    ins = o["fn"](e)
                    if o.get("sig"):
                        ins.then_inc(csem[ename], 1)
            if ename in dsem:
                K = self.n_dma_sems[ename]
                n = dcount[ename]
                for k in range(min(K, n)):
                    cnt = (n - 1 - k) // K + 1
                    e.wait_ge(dsem[ename][k], 16 * cnt)

        with nc.Block() as block:
            @block.sync
            def _(e):
                run_engine("sync", e)

            @block.tensor
            def _(e):
                run_engine("tensor", e)

            @block.vector
            def _(e):
                run_engine("vector", e)

            @block.scalar
            def _(e):
                run_engine("scalar", e)

            @block.gpsimd
            def _(e):
                run_engine("gpsimd", e)
        self.es.close()
        return nc
D = 1024
EPS = 1e-6


class Tok(Prog):
    def __init__(self, TB, nwsl=3):
        super().__init__()
        self.nwsl = nwsl
        self.TB = TB
        self.NT = TB // 128
        self.ident_d = self.dram("ident", [128, 128], F32, "ExternalInput")
        self.ident = self.sb("ident_sb", [128, 128], BF16)
        self.dma("gpsimd", self.ident[:], self.ident_d, w=["ident"])
        self.ones = self.sb("ones", [1, 128], F32)
        self.op("vector", lambda e: e.memset(self.ones[:], 1.0), w=["ones"])
        self.psA = [self.ps("psA%d" % i, [128, 512], F32) for i in range(3)]
        self.psY = [self.ps("psY%d" % i, [128, 1024], F32) for i in range(2)]
        self.psT = self.ps("psT", [128, 1024], BF16)
        self.wsl = [self.sb("wsl%d" % i, [128, 8192], BF16) for i in range(nwsl)]
        self.wi = 0
        self.ai = 0
        self.yi = 0
        self.tmpf = [self.sb("tmpf%d" % i, [128, 1024], F32) for i in range(2)]
        self.ti = 0
        self.hbf = [self.sb("hbf%d" % i, [128, 1024], BF16) for i in range(2)]
        self.hi = 0
        self.stat = self.sb("stat", [128, 64], F32)
        self.si = 0
        self.nrow = 1 if nwsl == 2 else 2
        self.rowbuf = self.sb("rowbuf", [1, 1024 * self.nrow], F32)
        self.si2 = 0
        self.csb = self.sb("csb", [128, 8], F32)
        self.csl = self.sb("csl", [128, 8], BF16)
        self.junk = self.sb("junk", [128, 1024], BF16)

    def nw(self):
        i = self.wi % self.nwsl
        self.wi += 1
        return self.wsl[i], "wsl%d" % i

    def nA(self):
        i = self.ai % 3
        self.ai += 1
        return self.psA[i], "psA%d" % i

    def nY(self):
        i = self.yi % 2
        self.yi += 1
        return self.psY[i], "psY%d" % i

    def ntmp(self):
        i = self.ti % 2
        self.ti += 1
        return self.tmpf[i], "tmpf%d" % i

    def nh(self):
        i = self.hi % 2
        self.hi += 1
        return self.hbf[i], "hbf%d" % i

    def nstat(self, n=1):
        if self.si + n > 64:
            self.si = 0
        c = self.si
        self.si += n
        return c

    def load_w(self, src, a, b):
        t, key = self.nw()
        dst = t[:, 0:a * b].rearrange("p (a b) -> p a b", a=a)
        self.dma("gpsimd", dst, src, w=[key])
        return dst, key

    def load_w_ind(self, Wrows, idx, ikey, a, b, elem_off):
        t, key = self.nw()
        dst = t[:, 0:a * b].rearrange("p (a b) -> p a b", a=a)
        for kc in range(a):
            self.op("gpsimd", lambda e, kc=kc: e.indirect_dma_start(out=dst[:, kc, :], out_offset=None, in_=Wrows, in_offset=bass.IndirectOffsetOnAxis(ap=idx[:, kc:kc + 1], axis=0), element_offset=elem_off),
                    r=[ikey], w=[key], dma=True)
        return dst, key

    def ffn_sp(self, nt, c, b, wg_l, wu_l, wd_l, IH):
        ntok = nt * 128
        hT, Y2 = c["hT"], c["Y2"]
        WS = c["WS"]
        first = True
        for q in range(4):
            wts = []
            for Wl in (wg_l, wu_l, wd_l):
                i = c["wsi"][0] % len(WS)
                c["wsi"][0] += 1
                wt, wk = WS[i], "WS%d" % i
                self.op("gpsimd", lambda e, wt=wt, Wl=Wl, q=q: e.indirect_dma_start(out=wt, out_offset=None, in_=Wl[0],
                                                                               in_offset=bass.IndirectOffsetOnAxis(ap=IH[:, b, q:q + 1], axis=0)),
                        r=["IH", "SLI", "EIDI", Wl[1]], w=[wk], dma=True)
                wts.append((wt, wk))
            (wtg, wgk), (wtu, wuk), (wtd, wdk) = wts
            vg = wtg.rearrange("p (k f) -> p k f", k=8)
            vu = wtu.rearrange("p (k f) -> p k f", k=8)
            vd = wtd.rearrange("p (k n) -> p k n", k=7)
            for fbh in range(2):
                nch = (4, 3)[fbh]
                j0 = fbh * 4
                sl = c["fbi"][0] % 2
                c["fbi"][0] += 1
                sg, sgk = c["sg"][sl], "sg%d" % sl
                gT, gk = c["gT"][sl], "gT%d" % sl
                for j in range(nch):
                    pa, pk = self.nA()
                    for kc in range(8):
                        self.op("tensor", lambda e, kc=kc, j=j, pa=pa, vg=vg, j0=j0: e.matmul(pa[:, 0:ntok], lhsT=vg[:, kc, (j0 + j) * 128:(j0 + j + 1) * 128], rhs=hT[:, kc, 0:ntok], start=(kc == 0), stop=(kc == 7)),
                                r=[wgk, "hT"], w=[pk])
                    self.op("scalar", lambda e, j=j, pa=pa, sg=sg: e.activation(out=sg[:, j, 0:ntok], in_=pa[:, 0:ntok], func=AF.Silu), r=[pk], w=[sgk])
                for j in range(nch):
                    pa, pk = self.nA()
                    for kc in range(8):
                        self.op("tensor", lambda e, kc=kc, j=j, pa=pa, vu=vu, j0=j0: e.matmul(pa[:, 0:ntok], lhsT=vu[:, kc, (j0 + j) * 128:(j0 + j + 1) * 128], rhs=hT[:, kc, 0:ntok], start=(kc == 0), stop=(kc == 7)),
                                r=[wuk, "hT"], w=[pk])
                    self.op("vector", lambda e, j=j, pa=pa, sg=sg, gT=gT: e.tensor_tensor(out=gT[:, j, 0:ntok], in0=sg[:, j, 0:ntok], in1=pa[:, 0:ntok], op=ALU.mult), r=[pk, sgk], w=[gk])
                for ti in range(nt):
                    py, pyk = self.nY()
                    for hf in range(2):
                        for j in range(nch):
                            self.op("tensor", lambda e, j=j, hf=hf, ti=ti, py=py, gT=gT, vd=vd, j0=j0, nch=nch: e.matmul(py[:, hf * 512:(hf + 1) * 512], lhsT=gT[:, j, ti * 128:(ti + 1) * 128], rhs=vd[:, j0 + j, hf * 512:(hf + 1) * 512], start=(j == 0), stop=(j == nch - 1)),
                                    r=[wdk, gk], w=[pyk])
                    yk = "Y2_%d" % ti
                    if first:
                        self.op("scalar", lambda e, ti=ti, py=py: e.copy(out=Y2[:, ti, :], in_=py[:, :]), r=[pyk], w=[yk])
                    else:
                        self.op("vector", lambda e, ti=ti, py=py: e.tensor_tensor(out=Y2[:, ti, :], in0=py[:, :], in1=Y2[:, ti, :], op=ALU.add), r=[pyk, yk], w=[yk])
                first = False

    def bc_dram(self, dst, dkey, src1d, off, n, q="sync"):
        ap = bass.AP(tensor=src1d.tensor, offset=off, ap=[[0, 128], [1, n]])
        self.dma(q, dst, ap, w=[dkey])

    def ada_mod(self, pref, cvec, ada_w, ada_b, normg, segs):
        self.dma("sync", self.csb[:], cvec, w=["csb"])
        self.op("scalar", lambda e: e.activation(out=self.csl[:], in_=self.csb[:], func=AF.Silu), r=["csb"], w=["csl"])
        wv = ada_w.rearrange("(kc p) n -> p kc n", p=128)
        bv = ada_b.rearrange("(o n) -> o n", o=1)
        rb = self.rowbuf
        out = {}
        for (seg, name, kind, gidx) in segs:
            t = self.get_tile(pref + name)
            tk = pref + name
            for hb in range(2):
                nb = seg * 2 + hb
                sl = self.si2 % self.nrow
                self.si2 += 1
                rk = "row%d" % sl
                self.dma("sync", rb[0:1, sl * 1024 + 512:sl * 1024 + 1024], bv[0:1, nb * 512:(nb + 1) * 512], w=[rk + "b"])
                wt, wk = self.load_w(wv[:, :, nb * 512:(nb + 1) * 512], 8, 512)
                pa, pk = self.nA()
                for kc in range(8):
                    self.op("tensor", lambda e, kc=kc, wt=wt, pa=pa: e.matmul(pa[0:1, :], lhsT=self.csl[:, kc:kc + 1], rhs=wt[:, kc, :], start=(kc == 0), stop=(kc == 7)),
                            r=[wk, "csl"], w=[pk])
                self.op("vector", lambda e, sl=sl, pa=pa: e.tensor_tensor(out=rb[0:1, sl * 1024:sl * 1024 + 512], in0=pa[0:1, :], in1=rb[0:1, sl * 1024 + 512:sl * 1024 + 1024], op=ALU.add),
                        r=[pk, rk + "b"], w=[rk])
                pb, pbk = self.nA()
                self.op("tensor", lambda e, sl=sl, pb=pb: e.matmul(pb[:, :], lhsT=self.ones[0:1, :], rhs=rb[0:1, sl * 1024:sl * 1024 + 512], start=True, stop=True),
                        r=[rk, "ones"], w=[pbk])
                self.op("scalar", lambda e, hb=hb, pb=pb, t=t: e.copy(out=t[:, hb * 512:(hb + 1) * 512], in_=pb[:, :]), r=[pbk], w=[tk])
            if kind != "raw":
                tmp, tmk = self.ntmp()
                self.bc_dram(tmp[:], tmk, normg, gidx * D, D)
                if kind == "mul1p":
                    self.op("vector", lambda e, t=t, tmp=tmp: e.scalar_tensor_tensor(out=t[:], in0=t[:], scalar=1.0, in1=tmp[:], op0=ALU.add, op1=ALU.mult), r=[tk, tmk], w=[tk])
                else:
                    self.op("vector", lambda e, t=t, tmp=tmp: e.tensor_tensor(out=t[:], in0=t[:], in1=tmp[:], op=ALU.mult), r=[tk, tmk], w=[tk])
            out[name] = (t, tk)
        return out

    def get_tile(self, name, shape=None, dt=F32):
        if not hasattr(self, "_tiles"):
            self._tiles = {}
        if name not in self._tiles:
            self._tiles[name] = self.sb(name, shape or [128, D], dt)
        return self._tiles[name]

    def rstd_of(self, srcs, skeys):
        n = len(srcs)
        c = self.nstat(n + 2)
        st = self.stat
        self.op("vector", lambda e: e.memset(st[:, c:c + n], 0.0), w=["stat"])
        for i, s in enumerate(srcs):
            self.op("scalar", lambda e, s=s, i=i: e.activation(out=self.junk[:, 0:s.shape[-1]], in_=s, func=AF.Square, accum_out=st[:, c + i:c + i + 1]),
                    r=list(skeys), w=["junk", "stat"])
        if n == 2:
            self.op("vector", lambda e: e.tensor_tensor(out=st[:, c:c + 1], in0=st[:, c:c + 1], in1=st[:, c + 1:c + 2], op=ALU.add), r=["stat"], w=["stat"])
        self.op("scalar", lambda e: e.activation(out=st[:, c + n:c + n + 1], in_=st[:, c:c + 1], func=AF.Sqrt, scale=1.0 / D, bias=EPS), r=["stat"], w=["stat"])
        self.op("vector", lambda e: e.reciprocal(out=st[:, c + n + 1:c + n + 2], in_=st[:, c + n:c + n + 1]), r=["stat"], w=["stat"])
        return st[:, c + n + 1:c + n + 2]

    def norm_mod(self, xt, xkey, Gt, SHt, out_bf, okey):
        rs = self.rstd_of([xt], [xkey])
        tmp, tk = self.ntmp()
        self.op("vector", lambda e: e.scalar_tensor_tensor(out=tmp[:], in0=xt, scalar=rs, in1=Gt[0][:], op0=ALU.mult, op1=ALU.mult),
                r=[xkey, "stat", Gt[1]], w=[tk])
        self.op("gpsimd", lambda e: e.tensor_tensor(out=out_bf, in0=tmp[:], in1=SHt[0][:], op=ALU.add), r=[tk, SHt[1]], w=[okey])

    def transpose_to(self, src_bf, skey, dstT, dkey, tcol, nk=8):
        pv = self.psT[:, 0:nk * 128].rearrange("p (a b) -> p a b", a=nk)
        for kc in range(nk):
            self.op("tensor", lambda e, kc=kc: e.transpose(out=pv[:, kc, :], in_=src_bf[:, kc * 128:(kc + 1) * 128], identity=self.ident[:]),
                    r=[skey, "ident"], w=["psT"])
        self.op("vector", lambda e: e.tensor_copy(out=dstT[:, 0:nk, tcol:tcol + 128], in_=pv), r=["psT"], w=[dkey])

    def post_res(self, ypsum, ykey, xt, xkey, GTt):
        rs = self.rstd_of([ypsum[:, 0:512], ypsum[:, 512:1024]], [ykey])
        tmp, tk = self.ntmp()
        self.op("vector", lambda e: e.scalar_tensor_tensor(out=tmp[:], in0=ypsum[:, :], scalar=rs, in1=GTt[0][:], op0=ALU.mult, op1=ALU.mult),
                r=[ykey, "stat", GTt[1]], w=[tk])
        self.op("gpsimd", lambda e: e.tensor_tensor(out=xt, in0=xt, in1=tmp[:], op=ALU.add), r=[tk, xkey], w=[xkey])

    def post_res_sb(self, ysb, ykey, xt, xkey, GTt):
        rs = self.rstd_of([ysb], [ykey, "uT"])
        tmp, tk = self.ntmp()
        self.op("vector", lambda e: e.scalar_tensor_tensor(out=tmp[:], in0=ysb, scalar=rs, in1=GTt[0][:], op0=ALU.mult, op1=ALU.mult),
                r=[ykey, "stat", GTt[1]], w=[tk])
        self.op("gpsimd", lambda e: e.tensor_tensor(out=xt, in0=xt, in1=tmp[:], op=ALU.add), r=[tk, xkey], w=[xkey])

    def lin_fm(self, W, KCn, hT, hkey, t0, ntok, nchunks, evac, group=4):
        def wsrc(a, b_):
            if callable(W):
                return lambda: W().rearrange("(kc p) n -> p kc n", p=128)[:, :, a:b_]
            return W.rearrange("(kc p) n -> p kc n", p=128)[:, :, a:b_]
        cols_per_load = 8192 // KCn // 128 * 128
        cols_per_load = min(cols_per_load, 512)
        i = 0
        while i < len(nchunks):
            n0 = nchunks[i]
            run = [n0]
            while len(run) * 128 < cols_per_load and i + len(run) < len(nchunks) and nchunks[i + len(run)] == run[-1] + 1:
                run.append(run[-1] + 1)
            if isinstance(W, dict):
                wt, wk = self.load_w_ind(W["rows"], W["idx"], W["ikey"], KCn, 512, (n0 // 4) * 1024 * 512)
            else:
                wt, wk = self.load_w(wsrc(n0 * 128, (n0 + len(run)) * 128), KCn, len(run) * 128)
            for j, ncx in enumerate(run):
                pa, pk = self.nA()
                for kc in range(KCn):
                    self.op("tensor", lambda e, kc=kc, j=j, wt=wt, pa=pa: e.matmul(pa[:, 0:ntok], lhsT=wt[:, kc, j * 128:(j + 1) * 128], rhs=hT[:, kc, t0:t0 + ntok], start=(kc == 0), stop=(kc == KCn - 1)),
                            r=[wk, hkey], w=[pk])
                evac(ncx, pa[:, 0:ntok], pk)
            i += len(run)

    def gelu_evac(self, ps, pk, out, okey):
        n = ps.shape[-1]
        tmp, tk = self.ntmp()
        t = tmp[:, 0:n]
        self.op("scalar", lambda e: e.activation(out=t, in_=ps, func=AF.Square), r=[pk], w=[tk])
        self.op("vector", lambda e: e.tensor_scalar(out=t, in0=t, scalar1=0.044715, scalar2=1.0, op0=ALU.mult, op1=ALU.add), r=[tk], w=[tk])
        self.op("vector", lambda e: e.tensor_tensor(out=t, in0=t, in1=ps, op=ALU.mult), r=[tk, pk], w=[tk])
        self.op("scalar", lambda e: e.activation(out=t, in_=t, func=AF.Sigmoid, scale=1.5957691216057308), r=[tk], w=[tk])
        self.op("vector", lambda e: e.tensor_tensor(out=out, in0=t, in1=ps, op=ALU.mult), r=[tk, pk], w=[okey])

    def load_block(self, X, srcs, pos=None):
        for ti, s in enumerate(srcs):
            self.dma("sync", X[:, ti, :], s, w=["X%d" % ti])
            if pos is not None:
                tmp, tk = self.ntmp()
                self.dma("sync", tmp[:], pos[ti], w=[tk])
                self.op("gpsimd", lambda e, ti=ti, tmp=tmp: e.tensor_tensor(out=X[:, ti, :], in0=X[:, ti, :], in1=tmp[:], op=ALU.add), r=[tk, "X%d" % ti], w=["X%d" % ti])

    def pre_T(self, X, nt, Gt, SHt, hT, want32=None):
        for ti in range(nt):
            hb, hk = self.nh()
            if want32 is None:
                self.norm_mod(X[:, ti, :], "X%d" % ti, Gt, SHt, hb[:], hk)
            else:
                h32, h32k = want32
                self.norm_mod(X[:, ti, :], "X%d" % ti, Gt, SHt, h32[:, ti, :], h32k + str(ti))
                self.op("scalar", lambda e, ti=ti, hb=hb: e.copy(out=hb[:], in_=h32[:, ti, :]), r=[h32k + str(ti)], w=[hk])
            self.transpose_to(hb, hk, hT, "hT", ti * 128)

    def gmlp(self, X, nt, c, mods):
        ntok = nt * 128
        hT, uT, vg = c["hT"], c["uT"], c["vg"]
        w_in, w_out = c["w_in"], c["w_out"]
        def ev_u(ncx, ps, pk):
            self.gelu_evac(ps, pk, uT[:, ncx, 0:ntok], "uT")
        self.lin_fm(w_in, 8, hT, "hT", 0, ntok, list(range(16)), ev_u)
        wv = w_in.rearrange("(kc p) n -> p kc n", p=128)
        for cb in range(4):
            wt, wk = self.load_w(wv[:, :, 2048 + cb * 512:2048 + (cb + 1) * 512], 8, 512)
            for ti in range(nt):
                pa, pk = self.nA()
                for kc in range(8):
                    self.op("tensor", lambda e, kc=kc, ti=ti, wt=wt, pa=pa: e.matmul(pa[:, :], lhsT=hT[:, kc, ti * 128:(ti + 1) * 128], rhs=wt[:, kc, :], start=(kc == 0), stop=(kc == 7)),
                            r=[wk, "hT"], w=[pk])
                self.gelu_evac(pa[:, :], pk, vg[:, ti, cb * 512:(cb + 1) * 512], "vg%d" % ti)
        bst = c["bst"]
        wsT = c["wsT"]
        for ti in range(nt):
            for cb in range(4):
                self.op("vector", lambda e, ti=ti, cb=cb: e.bn_stats(out=bst[:, cb * 6:(cb + 1) * 6], in_=vg[:, ti, cb * 512:(cb + 1) * 512]), r=["vg%d" % ti], w=["bst"])
            self.op("vector", lambda e: e.bn_aggr(out=bst[:, 24:26], in_=bst[:, 0:24]), r=["bst"], w=["bst"])
            self.op("scalar", lambda e: e.activation(out=bst[:, 26:27], in_=bst[:, 25:26], func=AF.Sqrt, scale=1.0, bias=EPS), r=["bst"], w=["bst"])
            self.op("vector", lambda e: e.reciprocal(out=bst[:, 27:28], in_=bst[:, 26:27]), r=["bst"], w=["bst"])
            vn, vk = c["vn"][ti % 2], "vn%d" % (ti % 2)
            for hf in range(2):
                tmp, tk = self.ntmp()
                self.op("vector", lambda e, ti=ti, hf=hf, tmp=tmp: e.tensor_scalar(out=tmp[:], in0=vg[:, ti, hf * 1024:(hf + 1) * 1024], scalar1=bst[:, 24:25], scalar2=bst[:, 27:28], op0=ALU.subtract, op1=ALU.mult),
                        r=["vg%d" % ti, "bst"], w=[tk])
                self.op("gpsimd", lambda e, hf=hf, tmp=tmp, vn=vn: e.tensor_tensor(out=vn[:, hf * 1024:(hf + 1) * 1024], in0=tmp[:], in1=c["GV"][:, hf * 1024:(hf + 1) * 1024], op=ALU.mult),
                        r=[tk, "GV"], w=[vk])
            for q4 in range(4):
                pa, pk = self.nA()
                for j in range(4):
                    cc = q4 * 4 + j
                    self.op("tensor", lambda e, j=j, cc=cc, pa=pa, vn=vn: e.matmul(pa[:, j * 128:(j + 1) * 128], lhsT=vn[:, cc * 128:(cc + 1) * 128], rhs=wsT[:, cc // 2, :], start=True, stop=True),
                            r=[vk, "wsT"], w=[pk])
                tmp, tk = self.ntmp()
                self.op("vector", lambda e, q4=q4, pa=pa, tmp=tmp: e.tensor_tensor(out=tmp[:, 0:512], in0=pa[:, :], in1=c["BS"][:, q4 * 512:(q4 + 1) * 512], op=ALU.add),
                        r=[pk, "BS"], w=[tk])
                self.op("gpsimd", lambda e, q4=q4, ti=ti, tmp=tmp: e.tensor_tensor(out=uT[:, q4 * 4:(q4 + 1) * 4, ti * 128:(ti + 1) * 128], in0=tmp[:, 0:512].rearrange("p (a b) -> p a b", a=4),
                                                                                    in1=uT[:, q4 * 4:(q4 + 1) * 4, ti * 128:(ti + 1) * 128], op=ALU.mult),
                        r=[tk, "uT"], w=["uT"])
        wo = w_out.rearrange("(kc p) n -> p kc n", p=128)
        wts = [self.load_w(wo[:, :, hf * 512:(hf + 1) * 512], 16, 512) for hf in range(2)]
        for ti in range(nt):
            py, pyk = self.nY()
            for hf in range(2):
                wt, wk = wts[hf]
                for kc in range(16):
                    self.op("tensor", lambda e, kc=kc, hf=hf, ti=ti, wt=wt, py=py: e.matmul(py[:, hf * 512:(hf + 1) * 512], lhsT=uT[:, kc, ti * 128:(ti + 1) * 128], rhs=wt[:, kc, :], start=(kc == 0), stop=(kc == 15)),
                            r=[wk, "uT"], w=[pyk])
            self.post_res(py, pyk, X[:, ti, :], "X%d" % ti, mods["GT1"])

    def mixer_out(self, X, nt, mTs, mkeys, w_out, KCn, GT):
        wo = w_out.rearrange("(kc p) n -> p kc n", p=128)
        wts = [self.load_w(wo[:, :, hf * 512:(hf + 1) * 512], KCn, 512) for hf in range(2)]
        nm = len(mTs)
        for ti in range(nt):
            py, pyk = self.nY()
            for hf in range(2):
                wt, wk = wts[hf]
                idx = 0
                for mi in range(nm):
                    for kc in range(KCn):
                        self.op("tensor", lambda e, kc=kc, hf=hf, ti=ti, wt=wt, py=py, mi=mi, idx=idx: e.matmul(py[:, hf * 512:(hf + 1) * 512], lhsT=mTs[mi][:, kc, ti * 128:(ti + 1) * 128], rhs=wt[:, kc, :], start=(idx == 0), stop=(idx == nm * KCn - 1)),
                                r=[wk, mkeys[mi]], w=[pyk])
                        idx += 1
            self.post_res(py, pyk, X[:, ti, :], "X%d" % ti, GT)

    def ffn(self, X, nt, c, GT, wg, wu, wd, FF, comb=None, first=True):
        ntok = nt * 128
        hT, Y2 = c["hT"], c["Y2"]
        nfb = (FF + 511) // 512
        def wdsrc(a, b_):
            if callable(wd):
                return lambda: wd().rearrange("(kc p) n -> p kc n", p=128)[:, a:b_, :]
            return wd.rearrange("(kc p) n -> p kc n", p=128)[:, a:b_, :]
        for fb in range(nfb):
            nch = min(4, FF // 128 - fb * 4)
            sg, sgk = c["sg"][fb % 2], "sg%d" % (fb % 2)
            gT, gk = c["gT"][fb % 2], "gT%d" % (fb % 2)
            chunks = list(range(fb * 4, fb * 4 + nch))
            def ev_g(ncx, ps, pk, sg=sg, sgk=sgk, fb=fb):
                self.op("scalar", lambda e: e.activation(out=sg[:, ncx - fb * 4, 0:ntok], in_=ps, func=AF.Silu), r=[pk], w=[sgk])
            self.lin_fm(wg, 8, hT, "hT", 0, ntok, chunks, ev_g)
            def ev_u(ncx, ps, pk, sg=sg, sgk=sgk, gT=gT, gk=gk, fb=fb):
                self.op("vector", lambda e: e.tensor_tensor(out=gT[:, ncx - fb * 4, 0:ntok], in0=sg[:, ncx - fb * 4, 0:ntok], in1=ps, op=ALU.mult), r=[pk, sgk], w=[gk])
            self.lin_fm(wu, 8, hT, "hT", 0, ntok, chunks, ev_u)
            if isinstance(wd, dict):
                wt, wk = self.load_w_ind(wd["rows"], wd["idx"], wd["ikey"], nch, 1024, fb * 512 * 1024)
            else:
                wt, wk = self.load_w(wdsrc(fb * 4, fb * 4 + nch), nch, 1024)
            for ti in range(nt):
                py, pyk = self.nY()
                for hf in range(2):
                    for kc in range(nch):
                        self.op("tensor", lambda e, kc=kc, hf=hf, ti=ti, wt=wt, py=py, gT=gT: e.matmul(py[:, hf * 512:(hf + 1) * 512], lhsT=gT[:, kc, ti * 128:(ti + 1) * 128], rhs=wt[:, kc, hf * 512:(hf + 1) * 512], start=(kc == 0), stop=(kc == nch - 1)),
                                r=[wk, gk], w=[pyk])
                yk = "Y2_%d" % ti
                if comb is None:
                    if first and fb == 0:
                        self.op("scalar", lambda e, ti=ti, py=py: e.copy(out=Y2[:, ti, :], in_=py[:, :]), r=[pyk], w=[yk, "uT"])
                    else:
                        self.op("vector", lambda e, ti=ti, py=py: e.tensor_tensor(out=Y2[:, ti, :], in0=py[:, :], in1=Y2[:, ti, :], op=ALU.add), r=[pyk, yk], w=[yk])
                else:
                    cap, ck = comb
                    if first and fb == 0:
                        self.op("vector", lambda e, ti=ti, py=py: e.tensor_scalar(out=Y2[:, ti, :], in0=py[:, :], scalar1=cap(ti), scalar2=None, op0=ALU.mult), r=[pyk, ck], w=[yk, "uT"])
                    else:
                        self.op("vector", lambda e, ti=ti, py=py: e.scalar_tensor_tensor(out=Y2[:, ti, :], in0=py[:, :], scalar=cap(ti), in1=Y2[:, ti, :], op0=ALU.mult, op1=ALU.add), r=[pyk, yk, ck], w=[yk])

    def ffn_post(self, X, nt, c, GT):
        for ti in range(nt):
            self.post_res_sb(c["Y2"][:, ti, :], "Y2_%d" % ti, X[:, ti, :], "X%d" % ti, GT)

    def out_block(self, X, nt, xo_rows, ho_rows=None, Gn=None, SHn=None):
        for ti in range(nt):
            if ho_rows is not None:
                hb, hk = self.nh()
                self.norm_mod(X[:, ti, :], "X%d" % ti, Gn, SHn, hb[:], hk)
                self.dma("sync", ho_rows[ti], hb[:], r=[hk])
            if xo_rows is not None:
                self.dma("sync", xo_rows[ti], X[:, ti, :], r=["X%d" % ti])

def build_S0(n_lat_tiles=32, TB=512):
    P = Tok(TB)
    NT = P.NT
    NTOK = n_lat_tiles * 128
    dI = lambda n, s, dt=F32: P.dram(n, s, dt, "ExternalInput")
    dO = lambda n, s, dt=F32: P.dram(n, s, dt, "ExternalOutput")
    x_d, pos_d, ctx_d = dI("x", [NTOK, D]), dI("pos", [NTOK, D]), dI("ctx", [128, D])
    c_d, cc_d = dI("c", [128, 8]), dI("c_ctx", [128, 8])
    aw0, ab0, ng0 = dI("ada_w0", [D, 6 * D]), dI("ada_b0", [6 * D]), dI("ng0", [4 * D])
    aw1, ab1, ng1 = dI("ada_w1", [D, 6 * D]), dI("ada_b1", [6 * D]), dI("ng1", [4 * D])
    g_win, g_gv, g_wsT, g_bs, g_wout = dI("g_win", [D, 4096]), dI("g_gv", [2048]), dI("g_wsT", [128, 1024]), dI("g_bs", [2048]), dI("g_wout", [2048, D])
    f_wg, f_wu, f_wd = dI("f_wg", [D, 2816]), dI("f_wu", [D, 2816]), dI("f_wd", [2816, D])
    xo, ho = dO("xo", [NTOK, D]), dO("ho", [NTOK, D], BF16)
    xco, hco = dO("xco", [128, D]), dO("hco", [128, D], BF16)
    X = P.sb("X", [128, NT, D], F32)
    c = dict(hT=P.sb("hT", [128, 8, TB], BF16), uT=None, vg=P.sb("vg", [128, NT, 2048], BF16),
             vn=[P.sb("vn%d" % i, [128, 2048], BF16) for i in range(2)], bst=P.sb("bst", [128, 32], F32),
             Y2=None, sg=[P.sb("sg%d" % i, [128, 4, TB], BF16) for i in range(2)],
             gT=[P.sb("gT%d" % i, [128, 4, TB], BF16) for i in range(2)],
             w_in=g_win, w_out=g_wout)
    uY = P.sb("uY", [128, NT * D], F32)
    c["Y2"] = uY[:].rearrange("p (a b) -> p a b", a=NT)
    c["uT"] = uY[:].bitcast(BF16).rearrange("p (a b) -> p a b", a=16)
    c["GV"] = P.sb("GV", [128, 2048], F32)
    c["BS"] = P.sb("BS", [128, 2048], F32)
    P.bc_dram(c["GV"][:], "GV", g_gv, 0, 2048)
    P.bc_dram(c["BS"][:], "BS", g_bs, 0, 2048)
    wsT = P.sb("wsT", [128, 8, 128], BF16)
    P.dma("gpsimd", wsT[:].rearrange("p a b -> p (a b)"), g_wsT, w=["wsT"])
    c["wsT"] = wsT

    def run_pass(pref, cvec, blocks):
        m = P.ada_mod("m", cvec, aw0, ab0, ng0, [(0, "SH1", "raw", 0), (1, "G1", "mul1p", 0), (2, "GT1", "mul", 1),
                                                  (3, "SH2", "raw", 0), (4, "G2", "mul1p", 2), (5, "GT2", "mul", 3)])
        mn = P.ada_mod("n", cvec, aw1, ab1, ng1, [(0, "SH1", "raw", 0), (1, "G1", "mul1p", 0)])
        for (srcs, poss, xos, hos) in blocks:
            nt = len(srcs)
            P.load_block(X, srcs, poss)
            P.pre_T(X, nt, m["G1"], m["SH1"], c["hT"])
            P.gmlp(X, nt, c, m)
            P.pre_T(X, nt, m["G2"], m["SH2"], c["hT"])
            P.ffn(X, nt, c, m["GT2"], f_wg, f_wu, f_wd, 2816)
            P.ffn_post(X, nt, c, m["GT2"])
            P.out_block(X, nt, xos, hos, mn["G1"], mn["SH1"])

    rows = lambda d, t: d[t * 128:(t + 1) * 128, :]
    blocks = []
    for b in range(n_lat_tiles // NT):
        ts = list(range(b * NT, (b + 1) * NT))
        blocks.append(([rows(x_d, t) for t in ts], [rows(pos_d, t) for t in ts], [rows(xo, t) for t in ts], [rows(ho, t) for t in ts]))
    run_pass("l", c_d, blocks)
    run_pass("c", cc_d, [([ctx_d], None, [xco], [hco])])
    return P

U32 = mybir.dt.uint32


def tok_router2(P, g, h32t, h32k, c):
    hT32, wr32, lg, mx8, identf, M1, M2, PW = c["hT32"], c["wr32"], c["lg"], c["mx8"], c["identf"], c["M1"], c["M2"], c["PW"]
    py, pyk = P.nY()
    pv = py[:, :].rearrange("p (a b) -> p a b", a=8)
    for kc in range(8):
        P.op("tensor", lambda e, kc=kc, pv=pv: e.transpose(out=pv[:, kc, :], in_=h32t[:, kc * 128:(kc + 1) * 128], identity=identf[:]), r=[h32k, "identf"], w=[pyk])
    P.op("vector", lambda e, pv=pv: e.tensor_copy(out=hT32[:], in_=pv), r=[pyk], w=["hT32"])
    pa, pk = P.nA()
    for kc in range(8):
        P.op("tensor", lambda e, kc=kc, pa=pa: e.matmul(pa[:, 0:8], lhsT=hT32[:, kc, :], rhs=wr32[:, kc, :], start=(kc == 0), stop=(kc == 7)), r=["hT32", "wr32"], w=[pk])
    P.op("vector", lambda e, pa=pa: e.tensor_copy(out=lg[:, 0:8], in_=pa[:, 0:8]), r=[pk], w=["lg"])
    P.op("vector", lambda e: e.max(out=mx8[:, 0:8], in_=lg[:, 0:8]), r=["lg"], w=["mx8"])
    P.op("vector", lambda e: e.tensor_tensor(out=mx8[:, 8:9], in0=mx8[:, 0:1], in1=mx8[:, 1:2], op=ALU.subtract), r=["mx8"], w=["mx8"])
    P.op("scalar", lambda e: e.activation(out=PW[:, g, 0:1], in_=mx8[:, 8:9], func=AF.Sigmoid, scale=1.0), r=["mx8"], w=["PW"])
    P.op("scalar", lambda e: e.activation(out=PW[:, g, 1:2], in_=mx8[:, 8:9], func=AF.Sigmoid, scale=-1.0), r=["mx8"], w=["PW"])
    P.op("vector", lambda e: e.tensor_scalar(out=M1[:, g, :], in0=lg[:, 0:8], scalar1=mx8[:, 0:1], scalar2=None, op0=ALU.is_equal), r=["lg", "mx8"], w=["M1"])
    P.op("vector", lambda e: e.tensor_scalar(out=M2[:, g, :], in0=lg[:, 0:8], scalar1=mx8[:, 1:2], scalar2=None, op0=ALU.is_equal), r=["lg", "mx8"], w=["M2"])


def moe_slots(P, NG, NBLK, c):
    M1, M2 = c["M1"], c["M2"]
    W = NG * 8
    fl = lambda T: T[:].rearrange("p g e -> p (g e)")
    ao = [3 * 7168]
    def sbf(n, w_, dt=F32):
        sl = c["arena"][:, ao[0]:ao[0] + 2 * w_]
        ao[0] += 2 * w_
        assert ao[0] <= 4 * 7168
        return sl.bitcast(dt)
    maskv, RK, TT, INC, EXC, SLOT, TMP = [sbf(n, W) for n in ("maskv", "RK", "TT", "INC", "EXC", "SLOT", "TMPs")]
    onesM, onesr = sbf("onesM", 128), sbf("onesr", NG)
    PADF, ENDF, BASEF = sbf("PADF", 8), sbf("ENDF", 8), sbf("BASEF", 8)
    PADI = sbf("PADI", 8, I32)
    SLF = sbf("SLF", 2 * NG)
    eidf = sbf("eidf", NBLK)
    U, blk = c["U"], c["blk"]
    SLI, EIDI = c["SLI"], c["EIDI"]
    P.op("vector", lambda e: e.memset(onesM[:], 1.0), w=["onesM"])
    P.op("vector", lambda e: e.memset(onesr[:], 1.0), w=["onesr"])
    P.op("vector", lambda e: e.tensor_tensor(out=maskv[:], in0=fl(M1), in1=fl(M2), op=ALU.add), r=["M1", "M2"], w=["maskv"])
    pa, pk = P.nA()
    P.op("tensor", lambda e: e.matmul(pa[:, 0:W], lhsT=U[:], rhs=maskv[:], start=True, stop=True), r=["U", "maskv"], w=[pk])
    P.op("vector", lambda e: e.tensor_copy(out=RK[:], in_=pa[:, 0:W]), r=[pk], w=["RK"])
    pb, pbk = P.nA()
    P.op("tensor", lambda e: e.matmul(pb[:, 0:W], lhsT=onesM[:], rhs=maskv[:], start=True, stop=True), r=["onesM", "maskv"], w=[pbk])
    P.op("vector", lambda e: e.tensor_copy(out=TT[:], in_=pb[:, 0:W]), r=[pbk], w=["TT"])
    ev = lambda T, e_: T[:].rearrange("p (g e) -> p e g", e=8)[:, e_, :]
    for e_ in range(8):
        P.op("vector", lambda e, e_=e_: e.tensor_tensor_scan(out=ev(INC, e_), data0=onesr[:], data1=ev(TT, e_), initial=0.0, op0=ALU.mult, op1=ALU.add), r=["TT", "onesr"], w=["INC"])
    P.op("vector", lambda e: e.tensor_tensor(out=EXC[:], in0=INC[:], in1=TT[:], op=ALU.subtract), r=["INC", "TT"], w=["EXC"])
    P.op("vector", lambda e: e.tensor_scalar(out=PADF[:], in0=INC[:, (NG - 1) * 8:NG * 8], scalar1=511.0, scalar2=None, op0=ALU.add), r=["INC"], w=["PADF"])
    P.op("vector", lambda e: e.tensor_copy(out=PADI[:], in_=PADF[:]), r=["PADF"], w=["PADI"])
    P.op("vector", lambda e: e.tensor_scalar(out=PADI[:], in0=PADI[:], scalar1=9, scalar2=9, op0=ALU.arith_shift_right, op1=ALU.logical_shift_left), r=["PADI"], w=["PADI"])
    P.op("vector", lambda e: e.tensor_copy(out=PADF[:], in_=PADI[:]), r=["PADI"], w=["PADF"])
    P.op("vector", lambda e: e.tensor_tensor_scan(out=ENDF[:], data0=onesr[:, 0:8], data1=PADF[:], initial=0.0, op0=ALU.mult, op1=ALU.add), r=["PADF", "onesr"], w=["ENDF"])
    P.op("vector", lambda e: e.tensor_tensor(out=BASEF[:], in0=ENDF[:], in1=PADF[:], op=ALU.subtract), r=["ENDF", "PADF"], w=["BASEF"])
    for e_ in range(8):
        P.op("vector", lambda e, e_=e_: e.scalar_tensor_tensor(out=ev(SLOT, e_), in0=ev(RK, e_), scalar=BASEF[:, e_:e_ + 1], in1=ev(EXC, e_), op0=ALU.add, op1=ALU.add), r=["RK", "EXC", "BASEF"], w=["SLOT"])
    for k, M in enumerate((M1, M2)):
        P.op("vector", lambda e, M=M: e.tensor_tensor(out=TMP[:], in0=fl(M), in1=SLOT[:], op=ALU.mult), r=["M1", "M2", "SLOT"], w=["TMPs"])
        P.op("vector", lambda e, k=k: e.tensor_reduce(out=SLF[:, k * NG:(k + 1) * NG], in_=TMP[:].rearrange("p (g e) -> p g e", e=8), axis=AX.X, op=ALU.add), r=["TMPs"], w=["SLF"])
    P.op("vector", lambda e: e.tensor_copy(out=SLI[:].rearrange("p k g -> p (k g)"), in_=SLF[:]), r=["SLF"], w=["SLI"])
    P.op("vector", lambda e: e.memset(eidf[:], 0.0), w=["eidf"])
    for e_ in range(8):
        P.op("vector", lambda e, e_=e_: e.scalar_tensor_tensor(out=eidf[:], in0=blk[:], scalar=ENDF[:, e_:e_ + 1], in1=eidf[:], op0=ALU.is_ge, op1=ALU.add), r=["blk", "ENDF", "eidf"], w=["eidf"])
    P.op("vector", lambda e: e.tensor_scalar(out=eidf[:], in0=eidf[:], scalar1=7.0, scalar2=None, op0=ALU.min), r=["eidf"], w=["eidf"])
    IH, rowc = c["IH"], c["rowc"]
    IHf = sbf("IHf", NBLK * 4).rearrange("p (a b) -> p a b", b=4)
    eg = sbf("eg", NBLK)
    P.op("vector", lambda e: e.tensor_scalar(out=eg[:], in0=eidf[:], scalar1=512.0, scalar2=None, op0=ALU.mult), r=["eidf"], w=["eg"])
    for b in range(NBLK):
        P.op("vector", lambda e, b=b: e.tensor_scalar(out=IHf[:, b, :], in0=rowc[:, 0:4], scalar1=eg[:, b:b + 1], scalar2=None, op0=ALU.add), r=["eg", "rowc"], w=["IHf"])
    P.op("vector", lambda e: e.tensor_copy(out=IH[:].rearrange("p a b -> p (a b)"), in_=IHf[:].rearrange("p a b -> p (a b)")), r=["IHf"], w=["IH"])


def moe_sparse(P, X, n_tiles, c, m, wg_d, wu_d, wd_d, FF, Xd, Hd, finish_tile):
    NG = n_tiles
    NTOK = NG * 128
    NBLK = (2 * NTOK + 8 * 511) // 512
    NS = NBLK * 512
    Hs = P.dram("Hs", [NS, D], BF16, "Internal")
    Ys = P.dram("Ys", [NS, D], F32, "Internal")
    SLI, EIDI = c["SLI"], c["EIDI"]
    moe_slots(P, NG, NBLK, c)
    for g in range(NG):
        hb, hk = P.nh()
        P.dma("sync", hb[:], Hd[g * 128:(g + 1) * 128, :], r=["Hd"], w=[hk])
        for k in range(2):
            P.op("gpsimd", lambda e, g=g, k=k, hb=hb: e.indirect_dma_start(out=Hs, out_offset=bass.IndirectOffsetOnAxis(ap=SLI[:, k, g:g + 1], axis=0), in_=hb[:], in_offset=None),
                 r=[hk, "SLI"], w=["Hs"], dma=True)
    NT = P.NT
    for b in range(NBLK):
        for ti in range(NT):
            hb, hk = P.nh()
            P.dma("sync", hb[:], Hs[(b * NT + ti) * 128:(b * NT + ti + 1) * 128, :], r=["Hs"], w=[hk])
            P.transpose_to(hb, hk, c["hT"], "hT", ti * 128)
        P.ffn_sp(NT, c, b, c["Wb"][0], c["Wb"][1], c["Wb"][2], c["IH"])
        for ti in range(NT):
            P.dma("sync", Ys[(b * NT + ti) * 128:(b * NT + ti + 1) * 128, :], c["Y2"][:, ti, :], r=["Y2_%d" % ti], w=["Ys"])
    YG = c["YG"]
    for g in range(NG):
        ti = g % NT
        P.dma("sync", X[:, ti, :], Xd[g * 128:(g + 1) * 128, :], r=["Xd"], w=["X%d" % ti])
        ys = []
        for k in range(2):
            yt, yk = YG[k], "YG%d" % k
            P.op("gpsimd", lambda e, g=g, k=k, yt=yt: e.indirect_dma_start(out=yt[:], out_offset=None, in_=Ys, in_offset=bass.IndirectOffsetOnAxis(ap=SLI[:, k, g:g + 1], axis=0)),
                 r=["Ys", "SLI"], w=[yk], dma=True)
            ys.append((yt, yk))
        (y1, y1k), (y2, y2k) = ys
        P.op("vector", lambda e, g=g, y1=y1: e.tensor_scalar(out=y1[:], in0=y1[:], scalar1=c["PW"][:, g, 0:1], scalar2=None, op0=ALU.mult), r=[y1k, "PW"], w=[y1k])
        P.op("vector", lambda e, g=g, y1=y1, y2=y2: e.scalar_tensor_tensor(out=y1[:], in0=y2[:], scalar=c["PW"][:, g, 1:2], in1=y1[:], op0=ALU.mult, op1=ALU.add), r=[y1k, y2k, "PW"], w=[y1k])
        P.post_res_sb(y1[:], y1k, X[:, ti, :], "X%d" % ti, m["GT2"])
        finish_tile(g, ti)

def tok_router(P, nt, c):
    h32, hT32, wr32, lg, mx8, comb, identf = c["h32"], c["hT32"], c["wr32"], c["lg"], c["mx8"], c["comb"], c["identf"]
    for ti in range(nt):
        py, pyk = P.nY()
        pv = py[:, :].rearrange("p (a b) -> p a b", a=8)
        for kc in range(8):
            P.op("tensor", lambda e, kc=kc, ti=ti, pv=pv: e.transpose(out=pv[:, kc, :], in_=h32[:, ti, kc * 128:(kc + 1) * 128], identity=identf[:]),
                 r=["h32_%d" % ti, "identf"], w=[pyk])
        P.op("vector", lambda e, pv=pv: e.tensor_copy(out=hT32[:], in_=pv), r=[pyk], w=["hT32"])
        pa, pk = P.nA()
        for kc in range(8):
            P.op("tensor", lambda e, kc=kc, pa=pa: e.matmul(pa[:, 0:8], lhsT=hT32[:, kc, :], rhs=wr32[:, kc, :], start=(kc == 0), stop=(kc == 7)),
                 r=["hT32", "wr32"], w=[pk])
        P.op("vector", lambda e, pa=pa: e.tensor_copy(out=lg[:, 0:8], in_=pa[:, 0:8]), r=[pk], w=["lg"])
        P.op("vector", lambda e: e.max(out=mx8[:, 0:8], in_=lg[:, 0:8]), r=["lg"], w=["mx8"])
        P.op("vector", lambda e: e.tensor_tensor(out=mx8[:, 8:9], in0=mx8[:, 0:1], in1=mx8[:, 1:2], op=ALU.subtract), r=["mx8"], w=["mx8"])
        P.op("scalar", lambda e: e.activation(out=mx8[:, 9:10], in_=mx8[:, 8:9], func=AF.Sigmoid, scale=1.0), r=["mx8"], w=["mx8"])
        P.op("scalar", lambda e: e.activation(out=mx8[:, 10:11], in_=mx8[:, 8:9], func=AF.Sigmoid, scale=-1.0), r=["mx8"], w=["mx8"])
        P.op("vector", lambda e: e.tensor_scalar(out=lg[:, 8:16], in0=lg[:, 0:8], scalar1=mx8[:, 0:1], scalar2=mx8[:, 9:10], op0=ALU.is_equal, op1=ALU.mult), r=["lg", "mx8"], w=["lg"])
        P.op("vector", lambda e: e.tensor_scalar(out=lg[:, 16:24], in0=lg[:, 0:8], scalar1=mx8[:, 1:2], scalar2=mx8[:, 10:11], op0=ALU.is_equal, op1=ALU.mult), r=["lg", "mx8"], w=["lg"])
        P.op("vector", lambda e, ti=ti: e.tensor_tensor(out=comb[:, ti, :], in0=lg[:, 8:16], in1=lg[:, 16:24], op=ALU.add), r=["lg"], w=["comb"])


def build_TOK2(kind, n_tiles=32, TB=512, sparse=True):
    P = Tok(TB, nwsl=(2 if (sparse and kind in ("S2", "S5")) else 3))
    NT = P.NT
    NTOK = n_tiles * 128
    dI = lambda n, s, dt=F32: P.dram(n, s, dt, "ExternalInput")
    dO = lambda n, s, dt=F32: P.dram(n, s, dt, "ExternalOutput")
    moe = kind in ("S2", "S5")
    sparse = sparse and moe
    has_next = kind in ("S2", "S4")
    x_d = dI("x", [NTOK, D])
    c_d = dI("c", [128, 8])
    aw, ab, ng = dI("ada_w", [D, 6 * D]), dI("ada_b", [6 * D]), dI("ng", [4 * D])
    if has_next:
        awn, abn, ngn = dI("ada_wn", [D, 6 * D]), dI("ada_bn", [6 * D]), dI("ngn", [4 * D])
        ho = dO("ho", [NTOK, D], BF16)
    xo = dO("xo", [NTOK, D])
    w_out = dI("w_out", [D, D])
    FF = 3584 if moe else 2816
    if sparse:
        wr_d = dI("w_router", [128, 64])
        wg_d, wu_d, wd_d = dI("wg", [4096, 4, 1792]), dI("wu", [4096, 4, 1792]), dI("wd", [4096, 4, 1792])
    elif moe:
        wr_d = dI("w_router", [128, 64])
        wg_d, wu_d, wd_d = dI("wg", [8, D, FF]), dI("wu", [8, D, FF]), dI("wd", [8, FF, D])
    else:
        wg_d, wu_d, wd_d = dI("wg", [D, FF]), dI("wu", [D, FF]), dI("wd", [FF, D])
    X = P.sb("X", [128, NT, D], F32)
    c = dict(hT=P.sb("hT", [128, 8, TB], BF16), Y2=P.sb("Y2", [128, NT, D], F32),
             sg=[P.sb("sg%d" % i, [128, 4, TB], BF16) for i in range(2)],
             gT=[P.sb("gT%d" % i, [128, 4, TB], BF16) for i in range(2)])
    if sparse:
        NBLKS = (2 * NTOK + 8 * 511) // 512
        U_d, blk_d = dI("Utri", [128, 128]), dI("blkrow", [128, NBLKS])
        Xd, Hd = P.dram("Xd", [NTOK, D], F32, "Internal"), P.dram("Hd", [NTOK, D], BF16, "Internal")
        c["M1"], c["M2"] = P.sb("M1", [128, n_tiles, 8], F32), P.sb("M2", [128, n_tiles, 8], F32)
        c["PW"] = P.sb("PW", [128, n_tiles, 2], F32)
        c["SLI"], c["EIDI"] = P.sb("SLI", [128, 2, n_tiles], U32), P.sb("EIDI", [128, NBLKS], I32)
        c["IH"] = P.sb("IH", [128, NBLKS, 4], U32)
        arena = P.sb("arena", [128, 4 * 7168], BF16)
        c["WS"] = [arena[:, i * 7168:(i + 1) * 7168] for i in range(4)]
        c["arena"] = arena
        c["Wb"] = []
        for wi_, Wsrc in enumerate((wg_d, wu_d, wd_d)):
            Wb = P.dram("Wb%d" % wi_, [4096, 7168], BF16, "Internal")
            for r_ in range(32):
                P.dma("gpsimd", Wb[r_ * 128:(r_ + 1) * 128, :].rearrange("r (a b) -> r a b", a=4), Wsrc[r_ * 128:(r_ + 1) * 128], w=["Wb%d" % wi_])
            c["Wb"].append((Wb, "Wb%d" % wi_))
        c["wsi"], c["fbi"] = [0], [0]
        c["rowc"] = P.sb("rowc_s", [128, 8], F32)
        rowc_d = dI("rowc", [128, 8])
        P.dma("sync", c["rowc"][:], rowc_d, w=["rowc"])
        c["U"], c["blk"] = P.sb("U_s", [128, 128], F32), P.sb("blk_s", [128, NBLKS], F32)
        P.dma("sync", c["U"][:], U_d, w=["U"])
        P.dma("sync", c["blk"][:], blk_d, w=["blk"])
        c["YG"] = [P.sb("YG%d" % i, [128, D], F32) for i in range(2)]
        h32r = [P.sb("h32r%d" % i, [128, D], F32) for i in range(1)]
    if moe:
        c["h32"] = P.sb("h32", [128, NT, D], F32) if not sparse else None
        c["hT32"] = P.sb("hT32", [128, 8, 128], F32)
        c["wr32"] = P.sb("wr32", [128, 8, 8], F32)
        c["lg"] = P.sb("lg", [128, 24], F32)
        c["mx8"] = P.sb("mx8", [128, 16], F32)
        c["comb"] = P.sb("comb", [128, NT, 8], F32)
        c["identf"] = P.sb("identf", [128, 128], F32)
        P.dma("sync", c["identf"][:], P.ident_d, w=["identf"])
        P.dma("sync", c["wr32"][:].rearrange("p a b -> p (a b)"), wr_d, w=["wr32"])
    if kind == "S2":
        mf_d, mb_d = dI("mf", [D, NTOK], BF16), dI("mb", [D, NTOK], BF16)
        if sparse:
            mTs = [arena[:, i * 8 * TB:(i + 1) * 8 * TB].rearrange("p (a b) -> p a b", a=8) for i in range(2)]
        else:
            mTs = [P.sb("mT0", [128, 8, TB], BF16), P.sb("mT1", [128, 8, TB], BF16)]
        msrc = [mf_d, mb_d]
    elif kind == "S4":
        mf_d = dI("mf", [D, NTOK], BF16)
        mTs = [P.sb("mT0", [128, 8, TB], BF16)]
        msrc = [mf_d]
    else:
        HW = 8
        h3_d = dI("h3T", [D, NTOK + 2 * HW], BF16)
        pw_in = dI("p_win", [D, D])
        pwg_d = dI("p_wg", [4, 256, 256])
        pbg_d, psc_d = dI("p_bg", [128, 8]), dI("p_scale", [128, 8])
        pfix_d = dI("p_fix", [128, 64])
        if sparse:
            NHp = TB + 2 * HW
            o = [0]
            def carve(n_el, a=None, f32=False):
                sl = arena[:, o[0]:o[0] + n_el]
                o[0] += n_el
                if f32:
                    return sl.bitcast(F32)
                return sl.rearrange("p (a b) -> p a b", a=a) if a else sl
            hTh, qT, yT = carve(8 * NHp, 8), carve(8 * TB, 8), carve(8 * TB, 8)
            pT, sA, sB = carve(2 * NHp, f32=True), carve(2 * NHp, f32=True), carve(2 * NHp, f32=True)
        else:
            hTh = P.sb("hTh", [128, 8, TB + 2 * HW], BF16)
            pT = P.sb("pT", [128, TB + 2 * HW], F32)
            sA = P.sb("sA", [128, TB + 2 * HW], F32)
            sB = P.sb("sB", [128, TB + 2 * HW], F32)
            qT = P.sb("qT", [128, 8, TB], BF16)
            yT = P.sb("yT", [128, 8, TB], BF16)
        wgs = P.sb("wgs", [128, 4, 2, 256], BF16)
        P.dma("gpsimd", wgs[:].rearrange("p g c d -> p (g c) d"), pwg_d.rearrange("g (cc p) d -> p (g cc) d", p=128), w=["wgs"])
        pbg, psc, pfix = P.sb("pbg", [128, 8], F32), P.sb("psc", [128, 8], F32), P.sb("pfix", [128, 4, 16], F32)
        P.dma("sync", pbg[:], pbg_d, w=["pbg"])
        P.dma("sync", psc[:], psc_d, w=["psc"])
        P.dma("sync", pfix[:].rearrange("p a b -> p (a b)"), pfix_d, w=["pfix"])

    segs = [(2, "GT1", "mul", 1), (3, "SH2", "raw", 0), (4, "G2", "mul1p", 2), (5, "GT2", "mul", 3)]
    m = P.ada_mod("m", c_d, aw, ab, ng, segs)
    if has_next:
        mn = P.ada_mod("n", c_d, awn, abn, ngn, [(0, "SH1", "raw", 0), (1, "G1", "mul1p", 0)])
    rows = lambda d, t: d[t * 128:(t + 1) * 128, :]
    nblk = n_tiles // NT
    for b in range(nblk):
        ts = list(range(b * NT, (b + 1) * NT))
        nt = NT
        t0 = b * TB
        P.load_block(X, [rows(x_d, t) for t in ts], None)
        if kind in ("S2", "S4"):
            for mi, src in enumerate(msrc):
                P.dma("sync", mTs[mi][:], src.rearrange("(kc p) t -> p kc t", p=128)[:, :, t0:t0 + TB], w=["mT%d" % mi])
            P.mixer_out(X, nt, mTs, ["mT%d" % i for i in range(len(mTs))], w_out, 8, m["GT1"])
        else:
            NH = TB + 2 * HW
            P.dma("sync", hTh[:], h3_d.rearrange("(kc p) t -> p kc t", p=128)[:, :, t0:t0 + NH], w=["hTh"])
            wv = pw_in.rearrange("(kc p) n -> p kc n", p=128)
            for cg in range(2):
                wt, wk = P.load_w(wv[:, :, cg * 512:(cg + 1) * 512], 8, 512)
                for j in range(4):
                    cc = cg * 4 + j
                    win = (2, 4, 8, 16)[cc // 2]
                    for hf in range(2):
                        pa, pk = P.nA()
                        c0 = hf * (NH // 2)
                        for kc in range(8):
                            P.op("tensor", lambda e, kc=kc, j=j, wt=wt, pa=pa, c0=c0: e.matmul(pa[:, 0:NH // 2], lhsT=wt[:, kc, j * 128:(j + 1) * 128], rhs=hTh[:, kc, c0:c0 + NH // 2], start=(kc == 0), stop=(kc == 7)),
                                 r=[wk, "hTh"], w=[pk])
                        P.op("scalar", lambda e, pa=pa, c0=c0: e.copy(out=pT[:, c0:c0 + NH // 2], in_=pa[:, 0:NH // 2]), r=[pk], w=["pT"])
                    P.op("vector", lambda e: e.tensor_tensor(out=sA[:, 1:NH], in0=pT[:, 0:NH - 1], in1=pT[:, 1:NH], op=ALU.add), r=["pT", "sB"], w=["sA"])
                    cur, ck, oth, ok = sA, "sA", sB, "sB"
                    sh = 1
                    lo, hi = 1, NH
                    while sh * 2 < win:
                        l2, h2 = lo + sh, hi - sh
                        P.op("vector", lambda e, cur=cur, oth=oth, sh=sh, l2=l2, h2=h2: e.tensor_tensor(out=oth[:, l2:h2], in0=cur[:, l2 - sh:h2 - sh], in1=cur[:, l2 + sh:h2 + sh], op=ALU.add),
                             r=[ck], w=[ok])
                        cur, ck, oth, ok = oth, ok, cur, ck
                        lo, hi = l2, h2
                        sh *= 2
                    wi = cc // 2
                    P.op("vector", lambda e, cur=cur, win=win: e.tensor_scalar(out=cur[:, HW:HW + TB], in0=cur[:, HW:HW + TB], scalar1=1.0 / win, scalar2=None, op0=ALU.mult), r=[ck], w=[ck])
                    if b == 0:
                        P.op("vector", lambda e, cur=cur, wi=wi: e.tensor_tensor(out=cur[:, HW:HW + 8], in0=cur[:, HW:HW + 8], in1=pfix[:, wi, 0:8], op=ALU.mult), r=[ck, "pfix"], w=[ck])
                    if b == nblk - 1:
                        P.op("vector", lambda e, cur=cur, wi=wi: e.tensor_tensor(out=cur[:, HW + TB - 8:HW + TB], in0=cur[:, HW + TB - 8:HW + TB], in1=pfix[:, wi, 8:16], op=ALU.mult), r=[ck, "pfix"], w=[ck])
                    P.op("vector", lambda e, cur=cur, cc=cc: e.tensor_tensor(out=qT[:, cc, :], in0=cur[:, HW:HW + TB], in1=pT[:, HW:HW + TB], op=ALU.subtract), r=[ck, "pT"], w=["qT", "sA", "sB"])
            for cc in range(8):
                g = cc // 2
                pa, pk = P.nA()
                for ci in range(2):
                    P.op("tensor", lambda e, ci=ci, g=g, cc=cc, pa=pa: e.matmul(pa[:, :], lhsT=wgs[:, g, ci, (cc % 2) * 128:(cc % 2 + 1) * 128], rhs=qT[:, 2 * g + ci, :], start=(ci == 0), stop=(ci == 1)),
                         r=["wgs", "qT"], w=[pk])
                P.op("vector", lambda e, cc=cc, pa=pa: e.tensor_scalar(out=yT[:, cc, :], in0=pa[:, :], scalar1=pbg[:, cc:cc + 1], scalar2=psc[:, cc:cc + 1], op0=ALU.add, op1=ALU.mult), r=[pk, "pbg", "psc"], w=["yT"])
            P.mixer_out(X, nt, [yT], ["yT"], w_out, 8, m["GT1"])
        if sparse:
            for ti in range(nt):
                g = b * NT + ti
                h32t, h32k = h32r[0], "h32r0"
                P.norm_mod(X[:, ti, :], "X%d" % ti, m["G2"], m["SH2"], h32t[:], h32k)
                hb, hk = P.nh()
                P.op("scalar", lambda e, hb=hb, h32t=h32t: e.copy(out=hb[:], in_=h32t[:]), r=[h32k], w=[hk])
                P.dma("sync", Hd[g * 128:(g + 1) * 128, :], hb[:], r=[hk], w=["Hd"])
                tok_router2(P, g, h32t, h32k, c)
                P.dma("sync", Xd[g * 128:(g + 1) * 128, :], X[:, ti, :], r=["X%d" % ti], w=["Xd"])
            continue
        if moe:
            P.pre_T(X, nt, m["G2"], m["SH2"], c["hT"], want32=(c["h32"], "h32_"))
            tok_router(P, nt, c)
            for ex in range(8):
                comb = (lambda ti, ex=ex: c["comb"][:, ti, ex:ex + 1], "comb")
                P.ffn(X, nt, c, m["GT2"], wg_d[ex], wu_d[ex], wd_d[ex], FF, comb=comb, first=(ex == 0))
        else:
            P.pre_T(X, nt, m["G2"], m["SH2"], c["hT"])
            P.ffn(X, nt, c, m["GT2"], wg_d, wu_d, wd_d, FF)
        P.ffn_post(X, nt, c, m["GT2"])
        if has_next:
            P.out_block(X, nt, [rows(xo, t) for t in ts], [rows(ho, t) for t in ts], mn["G1"], mn["SH1"])
        else:
            P.out_block(X, nt, [rows(xo, t) for t in ts])
    if sparse:
        def finish_tile(g, ti):
            if has_next:
                hb, hk = P.nh()
                P.norm_mod(X[:, ti, :], "X%d" % ti, mn["G1"], mn["SH1"], hb[:], hk)
                P.dma("sync", rows(ho, g), hb[:], r=[hk])
            P.dma("sync", rows(xo, g), X[:, ti, :], r=["X%d" % ti])
        moe_sparse(P, X, n_tiles, c, m, wg_d, wu_d, wd_d, FF, Xd, Hd, finish_tile)
    return P

def build_S1(L=16384, CTX=256):
    P = Prog()
    SEQP = 2 + CTX + 4 + L + 2
    dI = lambda n, s, dt=F32: P.dram(n, s, dt, "ExternalInput")
    hseq = dI("hseq", [4, D, SEQP], BF16)
    wg_d, wx_d = dI("w_gate", [D, 128]), dI("w_xb", [D, 128])
    cw_d, cb_d = dI("conv_w", [4 * 128]), dI("conv_b", [128, 1])
    wa_d, wxx_d = dI("w_a", [2, 128, 128]), dI("w_x", [2, 128, 128])
    ba_d, bx_d, lam_d = dI("b_a", [128, 2]), dI("b_x", [128, 2]), dI("lam", [128, 2])
    m_d = P.dram("m", [4, 128, L], BF16, "ExternalOutput")
    Wg = P.sb("Wg", [128, 8, 128], BF16)
    Wx = P.sb("Wx", [128, 8, 128], BF16)
    Wxk = P.sb("Wxk", [128, 8, 4, 128], BF16)
    cw = P.sb("cw", [128, 4, 128], F32)
    cb = P.sb("cb", [128, 1], F32)
    wa = P.sb("wa", [128, 2, 128], BF16)
    wxx = P.sb("wxx", [128, 2, 128], BF16)
    ba, bx, lam, cl = P.sb("ba", [128, 2], F32), P.sb("bx", [128, 2], F32), P.sb("lam_s", [128, 2], F32), P.sb("cl", [128, 2], F32)
    st = P.sb("st", [128, 1], F32)
    P.dma("gpsimd", Wg[:], wg_d.rearrange("(kc p) n -> p kc n", p=128), w=["Wg"])
    P.dma("gpsimd", Wx[:], wx_d.rearrange("(kc p) n -> p kc n", p=128), w=["Wx"])
    P.dma("sync", cw[:].rearrange("p a b -> p (a b)"), bass.AP(tensor=cw_d.tensor, offset=0, ap=[[0, 128], [1, 512]]), w=["cw"])
    P.dma("sync", cb[:], cb_d, w=["cb"])
    P.dma("gpsimd", wa[:], wa_d.rearrange("d i j -> i d j"), w=["wa"])
    P.dma("gpsimd", wxx[:], wxx_d.rearrange("d i j -> i d j"), w=["wxx"])
    P.dma("sync", ba[:], ba_d, w=["ba"])
    P.dma("sync", bx[:], bx_d, w=["bx"])
    P.dma("sync", lam[:], lam_d, w=["lam"])
    for k in range(4):
        for kc in range(8):
            P.op("vector", lambda e, k=k, kc=kc: e.tensor_tensor(out=Wxk[:, kc, k, :], in0=Wx[:, kc, :], in1=cw[:, k, :], op=ALU.mult), r=["Wx", "cw"], w=["Wxk"])
    P.op("scalar", lambda e: e.activation(out=cl[:], in_=lam[:], func=AF.Exp, scale=-1.0), r=["lam"], w=["cl"])
    P.op("scalar", lambda e: e.activation(out=cl[:], in_=cl[:], func=AF.Ln, scale=1.0, bias=1.0), r=["cl"], w=["cl"])
    P.op("vector", lambda e: e.tensor_scalar(out=cl[:], in0=cl[:], scalar1=-8.0, scalar2=None, op0=ALU.mult), r=["cl"], w=["cl"])
    ps = [P.ps("ps%d" % i, [128, 1024], F32) for i in range(4)]
    pi = [0]
    def nps():
        i = pi[0] % 4
        pi[0] += 1
        return ps[i], "ps%d" % i
    NB = 2
    hc = [P.sb("hc%d" % i, [128, 8, 1028], BF16) for i in range(NB)]
    bufs = {}
    for nm, dt in (("xc32", F32), ("xcb", BF16), ("r", F32), ("ii", F32), ("a", F32), ("sq", F32), ("bb", F32), ("hs", F32), ("t1", F32), ("mo", BF16)):
        bufs[nm] = [P.sb("%s%d" % (nm, i), [128, 1024], dt) for i in range(NB)]
    ci = 0
    for s in range(4):
        d = s // 2
        P.op("vector", lambda e: e.memset(st[:], 0.0), r=[], w=["st"])
        chunks = [(2, CTX, False, 0)] + [(2 + CTX + 4 + t0, min(1024, L - t0), True, t0) for t0 in range(0, L, 1024)]
        for (c0, n, is_lat, t0) in chunks:
            sl = ci % NB
            ci += 1
            B = {k: (v[sl], "%s%d" % (k, sl)) for k, v in bufs.items()}
            h, hk = hc[sl], "hc%d" % sl
            P.dma("sync", h[:, :, 0:n + 4], hseq[s].rearrange("(kc p) t -> p kc t", p=128)[:, :, c0 - 2:c0 + n + 2], w=[hk])
            px, pxk = nps()
            halves = [(h0, min(512, n - h0)) for h0 in range(0, n, 512)]
            for (h0, nn) in halves:
                idx = 0
                for k in range(4):
                    off = (k - 2) if d == 0 else (2 - k)
                    for kc in range(8):
                        P.op("tensor", lambda e, k=k, kc=kc, off=off, px=px, h=h, h0=h0, nn=nn, idx=idx: e.matmul(px[:, h0:h0 + nn], lhsT=Wxk[:, kc, k, :], rhs=h[:, kc, 2 + off + h0:2 + off + h0 + nn], start=(idx == 0), stop=(idx == 31)),
                             r=["Wxk", hk], w=[pxk])
                        idx += 1
            xc32, xk = B["xc32"]
            xcb, xbk = B["xcb"]
            P.op("scalar", lambda e, px=px, xc32=xc32, n=n: e.activation(out=xc32[:, 0:n], in_=px[:, 0:n], func=AF.Identity, bias=cb[:, 0:1], scale=1.0), r=[pxk, "cb"], w=[xk])
            P.op("vector", lambda e, xc32=xc32, xcb=xcb, n=n: e.tensor_copy(out=xcb[:, 0:n], in_=xc32[:, 0:n]), r=[xk], w=[xbk])
            pr, prk = nps()
            for (h0, nn) in halves:
                P.op("tensor", lambda e, pr=pr, xcb=xcb, h0=h0, nn=nn, d=d: e.matmul(pr[:, h0:h0 + nn], lhsT=wa[:, d, :], rhs=xcb[:, h0:h0 + nn], start=True, stop=True), r=["wa", xbk], w=[prk])
            pI, pik = nps()
            for (h0, nn) in halves:
                P.op("tensor", lambda e, pI=pI, xcb=xcb, h0=h0, nn=nn, d=d: e.matmul(pI[:, h0:h0 + nn], lhsT=wxx[:, d, :], rhs=xcb[:, h0:h0 + nn], start=True, stop=True), r=["wxx", xbk], w=[pik])
            r_, rk = B["r"]
            i_, ik = B["ii"]
            a_, ak = B["a"]
            sq, sqk = B["sq"]
            bb, bbk = B["bb"]
            hs, hsk = B["hs"]
            P.op("scalar", lambda e, pr=pr, r_=r_, n=n, d=d: e.activation(out=r_[:, 0:n], in_=pr[:, 0:n], func=AF.Sigmoid, bias=ba[:, d:d + 1], scale=1.0), r=[prk, "ba"], w=[rk])
            P.op("scalar", lambda e, pI=pI, i_=i_, n=n, d=d: e.activation(out=i_[:, 0:n], in_=pI[:, 0:n], func=AF.Sigmoid, bias=bx[:, d:d + 1], scale=1.0), r=[pik, "bx"], w=[ik])
            P.op("scalar", lambda e, r_=r_, a_=a_, n=n, d=d: e.activation(out=a_[:, 0:n], in_=r_[:, 0:n], func=AF.Exp, scale=cl[:, d:d + 1]), r=[rk, "cl"], w=[ak])
            P.op("scalar", lambda e, a_=a_, sq=sq, n=n: e.activation(out=sq[:, 0:n], in_=a_[:, 0:n], func=AF.Square), r=[ak], w=[sqk])
            P.op("scalar", lambda e, sq=sq, n=n: e.activation(out=sq[:, 0:n], in_=sq[:, 0:n], func=AF.Sqrt, scale=-1.0, bias=1.0), r=[sqk], w=[sqk])
            P.op("vector", lambda e, i_=i_, xc32=xc32, bb=bb, n=n: e.tensor_tensor(out=bb[:, 0:n], in0=i_[:, 0:n], in1=xc32[:, 0:n], op=ALU.mult), r=[ik, xk], w=[bbk])
            P.op("vector", lambda e, sq=sq, bb=bb, n=n: e.tensor_tensor(out=bb[:, 0:n], in0=bb[:, 0:n], in1=sq[:, 0:n], op=ALU.mult), r=[sqk, bbk], w=[bbk])
            P.op("vector", lambda e, a_=a_, bb=bb, hs=hs, n=n: e.tensor_tensor_scan(out=hs[:, 0:n], data0=a_[:, 0:n], data1=bb[:, 0:n], initial=st[:, 0:1], op0=ALU.mult, op1=ALU.add), r=[ak, bbk, "st"], w=[hsk])
            P.op("vector", lambda e, hs=hs, n=n: e.tensor_copy(out=st[:, 0:1], in_=hs[:, n - 1:n]), r=[hsk], w=["st"])
            if is_lat:
                pg, pgk = nps()
                for (h0, nn) in halves:
                    for kc in range(8):
                        P.op("tensor", lambda e, kc=kc, pg=pg, h=h, h0=h0, nn=nn: e.matmul(pg[:, h0:h0 + nn], lhsT=Wg[:, kc, :], rhs=h[:, kc, 2 + h0:2 + h0 + nn], start=(kc == 0), stop=(kc == 7)), r=["Wg", hk], w=[pgk])
                t1, t1k = B["t1"]
                mo, mok = B["mo"]
                g = pg[:, 0:n]
                t = t1[:, 0:n]
                P.op("scalar", lambda e, t=t, g=g: e.activation(out=t, in_=g, func=AF.Square), r=[pgk], w=[t1k])
                P.op("vector", lambda e, t=t: e.tensor_scalar(out=t, in0=t, scalar1=0.044715, scalar2=1.0, op0=ALU.mult, op1=ALU.add), r=[t1k], w=[t1k])
                P.op("vector", lambda e, t=t, g=g: e.tensor_tensor(out=t, in0=t, in1=g, op=ALU.mult), r=[t1k, pgk], w=[t1k])
                P.op("scalar", lambda e, t=t: e.activation(out=t, in_=t, func=AF.Sigmoid, scale=1.5957691216057308), r=[t1k], w=[t1k])
                P.op("vector", lambda e, t=t, g=g: e.tensor_tensor(out=t, in0=t, in1=g, op=ALU.mult), r=[t1k, pgk], w=[t1k])
                P.op("vector", lambda e, t=t, hs=hs, mo=mo, n=n: e.tensor_tensor(out=mo[:, 0:n], in0=t, in1=hs[:, 0:n], op=ALU.mult), r=[t1k, hsk], w=[mok])
                P.dma("sync", m_d[s][:, t0:t0 + n], mo[:, 0:n], r=[mok])
    return P

def build_S3a(n_tiles=32):
    P = Prog()
    NTOK = n_tiles * 128
    dI = lambda n, s, dt=F32: P.dram(n, s, dt, "ExternalInput")
    hT_d = dI("hT", [D, NTOK + 2], BF16)
    w_d, cw_d, cb_d = dI("w_in", [D, 3072]), dI("conv_w", [3 * 3072]), dI("conv_b", [3072])
    z_d = P.dram("z", [NTOK, 3072], F32, "ExternalOutput")
    hT = P.sb("hT_s", [128, 8, NTOK + 2], BF16)
    P.dma("sync", hT[:], hT_d.rearrange("(kc p) t -> p kc t", p=128), w=["hT"])
    W = [P.sb("W%d" % i, [128, 8, 512], BF16) for i in range(2)]
    Wk = [P.sb("Wk%d" % i, [128, 3, 8, 512], BF16) for i in range(2)]
    cwb = [P.sb("cwb%d" % i, [128, 3, 512], F32) for i in range(2)]
    bb = [P.sb("bb%d" % i, [128, 512], F32) for i in range(2)]
    zo = [P.sb("zo%d" % i, [128, 512], F32) for i in range(3)]
    ps = [P.ps("ps%d" % i, [128, 512], F32) for i in range(4)]
    wv = w_d.rearrange("(kc p) n -> p kc n", p=128)
    n = 0
    for cb in range(6):
        s = cb % 2
        P.dma("gpsimd", W[s][:], wv[:, :, cb * 512:(cb + 1) * 512], w=["W%d" % s])
        for k in range(3):
            P.dma("sync", cwb[s][:, k, :], bass.AP(tensor=cw_d.tensor, offset=k * 3072 + cb * 512, ap=[[0, 128], [1, 512]]), w=["cwb%d_%d" % (s, k)])
        P.dma("sync", bb[s][:], bass.AP(tensor=cb_d.tensor, offset=cb * 512, ap=[[0, 128], [1, 512]]), w=["bb%d" % s])
        for k in range(3):
            for kc in range(8):
                eng = "vector" if (kc % 2 == 0) else "gpsimd"
                P.op(eng, lambda e, s=s, k=k, kc=kc: e.tensor_tensor(out=Wk[s][:, k, kc, :], in0=W[s][:, kc, :], in1=cwb[s][:, k, :], op=ALU.mult),
                     r=["W%d" % s, "cwb%d_%d" % (s, k)], w=["Wk%d_%d" % (s, k)])
        for ti in range(n_tiles):
            pa, pk = ps[n % 4], "ps%d" % (n % 4)
            zt, zk = zo[n % 3], "zo%d" % (n % 3)
            n += 1
            idx = 0
            for k in range(3):
                for kc in range(8):
                    P.op("tensor", lambda e, s=s, k=k, kc=kc, ti=ti, pa=pa, idx=idx: e.matmul(pa[:, :], lhsT=hT[:, kc, ti * 128 + k:ti * 128 + k + 128], rhs=Wk[s][:, k, kc, :], start=(idx == 0), stop=(idx == 23)),
                         r=["hT", "Wk%d_%d" % (s, k)], w=[pk])
                    idx += 1
            P.op("vector", lambda e, pa=pa, zt=zt, s=s: e.tensor_tensor(out=zt[:], in0=pa[:, :], in1=bb[s][:], op=ALU.add), r=[pk, "bb%d" % s], w=[zk])
            P.dma("sync", z_d[ti * 128:(ti + 1) * 128, cb * 512:(cb + 1) * 512], zt[:], r=[zk])
    return P


def build_S3b(L=16384, NCH=128):
    P = Prog()
    NB = L // 128
    NC2 = 2 * NB
    L2 = 2 * L
    NBLK = L2 // 512
    dI = lambda n, s, dt=F32: P.dram(n, s, dt, "ExternalInput")
    vrev_d, vnat_d = dI("vrev", [NCH, 128, NC2]), dI("vnat", [NCH, 128, NC2])
    x1_d, x2_d = dI("x1nat", [NCH, 128, NC2]), dI("x2rev", [NCH, 128, NC2])
    zT_d = [dI("zT1", [33, L]), dI("zT2", [33, L])]
    win_d = [dI("win1", [128, L]), dI("win2", [128, L])]
    w1_d, w2_d, w3_d = dI("f_w1", [33, 64]), dI("f_w2", [64, 64]), dI("f_w3", [64, 64])
    fb_d, fr_d = dI("f_b", [64, 3]), dI("f_freq", [64, 3])
    wo_d = dI("f_wout", [64, 4 * 128])
    hb_d = dI("hbias", [2 * 128])
    J_d = dI("J", [128, 128])
    y2_d = P.dram("y2rev", [NCH, 128, NC2], BF16, "ExternalOutput")
    Ktab = [P.dram("Ktab%d" % i, [128, L2], BF16, "Internal") for i in range(2)]
    Y1d = P.dram("Y1d", [NCH, 128, NC2], BF16, "Internal")
    w1, w2, w3 = P.sb("w1", [33, 64], F32), P.sb("w2", [64, 64], F32), P.sb("w3", [64, 64], F32)
    fb, fr, wo = P.sb("fb", [64, 3], F32), P.sb("fr", [64, 3], F32), P.sb("wo", [64, 4, 128], F32)
    hbias = P.sb("hbias_s", [128, 2, 128], F32)
    J = P.sb("J_s", [128, 128], BF16)
    for t, d_, k in ((w1, w1_d, "w1"), (w2, w2_d, "w2"), (w3, w3_d, "w3"), (fb, fb_d, "fb"), (fr, fr_d, "fr")):
        P.dma("sync", t[:], d_, w=[k])
    P.dma("sync", wo[:].rearrange("p a b -> p (a b)"), wo_d, w=["wo"])
    P.dma("sync", hbias[:].rearrange("p a b -> p (a b)"), bass.AP(tensor=hb_d.tensor, offset=0, ap=[[0, 128], [1, 256]]), w=["hbias"])
    P.dma("gpsimd", J[:], J_d, w=["J"])
    ps = [P.ps("ps%d" % i, [128, 512], F32) for i in range(4)]
    pi = [0]
    def nps():
        i = pi[0] % 4
        pi[0] += 1
        return ps[i], "ps%d" % i
    GW = min(2048, L)
    NGR = L // GW
    NJ = GW // 512
    psF = P.ps("psF", [128, GW], F32)
    zt = P.sb("zt0", [33, GW], F32)
    wn = P.sb("wn0", [128, GW], F32)
    hd = P.sb("hd0", [64, GW], F32)
    wi = P.sb("wi0", [64, GW], I32)
    g1 = P.sb("g1_0", [64, GW], F32)
    kb = P.sb("kb0", [128, 2, GW], BF16)
    fr2 = P.sb("fr2", [64, 3], F32)
    TWO_PI = 6.283185307179586
    P.op("vector", lambda e: e.tensor_scalar(out=fr2[:], in0=fr[:], scalar1=1.0 / TWO_PI, scalar2=None, op0=ALU.mult), r=["fr"], w=["fr2"])
    for pas in range(2):
        for gr in range(NGR):
            m0 = gr * GW
            P.dma("sync", zt[:], zT_d[pas][:, m0:m0 + GW], w=["zt"])
            P.dma("sync", wn[:], win_d[pas][:, m0:m0 + GW], w=["wn"])
            src, skey, W_ = zt, "zt", [w1, w2, w3]
            for ly in range(3):
                nk = 33 if ly == 0 else 64
                for j in range(NJ):
                    P.op("tensor", lambda e, src=src, ly=ly, W_=W_, nk=nk, j=j: e.matmul(psF[0:64, j * 512:(j + 1) * 512], lhsT=W_[ly][0:nk, :], rhs=src[0:nk, j * 512:(j + 1) * 512], start=True, stop=True),
                         r=[skey, "w%d" % (ly + 1)], w=["psF"])
                h, hk = hd, "hd"
                P.op("vector", lambda e, h=h, ly=ly: e.tensor_scalar(out=h[:], in0=psF[0:64, :], scalar1=fb[:, ly:ly + 1], scalar2=fr2[:, ly:ly + 1], op0=ALU.add, op1=ALU.mult), r=["psF", "fb", "fr2"], w=[hk])
                P.op("vector", lambda e, h=h: e.tensor_copy(out=wi[:], in_=h[:]), r=[hk], w=["wi"])
                P.op("vector", lambda e: e.tensor_copy(out=g1[:], in_=wi[:]), r=["wi"], w=["g1"])
                P.op("vector", lambda e, h=h: e.tensor_tensor(out=h[:], in0=h[:], in1=g1[:], op=ALU.subtract), r=[hk, "g1"], w=[hk])
                P.op("vector", lambda e, h=h: e.tensor_scalar(out=g1[:], in0=h[:], scalar1=0.5, scalar2=None, op0=ALU.is_gt), r=[hk], w=["g1"])
                P.op("vector", lambda e, h=h: e.tensor_tensor(out=h[:], in0=h[:], in1=g1[:], op=ALU.subtract), r=[hk, "g1"], w=[hk])
                P.op("vector", lambda e, h=h: e.tensor_scalar(out=g1[:], in0=h[:], scalar1=-0.5, scalar2=None, op0=ALU.is_lt), r=[hk], w=["g1"])
                P.op("vector", lambda e, h=h: e.tensor_tensor(out=h[:], in0=h[:], in1=g1[:], op=ALU.add), r=[hk, "g1"], w=[hk])
                P.op("scalar", lambda e, h=h: e.activation(out=h[:], in_=h[:], func=AF.Sin, scale=TWO_PI), r=[hk], w=[hk])
                src, skey = h, hk
            for tab in range(2):
                dr = (1, 0)[tab] if pas == 0 else (0, 1)[tab]
                for j in range(NJ):
                    P.op("tensor", lambda e, src=src, tab=tab, dr=dr, j=j: e.matmul(psF[:, j * 512:(j + 1) * 512], lhsT=wo[:, tab * 2 + dr, :], rhs=src[:, j * 512:(j + 1) * 512], start=True, stop=True), r=[skey, "wo"], w=["psF"])
                P.op("vector", lambda e, tab=tab: e.tensor_tensor(out=kb[:, tab, :], in0=psF[:, :], in1=wn[:], op=ALU.mult), r=["psF", "wn"], w=["kb_%d" % tab])
                c0, c1 = 0, GW
                if pas == 0:
                    col0 = m0
                    if tab == 0 and gr == NGR - 1:
                        c1 = GW - 1
                else:
                    col0 = (L - 1) + m0
                    if tab == 1 and gr == 0:
                        c0 = 1
                P.dma("sync", Ktab[tab][:, col0 + c0:col0 + c1], kb[:, tab, c0:c1], r=["kb_%d" % tab], w=["Ktab%d" % tab])
    HALF = NB * 128
    T = [P.sb("T%d" % i, [128, HALF], BF16) for i in range(4)]
    vb = [P.sb("vb%d" % i, [128, NC2], BF16) for i in range(2)]
    gA = [P.sb("gA%d" % i, [128, NC2], F32) for i in range(2)]
    gB = [P.sb("gB%d" % i, [128, NC2], F32) for i in range(2)]
    tt = [P.sb("tt%d" % i, [128, NC2], F32) for i in range(2)]
    yo = [P.sb("yo%d" % i, [128, NC2], BF16) for i in range(2)]
    tn = 0
    n = 0
    for stage in range(2):
        for c in range(NCH):
            s = n % 2
            n += 1
            if stage == 0:
                P.dma("gpsimd", vb[s][:], vrev_d[c], w=["vb%d" % s])
                P.dma("sync", gA[s][:], vnat_d[c], w=["gA%d" % s])
                P.dma("sync", gB[s][:], x1_d[c], w=["gB%d" % s])
            else:
                P.dma("sync", vb[s][:], Y1d[c], r=["Y1d"], w=["vb%d" % s])
                P.dma("sync", gB[s][:], x2_d[c], w=["gB%d" % s])
                pj, pjk = nps()
                P.op("tensor", lambda e, pj=pj, s=s: e.matmul(pj[:, 0:NC2], lhsT=J[:], rhs=vb[s][:], start=True, stop=True), r=["J", "vb%d" % s], w=[pjk])
                P.op("scalar", lambda e, pj=pj, s=s: e.copy(out=gA[s][:], in_=pj[:, 0:NC2]), r=[pjk], w=["gA%d" % s])
            pa, pk = nps()
            pav = pa[:, 0:NC2].rearrange("p (b i) -> p b i", b=2)
            vv = vb[s][:].rearrange("p (b i) -> p b i", b=2)
            order = [NB - 1] + [k for k in range(2 * NB - 1) if k != NB - 1]
            loaded = {}
            cnt = 0
            for k in order:
                hf = 0 if k < NB else 1
                if hf not in loaded:
                    ts_ = tn % 4
                    tn += 1
                    nel = HALF if hf == 0 else HALF - 128
                    P.dma(("sync", "scalar")[tn % 2], T[ts_][:, 0:nel], bass.AP(tensor=Ktab[stage].tensor, offset=c * L2 + hf * HALF, ap=[[1, 128], [1, nel]]),
                          r=["Ktab%d" % stage], w=["T%d" % ts_])
                    loaded[hf] = ts_
                ts_ = loaded[hf]
                delta = (k - (NB - 1)) if stage == 0 else ((NB - 1) - k)
                i0, i1 = max(0, delta), min(NB, NB + delta)
                kk = k - hf * NB
                P.op("tensor", lambda e, pav=pav, vv=vv, ts_=ts_, kk=kk, i0=i0, i1=i1, delta=delta, cnt=cnt: e.matmul(pav[:, :, i0:i1], lhsT=T[ts_][:, kk * 128:(kk + 1) * 128], rhs=vv[:, :, i0 - delta:i1 - delta], start=(cnt == 0), stop=(cnt == 2 * NB - 2)),
                     r=["T%d" % ts_, "vb%d" % s], w=[pk])
                cnt += 1
            P.op("vector", lambda e, pa=pa, s=s, stage=stage, c=c: e.scalar_tensor_tensor(out=tt[s][:], in0=gA[s][:], scalar=hbias[:, stage, c:c + 1], in1=pa[:, 0:NC2], op0=ALU.mult, op1=ALU.add),
                 r=[pk, "gA%d" % s, "hbias"], w=["tt%d" % s])
            P.op("gpsimd", lambda e, s=s: e.tensor_tensor(out=yo[s][:], in0=tt[s][:], in1=gB[s][:], op=ALU.mult), r=["tt%d" % s, "gB%d" % s], w=["yo%d" % s])
            if stage == 0:
                P.dma("sync", Y1d[c], yo[s][:], r=["yo%d" % s], w=["Y1d"])
            else:
                P.dma("sync", y2_d[c], yo[s][:], r=["yo%d" % s])
    return P

def hyena_consts(L, ch0, nch):
    f32 = np.float32
    t = np.linspace(0.0, 1.0, L, dtype=f32)[:, None]
    w = (2.0 * np.pi * np.arange(L, dtype=f32)[:, None] / L).astype(f32)
    bands = np.linspace(1e-4, 15, 16, dtype=f32)[None, :]
    z = np.concatenate([t, np.cos(bands * w), np.sin(-bands * w)], axis=-1).astype(f32)
    max_decay = math.log(1e-2) / 0.3
    min_decay = math.log(1e-2) / 1.5
    deltas = np.abs(np.linspace(min_decay, max_decay, 1024, dtype=f32))
    window = np.exp(-t * deltas[None, ch0:ch0 + nch]).astype(f32)
    zT1 = np.ascontiguousarray(z[::-1].T)
    zT2 = np.ascontiguousarray(z.T)
    win1 = np.zeros((128, L), f32)
    win2 = np.zeros((128, L), f32)
    win1[:nch] = window[::-1].T
    win2[:nch] = window.T
    return zT1, zT2, win1, win2


def moe_lay_gu(w):
    return np.ascontiguousarray(w.reshape(8, 8, 128, 4, 896).transpose(0, 3, 2, 1, 4)).reshape(4096, 4, 1792)


def moe_lay_d(w):
    return np.ascontiguousarray(w.reshape(8, 4, 7, 128, 1024).transpose(0, 1, 3, 2, 4)).reshape(4096, 4, 1792)

def _pos_embed(rows):
    f32 = np.float32
    r, col = np.meshgrid(np.arange(rows, dtype=f32), np.arange(64, dtype=f32), indexing='ij')
    quarter = D // 4
    omega = (1.0 / (f32(10000.0) ** (np.arange(quarter, dtype=f32) / f32(quarter)))).astype(f32)
    def sincos(p):
        ang = (p.reshape(-1, 1) * omega[None, :]).astype(f32)
        return np.concatenate([np.sin(ang), np.cos(ang)], axis=-1)
    return np.concatenate([sincos(r), sincos(col)], axis=-1).astype(f32)


_LAST_DBG = {}


def _run(P, in_maps):
    nc = P.emit()
    res = run_bass_kernel_spmd(nc, in_maps, core_ids=list(range(8)))
    return res.results


def _col8(v):
    return np.ascontiguousarray(np.asarray(v, np.float32).reshape(8, 128).T)


def kernel(**inp):
    f32 = np.float32
    bf = ml_dtypes.bfloat16
    inp = {k: np.asarray(v) for k, v in inp.items()}
    B, L = 2, 16384
    TPC = 4096
    ident = np.eye(128, dtype=f32)
    pos = _pos_embed(L // 64)
    dbg = _LAST_DBG
    dbg.clear()
    cb = lambda k: (k // 4, (k % 4) * TPC)
    wsT = np.ascontiguousarray(inp['gmlp_w_s'][0].transpose(2, 0, 1)).reshape(128, 1024)
    bs16 = np.ascontiguousarray(np.repeat(inp['gmlp_b_s'][0], 2, axis=0).reshape(-1))
    maps = []
    for k in range(8):
        b, t0 = cb(k)
        bc_, half = (k % 4) // 2, (k % 4) % 2
        maps.append(dict(ident=ident, x=inp['x'][b, t0:t0 + TPC], pos=pos[t0:t0 + TPC], ctx=inp['ctx'][bc_, half * 128:(half + 1) * 128],
                         c=_col8(inp['c'][b]), c_ctx=_col8(inp['c_ctx']),
                         ada_w0=inp['ada_w'][0], ada_b0=inp['ada_b'][0], ng0=inp['norm_g'][0].reshape(-1),
                         ada_w1=inp['ada_w'][1], ada_b1=inp['ada_b'][1], ng1=inp['norm_g'][1].reshape(-1),
                         g_win=inp['gmlp_w_in'][0], g_gv=inp['gmlp_g_v'][0], g_wsT=wsT, g_bs=bs16, g_wout=inp['gmlp_w_out'][0],
                         f_wg=inp['ffn_w_gate'][0], f_wu=inp['ffn_w_up'][0], f_wd=inp['ffn_w_down'][0]))
    r = _run(build_S0(), maps)
    x1 = np.stack([np.concatenate([r[b * 4 + j]['xo'] for j in range(4)], 0) for b in range(2)])
    h1 = np.stack([np.concatenate([r[b * 4 + j]['ho'] for j in range(4)], 0) for b in range(2)])
    h1c = np.stack([np.concatenate([r[bc_ * 2 + half]['hco'] for half in range(2)], 0) for bc_ in range(2)])
    dbg['x0'] = x1
    del r
    CTX = 256
    SEQP = 2 + CTX + 4 + L + 2
    hseq = np.zeros((4, D, SEQP), dtype=bf)
    for d in range(2):
        for b in range(2):
            cs, ls = h1c[b], h1[b]
            if d == 1:
                cs, ls = cs[::-1], ls[::-1]
            hseq[d * 2 + b, :, 2:2 + CTX] = cs.T
            hseq[d * 2 + b, :, 2 + CTX + 4:2 + CTX + 4 + L] = ls.T
    maps = []
    for k in range(8):
        sl = slice(k * 128, (k + 1) * 128)
        maps.append(dict(hseq=hseq, w_gate=np.ascontiguousarray(inp['lru_w_in'][0][:, sl]), w_xb=np.ascontiguousarray(inp['lru_w_in'][0][:, 1024 + k * 128:1024 + (k + 1) * 128]),
                         conv_w=np.ascontiguousarray(inp['lru_conv_w'][0][:, sl]).reshape(-1), conv_b=np.ascontiguousarray(inp['lru_conv_b'][0][sl].reshape(128, 1)),
                         w_a=np.ascontiguousarray(inp['lru_w_a'][0][:, k]), w_x=np.ascontiguousarray(inp['lru_w_x'][0][:, k]),
                         b_a=np.ascontiguousarray(inp['lru_b_a'][0][:, sl].T), b_x=np.ascontiguousarray(inp['lru_b_x'][0][:, sl].T),
                         lam=np.ascontiguousarray(inp['lru_lam'][0][:, sl].T)))
    r = _run(build_S1(L=L, CTX=CTX), maps)
    del hseq
    mf = [np.concatenate([r[k]['m'][b] for k in range(8)], 0) for b in range(2)]
    mb = [np.concatenate([r[k]['m'][2 + b][:, ::-1] for k in range(8)], 0) for b in range(2)]
    del r
    NBLK = (2 * TPC + 8 * 511) // 512
    Utri = np.triu(np.ones((128, 128), f32), 1)
    blkrow = np.ascontiguousarray(np.broadcast_to((512.0 * np.arange(NBLK, dtype=f32))[None], (128, NBLK)))
    rowc = np.ascontiguousarray(np.concatenate([np.arange(128, dtype=f32)[:, None] + 128.0 * np.arange(4, dtype=f32)[None, :], np.zeros((128, 4), f32)], 1))
    moe_w = [(moe_lay_gu(inp['moe_w_gate'][k_]), moe_lay_gu(inp['moe_w_up'][k_]), moe_lay_d(inp['moe_w_down'][k_])) for k_ in range(2)]
    def wr_lay(w):
        return np.ascontiguousarray(w.reshape(8, 128, 8).transpose(1, 0, 2).reshape(128, 64))
    maps = []
    for k in range(8):
        b, t0 = cb(k)
        maps.append(dict(ident=ident, x=x1[b, t0:t0 + TPC], c=_col8(inp['c'][b]),
                         ada_w=inp['ada_w'][1], ada_b=inp['ada_b'][1], ng=inp['norm_g'][1].reshape(-1),
                         ada_wn=inp['ada_w'][2], ada_bn=inp['ada_b'][2], ngn=inp['norm_g'][2].reshape(-1),
                         w_out=inp['lru_w_out'][0], w_router=wr_lay(inp['moe_w_router'][0]),
                         wg=moe_w[0][0], wu=moe_w[0][1], wd=moe_w[0][2], Utri=Utri, blkrow=blkrow, rowc=rowc,
                         mf=np.ascontiguousarray(mf[b][:, t0:t0 + TPC]), mb=np.ascontiguousarray(mb[b][:, t0:t0 + TPC])))
    r = _run(build_TOK2("S2"), maps)
    x2 = np.stack([np.concatenate([r[b * 4 + j]['xo'] for j in range(4)], 0) for b in range(2)])
    h2 = np.stack([np.concatenate([r[b * 4 + j]['ho'] for j in range(4)], 0) for b in range(2)])
    dbg['x1'] = x2
    del r, mf, mb, x1, h1
    maps = []
    for k in range(8):
        b, t0 = cb(k)
        hp = np.zeros((D, TPC + 2), dtype=bf)
        lo, hi = max(t0 - 1, 0), min(t0 + TPC + 1, L)
        hp[:, lo - (t0 - 1):hi - (t0 - 1)] = h2[b, lo:hi].T
        maps.append(dict(hT=hp, w_in=inp['hyena_w_in'][0], conv_w=inp['hyena_conv_w'][0].reshape(-1), conv_b=inp['hyena_conv_b'][0]))
    r = _run(build_S3a(), maps)
    z = np.stack([np.concatenate([r[b * 4 + j]['z'] for j in range(4)], 0) for b in range(2)])
    del r, h2
    NB = L // 128
    def lay(a, rev):
        t = a.reshape(2, NB, 128, 128).transpose(3, 2, 0, 1)
        if rev:
            t = t[:, ::-1]
        return np.ascontiguousarray(t.reshape(128, 128, 2 * NB))
    fbias = np.ascontiguousarray(np.stack([inp['hyena_f_b1'][0], inp['hyena_f_b2'][0], inp['hyena_f_b3'][0]], 1))
    ffreq = np.ascontiguousarray(inp['hyena_f_freq'][0].T)
    Jm = np.ascontiguousarray(np.eye(128, dtype=f32)[::-1])
    maps = []
    for k in range(8):
        ch0 = k * 128
        zT1, zT2, win1, win2 = hyena_consts(L, ch0, 128)
        wo = np.ascontiguousarray(inp['hyena_f_wout'][0].reshape(64, 4, 1024)[:, :, ch0:ch0 + 128]).reshape(64, 512)
        maps.append(dict(vrev=lay(z[:, :, 2048 + ch0:2048 + ch0 + 128], True), vnat=lay(z[:, :, 2048 + ch0:2048 + ch0 + 128], False),
                         x1nat=lay(z[:, :, ch0:ch0 + 128], False), x2rev=lay(z[:, :, 1024 + ch0:1024 + ch0 + 128], True),
                         zT1=zT1, zT2=zT2, win1=win1, win2=win2, f_w1=inp['hyena_f_w1'][0], f_w2=inp['hyena_f_w2'][0], f_w3=inp['hyena_f_w3'][0],
                         f_b=fbias, f_freq=ffreq, f_wout=wo, hbias=np.ascontiguousarray(inp['hyena_bias'][0][:, ch0:ch0 + 128]).reshape(-1), J=Jm))
    r = _run(build_S3b(L=L, NCH=128), maps)
    del z
    m2 = np.zeros((2, D, L), dtype=bf)
    for k in range(8):
        y = r[k]['y2rev'][:, ::-1]
        m2[:, k * 128:(k + 1) * 128, :] = y.reshape(128, 128, 2, NB).transpose(2, 0, 3, 1).reshape(2, 128, L)
    del r
    maps = []
    for k in range(8):
        b, t0 = cb(k)
        maps.append(dict(ident=ident, x=x2[b, t0:t0 + TPC], c=_col8(inp['c'][b]),
                         ada_w=inp['ada_w'][2], ada_b=inp['ada_b'][2], ng=inp['norm_g'][2].reshape(-1),
                         ada_wn=inp['ada_w'][3], ada_bn=inp['ada_b'][3], ngn=inp['norm_g'][3].reshape(-1),
                         w_out=inp['hyena_w_out'][0], wg=inp['ffn_w_gate'][1], wu=inp['ffn_w_up'][1], wd=inp['ffn_w_down'][1],
                         mf=np.ascontiguousarray(m2[b][:, t0:t0 + TPC])))
    r = _run(build_TOK2("S4"), maps)
    x3 = np.stack([np.concatenate([r[b * 4 + j]['xo'] for j in range(4)], 0) for b in range(2)])
    h3 = np.stack([np.concatenate([r[b * 4 + j]['ho'] for j in range(4)], 0) for b in range(2)])
    dbg['x2'] = x3
    del r, m2, x2
    HW = 8
    maps = []
    for k in range(8):
        b, t0 = cb(k)
        hp = np.zeros((D, TPC + 2 * HW), dtype=bf)
        lo, hi = max(t0 - HW, 0), min(t0 + TPC + HW, L)
        hp[:, lo - (t0 - HW):hi - (t0 - HW)] = h3[b, lo:hi].T
        fix = np.ones((4, 16), f32)
        for wi_, win in enumerate((2, 4, 8, 16)):
            for j in range(8):
                if k % 4 == 0:
                    t = j
                    fix[wi_, j] = win / float(min(t + win // 2, L) - max(t - win // 2, 0))
                if k % 4 == 3:
                    t = L - 8 + j
                    fix[wi_, 8 + j] = win / float(min(t + win // 2, L) - max(t - win // 2, 0))
        maps.append(dict(ident=ident, x=x3[b, t0:t0 + TPC], c=_col8(inp['c'][b]),
                         ada_w=inp['ada_w'][3], ada_b=inp['ada_b'][3], ng=inp['norm_g'][3].reshape(-1),
                         w_out=inp['pool_w_out'][0], w_router=wr_lay(inp['moe_w_router'][1]),
                         wg=moe_w[1][0], wu=moe_w[1][1], wd=moe_w[1][2], Utri=Utri, blkrow=blkrow, rowc=rowc,
                         h3T=hp, p_win=inp['pool_w_in'][0], p_wg=inp['pool_w_g'][0], p_bg=_col8(inp['pool_b_g'][0].reshape(-1)),
                         p_scale=_col8(inp['pool_scale'][0]), p_fix=np.ascontiguousarray(np.broadcast_to(fix.reshape(1, 64), (128, 64)))))
    r = _run(build_TOK2("S5"), maps)
    out = np.stack([np.concatenate([r[b * 4 + j]['xo'] for j in range(4)], 0) for b in range(2)]).astype(f32)
    return out
```
